# Optimizing a Trainium2 kernel written in Bass

```python
import math
import jax, jax.numpy as jnp
from jax import lax
import numpy as np

D_MODEL = 1024
BATCH = 2
SEQ = 8192
DEPTH = 1

GRID_W = 64
CTX_LEN = 256
N_HEADS = 8
HEAD_DIM = 128
D_GDN = N_HEADS * HEAD_DIM
CONV_K = 5
GDN_CHUNK = 64
SGU_GROUPS = 8
SGU_GROUP_DIM = 128
D_SGU = SGU_GROUPS * SGU_GROUP_DIM
SGU_CHUNK = 128
ROWS_PER_CHUNK = SGU_CHUNK // GRID_W
N_EXPERT_GROUPS = 4
EXPERTS_PER_GROUP = 8
N_EXPERTS = N_EXPERT_GROUPS * EXPERTS_PER_GROUP
TOP_K = 2
D_EXPERT = 256
N_MOD = 6
EPS = 1e-6
COL_BETA = 3 * D_GDN
COL_A = COL_BETA + 2 * N_HEADS
COL_Z = COL_A + 2 * N_HEADS
COL_U = COL_Z + D_GDN
COL_V = COL_U + D_SGU
COL_GATE = COL_V + D_SGU
D_IN = COL_GATE + 2 * D_MODEL

kernel_name = 'hybrid_gdn_sgu_hmoe_dit_block'


def rms_norm(x, gain):
    xf = x.astype(jnp.float32)
    y = xf * lax.rsqrt(jnp.mean(xf * xf, axis=-1, keepdims=True) + EPS)
    return (y * gain.astype(jnp.float32)).astype(x.dtype)


def layer_norm(x, gain, bias):
    xf = x.astype(jnp.float32)
    mu = jnp.mean(xf, axis=-1, keepdims=True)
    var = jnp.mean(jnp.square(xf - mu), axis=-1, keepdims=True)
    y = (xf - mu) * lax.rsqrt(var + EPS)
    return (y * gain.astype(jnp.float32) + bias.astype(jnp.float32)).astype(x.dtype)


def l2_normalize(t):
    return t * lax.rsqrt(jnp.sum(t * t, axis=-1, keepdims=True) + EPS)


def modulate(h, shift, scale):
    return h * (1.0 + scale[:, None, :]) + shift[:, None, :]


def adaln_params(cond, w, b):
    m = jax.nn.silu(cond) @ w + b
    return m.reshape(cond.shape[0], N_MOD, D_MODEL)


def centred_depthwise_conv(x, w):
    C = x.shape[-1]
    pad = CONV_K // 2
    return lax.conv_general_dilated(
        x, w.reshape(CONV_K, 1, C).astype(x.dtype), window_strides=(1,), padding=((pad, pad),),
        dimension_numbers=('NWC', 'WIO', 'NWC'), feature_group_count=C)


def gated_delta_chunked(q, k, v, g, beta, s0):
    B, H, L, dk = q.shape
    n = L // GDN_CHUNK
    C = GDN_CHUNK
    rs = lambda t: t.reshape(B, H, n, C, *t.shape[3:])
    q, k, v, g, beta = rs(q), rs(k), rs(v), rs(g), rs(beta)
    G = jnp.cumsum(g, axis=-1)
    incl = jnp.tril(jnp.ones((C, C), dtype=bool))
    strict = jnp.tril(jnp.ones((C, C), dtype=bool), -1)
    decay = jnp.exp(jnp.where(incl, G[..., :, None] - G[..., None, :], -jnp.inf))
    kb = k * beta[..., None]
    low = jnp.where(strict, jnp.einsum('bhncd,bhnsd->bhncs', kb, k) * decay, 0.0)
    eye = jnp.eye(C, dtype=q.dtype)
    T = lax.linalg.triangular_solve(eye + low, jnp.broadcast_to(eye, low.shape), left_side=True, lower=True)
    u = T @ (v * beta[..., None])
    w = T @ (kb * jnp.exp(G)[..., None])
    qk_intra = jnp.einsum('bhncd,bhnsd->bhncs', q, k) * decay
    q_dec = q * jnp.exp(G)[..., None]
    k_dec = k * jnp.exp(G[..., -1:] - G)[..., None]
    g_last = jnp.exp(G[..., -1])

    def step(s, inp):
        u_c, w_c, qk_c, qd_c, kd_c, gl_c = inp
        v_new = u_c - w_c @ s
        o_c = qd_c @ s + qk_c @ v_new
        s = s * gl_c[..., None, None] + jnp.swapaxes(kd_c, -1, -2) @ v_new
        return s, o_c

    xs = tuple(jnp.moveaxis(t, 2, 0) for t in (u, w, qk_intra, q_dec, k_dec, g_last))
    s_final, o = lax.scan(step, s0, xs)
    o = jnp.moveaxis(o, 0, 2).reshape(B, H, L, v.shape[-1])
    return o, s_final


def gdn_scan(p_qkv, p_beta, p_a, conv_w, a_log, dt_bias, s_init):
    B, L, _ = p_qkv.shape
    qkv = jax.nn.silu(centred_depthwise_conv(p_qkv, conv_w)).astype(jnp.float32)
    heads = lambda t: jnp.swapaxes(t.reshape(B, L, N_HEADS, HEAD_DIM), 1, 2)
    q, k, v = (heads(t) for t in jnp.split(qkv, 3, axis=-1))
    q = l2_normalize(q) * (HEAD_DIM ** -0.5)
    k = l2_normalize(k)
    dirs = lambda t: t.astype(jnp.float32).reshape(B, L, 2, N_HEADS).transpose(2, 0, 3, 1)
    beta = jax.nn.sigmoid(dirs(p_beta))
    g = -jnp.exp(a_log.astype(jnp.float32))[:, None, :, None] * jax.nn.softplus(
        dirs(p_a) + dt_bias.astype(jnp.float32)[:, None, :, None])
    o_f, s_f = gated_delta_chunked(q, k, v, g[0], beta[0], s_init[0])
    flip = lambda t: jnp.flip(t, axis=2)
    o_b, s_b = gated_delta_chunked(flip(q), flip(k), flip(v), flip(g[1]), flip(beta[1]), s_init[1])
    o = jnp.swapaxes(o_f + flip(o_b), 1, 2)
    return o, jnp.stack([s_f, s_b])


def gdn_output(o, p_z, norm_g):
    B, L = o.shape[:2]
    z = p_z.astype(jnp.float32).reshape(B, L, N_HEADS, HEAD_DIM)
    y = rms_norm(o, norm_g) * jax.nn.silu(z)
    return y.reshape(B, L, D_GDN).astype(p_z.dtype)


def sgu_mixer(p_u, p_v, n_chunks, ln_g, ln_b, w_s, b_s):
    B, L, _ = p_u.shape
    u = jax.nn.gelu(p_u)
    v = layer_norm(jax.nn.gelu(p_v), ln_g, ln_b).reshape(B, n_chunks, SGU_CHUNK, SGU_GROUPS, SGU_GROUP_DIM)
    mixed = jnp.einsum('gpq,bnqgc->bnpgc', w_s, v) + b_s.T[:, :, None]
    return u * mixed.reshape(B, L, D_SGU)


def merge_branches(y_gdn, y_sgu, p_gates, w_a, w_b, w_o):
    g_a, g_b = jnp.split(jax.nn.sigmoid(p_gates), 2, axis=-1)
    return (g_a * (y_gdn @ w_a) + g_b * (y_sgu @ w_b)) @ w_o


def hier_moe(h, w_rg, b_rg, w_re, b_re, w1, w3, w2):
    B, L, D = h.shape
    t = h.reshape(B * L, D)
    p_group = jax.nn.softmax((t @ w_rg + b_rg).astype(jnp.float32), axis=-1)
    grp = jnp.argmax(p_group, axis=-1)
    p_g = jnp.take_along_axis(p_group, grp[:, None], axis=-1)
    logits_e = (t @ w_re + b_re).astype(jnp.float32).reshape(-1, N_EXPERT_GROUPS, EXPERTS_PER_GROUP)
    logits_in = jnp.take_along_axis(logits_e, grp[:, None, None], axis=1)[:, 0]
    top_p, top_i = lax.top_k(jax.nn.softmax(logits_in, axis=-1), TOP_K)
    top_p = top_p / jnp.sum(top_p, axis=-1, keepdims=True) * p_g
    expert_id = grp[:, None] * EXPERTS_PER_GROUP + top_i
    gate = jnp.sum(jax.nn.one_hot(expert_id, N_EXPERTS, dtype=jnp.float32) * top_p[..., None], axis=1)
    gate = gate.astype(h.dtype).reshape(-1, N_EXPERT_GROUPS, EXPERTS_PER_GROUP)
    out = jnp.zeros_like(t)
    for gi in range(N_EXPERT_GROUPS):
        sl = slice(gi * EXPERTS_PER_GROUP, (gi + 1) * EXPERTS_PER_GROUP)
        hid = jax.nn.silu(jnp.einsum('td,edf->tef', t, w1[sl])) * jnp.einsum('td,edf->tef', t, w3[sl])
        out = out + jnp.einsum('tef,efd->td', hid * gate[:, gi, :, None], w2[sl])
    return out.reshape(B, L, D)


def setup_inputs(seed: int = 0) -> dict:
    key = jax.random.key(seed)
    ks = jax.random.split(key, 32)
    f32 = jnp.float32
    nrm = lambda k, shape, scale: jax.random.normal(k, shape, f32) * scale
    gain = lambda k, shape: 1.0 + 0.1 * jax.random.normal(k, shape, f32)
    dt = jnp.exp(jax.random.uniform(ks[9], (DEPTH, 2, N_HEADS), f32, math.log(1e-3), math.log(1e-1)))
    return {
        'x': nrm(ks[0], (BATCH, SEQ, D_MODEL), 1.0),
        'c': nrm(ks[1], (BATCH, D_MODEL), 1.0),
        'ctx': nrm(ks[2], (BATCH, CTX_LEN, D_MODEL), 1.0),
        'c_ctx': nrm(ks[3], (D_MODEL,), 1.0),
        'ada_w': nrm(ks[4], (DEPTH, D_MODEL, N_MOD * D_MODEL), 0.5 * D_MODEL ** -0.5),
        'ada_b': nrm(ks[5], (DEPTH, N_MOD * D_MODEL), 0.02),
        'norm_mix_g': gain(ks[6], (DEPTH, D_MODEL)),
        'w_in': nrm(ks[7], (DEPTH, D_MODEL, D_IN), D_MODEL ** -0.5),
        'conv_w': nrm(ks[8], (DEPTH, CONV_K, 3 * D_GDN), CONV_K ** -0.5),
        'a_log': jnp.log(jax.random.uniform(ks[10], (DEPTH, 2, N_HEADS), f32, 1.0, 16.0)),
        'dt_bias': dt + jnp.log(-jnp.expm1(-dt)),
        'gdn_norm_g': gain(ks[11], (DEPTH, HEAD_DIM)),
        'sgu_ln_g': gain(ks[12], (DEPTH, D_SGU)),
        'sgu_ln_b': nrm(ks[13], (DEPTH, D_SGU), 0.02),
        'sgu_w': nrm(ks[14], (DEPTH, SGU_GROUPS, SGU_CHUNK, SGU_CHUNK), SGU_CHUNK ** -0.5),
        'sgu_b': gain(ks[15], (DEPTH, SGU_GROUPS, SGU_CHUNK)),
        'w_branch_a': nrm(ks[16], (DEPTH, D_GDN, D_MODEL), D_GDN ** -0.5),
        'w_branch_b': nrm(ks[17], (DEPTH, D_SGU, D_MODEL), D_SGU ** -0.5),
        'w_out': nrm(ks[18], (DEPTH, D_MODEL, D_MODEL), D_MODEL ** -0.5),
        'norm_ffn_g': gain(ks[19], (DEPTH, D_MODEL)),
        'router_group_w': nrm(ks[20], (DEPTH, D_MODEL, N_EXPERT_GROUPS), D_MODEL ** -0.5),
        'router_group_b': nrm(ks[21], (DEPTH, N_EXPERT_GROUPS), 0.01),
        'router_expert_w': nrm(ks[22], (DEPTH, D_MODEL, N_EXPERTS), D_MODEL ** -0.5),
        'router_expert_b': nrm(ks[23], (DEPTH, N_EXPERTS), 0.01),
        'expert_w1': nrm(ks[24], (DEPTH, N_EXPERTS, D_MODEL, D_EXPERT), D_MODEL ** -0.5),
        'expert_w3': nrm(ks[25], (DEPTH, N_EXPERTS, D_MODEL, D_EXPERT), D_MODEL ** -0.5),
        'expert_w2': nrm(ks[26], (DEPTH, N_EXPERTS, D_EXPERT, D_MODEL), D_EXPERT ** -0.5),
        'final_norm_g': gain(ks[27], (D_MODEL,)),
    }


def reference(x, c, ctx, c_ctx, ada_w, ada_b, norm_mix_g, w_in, conv_w, a_log, dt_bias, gdn_norm_g,
              sgu_ln_g, sgu_ln_b, sgu_w, sgu_b, w_branch_a, w_branch_b, w_out, norm_ffn_g,
              router_group_w, router_group_b, router_expert_w, router_expert_b,
              expert_w1, expert_w3, expert_w2, final_norm_g):
    B = x.shape[0]
    rows = x.shape[1] // GRID_W
    lat_chunks = rows // ROWS_PER_CHUNK
    ctx_chunks = ctx.shape[1] // SGU_CHUNK
    s_zero = jnp.zeros((2, B, N_HEADS, HEAD_DIM, HEAD_DIM), jnp.float32)
    for l in range(DEPTH):
        mod_x = adaln_params(c, ada_w[l], ada_b[l])
        mod_c = adaln_params(c_ctx[None, :], ada_w[l], ada_b[l])
        hc = modulate(rms_norm(ctx, norm_mix_g[l]), mod_c[:, 0], mod_c[:, 1])
        hx = modulate(rms_norm(x, norm_mix_g[l]), mod_x[:, 0], mod_x[:, 1])
        c_qkv, c_beta, c_a = jnp.split(hc @ w_in[l][:, :COL_Z], (COL_BETA, COL_A), axis=-1)
        o_c, s_ctx = gdn_scan(c_qkv, c_beta, c_a, conv_w[l], a_log[l], dt_bias[l], s_zero)
        x_qkv, x_beta, x_a, x_z, x_u, x_v, x_gates = jnp.split(
            hx @ w_in[l], (COL_BETA, COL_A, COL_Z, COL_U, COL_V, COL_GATE), axis=-1)
        o_x, _ = gdn_scan(x_qkv, x_beta, x_a, conv_w[l], a_log[l], dt_bias[l], s_ctx)
        y_gdn = gdn_output(o_x, x_z, gdn_norm_g[l])
        y_sgu = sgu_mixer(x_u, x_v, lat_chunks, sgu_ln_g[l], sgu_ln_b[l], sgu_w[l], sgu_b[l])
        x = x + mod_x[:, 2][:, None, :] * merge_branches(y_gdn, y_sgu, x_gates, w_branch_a[l], w_branch_b[l], w_out[l])
        hx = modulate(rms_norm(x, norm_ffn_g[l]), mod_x[:, 3], mod_x[:, 4])
        x = x + mod_x[:, 5][:, None, :] * hier_moe(hx, router_group_w[l], router_group_b[l], router_expert_w[l],
                                                   router_expert_b[l], expert_w1[l], expert_w3[l], expert_w2[l])
        if l < DEPTH - 1:
            c_z, c_u, c_v, c_gates = jnp.split(hc @ w_in[l][:, COL_Z:], (D_GDN, D_GDN + D_SGU, D_GDN + 2 * D_SGU), axis=-1)
            yc_gdn = gdn_output(o_c, c_z, gdn_norm_g[l])
            yc_sgu = sgu_mixer(c_u, c_v, ctx_chunks, sgu_ln_g[l], sgu_ln_b[l], sgu_w[l], sgu_b[l])
            ctx = ctx + mod_c[:, 2][:, None, :] * merge_branches(yc_gdn, yc_sgu, c_gates, w_branch_a[l], w_branch_b[l], w_out[l])
            hc = modulate(rms_norm(ctx, norm_ffn_g[l]), mod_c[:, 3], mod_c[:, 4])
            ctx = ctx + mod_c[:, 5][:, None, :] * hier_moe(hc, router_group_w[l], router_group_b[l], router_expert_w[l],
                                                           router_expert_b[l], expert_w1[l], expert_w3[l], expert_w2[l])
    return rms_norm(x, final_norm_g)
```

```python
import contextlib
import os
import numpy as np
import concourse.bass as bass
import concourse.mybir as mybir
from concourse.bass_utils import run_bass_kernel_spmd

F32 = mybir.dt.float32
BF16 = mybir.dt.bfloat16
ALU = mybir.AluOpType
AF = mybir.ActivationFunctionType

D = 1024
SEQ = 8192
CTX = 256
NT = 66
OWN = 2048
NOT = 16
NE = 32
DE = 256
COL_BETA = 3072
COL_A = COL_BETA + 16
COL_Z = COL_A + 16
EPS = 1e-6
ARENA = 53000


SAME_ENGINE_WAITS = os.environ.get('SAMEENG', '1') == '1'


class Tk:
    __slots__ = ("name", "w", "rd", "excl")

    def __init__(self, name="", excl=False):
        self.name = name
        self.w = None
        self.rd = []
        self.excl = excl


class Op:
    __slots__ = ("eng", "fn", "deps", "used", "sem", "val", "dma", "key", "idx", "inc")


class Sched:
    ENGS = ("pe", "act", "dve", "pool", "sp")

    def __init__(self, nc):
        self.nc = nc
        self.ops = {e: [] for e in self.ENGS}
        self.all = []
        self.dma_since_barrier = []

    def op(self, eng, fn, reads=(), writes=(), dma=0, key=None, extra=(), inc=16):
        o = Op()
        o.eng = eng; o.fn = fn; o.used = False; o.sem = None; o.val = None
        o.dma = dma; o.key = key; o.idx = len(self.all); o.inc = inc
        deps = set(extra)
        reads = list(reads); writes = list(writes)
        for r in list(reads):
            if r.excl and r not in writes:
                writes.append(r)
        for r in reads:
            if r.w is not None:
                deps.add(r.w)
        for w in writes:
            if w.w is not None:
                deps.add(w.w)
            for x in w.rd:
                deps.add(x)
        for r in reads:
            r.rd.append(o)
        for w in writes:
            w.w = o
            w.rd = []
        o.deps = [d for d in deps if d is not o and not (d.eng == "pe" and eng == "pe" and not d.dma and not dma)
                  and not (SAME_ENGINE_WAITS is False and d.eng == eng and eng in ("act", "dve", "pool") and not d.dma and not dma)]
        for d in o.deps:
            d.used = True
        if dma:
            assert key is not None
            self.dma_since_barrier.append(o)
        self.ops[eng].append(o)
        self.all.append(o)
        return o

    def barrier(self):
        last = [self.ops[e][-1] for e in self.ENGS if self.ops[e] and self.ops[e][-1].fn is not None]
        dmas = list(self.dma_since_barrier)
        self.dma_since_barrier = []
        for e in self.ENGS:
            self.op(e, None, extra=[x for x in last if x.eng != e] + dmas)

    def emit(self, block, sems):
        nc = self.nc
        sems = list(sems)
        eng_sem = {e: sems.pop() for e in ("pe", "act", "dve", "pool")}
        keysem = {}
        cnt = {e: 0 for e in eng_sem}
        kcnt = {}
        for o in self.all:
            if o.dma:
                if o.key not in keysem:
                    keysem[o.key] = sems.pop()
                    kcnt[o.key] = 0
                kcnt[o.key] += o.inc * o.dma
                o.sem = keysem[o.key]; o.val = kcnt[o.key]
            elif o.used:
                assert o.fn is not None
                cnt[o.eng] += 1
                o.sem = eng_sem[o.eng]; o.val = cnt[o.eng]
        engobj = {"pe": nc.tensor, "act": nc.scalar, "dve": nc.vector, "pool": nc.gpsimd, "sp": nc.sync}
        deco = {"pe": block.tensor, "act": block.scalar, "dve": block.vector, "pool": block.gpsimd, "sp": block.sync}

        def run(ename):
            def body(e):
                known = {}
                for o in self.ops[ename]:
                    for d in sorted(o.deps, key=lambda x: x.idx):
                        sid = id(d.sem)
                        if known.get(sid, 0) >= d.val:
                            continue
                        e.wait_ge(d.sem, d.val)
                        known[sid] = d.val
                    if o.fn is None:
                        continue
                    if o.dma:
                        n = [0]

                        def sig(inst, o=o, n=n):
                            if o.inc == 16:
                                inst.then_inc(o.sem, 16)
                            else:
                                inst.then_inc(o.sem)
                            n[0] += 1
                            return inst
                        o.fn(e, sig)
                        assert n[0] == o.dma, (n[0], o.dma)
                    else:
                        inst = o.fn(e)
                        if o.used:
                            inst.then_inc(o.sem, 1)
            return body
        for ename in self.ENGS:
            deco[ename](run(ename))


class Arena:
    def __init__(self, ap_f32, nf32, base=0):
        self.ap = ap_f32
        self.n = base + nf32
        self.off = base
        self.hi = 0

    def mark(self):
        return self.off

    def release(self, m):
        self.off = m

    def alloc(self, free_shape, dtype=F32):
        n = int(np.prod(free_shape))
        nf = n if dtype == F32 else (n + 1) // 2
        nf = (nf + 1) // 2 * 2
        assert self.off + nf <= self.n, ("arena overflow", self.off, nf, self.n)
        v = self.ap[:, self.off:self.off + nf]
        self.off += nf
        self.hi = max(self.hi, self.off)
        if dtype != F32:
            v = v.bitcast(dtype)[:, 0:n]
        else:
            v = v[:, 0:n]
        if len(free_shape) == 2:
            v = v.rearrange("p (a b) -> p a b", a=free_shape[0])
        elif len(free_shape) == 3:
            v = v.rearrange("p (a b c) -> p a b c", a=free_shape[0], b=free_shape[1])
        return v


class _Stop(Exception):
    pass


def build_program(debug=False, stage=99):
    KCUT = int(os.environ.get('KCUT', '0')); NTL = int(os.environ.get('NTL', str(NT)))
    nc = bass.Bass("TRN2", target_bir_lowering=False)

    def din(name, shape, dt=F32):
        return nc.dram_tensor(name, list(shape), dt, kind="ExternalInput")

    xb = din("xb", [SEQ, D]); ctxb = din("ctxb", [CTX, D]); xo = din("xo", [OWN, D])
    cvT = din("cvT", [128, 8, 2]); ada_w = din("ada_w", [D, 6 * D]); ada_b = din("ada_b", [1, 6 * D])
    gcols = din("gcols", [128, 8, 2])
    w_qkv = din("w_qkv", [D, 768]); w_ba = din("w_ba", [D, 8])
    gsc66 = din("gsc66", [128, 2, NT, 4])
    conv_c = din("conv_c", [128, 6, 5]); gsc = din("gsc", [128, 8]); gng = din("gng", [128, 1])
    dcm_d = din("dcm", [9, 128, 128], BF16)
    ident_d = din("ident", [128, 128]); tri_d = din("tri", [4, 128, 128]); neg_d = din("negm", [4, 128, 128])
    w_rest = din("w_rest", [D, 5120]); sgu_wT = din("sgu_wT", [8, 128, 128]); sgu_bb = din("sgu_bb", [128, 8, 128])
    lng_c = din("lng_c", [128, 8]); lnb_bc = din("lnb_bc", [128, D])
    w_a = din("w_a", [D, D]); w_b = din("w_b", [D, D]); w_out = din("w_out", [D, D])
    rw = din("rw", [D, 36]); rb = din("rb", [128, 36])
    ew1 = din("ew1", [NE, D, DE]); ew3 = din("ew3", [NE, D, DE]); ew2 = din("ew2", [NE, DE, D])
    fng_bc = din("fng_bc", [128, D]); qmask_d = din("qmask", [128, 4])
    y = nc.dram_tensor("y", [OWN, D], F32, kind="ExternalOutput")
    snd = [nc.dram_tensor(f"snd{j}", [2 * 128, 1024], F32) for j in range(4)]
    rcv = [nc.dram_tensor(f"rcv{j}", [4 * 2 * 128, 1024], F32) for j in range(4)]
    x1d = nc.dram_tensor("x1d", [OWN, D], F32)
    dbg = None
    if debug:
        dbg = nc.dram_tensor("dbg", [128, 4096], F32, kind="ExternalOutput")
        dbgb = nc.dram_tensor("dbgb", [768, SEQ], BF16, kind="ExternalOutput")

    es = contextlib.ExitStack()
    with es:
        arena_t = es.enter_context(nc.sbuf_tensor("arena", [128, ARENA], F32))
        psb = [es.enter_context(nc.psum_tensor(f"psb{i}", [128, 512], F32)) for i in range(8)]
        sems = [es.enter_context(nc.semaphore(f"s{i}")) for i in range(100)]
        block = es.enter_context(nc.Block())
        A = Arena(arena_t, ARENA)
        S = Sched(nc)

        dump_ops = []

        def dump(ap, col0, ncols, reads=()):
            o_ = S.op("sp", lambda e, sig: sig(e.dma_start(out=dbg[:, col0:col0 + ncols], in_=ap)), reads=list(reads), dma=1, key="dbg")
            dump_ops.append(o_)

        def cut(k, dumps):
            if stage == k:
                S.barrier()
                for d_ in dumps():
                    dump(*d_)
                raise _Stop()

        def dumpb(ap, row0, nrows, col0, ncols, reads=(), extra=()):
            o_ = S.op("sp", lambda e, sig: sig(e.dma_start(out=dbgb[row0:row0 + nrows, col0:col0 + ncols], in_=ap)), reads=list(reads), extra=list(extra), dma=1, key="dbg")
            dump_ops.append(o_)

        def author():
            nonlocal A
            class Pool:
                def __init__(self, items):
                    self.items = items
                    self.i = 0

                def get(self):
                    it = self.items[self.i % len(self.items)]
                    self.i += 1
                    return it
            p128 = Pool([(psb[b][:, 0:128], Tk(f"p128_{b}", excl=True)) for b in range(4)])
            p512 = Pool([(psb[b][:, :], Tk(f"p512_{b}", excl=True)) for b in range(4, 8)])

            def bfv(ap):
                n = ap.shape[-1]
                return ap.bitcast(BF16)[:, 0:n]

            def load(dst, src, tk, key):
                return S.op("sp", lambda e, sig: sig(e.dma_start(out=dst, in_=src)), writes=[tk], dma=1, key=key)

            identf = A.alloc([128]); t_identf = Tk(); load(identf, ident_d[:, :], t_identf, "c0")
            identb = A.alloc([128], BF16); t_identb = Tk()
            S.op("pool", lambda e: e.tensor_copy(out=identb, in_=identf), reads=[t_identf], writes=[t_identb])
            trif = A.alloc([4, 128]); t_trif = Tk(); load(trif, tri_d.ap().rearrange("a p q -> p a q"), t_trif, "c1")
            negf = A.alloc([4, 128]); t_negf = Tk(); load(negf, neg_d.ap().rearrange("a p q -> p a q"), t_negf, "c2")
            negb = A.alloc([4, 128], BF16); t_negb = Tk()
            S.op("pool", lambda e: e.tensor_copy(out=negb, in_=negf), reads=[t_negf], writes=[t_negb])
            dcm = A.alloc([9, 128], BF16); t_dcm = Tk(); load(dcm, dcm_d.ap().rearrange("a p q -> p a q"), t_dcm, "c2b")
            onesf = A.alloc([128]); t_onesf = Tk()
            S.op("pool", lambda e: e.memset(onesf, 1.0), writes=[t_onesf])
            Uf, Vf, Ub, Vb = (trif[:, i, :] for i in range(4))
            gscs = A.alloc([8]); t_gscs = Tk(); load(gscs, gsc[:, :], t_gscs, "c3")
            negA = A.alloc([4]); t_negA = Tk()
            S.op("act", lambda e: e.activation(out=negA, in_=gscs[:, 0:4], func=AF.Exp), reads=[t_gscs], writes=[t_negA])
            S.op("dve", lambda e: e.tensor_scalar(out=negA, in0=negA, scalar1=-1.0, scalar2=None, op0=ALU.mult), reads=[t_negA], writes=[t_negA])
            dtb = gscs[:, 4:8]
            convc = A.alloc([6, 5]); t_convc = Tk(); load(convc, conv_c[:, :, :], t_convc, "c4")
            gngs = A.alloc([1]); t_gngs = Tk(); load(gngs, gng[:, :], t_gngs, "c5")
            qmask = A.alloc([4]); t_qmask = Tk(); load(qmask, qmask_d[:, :], t_qmask, "c6")
            gcol = A.alloc([8, 2]); t_gcol = Tk(); load(gcol, gcols[:, :, :], t_gcol, "c7")
            modc = A.alloc([6, 8]); t_modc = Tk("modc")
            gmix = A.alloc([D]); t_gmix = Tk("gmix")
            gffn = A.alloc([D]); t_gffn = Tk("gffn")
            GT = A.alloc([NOT, 32]); t_GT = [Tk() for _ in range(NOT)]

            m0 = A.mark()
            scT = A.alloc([8, 2]); t_scT = Tk(); load(scT, cvT[:, :, :], t_scT, "c8")
            S.op("act", lambda e: e.activation(out=scT, in_=scT, func=AF.Silu), reads=[t_scT], writes=[t_scT])
            modrow = A.alloc([6 * D]); t_modrow = Tk("modrow")
            modrow_c = A.alloc([6 * D]); t_modrow_c = Tk("modrow_c")
            adb = A.alloc([6 * D]); t_adb = Tk()
            S.op("sp", lambda e, sig: (sig(e.dma_start(out=adb[0:1, :], in_=ada_b[:, :])), sig(e.dma_start(out=adb[1:2, :], in_=ada_b[:, :]))),
                 writes=[t_adb], dma=2, key="c9")
            adw = [A.alloc([8, 512]) for _ in range(2)]; t_adw = [Tk(), Tk()]
            ada_v = ada_w.ap().rearrange("(k p) n -> p k n", p=128)
            for nb in range(12):
                sl = nb % 2
                S.op("sp", lambda e, sig, nb=nb, sl=sl: (sig(e.dma_start(out=adw[sl][:, 0:4, :], in_=ada_v[:, 0:4, nb * 512:(nb + 1) * 512])),
                                                        sig(e.dma_start(out=adw[sl][:, 4:8, :], in_=ada_v[:, 4:8, nb * 512:(nb + 1) * 512]))),
                     writes=[t_adw[sl]], dma=2, key=f"adw{sl}")
                pp, tp = p512.get()

                def f(e, sl=sl, pp=pp):
                    for k in range(8):
                        i = e.matmul(pp[0:2, :], lhsT=scT[:, k, :], rhs=adw[sl][:, k, :], start=(k == 0), stop=(k == 7))
                    return i
                S.op("pe", f, reads=[t_scT, t_adw[sl]], writes=[tp])
                S.op("dve", lambda e, nb=nb, pp=pp: e.tensor_tensor(out=modrow[0:2, nb * 512:(nb + 1) * 512], in0=pp[0:2, :], in1=adb[0:2, nb * 512:(nb + 1) * 512], op=ALU.add),
                     reads=[tp, t_adb], writes=[t_modrow])
            S.op("sp", lambda e, sig: sig(e.dma_start(out=modrow_c[0:1, :], in_=modrow[1:2, :])), reads=[t_modrow], writes=[t_modrow_c], dma=1, key="c10")
            pp, tp = p128.get()
            vecs = [(modrow, 0), (modrow, 1), (modrow_c, 0), (modrow_c, 1), (modrow, 3), (modrow, 4)]

            def f(e, pp=pp):
                for vi, (row, m) in enumerate(vecs):
                    for k in range(8):
                        i = e.matmul(pp[:, vi * 8 + k:vi * 8 + k + 1], lhsT=row[0:1, m * D + k * 128:m * D + (k + 1) * 128], rhs=onesf[0:1, 0:1], start=True, stop=True)
                return i
            S.op("pe", f, reads=[t_modrow, t_modrow_c, t_onesf], writes=[tp])
            S.op("dve", lambda e, pp=pp: e.tensor_copy(out=modc.rearrange("p a b -> p (a b)"), in_=pp[:, 0:48]), reads=[tp], writes=[t_modc])
            for vi, gi in ((1, 0), (3, 0), (5, 1)):
                S.op("dve", lambda e, vi=vi, gi=gi: e.scalar_tensor_tensor(out=modc[:, vi, :], in0=modc[:, vi, :], scalar=1.0, in1=gcol[:, :, gi], op0=ALU.add, op1=ALU.mult),
                     reads=[t_modc, t_gcol], writes=[t_modc])
            for dst, tdst, m in ((gmix, t_gmix, 2), (gffn, t_gffn, 5)):
                for h in range(2):
                    pp, tp = p512.get()
                    S.op("pe", lambda e, pp=pp, m=m, h=h: e.matmul(pp, lhsT=onesf[0:1, :], rhs=modrow[0:1, m * D + h * 512:m * D + (h + 1) * 512], start=True, stop=True),
                         reads=[t_modrow, t_onesf], writes=[tp])
                    S.op("act", lambda e, pp=pp, dst=dst, h=h: e.copy(out=dst[:, h * 512:(h + 1) * 512], in_=pp), reads=[tp], writes=[tdst])
            S.barrier()
            A.release(m0)
            if stage == 0:
                dump(modc.rearrange("p a b -> p (a b)"), 0, 48); dump(gmix[:, 0:256], 64, 256); dump(gffn[:, 0:256], 320, 256)
                raise _Stop()

            mA = A.mark()
            SC = A.alloc([NT, 28]); t_SC = [Tk("SC")] * NT
            QN = A.alloc([NT, 2, 128], BF16); KN = A.alloc([NT, 2, 128], BF16); VV = A.alloc([NT, 2, 128], BF16)
            t_QKV = [[Tk() for _ in range(6)] for t in range(NT)]
            mA1 = A.mark()
            wst = A.alloc([8, 776]); t_wst = Tk()
            wq = A.alloc([8, 896], BF16); t_wq = Tk()
            S.op("sp", lambda e, sig: (sig(e.dma_start(out=wst[:, :, 0:768], in_=w_qkv.ap().rearrange("(k p) n -> p k n", p=128))),
                                       sig(e.dma_start(out=wst[:, :, 768:776], in_=w_ba.ap().rearrange("(k p) n -> p k n", p=128)))),
                 writes=[t_wst], dma=2, key="wst")
            S.op("pool", lambda e: e.tensor_copy(out=wq[:, :, 0:776], in_=wst), reads=[t_wst], writes=[t_wq])
            xt_A = [A.alloc([D]) for _ in range(3)]; t_xt_A = [Tk() for _ in range(3)]
            junk_A = A.alloc([D]); t_junk_A = Tk()
            ssq_A = [A.alloc([1]) for _ in range(3)]; t_ssq_A = [Tk() for _ in range(3)]
            xs_A = [A.alloc([8, 128], BF16) for _ in range(2)]; t_xs_A = [Tk(), Tk()]
            hxT = [A.alloc([8, 128], BF16) for _ in range(2)]; t_hxT = [Tk(), Tk()]; t_hxTb = [Tk(), Tk()]
            PRE = [A.alloc([6, 132]) for _ in range(3)]; t_PRE = [Tk() for _ in range(3)]
            CV = A.alloc([6, 128]); t_CVc = [Tk() for _ in range(6)]
            SQ = A.alloc([6, 128], BF16); t_SQ = Tk()
            sm = [A.alloc([40]) for _ in range(2)]; t_sm = [Tk(), Tk()]
            nr = [A.alloc([8]) for _ in range(2)]; t_nr = [Tk(), Tk()]

            wst_flat = wst.rearrange("p a b -> p (a b)")
            BA = wst_flat[:, 0:NT * 8].rearrange("p (a b) -> p a b", b=8); t_BA = Tk("BA")
            g66 = A.alloc([2, NT, 4]); t_g66 = Tk(); load(g66, gsc66[:, :, :, :], t_g66, "c3b")
            Mx = [wst_flat[:, 1024 + i_ * 512:1024 + i_ * 512 + NT * 4].rearrange("p (a b) -> p a b", b=4) for i_ in range(4)]; t_Mx = [Tk() for _ in range(4)]

            def rows_of(t):
                if t < 2:
                    return ctxb[t * 128:(t + 1) * 128, :]
                return xb[(t - 2) * 128:(t - 1) * 128, :]

            def front(t):
                s3 = t % 3; s2 = t % 2
                isctx = t < 2
                shc = modc[:, 2 if isctx else 0, :]; scc = modc[:, 3 if isctx else 1, :]
                S.op("sp", lambda e, sig: sig(e.dma_start(out=xt_A[s3], in_=rows_of(t))), writes=[t_xt_A[s3]], dma=1, key=f"xt_A{s3}")
                S.op("act", lambda e: e.activation(out=junk_A, in_=xt_A[s3], func=AF.Square, accum_out=ssq_A[s3]), reads=[t_xt_A[s3]], writes=[t_ssq_A[s3]])
                S.op("act", lambda e: e.activation(out=ssq_A[s3], in_=ssq_A[s3], func=AF.Sqrt, scale=1.0 / D, bias=EPS), reads=[t_ssq_A[s3]], writes=[t_ssq_A[s3]])
                S.op("dve", lambda e: e.reciprocal(out=ssq_A[s3], in_=ssq_A[s3]), reads=[t_ssq_A[s3]], writes=[t_ssq_A[s3]])
                S.op("pool", lambda e: e.tensor_scalar(out=xs_A[s2].rearrange("p a b -> p (a b)"), in0=xt_A[s3], scalar1=ssq_A[s3], scalar2=None, op0=ALU.mult),
                     reads=[t_xt_A[s3], t_ssq_A[s3]], writes=[t_xs_A[s2]])
                pp, tp = p512.get()
                ppb = pp.bitcast(BF16)

                def tr(e):
                    for k in range(8):
                        i = e.transpose(out=ppb[:, k * 128:(k + 1) * 128], in_=xs_A[s2][:, k, :], identity=identb)
                    return i
                S.op("pe", tr, reads=[t_xs_A[s2], t_identb], writes=[tp])

                def ev_d(e):
                    for k in range(8):
                        i = e.tensor_scalar(out=hxT[s2][:, k, :], in0=ppb[:, k * 128:(k + 1) * 128], scalar1=scc[:, k:k + 1], scalar2=shc[:, k:k + 1], op0=ALU.mult, op1=ALU.add)
                    return i
                t_h2 = t_hxTb[s2]
                S.op("dve", ev_d, reads=[tp, t_modc], writes=[t_hxT[s2], t_h2])
                if KCUT == 1:
                    return
                pA, tA = p512.get()
                pB, tB = p512.get()

                def pjA(e):
                    for ch in range(4):
                        for k in range(8):
                            i = e.matmul(pA[:, ch * 128:(ch + 1) * 128], lhsT=wq[:, k, ch * 128:(ch + 1) * 128], rhs=hxT[s2][:, k, :], start=(k == 0), stop=(k == 7))
                    return i

                def pjB(e):
                    for ch in range(4, 6):
                        for k in range(8):
                            i = e.matmul(pB[:, (ch - 4) * 128:(ch - 3) * 128], lhsT=wq[:, k, ch * 128:(ch + 1) * 128], rhs=hxT[s2][:, k, :], start=(k == 0), stop=(k == 7))
                    for k in range(8):
                        i = e.matmul(pB[:, 256:264], lhsT=hxT[s2][:, k, :], rhs=wq[:, k, 768:776], start=(k == 0), stop=(k == 7))
                    return i
                S.op("pe", pjA, reads=[t_wq, t_hxT[s2]], writes=[tA])
                S.op("pe", pjB, reads=[t_wq, t_hxT[s2]], writes=[tB])
                pba = pB[:, 256:264]; tba = tB
                S.op("dve", lambda e: e.tensor_copy(out=PRE[s3][:, 0:4, 2:130], in_=pA.rearrange("p (a b) -> p a b", a=4)), reads=[tA], writes=[t_PRE[s3]])
                S.op("dve", lambda e: e.tensor_copy(out=PRE[s3][:, 4:6, 2:130], in_=pB[:, 0:256].rearrange("p (a b) -> p a b", a=2)), reads=[tB], writes=[t_PRE[s3]])
                if KCUT == 2:
                    return
                first = t in (0, 2); last = t in (1, NT - 1)
                if first:
                    S.op("pool", lambda e: e.memset(PRE[s3][:, :, 0:2], 0.0), writes=[t_PRE[s3]])
                else:
                    sp_ = (t - 1) % 3
                    S.op("pool", lambda e: e.tensor_copy(out=PRE[sp_][:, :, 130:132], in_=PRE[s3][:, :, 2:4]), reads=[t_PRE[s3]], writes=[t_PRE[sp_]])
                if last:
                    S.op("pool", lambda e: e.memset(PRE[s3][:, :, 130:132], 0.0), writes=[t_PRE[s3]])
                else:
                    sn = (t + 1) % 3
                    S.op("pool", lambda e: e.tensor_copy(out=PRE[sn][:, :, 0:2], in_=PRE[s3][:, :, 128:130]), reads=[t_PRE[s3]], writes=[t_PRE[sn]])
                S.op("dve", lambda e: e.tensor_copy(out=BA[:, t, :], in_=pba), reads=[tba], writes=[t_BA])

            def lag(t):
                s3 = t % 3; s2 = t % 2
                for ch in range(6):
                    def cv(e, ch=ch):
                        i = e.tensor_scalar(out=CV[:, ch, :], in0=PRE[s3][:, ch, 0:128], scalar1=convc[:, ch, 0:1], scalar2=None, op0=ALU.mult)
                        for j in range(1, 5):
                            i = e.scalar_tensor_tensor(out=CV[:, ch, :], in0=PRE[s3][:, ch, j:j + 128], scalar=convc[:, ch, j:j + 1], in1=CV[:, ch, :], op0=ALU.mult, op1=ALU.add)
                        return i
                    tcv = t_CVc[ch]
                    for j in range(5):
                        def cvj(e, ch=ch, j=j):
                            if j == 0:
                                return e.tensor_scalar(out=CV[:, ch, :], in0=PRE[s3][:, ch, 0:128], scalar1=convc[:, ch, 0:1], scalar2=None, op0=ALU.mult)
                            return e.scalar_tensor_tensor(out=CV[:, ch, :], in0=PRE[s3][:, ch, j:j + 128], scalar=convc[:, ch, j:j + 1], in1=CV[:, ch, :], op0=ALU.mult, op1=ALU.add)
                        S.op("dve", cvj, reads=[t_PRE[s3], t_convc] + ([tcv] if j else []), writes=[tcv])
                S.op("act", lambda e: e.activation(out=SQ, in_=CV, func=AF.Silu), reads=t_CVc, writes=[t_SQ])
                pT, tT = p512.get()
                pTb = pT.bitcast(BF16)

                def trs(e):
                    for ch in range(6):
                        i = e.transpose(out=pTb[:, ch * 128:(ch + 1) * 128], in_=SQ[:, ch, :], identity=identb)
                    return i
                S.op("pe", trs, reads=[t_SQ, t_identb], writes=[tT])
                pts = [(pTb[:, ch * 128:(ch + 1) * 128], tT) for ch in range(6)]
                n = nr[s2]; tn = t_nr[s2]
                for ch in range(4):
                    S.op("act", lambda e, ch=ch: e.activation(out=junk_A[:, 0:128], in_=pts[ch][0], func=AF.Square, accum_out=n[:, ch:ch + 1]),
                         reads=[pts[ch][1]], writes=[tn])
                S.op("act", lambda e: e.activation(out=n[:, 0:4], in_=n[:, 0:4], func=AF.Sqrt, bias=EPS), reads=[tn], writes=[tn])
                S.op("dve", lambda e: e.reciprocal(out=n[:, 0:4], in_=n[:, 0:4]), reads=[tn], writes=[tn])
                S.op("dve", lambda e: e.tensor_scalar(out=n[:, 0:2], in0=n[:, 0:2], scalar1=128.0 ** -0.5, scalar2=None, op0=ALU.mult), reads=[tn], writes=[tn])
                dsts = [QN[:, t, 0, :], QN[:, t, 1, :], KN[:, t, 0, :], KN[:, t, 1, :], VV[:, t, 0, :], VV[:, t, 1, :]]
                for ch in range(6):
                    if ch < 4:
                        S.op("dve", lambda e, ch=ch: e.tensor_scalar(out=dsts[ch], in0=pts[ch][0], scalar1=n[:, ch:ch + 1], scalar2=None, op0=ALU.mult),
                             reads=[pts[ch][1], tn], writes=[t_QKV[t][ch]])
                    else:
                        S.op("act", lambda e, ch=ch: e.copy(out=dsts[ch], in_=pts[ch][0]), reads=[pts[ch][1]], writes=[t_QKV[t][ch]])

            for i in range(NTL + 1):
                if i < NTL:
                    front(i)
                if i >= 1 and KCUT in (0, 5):
                    lag(i - 1)
            tS = t_SC[0]
            SCk = lambda k: SC[:, :, k * 4:(k + 1) * 4]
            M0, M1, M2, M3 = Mx; tM0, tM1, tM2, tM3 = t_Mx
            S.op("act", lambda e: e.activation(out=g66[:, 0, :, :], in_=g66[:, 0, :, :], func=AF.Exp), reads=[t_g66], writes=[t_g66])
            S.op("act", lambda e: e.activation(out=SCk(3), in_=BA[:, :, 0:4], func=AF.Sigmoid), reads=[t_BA], writes=[tS])
            S.op("dve", lambda e: e.tensor_tensor(out=M0, in0=BA[:, :, 4:8], in1=g66[:, 1, :, :], op=ALU.add), reads=[t_BA, t_g66], writes=[tM0])
            S.op("dve", lambda e: e.tensor_scalar(out=M1, in0=M0, scalar1=-1.0, scalar2=None, op0=ALU.mult), reads=[tM0], writes=[tM1])
            S.op("dve", lambda e: e.tensor_tensor(out=M1, in0=M1, in1=M0, op=ALU.min), reads=[tM0, tM1], writes=[tM1])
            S.op("act", lambda e: e.activation(out=M1, in_=M1, func=AF.Exp), reads=[tM1], writes=[tM1])
            S.op("act", lambda e: e.activation(out=M1, in_=M1, func=AF.Ln, bias=1.0), reads=[tM1], writes=[tM1])
            S.op("dve", lambda e: e.tensor_scalar(out=M2, in0=M0, scalar1=0.0, scalar2=None, op0=ALU.max), reads=[tM0], writes=[tM2])
            S.op("dve", lambda e: e.tensor_tensor(out=M2, in0=M2, in1=M1, op=ALU.add), reads=[tM1, tM2], writes=[tM2])
            S.op("dve", lambda e: e.scalar_tensor_tensor(out=SCk(6), in0=M2, scalar=-1.0, in1=g66[:, 0, :, :], op0=ALU.mult, op1=ALU.mult), reads=[tM2, t_g66], writes=[tS])
            pcA, tcA = p512.get(); pcB, tcB = p512.get()

            def cums(e):
                e.matmul(pcA[:, 0:2 * NT], lhsT=Uf, rhs=SC[:, :, 24:26], start=True, stop=True)
                return e.matmul(pcA[:, 2 * NT:4 * NT], lhsT=Ub, rhs=SC[:, :, 26:28], start=True, stop=True)
            S.op("pe", cums, reads=[tS, t_trif], writes=[tcA])
            S.op("pe", lambda e: e.matmul(pcB[:, 0:4 * NT], lhsT=onesf, rhs=SC[:, :, 24:28], start=True, stop=True), reads=[tS, t_onesf], writes=[tcB])
            Gf_ps = pcA[:, 0:2 * NT].rearrange("p (a b) -> p a b", b=2); Gb_ps = pcA[:, 2 * NT:4 * NT].rearrange("p (a b) -> p a b", b=2)
            Gt_ps = pcB[:, 0:4 * NT].rearrange("p (a b) -> p a b", b=4)
            S.op("act", lambda e: e.activation(out=SC[:, :, 16:18], in_=Gf_ps, func=AF.Exp), reads=[tcA], writes=[tS])
            S.op("act", lambda e: e.activation(out=SC[:, :, 18:20], in_=Gb_ps, func=AF.Exp), reads=[tcA], writes=[tS])
            S.op("act", lambda e: e.activation(out=SCk(5), in_=Gt_ps, func=AF.Exp), reads=[tcB], writes=[tS])
            S.op("dve", lambda e: e.tensor_copy(out=M3[:, :, 0:2], in_=Gf_ps), reads=[tcA], writes=[tM3])
            S.op("dve", lambda e: e.tensor_copy(out=M3[:, :, 2:4], in_=Gb_ps), reads=[tcA], writes=[tM3])
            S.op("dve", lambda e: e.tensor_tensor(out=M3, in0=Gt_ps, in1=M3, op=ALU.subtract), reads=[tcB, tM3], writes=[tM3])
            S.op("act", lambda e: e.activation(out=SCk(2), in_=M3, func=AF.Exp), reads=[tM3], writes=[tS])
            S.op("dve", lambda e: e.tensor_scalar(out=SCk(0), in0=SCk(3), scalar1=-1.0, scalar2=None, op0=ALU.mult), reads=[tS], writes=[tS])
            S.op("dve", lambda e: e.tensor_tensor(out=SCk(1), in0=SCk(3), in1=SCk(4), op=ALU.mult), reads=[tS], writes=[tS])
            S.barrier()
            A.release(mA1)
            if stage == 1:
                for i_, t_ in enumerate((0, 1, 2, 3, 33, 65)):
                    dump(SC[:, t_, :], i_ * 32, 28)
                    dumpb(QN[:, t_, :, :].rearrange("p a b -> p (a b)"), 0, 128, i_ * 768, 256)
                    dumpb(KN[:, t_, :, :].rearrange("p a b -> p (a b)"), 0, 128, i_ * 768 + 256, 256)
                    dumpb(VV[:, t_, :, :].rearrange("p a b -> p (a b)"), 0, 128, i_ * 768 + 512, 256)
                raise _Stop()

            p128_small = p128
            p128 = Pool([(psb[b_][:, 0:128], Tk(f"p128x_{b_}", excl=True)) for b_ in range(8)])
            chains = [(hl, d) for hl in range(2) for d in range(2)]
            cb = {}
            for c in chains:
                b = {}
                for nm in ("knT", "qnT", "qdT", "X0", "XT0", "Xb", "XbT", "X0s", "XT0s", "X1s", "XT1s", "P0", "P1", "PT0", "PT1", "No", "NoT", "Wb", "Vb", "kbg", "kd", "vb", "nwT", "vnew", "QKm", "qd", "Sb"):
                    b[nm] = A.alloc([128], BF16); b["t_" + nm] = Tk(nm)
                for nm in ("gV", "gU", "E", "ET", "S"):
                    b[nm] = A.alloc([128]); b["t_" + nm] = Tk(nm)
                cb[c] = b
                S.op("pool", lambda e, b=b: e.memset(b["S"], 0.0), writes=[b["t_S"]])
                S.op("pool", lambda e, b=b: e.memset(b["Sb"], 0.0), writes=[b["t_Sb"]])
            OACC = A.alloc([64, 2, 128], BF16); t_OACC = [[Tk() for _ in range(2)] for _ in range(64)]
            ofin = [A.alloc([128]) for _ in range(2)]; t_ofin = [Tk(), Tk()]
            onb = [A.alloc([128], BF16) for _ in range(2)]; t_onb = [Tk(), Tk()]
            onT = [A.alloc([128], BF16) for _ in range(4)]; t_onT = [Tk() for _ in range(4)]
            fsm = [A.alloc([4]) for _ in range(2)]; t_fsm = [Tk(), Tk()]
            junk_B = A.alloc([128])
            visited = set()
            snd_ops = []
            fin_cnt = [0]
            evq = [0]

            def evac_copy(dst, src, rd, wr, scale=None):
                evq[0] += 1
                if evq[0] % 2 == 0:
                    if scale is None:
                        return S.op("act", lambda e: e.copy(out=dst, in_=src), reads=rd, writes=wr)
                    return S.op("act", lambda e: e.activation(out=dst, in_=src, func=AF.Copy, scale=scale), reads=rd, writes=wr)
                if scale is None:
                    return S.op("dve", lambda e: e.tensor_copy(out=dst, in_=src), reads=rd, writes=wr)
                return S.op("dve", lambda e: e.tensor_scalar(out=dst, in0=src, scalar1=scale, scalar2=None, op0=ALU.mult), reads=rd, writes=wr)

            def chain_step(c, t):
                hl, d = c
                b = cb[c]
                col = d * 2 + hl
                latent = t >= 2
                kn = KN[:, t, hl, :]; qn = QN[:, t, hl, :]; vv = VV[:, t, hl, :]
                tqq = t_QKV[t][hl]; tqk = t_QKV[t][2 + hl]; tqv = t_QKV[t][4 + hl]; tsc = t_SC[t]

                def scol(kind):
                    return SC[:, t, kind * 4 + col:kind * 4 + col + 1]
                U_, V_ = (Uf, Vf) if d == 0 else (Ub, Vb)
                negs = negb[:, 0 if d == 0 else 2, :]; negi = negb[:, 1 if d == 0 else 3, :]
                pk, tpk = p128.get()
                S.op("pe", lambda e: e.transpose(out=bfv(pk), in_=kn, identity=identb), reads=[tqk, t_identb], writes=[tpk])
                evac_copy(b["knT"], bfv(pk), [tpk], [b["t_knT"]])
                S.op("pool", lambda e: e.tensor_scalar(out=b["gV"], in0=V_, scalar1=scol(6), scalar2=None, op0=ALU.mult), reads=[t_trif, tsc], writes=[b["t_gV"]])
                S.op("pool", lambda e: e.tensor_scalar(out=b["gU"], in0=U_, scalar1=scol(6), scalar2=None, op0=ALU.mult), reads=[t_trif, tsc], writes=[b["t_gU"]])
                S.op("pool", lambda e: e.tensor_scalar(out=b["kbg"], in0=kn, scalar1=scol(1), scalar2=None, op0=ALU.mult), reads=[tqk, tsc], writes=[b["t_kbg"]])
                S.op("pool", lambda e: e.tensor_scalar(out=b["kd"], in0=kn, scalar1=scol(2), scalar2=None, op0=ALU.mult), reads=[tqk, tsc], writes=[b["t_kd"]])
                S.op("pool", lambda e: e.tensor_scalar(out=b["vb"], in0=vv, scalar1=scol(3), scalar2=None, op0=ALU.mult), reads=[tqv, tsc], writes=[b["t_vb"]])
                yield
                pd, tpd = p128.get()

                def dm(e):
                    e.matmul(pd, lhsT=identb, rhs=negs, start=True, stop=False)
                    return e.matmul(pd, lhsT=U_, rhs=b["gV"], start=False, stop=True)
                S.op("pe", dm, reads=[t_identb, t_negb, t_trif, b["t_gV"]], writes=[tpd])
                S.op("act", lambda e: e.activation(out=b["E"], in_=pd, func=AF.Exp), reads=[tpd], writes=[b["t_E"]])
                pkk, tpkk = p128.get()
                S.op("pe", lambda e: e.matmul(pkk, lhsT=b["knT"], rhs=b["knT"], start=True, stop=True), reads=[b["t_knT"]], writes=[tpkk])
                S.op("dve", lambda e: e.scalar_tensor_tensor(out=b["X0"], in0=pkk, scalar=scol(0), in1=b["E"], op0=ALU.mult, op1=ALU.mult),
                     reads=[tpkk, tsc, b["t_E"]], writes=[b["t_X0"]])
                if latent:
                    pdt, tpdt = p128.get()

                    def dmt(e):
                        e.matmul(pdt, lhsT=identb, rhs=negi, start=True, stop=False)
                        return e.matmul(pdt, lhsT=V_, rhs=b["gU"], start=False, stop=True)
                    S.op("pe", dmt, reads=[t_identb, t_negb, t_trif, b["t_gU"]], writes=[tpdt])
                    S.op("act", lambda e: e.activation(out=b["ET"], in_=pdt, func=AF.Exp), reads=[tpdt], writes=[b["t_ET"]])
                    pq_, tpq = p128.get()
                    S.op("pe", lambda e: e.transpose(out=bfv(pq_), in_=qn, identity=identb), reads=[tqq, t_identb], writes=[tpq])
                    evac_copy(b["qnT"], bfv(pq_), [tpq], [b["t_qnT"]])
                    S.op("pool", lambda e: e.tensor_scalar(out=b["qd"], in0=qn, scalar1=scol(4), scalar2=None, op0=ALU.mult), reads=[tqq, tsc], writes=[b["t_qd"]])
                yield
                px, tpx = p128.get()
                S.op("pe", lambda e: e.transpose(out=bfv(px), in_=b["X0"], identity=identb), reads=[b["t_X0"], t_identb], writes=[tpx])
                evac_copy(b["XT0"], bfv(px), [tpx], [b["t_XT0"]])
                if latent:
                    pqd, tpqd = p128.get()
                    S.op("pe", lambda e: e.transpose(out=bfv(pqd), in_=b["qd"], identity=identb), reads=[b["t_qd"], t_identb], writes=[tpqd])
                    evac_copy(b["qdT"], bfv(pqd), [tpqd], [b["t_qdT"]])
                    pqk, tpqk = p128.get()
                    S.op("pe", lambda e: e.matmul(pqk, lhsT=b["knT"], rhs=b["qnT"], start=True, stop=True), reads=[b["t_knT"], b["t_qnT"]], writes=[tpqk])
                    S.op("dve", lambda e: e.tensor_tensor(out=b["QKm"], in0=pqk, in1=b["ET"], op=ALU.mult), reads=[tpqk, b["t_ET"]], writes=[b["t_QKm"]])
                yield
                mk = lambda nm: (b[nm], b["t_" + nm])
                Xb, tXb = mk("Xb"); XbT, tXbT = mk("XbT")
                S.op("pool", lambda e: e.tensor_tensor(out=Xb, in0=b["X0"], in1=dcm[:, 0, :], op=ALU.mult), reads=[b["t_X0"], t_dcm], writes=[tXb])
                S.op("pool", lambda e: e.tensor_tensor(out=XbT, in0=b["XT0"], in1=dcm[:, 0, :], op=ALU.mult), reads=[b["t_XT0"], t_dcm], writes=[tXbT])
                S.op("pool", lambda e: e.tensor_tensor(out=b["P0"], in0=Xb, in1=identb, op=ALU.add), reads=[tXb, t_identb], writes=[b["t_P0"]])
                S.op("pool", lambda e: e.tensor_tensor(out=b["PT0"], in0=XbT, in1=identb, op=ALU.add), reads=[tXbT, t_identb], writes=[b["t_PT0"]])
                yield
                cur = 0
                cX, tcX, cXT, tcXT = Xb, tXb, XbT, tXbT
                for lev in range(2):
                    nX, tnX = mk(f"X{lev}s"); nXT, tnXT = mk(f"XT{lev}s")
                    p1, tp1 = p128.get()
                    S.op("pe", lambda e, p1=p1, cX=cX, cXT=cXT: e.matmul(p1, lhsT=cXT, rhs=cX, start=True, stop=True), reads=[tcX, tcXT], writes=[tp1])
                    evac_copy(nX, p1, [tp1], [tnX])
                    p2, tp2 = p128.get()
                    S.op("pe", lambda e, p2=p2, cX=cX, cXT=cXT: e.matmul(p2, lhsT=cX, rhs=cXT, start=True, stop=True), reads=[tcX, tcXT], writes=[tp2])
                    evac_copy(nXT, p2, [tp2], [tnXT])
                    yield
                    P = b[f"P{cur}"]; tP = b[f"t_P{cur}"]; nP = b[f"P{1 - cur}"]; tnP = b[f"t_P{1 - cur}"]
                    PT = b[f"PT{cur}"]; tPT = b[f"t_PT{cur}"]; nPT = b[f"PT{1 - cur}"]; tnPT = b[f"t_PT{1 - cur}"]
                    p3, tp3 = p128.get()
                    S.op("pe", lambda e, p3=p3, nXT=nXT, P=P: e.matmul(p3, lhsT=nXT, rhs=P, start=True, stop=True), reads=[tnXT, tP], writes=[tp3])
                    S.op("dve", lambda e, p3=p3, P=P, nP=nP: e.tensor_tensor(out=nP, in0=p3, in1=P, op=ALU.add), reads=[tp3, tP], writes=[tnP])
                    p4, tp4 = p128.get()
                    S.op("pe", lambda e, p4=p4, nX=nX, PT=PT: e.matmul(p4, lhsT=nX, rhs=PT, start=True, stop=True), reads=[tnX, tPT], writes=[tp4])
                    S.op("dve", lambda e, p4=p4, PT=PT, nPT=nPT: e.tensor_tensor(out=nPT, in0=p4, in1=PT, op=ALU.add), reads=[tp4, tPT], writes=[tnPT])
                    cur = 1 - cur
                    cX, tcX, cXT, tcXT = nX, tnX, nXT, tnXT
                    yield
                for li in range(4):
                    mi = 1 + 2 * li + (0 if d == 0 else 1)
                    miT = 1 + 2 * li + (1 if d == 0 else 0)
                    No, tNo = mk("No"); NoT, tNoT = mk("NoT")
                    P = b[f"P{cur}"]; tP = b[f"t_P{cur}"]; nP = b[f"P{1 - cur}"]; tnP = b[f"t_P{1 - cur}"]
                    PT = b[f"PT{cur}"]; tPT = b[f"t_PT{cur}"]; nPT = b[f"PT{1 - cur}"]; tnPT = b[f"t_PT{1 - cur}"]
                    S.op("pool", lambda e, mi=mi, No=No: e.tensor_tensor(out=No, in0=b["X0"], in1=dcm[:, mi, :], op=ALU.mult), reads=[b["t_X0"], t_dcm], writes=[tNo])
                    pw_, tpw_ = p128.get()
                    S.op("pe", lambda e, pw_=pw_, No=No, PT=PT: e.matmul(pw_, lhsT=No, rhs=PT, start=True, stop=True), reads=[tNo, tPT], writes=[tpw_])
                    Wb, tWb = mk("Wb")
                    evac_copy(Wb, pw_, [tpw_], [tWb])
                    if li < 3:
                        S.op("pool", lambda e, miT=miT, NoT=NoT: e.tensor_tensor(out=NoT, in0=b["XT0"], in1=dcm[:, miT, :], op=ALU.mult), reads=[b["t_XT0"], t_dcm], writes=[tNoT])
                        pv_, tpv_ = p128.get()
                        S.op("pe", lambda e, pv_=pv_, NoT=NoT, P=P: e.matmul(pv_, lhsT=NoT, rhs=P, start=True, stop=True), reads=[tNoT, tP], writes=[tpv_])
                        Vb_, tVb_ = mk("Vb")
                        evac_copy(Vb_, pv_, [tpv_], [tVb_])
                    yield
                    p5, tp5 = p128.get()
                    S.op("pe", lambda e, p5=p5, P=P, Wb=Wb: e.matmul(p5, lhsT=P, rhs=Wb, start=True, stop=True), reads=[tP, tWb], writes=[tp5])
                    S.op("dve", lambda e, p5=p5, PT=PT, nPT=nPT: e.tensor_tensor(out=nPT, in0=p5, in1=PT, op=ALU.add), reads=[tp5, tPT], writes=[tnPT])
                    if li < 3:
                        p6, tp6 = p128.get()
                        S.op("pe", lambda e, p6=p6, PT=PT, Vb_=Vb_: e.matmul(p6, lhsT=PT, rhs=Vb_, start=True, stop=True), reads=[tPT, tVb_], writes=[tp6])
                        S.op("dve", lambda e, p6=p6, P=P, nP=nP: e.tensor_tensor(out=nP, in0=p6, in1=P, op=ALU.add), reads=[tp6, tP], writes=[tnP])
                    cur = 1 - cur
                    yield
                TT = b[f"PT{cur}"]; tTT = b[f"t_PT{cur}"]
                pw, tpw = p128.get()
                S.op("pe", lambda e: e.matmul(pw, lhsT=b["kbg"], rhs=TT, start=True, stop=True), reads=[b["t_kbg"], tTT], writes=[tpw])
                evac_copy(b["nwT"], pw, [tpw], [b["t_nwT"]], scale=-1.0)
                yield
                pv, tpv = p128.get()

                def vn(e):
                    e.matmul(pv, lhsT=TT, rhs=b["vb"], start=True, stop=False)
                    return e.matmul(pv, lhsT=b["nwT"], rhs=b["Sb"], start=False, stop=True)
                S.op("pe", vn, reads=[tTT, b["t_vb"], b["t_nwT"], b["t_Sb"]], writes=[tpv])
                evac_copy(b["vnew"], pv, [tpv], [b["t_vnew"]])
                yield
                if latent:
                    lt = t - 2
                    po, tpo = p128.get()

                    def om(e):
                        e.matmul(po, lhsT=b["qdT"], rhs=b["Sb"], start=True, stop=False)
                        return e.matmul(po, lhsT=b["QKm"], rhs=b["vnew"], start=False, stop=True)
                    S.op("pe", om, reads=[b["t_qdT"], b["t_Sb"], b["t_QKm"], b["t_vnew"]], writes=[tpo])
                    if (lt, hl) not in visited:
                        visited.add((lt, hl))
                        evac_copy(OACC[:, lt, hl, :], po, [tpo], [t_OACC[lt][hl]])
                    else:
                        k2 = fin_cnt[0] % 2; k4 = fin_cnt[0] % 4
                        fin_cnt[0] += 1
                        of = ofin[k2]; tof = t_ofin[k2]; fs = fsm[k2]; tfs = t_fsm[k2]
                        S.op("dve", lambda e: e.tensor_tensor(out=of, in0=po, in1=OACC[:, lt, hl, :], op=ALU.add), reads=[tpo, t_OACC[lt][hl]], writes=[tof])
                        S.op("act", lambda e: e.activation(out=junk_B, in_=of, func=AF.Square, accum_out=fs[:, 0:1]), reads=[tof], writes=[tfs])
                        S.op("act", lambda e: e.activation(out=fs[:, 0:1], in_=fs[:, 0:1], func=AF.Sqrt, scale=1.0 / 128, bias=EPS), reads=[tfs], writes=[tfs])
                        S.op("dve", lambda e: e.reciprocal(out=fs[:, 0:1], in_=fs[:, 0:1]), reads=[tfs], writes=[tfs])
                        S.op("dve", lambda e: e.tensor_scalar(out=onb[k2], in0=of, scalar1=fs[:, 0:1], scalar2=None, op0=ALU.mult), reads=[tof, tfs], writes=[t_onb[k2]])
                        pt_, tpt = p128.get()
                        S.op("pe", lambda e: e.transpose(out=bfv(pt_), in_=onb[k2], identity=identb), reads=[t_onb[k2], t_identb], writes=[tpt])
                        evac_copy(onT[k4], bfv(pt_), [tpt], [t_onT[k4]])
                        o_ = S.op("pool", lambda e, sig: sig(e.dma_start(out=snd[lt // 16][hl * 128:(hl + 1) * 128, (lt % 16) * 64:(lt % 16 + 1) * 64], in_=onT[k4].bitcast(F32))),
                                  reads=[t_onT[k4]], dma=1, key=f"snd{k4}")
                        snd_ops.append(o_)
                ps_, tps = p128.get()
                S.op("pe", lambda e: e.matmul(ps_, lhsT=b["kd"], rhs=b["vnew"], start=True, stop=True), reads=[b["t_kd"], b["t_vnew"]], writes=[tps])
                S.op("dve", lambda e: e.scalar_tensor_tensor(out=b["S"], in0=b["S"], scalar=scol(5), in1=ps_, op0=ALU.mult, op1=ALU.add),
                     reads=[b["t_S"], tsc, tps], writes=[b["t_S"]])
                S.op("act", lambda e: e.copy(out=b["Sb"], in_=b["S"]), reads=[b["t_S"]], writes=[b["t_Sb"]])
                yield

            def bwd_tile(i):
                return 1 - i if i < 2 else NT + 1 - i

            for i in range(NT):
                gens = []
                for c in chains:
                    t = i if c[1] == 0 else bwd_tile(i)
                    gens.append(chain_step(c, t))
                alive = list(gens)
                while alive:
                    nxt = []
                    for g in alive:
                        try:
                            next(g)
                            nxt.append(g)
                        except StopIteration:
                            pass
                    alive = nxt
            if stage == 2:
                for i_ in range(4):
                    o_ = S.op("sp", lambda e, sig, i_=i_: sig(e.dma_start(out=dbgb[0:256, i_ * 2048:(i_ + 1) * 2048].bitcast(F32), in_=snd[i_][:, :])), extra=snd_ops, dma=1, key="dbg")
                    dump_ops.append(o_)
                for i_, c_ in enumerate(chains):
                    dump(cb[c_]["S"], i_ * 128, 128, reads=[cb[c_]["t_S"]])
                raise _Stop()
            p128 = p128_small
            ccs = []
            for j in range(4):
                ccs.append(S.op("pool", lambda e, sig, j=j: sig(e.collective_compute("AllGather", ALU.bypass, replica_groups=[[0, 1, 2, 3], [4, 5, 6, 7]],
                                                                                 ins=[snd[j].ap().opt()], outs=[rcv[j].ap().opt()])),
                                extra=snd_ops, dma=1, key=f"cc{j}", inc=1))
            S.barrier()
            A.release(mA)
            if stage == 3:
                raise _Stop()

            hxo = A.alloc([8, OWN], BF16); t_hxo = [Tk() for _ in range(NOT)]
            markH = A.mark()
            offS = A.mark()
            SZ = A.alloc([8, OWN], BF16); t_SZ = [[Tk() for _ in range(4)] for _ in range(8)]
            GU = A.alloc([8, OWN], BF16); t_GU = [[Tk() for _ in range(4)] for _ in range(8)]
            wstg = [A.alloc([8, 512]) for _ in range(2)]; t_wstg = [Tk(), Tk()]
            wbf = [A.alloc([8, 512], BF16) for _ in range(2)]; t_wbf = [Tk(), Tk()]
            mC1 = A.mark()
            xt_C = [A.alloc([D]) for _ in range(2)]; t_xt_C = [Tk(), Tk()]
            junk_C = A.alloc([D]); t_junk_C = Tk()
            ssq_C = [A.alloc([1]) for _ in range(2)]; t_ssq_C = [Tk(), Tk()]
            xs_C = [A.alloc([8, 128], BF16) for _ in range(2)]; t_xs_C = [Tk(), Tk()]
            for t in range(NOT):
                s2 = t % 2
                S.op("sp", lambda e, sig, t=t, s2=s2: sig(e.dma_start(out=xt_C[s2], in_=xo[t * 128:(t + 1) * 128, :])), writes=[t_xt_C[s2]], dma=1, key=f"cxt{s2}")
                S.op("act", lambda e, s2=s2: e.activation(out=junk_C, in_=xt_C[s2], func=AF.Square, accum_out=ssq_C[s2]), reads=[t_xt_C[s2]], writes=[t_ssq_C[s2]])
                S.op("act", lambda e, s2=s2: e.activation(out=ssq_C[s2], in_=ssq_C[s2], func=AF.Sqrt, scale=1.0 / D, bias=EPS), reads=[t_ssq_C[s2]], writes=[t_ssq_C[s2]])
                S.op("dve", lambda e, s2=s2: e.reciprocal(out=ssq_C[s2], in_=ssq_C[s2]), reads=[t_ssq_C[s2]], writes=[t_ssq_C[s2]])
                S.op("pool", lambda e, s2=s2: e.tensor_scalar(out=xs_C[s2].rearrange("p a b -> p (a b)"), in0=xt_C[s2], scalar1=ssq_C[s2], scalar2=None, op0=ALU.mult),
                     reads=[t_xt_C[s2], t_ssq_C[s2]], writes=[t_xs_C[s2]])
                pp, tp = p512.get()
                ppb = pp.bitcast(BF16)

                def tr(e, s2=s2, ppb=ppb):
                    for k in range(8):
                        i = e.transpose(out=ppb[:, k * 128:(k + 1) * 128], in_=xs_C[s2][:, k, :], identity=identb)
                    return i
                S.op("pe", tr, reads=[t_xs_C[s2], t_identb], writes=[tp])

                def ev_d(e, t=t, ppb=ppb):
                    for k in range(8):
                        i = e.tensor_scalar(out=hxo[:, k, t * 128:(t + 1) * 128], in0=ppb[:, k * 128:(k + 1) * 128], scalar1=modc[:, 1, k:k + 1], scalar2=modc[:, 0, k:k + 1], op0=ALU.mult, op1=ALU.add)
                    return i
                S.op("dve", ev_d, reads=[tp, t_modc], writes=[t_hxo[t]])
            S.barrier()
            A.release(mC1)
            wcnt = [0]

            def stream_w(src_ap_cols, ncols):
                s = wcnt[0] % 2
                wcnt[0] += 1
                v = src_ap_cols.rearrange("(k p) n -> p k n", p=128)
                S.op("sp", lambda e, sig: (sig(e.dma_start(out=wstg[s][:, 0:4, 0:ncols], in_=v[:, 0:4, :])), sig(e.dma_start(out=wstg[s][:, 4:8, 0:ncols], in_=v[:, 4:8, :]))),
                     writes=[t_wstg[s]], dma=2, key=f"wstg{s}")
                S.op("pool", lambda e: e.tensor_copy(out=wbf[s][:, :, 0:ncols], in_=wstg[s][:, :, 0:ncols]), reads=[t_wstg[s]], writes=[t_wbf[s]])
                return wbf[s], t_wbf[s]
            for cbk in range(4):
                wv, twv = stream_w(w_rest[:, cbk * 512:(cbk + 1) * 512], 512)
                dst, tdst, fn = (SZ, t_SZ, AF.Silu) if cbk < 2 else (GU, t_GU, AF.Gelu)
                for cc_ in range(4):
                    chn = (cbk % 2) * 4 + cc_
                    for tb in range(4):
                        pp, tp = p512.get()

                        def f(e, pp=pp, wv=wv, cc_=cc_, tb=tb):
                            for k in range(8):
                                i = e.matmul(pp, lhsT=wv[:, k, cc_ * 128:(cc_ + 1) * 128], rhs=hxo[:, k, tb * 512:(tb + 1) * 512], start=(k == 0), stop=(k == 7))
                            return i
                        S.op("pe", f, reads=[twv] + t_hxo[tb * 4:(tb + 1) * 4], writes=[tp])
                        S.op("act", lambda e, pp=pp, dst=dst, chn=chn, tb=tb, fn=fn: e.activation(out=dst[:, chn, tb * 512:(tb + 1) * 512], in_=pp, func=fn),
                             reads=[tp], writes=[tdst[chn][tb]])
            def cut4(k):
                if stage == 4 and int(os.environ.get("CUT4", "0")) == k:
                    S.barrier()
                    for i_, buf_ in enumerate((SZ, GU, hxo)):
                        for h_ in range(2):
                            dumpb(buf_[:, h_ * 4:(h_ + 1) * 4, :].rearrange("p a b -> p (a b)"), i_ * 256 + h_ * 128, 128, 0, SEQ)
                    raise _Stop()
            cut4(1)
            mV = A.mark()
            junk_V = A.alloc([D])
            wcnt[0] = 0
            wvh = [stream_w(w_rest[:, 2048 + hh * 512:2048 + (hh + 1) * 512], 512) for hh in range(2)]
            swf = A.alloc([8, 128]); t_swf = Tk(); load(swf, sgu_wT.ap().rearrange("g q p -> q g p"), t_swf, "c11")
            swb = A.alloc([8, 128], BF16); t_swb = Tk()
            S.op("pool", lambda e: e.tensor_copy(out=swb, in_=swf), reads=[t_swf], writes=[t_swb])
            lnbb = A.alloc([D]); t_lnbb = Tk(); load(lnbb, lnb_bc[:, :], t_lnbb, "c12")
            sbb = A.alloc([8, 128]); t_sbb = Tk(); load(sbb, sgu_bb[:, :, :], t_sbb, "c13")
            lngc = A.alloc([8]); t_lngc = Tk(); load(lngc, lng_c[:, :], t_lngc, "c14")
            BIAS = A.alloc([8, 128]); t_BIAS = Tk()
            for g in range(8):
                pp, tp = p128.get()
                S.op("pe", lambda e, pp=pp, g=g: e.matmul(pp, lhsT=lnbb[:, g * 128:(g + 1) * 128], rhs=swf[:, g, :], start=True, stop=True), reads=[t_lnbb, t_swf], writes=[tp])
                S.op("dve", lambda e, pp=pp, g=g: e.tensor_tensor(out=BIAS[:, g, :], in0=pp, in1=sbb[:, g, :], op=ALU.add), reads=[tp, t_sbb], writes=[t_BIAS])
            gv = [A.alloc([D])] * 2; t_gv = [Tk()] * 2
            vnb = [A.alloc([D], BF16) for _ in range(2)]; t_vnb = [Tk(), Tk()]
            lst = [A.alloc([8]) for _ in range(2)]; t_lst = [Tk(), Tk()]
            mtmp = [A.alloc([128]) for _ in range(2)]; t_mtmp = [Tk(), Tk()]
            for t in range(NOT):
                s2 = t % 2
                for hh in range(2):
                    pp, tp = p512.get()

                    def f(e, pp=pp, hh=hh, t=t):
                        for k in range(8):
                            i = e.matmul(pp, lhsT=hxo[:, k, t * 128:(t + 1) * 128], rhs=wvh[hh][0][:, k, :], start=(k == 0), stop=(k == 7))
                        return i
                    S.op("pe", f, reads=[wvh[hh][1], t_hxo[t]], writes=[tp])
                    S.op("act", lambda e, pp=pp, hh=hh, s2=s2: e.activation(out=gv[s2][:, hh * 512:(hh + 1) * 512], in_=pp, func=AF.Gelu), reads=[tp], writes=[t_gv[s2]])
                ls = lst[s2]; tls = t_lst[s2]
                S.op("dve", lambda e, s2=s2, ls=ls: e.tensor_reduce(out=ls[:, 0:1], in_=gv[s2], axis=mybir.AxisListType.X, op=ALU.add), reads=[t_gv[s2]], writes=[tls])
                S.op("act", lambda e, s2=s2, ls=ls: e.activation(out=junk_V, in_=gv[s2], func=AF.Square, accum_out=ls[:, 1:2]), reads=[t_gv[s2]], writes=[tls])
                S.op("dve", lambda e, ls=ls: e.tensor_scalar(out=ls[:, 0:2], in0=ls[:, 0:2], scalar1=1.0 / D, scalar2=None, op0=ALU.mult), reads=[tls], writes=[tls])
                S.op("dve", lambda e, ls=ls: e.tensor_tensor(out=ls[:, 2:3], in0=ls[:, 0:1], in1=ls[:, 0:1], op=ALU.mult), reads=[tls], writes=[tls])
                S.op("dve", lambda e, ls=ls: e.tensor_tensor(out=ls[:, 2:3], in0=ls[:, 1:2], in1=ls[:, 2:3], op=ALU.subtract), reads=[tls], writes=[tls])
                S.op("act", lambda e, ls=ls: e.activation(out=ls[:, 2:3], in_=ls[:, 2:3], func=AF.Sqrt, bias=EPS), reads=[tls], writes=[tls])
                S.op("dve", lambda e, ls=ls: e.reciprocal(out=ls[:, 2:3], in_=ls[:, 2:3]), reads=[tls], writes=[tls])
                S.op("dve", lambda e, s2=s2, ls=ls: e.tensor_scalar(out=vnb[s2], in0=gv[s2], scalar1=ls[:, 0:1], scalar2=ls[:, 2:3], op0=ALU.subtract, op1=ALU.mult),
                     reads=[t_gv[s2], tls], writes=[t_vnb[s2]])
                for g in range(8):
                    pp, tp = p128.get()
                    S.op("pe", lambda e, pp=pp, g=g, s2=s2: e.matmul(pp, lhsT=vnb[s2][:, g * 128:(g + 1) * 128], rhs=swb[:, g, :], start=True, stop=True),
                         reads=[t_vnb[s2], t_swb], writes=[tp])
                    mt = mtmp[g % 2]; tmt = t_mtmp[g % 2]
                    S.op("dve", lambda e, pp=pp, g=g, mt=mt: e.scalar_tensor_tensor(out=mt, in0=pp, scalar=lngc[:, g:g + 1], in1=BIAS[:, g, :], op0=ALU.mult, op1=ALU.add),
                         reads=[tp, t_lngc, t_BIAS], writes=[tmt])
                    S.op("pool", lambda e, g=g, t=t, mt=mt: e.tensor_tensor(out=GU[:, g, t * 128:(t + 1) * 128], in0=GU[:, g, t * 128:(t + 1) * 128], in1=mt, op=ALU.mult),
                         reads=[tmt, t_GU[g][t // 4]], writes=[t_GU[g][t // 4]])
            S.barrier()
            A.release(mV)
            cut4(2)
            mY = A.mark()
            rq = [A.alloc([4, 512], BF16) for _ in range(2)]; t_rq = [Tk(), Tk()]
            acc = [A.alloc([512]) for _ in range(2)]; t_acc = [Tk(), Tk()]
            cntr = 0
            for hp in range(4):
                for hl in range(2):
                    chn = hp * 2 + hl
                    for tb in range(4):
                        s = cntr % 2; cntr += 1
                        S.op("sp", lambda e, sig, s=s, chn=chn, tb=tb: tuple(sig(e.dma_start(out=rq[s][:, j, :].bitcast(F32), in_=rcv[j][chn * 128:(chn + 1) * 128, tb * 256:(tb + 1) * 256])) for j in range(4)),
                             extra=ccs, writes=[t_rq[s]], dma=4, key=f"rq{s}")
                        for j in range(4):
                            def f(e, s=s, j=j):
                                if j == 0:
                                    return e.tensor_scalar(out=acc[s], in0=rq[s][:, 0, :], scalar1=qmask[:, 0:1], scalar2=None, op0=ALU.mult)
                                return e.scalar_tensor_tensor(out=acc[s], in0=rq[s][:, j, :], scalar=qmask[:, j:j + 1], in1=acc[s], op0=ALU.mult, op1=ALU.add)
                            S.op("dve", f, reads=[t_rq[s], t_qmask, t_acc[s]] if j else [t_rq[s], t_qmask], writes=[t_acc[s]])
                        S.op("dve", lambda e, s=s, chn=chn, tb=tb: e.scalar_tensor_tensor(out=SZ[:, chn, tb * 512:(tb + 1) * 512], in0=acc[s], scalar=gngs[:, 0:1], in1=SZ[:, chn, tb * 512:(tb + 1) * 512], op0=ALU.mult, op1=ALU.mult),
                             reads=[t_acc[s], t_gngs, t_SZ[chn][tb]], writes=[t_SZ[chn][tb]])
            S.barrier()
            A.release(mY)
            cut4(3)
            S.barrier()
            mM = A.mark()
            MG = A.alloc([8, OWN], BF16); t_MG = [[Tk() for _ in range(4)] for _ in range(8)]
            sga = [A.alloc([512], BF16) for _ in range(2)]; t_sga = [Tk(), Tk()]
            sgb = [A.alloc([512], BF16) for _ in range(2)]; t_sgb = [Tk(), Tk()]
            m1 = [A.alloc([512]) for _ in range(2)]; t_m1 = [Tk(), Tk()]
            m2 = [A.alloc([512]) for _ in range(2)]; t_m2 = [Tk(), Tk()]
            sub = [(wstg[i][:, :, j * 128:(j + 1) * 128], wbf[i][:, :, j * 128:(j + 1) * 128], Tk(), Tk()) for i in range(2) for j in range(4)]
            subc = [0]

            def stream_small(src_cols):
                stg, bfw, tst, tbf = sub[subc[0] % 8]
                subc[0] += 1
                v = src_cols.rearrange("(k p) n -> p k n", p=128)
                S.op("sp", lambda e, sig: sig(e.dma_start(out=stg, in_=v)), writes=[tst], dma=1, key=f"sub{(subc[0] - 1) % 8}")
                S.op("pool", lambda e: e.tensor_copy(out=bfw, in_=stg), reads=[tst], writes=[tbf])
                return bfw, tbf
            cntr = 0
            for dc in range(8):
                ws = [stream_small(w_a[:, dc * 128:(dc + 1) * 128]), stream_small(w_b[:, dc * 128:(dc + 1) * 128]),
                      stream_small(w_rest[:, 3072 + dc * 128:3072 + (dc + 1) * 128]), stream_small(w_rest[:, 4096 + dc * 128:4096 + (dc + 1) * 128])]
                for tb in range(4):
                    s = cntr % 2; cntr += 1
                    outs = []
                    for wi, (src, tsrc) in enumerate(((SZ, t_SZ), (GU, t_GU), (hxo, None), (hxo, None))):
                        wv_, tw_ = ws[wi]
                        pp, tp = p512.get()

                        def f(e, pp=pp, wv_=wv_, src=src, tb=tb):
                            for k in range(8):
                                i = e.matmul(pp, lhsT=wv_[:, k, :], rhs=src[:, k, tb * 512:(tb + 1) * 512], start=(k == 0), stop=(k == 7))
                            return i
                        rds = [tw_] + ([tsrc[k][tb] for k in range(8)] if tsrc is not None else t_hxo[tb * 4:(tb + 1) * 4])
                        S.op("pe", f, reads=rds, writes=[tp])
                        outs.append((pp, tp))
                    S.op("act", lambda e, s=s, pp=outs[2][0]: e.activation(out=sga[s], in_=pp, func=AF.Sigmoid), reads=[outs[2][1]], writes=[t_sga[s]])
                    S.op("act", lambda e, s=s, pp=outs[3][0]: e.activation(out=sgb[s], in_=pp, func=AF.Sigmoid), reads=[outs[3][1]], writes=[t_sgb[s]])
                    S.op("dve", lambda e, s=s, pp=outs[0][0]: e.tensor_tensor(out=m1[s], in0=pp, in1=sga[s], op=ALU.mult), reads=[outs[0][1], t_sga[s]], writes=[t_m1[s]])
                    S.op("dve", lambda e, s=s, pp=outs[1][0]: e.tensor_tensor(out=m2[s], in0=pp, in1=sgb[s], op=ALU.mult), reads=[outs[1][1], t_sgb[s]], writes=[t_m2[s]])
                    S.op("pool", lambda e, s=s, dc=dc, tb=tb: e.tensor_tensor(out=MG[:, dc, tb * 512:(tb + 1) * 512], in0=m1[s], in1=m2[s], op=ALU.add),
                         reads=[t_m1[s], t_m2[s]], writes=[t_MG[dc][tb]])
            S.barrier()
            if stage == 4:
                for i_, (buf_, tk_) in enumerate(((SZ, t_SZ), (GU, t_GU), (MG, t_MG))):
                    for h_ in range(2):
                        dumpb(buf_[:, h_ * 4:(h_ + 1) * 4, :].rearrange("p a b -> p (a b)"), i_ * 256 + h_ * 128, 128, 0, SEQ)
                raise _Stop()
            hx2 = hxo; t_hx2 = [Tk() for _ in range(NOT)]
            Amain = A
            A = Arena(arena_t, 16384, base=offS)
            wo_b = A.alloc([8, D], BF16); t_wo_b = Tk()
            for hh in range(2):
                sl = hh
                S.op("sp", lambda e, sig, hh=hh, sl=sl: (sig(e.dma_start(out=wstg[sl][:, 0:4, :], in_=w_out.ap().rearrange("(k p) n -> p k n", p=128)[:, 0:4, hh * 512:(hh + 1) * 512])),
                                                        sig(e.dma_start(out=wstg[sl][:, 4:8, :], in_=w_out.ap().rearrange("(k p) n -> p k n", p=128)[:, 4:8, hh * 512:(hh + 1) * 512]))),
                     writes=[t_wstg[sl]], dma=2, key=f"wstg{sl}")
                for k in range(8):
                    S.op("pool", lambda e, k=k, hh=hh, sl=sl: e.tensor_tensor(out=wo_b[:, k, hh * 512:(hh + 1) * 512], in0=wstg[sl][:, k, :], in1=gmix[:, hh * 512:(hh + 1) * 512], op=ALU.mult),
                         reads=[t_wstg[sl], t_gmix], writes=[t_wo_b])
            rwf = A.alloc([8, 36]); t_rwf = Tk(); load(rwf, rw.ap().rearrange("(k p) n -> p k n", p=128), t_rwf, "c15")
            rbb = A.alloc([36]); t_rbb = Tk(); load(rbb, rb[:, :], t_rbb, "c16")
            x1 = [A.alloc([D]) for _ in range(2)]; t_x1 = [Tk(), Tk()]
            xt_D = [A.alloc([D]) for _ in range(2)]; t_xt_D = [Tk(), Tk()]
            junk_D = A.alloc([D]); t_junk_D = Tk()
            ssq_D = [A.alloc([1]) for _ in range(2)]; t_ssq_D = [Tk(), Tk()]
            xsf = [A.alloc([8, 128]) for _ in range(2)]; t_xsf = [Tk(), Tk()]
            hxf = [A.alloc([8, 128]) for _ in range(2)]; t_hxf = [Tk(), Tk()]
            rs_ = [A.alloc([64]) for _ in range(2)]; t_rs = [Tk(), Tk()]
            x1_ops = []
            for t in range(NOT):
                s2 = t % 2
                S.op("sp", lambda e, sig, t=t, s2=s2: sig(e.dma_start(out=xt_D[s2], in_=xo[t * 128:(t + 1) * 128, :])), writes=[t_xt_D[s2]], dma=1, key=f"dxt{s2}")
                for hh in range(2):
                    pp, tp = p512.get()

                    def f(e, pp=pp, hh=hh, t=t):
                        for k in range(8):
                            i = e.matmul(pp, lhsT=MG[:, k, t * 128:(t + 1) * 128], rhs=wo_b[:, k, hh * 512:(hh + 1) * 512], start=(k == 0), stop=(k == 7))
                        return i
                    S.op("pe", f, reads=[t_wo_b] + [t_MG[k][t // 4] for k in range(8)], writes=[tp])
                    S.op("dve", lambda e, pp=pp, hh=hh, s2=s2: e.tensor_tensor(out=x1[s2][:, hh * 512:(hh + 1) * 512], in0=pp, in1=xt_D[s2][:, hh * 512:(hh + 1) * 512], op=ALU.add),
                         reads=[tp, t_xt_D[s2]], writes=[t_x1[s2]])
                o_ = S.op("pool", lambda e, sig, t=t, s2=s2: sig(e.dma_start(out=x1d[t * 128:(t + 1) * 128, :], in_=x1[s2])), reads=[t_x1[s2]], dma=1, key=f"x1d{s2}")
                x1_ops.append(o_)
                S.op("act", lambda e, s2=s2: e.activation(out=junk_D, in_=x1[s2], func=AF.Square, accum_out=ssq_D[s2]), reads=[t_x1[s2]], writes=[t_ssq_D[s2]])
                S.op("act", lambda e, s2=s2: e.activation(out=ssq_D[s2], in_=ssq_D[s2], func=AF.Sqrt, scale=1.0 / D, bias=EPS), reads=[t_ssq_D[s2]], writes=[t_ssq_D[s2]])
                S.op("dve", lambda e, s2=s2: e.reciprocal(out=ssq_D[s2], in_=ssq_D[s2]), reads=[t_ssq_D[s2]], writes=[t_ssq_D[s2]])
                S.op("pool", lambda e, s2=s2: e.tensor_scalar(out=xsf[s2].rearrange("p a b -> p (a b)"), in0=x1[s2], scalar1=ssq_D[s2], scalar2=None, op0=ALU.mult),
                     reads=[t_x1[s2], t_ssq_D[s2]], writes=[t_xsf[s2]])
                for q4 in range(2):
                    pp, tp = p512.get()

                    def tr(e, pp=pp, q4=q4, s2=s2):
                        for k in range(4):
                            i = e.transpose(out=pp[:, k * 128:(k + 1) * 128], in_=xsf[s2][:, q4 * 4 + k, :], identity=identf)
                        return i
                    S.op("pe", tr, reads=[t_xsf[s2], t_identf], writes=[tp])

                    def ev(e, pp=pp, q4=q4, s2=s2, t=t):
                        for k in range(4):
                            kk = q4 * 4 + k
                            i = e.tensor_scalar(out=hxf[s2][:, kk, :], in0=pp[:, k * 128:(k + 1) * 128], scalar1=modc[:, 5, kk:kk + 1], scalar2=modc[:, 4, kk:kk + 1], op0=ALU.mult, op1=ALU.add)
                        return i
                    S.op("dve", ev, reads=[tp, t_modc], writes=[t_hxf[s2]])
                S.op("pool", lambda e, s2=s2, t=t: e.tensor_copy(out=hx2[:, :, t * 128:(t + 1) * 128], in_=hxf[s2]), reads=[t_hxf[s2]], writes=[t_hx2[t]])
                pr, tpr = p128.get()

                def rt(e, pr=pr, s2=s2):
                    for k in range(8):
                        i = e.matmul(pr[:, 0:36], lhsT=hxf[s2][:, k, :], rhs=rwf[:, k, :], start=(k == 0), stop=(k == 7))
                    return i
                S.op("pe", rt, reads=[t_hxf[s2], t_rwf], writes=[tpr])
                r = rs_[s2]; tr_ = t_rs[s2]
                S.op("dve", lambda e, pr=pr, r=r: e.tensor_tensor(out=r[:, 0:36], in0=pr[:, 0:36], in1=rbb, op=ALU.add), reads=[tpr, t_rbb], writes=[tr_])
                S.op("dve", lambda e, r=r: e.tensor_reduce(out=r[:, 40:41], in_=r[:, 0:4], axis=mybir.AxisListType.X, op=ALU.max), reads=[tr_], writes=[tr_])
                S.op("dve", lambda e, r=r: e.tensor_scalar(out=r[:, 36:40], in0=r[:, 0:4], scalar1=r[:, 40:41], scalar2=None, op0=ALU.is_ge), reads=[tr_], writes=[tr_])
                S.op("dve", lambda e, r=r: e.tensor_scalar(out=r[:, 60:64], in0=r[:, 0:4], scalar1=r[:, 40:41], scalar2=None, op0=ALU.subtract), reads=[tr_], writes=[tr_])
                S.op("act", lambda e, r=r: e.activation(out=r[:, 60:64], in_=r[:, 60:64], func=AF.Exp, accum_out=r[:, 41:42]), reads=[tr_], writes=[tr_])
                S.op("dve", lambda e, r=r: e.reciprocal(out=r[:, 41:42], in_=r[:, 41:42]), reads=[tr_], writes=[tr_])
                S.op("dve", lambda e, r=r: e.tensor_scalar(out=r[:, 42:50], in0=r[:, 4:12], scalar1=r[:, 36:37], scalar2=None, op0=ALU.mult), reads=[tr_], writes=[tr_])
                for g in range(1, 4):
                    S.op("dve", lambda e, r=r, g=g: e.scalar_tensor_tensor(out=r[:, 42:50], in0=r[:, 4 + 8 * g:12 + 8 * g], scalar=r[:, 36 + g:37 + g], in1=r[:, 42:50], op0=ALU.mult, op1=ALU.add),
                         reads=[tr_], writes=[tr_])
                S.op("dve", lambda e, r=r: e.tensor_reduce(out=r[:, 50:51], in_=r[:, 42:50], axis=mybir.AxisListType.X, op=ALU.max), reads=[tr_], writes=[tr_])
                S.op("dve", lambda e, r=r: e.tensor_scalar(out=r[:, 42:50], in0=r[:, 42:50], scalar1=r[:, 50:51], scalar2=None, op0=ALU.subtract), reads=[tr_], writes=[tr_])
                S.op("act", lambda e, r=r: e.activation(out=r[:, 42:50], in_=r[:, 42:50], func=AF.Exp), reads=[tr_], writes=[tr_])
                S.op("dve", lambda e, r=r: e.tensor_scalar(out=r[:, 52:60], in0=r[:, 42:50], scalar1=1.0, scalar2=None, op0=ALU.is_ge), reads=[tr_], writes=[tr_])
                S.op("dve", lambda e, r=r: e.scalar_tensor_tensor(out=r[:, 4:12], in0=r[:, 52:60], scalar=-2.0, in1=r[:, 42:50], op0=ALU.mult, op1=ALU.add), reads=[tr_], writes=[tr_])
                S.op("dve", lambda e, r=r: e.tensor_reduce(out=r[:, 51:52], in_=r[:, 4:12], axis=mybir.AxisListType.X, op=ALU.max), reads=[tr_], writes=[tr_])
                S.op("dve", lambda e, r=r: e.tensor_scalar(out=r[:, 12:20], in0=r[:, 4:12], scalar1=r[:, 51:52], scalar2=None, op0=ALU.is_ge), reads=[tr_], writes=[tr_])
                S.op("dve", lambda e, r=r: e.scalar_tensor_tensor(out=r[:, 20:28], in0=r[:, 12:20], scalar=r[:, 51:52], in1=r[:, 52:60], op0=ALU.mult, op1=ALU.add), reads=[tr_], writes=[tr_])
                S.op("dve", lambda e, r=r: e.tensor_scalar(out=r[:, 50:51], in0=r[:, 51:52], scalar1=1.0, scalar2=None, op0=ALU.add), reads=[tr_], writes=[tr_])
                S.op("dve", lambda e, r=r: e.reciprocal(out=r[:, 50:51], in_=r[:, 50:51]), reads=[tr_], writes=[tr_])
                S.op("dve", lambda e, r=r: e.tensor_tensor(out=r[:, 50:51], in0=r[:, 50:51], in1=r[:, 41:42], op=ALU.mult), reads=[tr_], writes=[tr_])
                S.op("dve", lambda e, r=r: e.tensor_scalar(out=r[:, 20:28], in0=r[:, 20:28], scalar1=r[:, 50:51], scalar2=None, op0=ALU.mult), reads=[tr_], writes=[tr_])
                for g in range(4):
                    S.op("dve", lambda e, r=r, g=g, t=t: e.tensor_scalar(out=GT[:, t, g * 8:(g + 1) * 8], in0=r[:, 20:28], scalar1=r[:, 36 + g:37 + g], scalar2=None, op0=ALU.mult),
                         reads=[tr_], writes=[t_GT[t]])
            S.barrier()
            A = Amain
            A.release(markH)
            if stage == 5:
                dump(GT.rearrange("p a b -> p (a b)"), 0, 512)
                for i_ in range(4):
                    o_ = S.op("sp", lambda e, sig, i_=i_: sig(e.dma_start(out=y[i_ * 512:(i_ + 1) * 512, :], in_=x1d[i_ * 512:(i_ + 1) * 512, :])), extra=x1_ops, dma=1, key="dbg")
                    dump_ops.append(o_)
                dumpb(hx2[:, 0:4, :].rearrange("p a b -> p (a b)"), 0, 128, 0, SEQ)
                raise _Stop()
            ACC = A.alloc([NOT, D]); t_ACC = [[Tk(), Tk()] for _ in range(NOT)]
            for t in range(NOT):
                S.op("pool", lambda e, t=t: e.memset(ACC[:, t, :], 0.0), writes=t_ACC[t])
            e1f = A.alloc([8, 512]); t_e1f = Tk()
            e2f = A.alloc([2, D]); t_e2f = Tk()
            e1b = [A.alloc([8, 512], BF16) for _ in range(2)]; t_e1b = [Tk(), Tk()]
            e2b = [A.alloc([2, D], BF16) for _ in range(2)]; t_e2b = [Tk(), Tk()]
            sil = [A.alloc([512], BF16) for _ in range(2)]; t_sil = [Tk(), Tk()]
            hid = [A.alloc([512], BF16) for _ in range(4)]; t_hid = [Tk() for _ in range(4)]
            hc = 0
            for ex in range(NE):
                s = ex % 2
                v1 = ew1[ex].rearrange("(k p) n -> p k n", p=128); v3 = ew3[ex].rearrange("(k p) n -> p k n", p=128)
                v2 = ew2[ex].rearrange("(k p) n -> p k n", p=128)
                S.op("sp", lambda e, sig, v1=v1, v3=v3: (sig(e.dma_start(out=e1f[:, :, 0:256], in_=v1)), sig(e.dma_start(out=e1f[:, :, 256:512], in_=v3))), writes=[t_e1f], dma=2, key="e1f")
                S.op("sp", lambda e, sig, v2=v2: sig(e.dma_start(out=e2f, in_=v2)), writes=[t_e2f], dma=1, key="e2f")
                S.op("pool", lambda e, s=s: e.tensor_copy(out=e1b[s], in_=e1f), reads=[t_e1f], writes=[t_e1b[s]])
                S.op("pool", lambda e, s=s: e.tensor_copy(out=e2b[s], in_=e2f), reads=[t_e2f], writes=[t_e2b[s]])
                for tb in range(4):
                    hs = []
                    for fc in range(2):
                        p1, tp1 = p512.get(); p3, tp3 = p512.get()

                        def f1(e, p1=p1, s=s, fc=fc, tb=tb):
                            for k in range(8):
                                i = e.matmul(p1, lhsT=e1b[s][:, k, fc * 128:(fc + 1) * 128], rhs=hx2[:, k, tb * 512:(tb + 1) * 512], start=(k == 0), stop=(k == 7))
                            return i

                        def f3(e, p3=p3, s=s, fc=fc, tb=tb):
                            for k in range(8):
                                i = e.matmul(p3, lhsT=e1b[s][:, k, 256 + fc * 128:256 + (fc + 1) * 128], rhs=hx2[:, k, tb * 512:(tb + 1) * 512], start=(k == 0), stop=(k == 7))
                            return i
                        S.op("pe", f1, reads=[t_e1b[s]] + t_hx2[tb * 4:(tb + 1) * 4], writes=[tp1])
                        S.op("pe", f3, reads=[t_e1b[s]] + t_hx2[tb * 4:(tb + 1) * 4], writes=[tp3])
                        ss = hc % 2; h4 = hc % 4; hc += 1
                        S.op("act", lambda e, p1=p1, ss=ss: e.activation(out=sil[ss], in_=p1, func=AF.Silu), reads=[tp1], writes=[t_sil[ss]])
                        S.op("dve", lambda e, p3=p3, ss=ss, h4=h4: e.tensor_tensor(out=hid[h4], in0=p3, in1=sil[ss], op=ALU.mult), reads=[tp3, t_sil[ss]], writes=[t_hid[h4]])
                        hs.append(h4)
                    for tt in range(4):
                        t = tb * 4 + tt
                        for hh in range(2):
                            po, tpo = p512.get()

                            def f2(e, po=po, s=s, tt=tt, hh=hh, hs=tuple(hs)):
                                for fc in range(2):
                                    i = e.matmul(po, lhsT=hid[hs[fc]][:, tt * 128:(tt + 1) * 128], rhs=e2b[s][:, fc, hh * 512:(hh + 1) * 512], start=(fc == 0), stop=(fc == 1))
                                return i
                            S.op("pe", f2, reads=[t_hid[hs[0]], t_hid[hs[1]], t_e2b[s]], writes=[tpo])
                            S.op("dve", lambda e, po=po, t=t, hh=hh, ex=ex: e.scalar_tensor_tensor(out=ACC[:, t, hh * 512:(hh + 1) * 512], in0=po, scalar=GT[:, t, ex:ex + 1], in1=ACC[:, t, hh * 512:(hh + 1) * 512], op0=ALU.mult, op1=ALU.add),
                                 reads=[tpo, t_ACC[t][hh]], writes=[t_ACC[t][hh]])
            fngb = A.alloc([D]); t_fngb = Tk(); load(fngb, fng_bc[:, :], t_fngb, "c17")
            x1r = [A.alloc([D]) for _ in range(2)]; t_x1r = [Tk(), Tk()]
            x2 = [A.alloc([D]) for _ in range(2)]; t_x2 = [Tk(), Tk()]
            junk_E = A.alloc([D]); t_junk_E = Tk()
            ssq_E = [A.alloc([1]) for _ in range(2)]; t_ssq_E = [Tk(), Tk()]
            outs_ = []
            for t in range(NOT):
                s2 = t % 2
                S.op("sp", lambda e, sig, t=t, s2=s2: sig(e.dma_start(out=x1r[s2], in_=x1d[t * 128:(t + 1) * 128, :])), extra=[x1_ops[t]], writes=[t_x1r[s2]], dma=1, key=f"x1r{s2}")
                S.op("dve", lambda e, t=t, s2=s2: e.tensor_tensor(out=x2[s2], in0=ACC[:, t, :], in1=gffn, op=ALU.mult), reads=t_ACC[t] + [t_gffn], writes=[t_x2[s2]])
                S.op("dve", lambda e, s2=s2: e.tensor_tensor(out=x2[s2], in0=x2[s2], in1=x1r[s2], op=ALU.add), reads=[t_x2[s2], t_x1r[s2]], writes=[t_x2[s2]])
                S.op("act", lambda e, s2=s2: e.activation(out=junk_E, in_=x2[s2], func=AF.Square, accum_out=ssq_E[s2]), reads=[t_x2[s2]], writes=[t_ssq_E[s2]])
                S.op("act", lambda e, s2=s2: e.activation(out=ssq_E[s2], in_=ssq_E[s2], func=AF.Sqrt, scale=1.0 / D, bias=EPS), reads=[t_ssq_E[s2]], writes=[t_ssq_E[s2]])
                S.op("dve", lambda e, s2=s2: e.reciprocal(out=ssq_E[s2], in_=ssq_E[s2]), reads=[t_ssq_E[s2]], writes=[t_ssq_E[s2]])
                S.op("dve", lambda e, s2=s2: e.scalar_tensor_tensor(out=x2[s2], in0=x2[s2], scalar=ssq_E[s2], in1=fngb, op0=ALU.mult, op1=ALU.mult), reads=[t_x2[s2], t_ssq_E[s2], t_fngb], writes=[t_x2[s2]])
                o_ = S.op("sp", lambda e, sig, t=t, s2=s2: sig(e.dma_start(out=y[t * 128:(t + 1) * 128, :], in_=x2[s2])), reads=[t_x2[s2]], dma=1, key=f"yo{s2}")
                outs_.append(o_)
            S.op("sp", None, extra=outs_)
        try:
            author()
        except _Stop:
            pass
        if dump_ops:
            S.op("sp", None, extra=dump_ops)
        S.emit(block, sems)
    return nc


def _prep(inputs):
    f = lambda a: np.ascontiguousarray(np.asarray(a, dtype=np.float32))
    x = f(inputs["x"]); c = f(inputs["c"]); ctx = f(inputs["ctx"]); c_ctx = f(inputs["c_ctx"])
    w_in = f(inputs["w_in"])[0]
    conv_w = f(inputs["conv_w"])[0]
    a_log = f(inputs["a_log"])[0]; dt_bias = f(inputs["dt_bias"])[0]
    idx = np.arange(128)
    tri = np.stack([(idx[:, None] <= idx[None, :]), (idx[:, None] > idx[None, :]), (idx[:, None] >= idx[None, :]), (idx[:, None] < idx[None, :])]).astype(np.float32)
    negm = np.stack([(idx[:, None] <= idx[None, :]), (idx[None, :] < idx[:, None]), (idx[:, None] >= idx[None, :]), (idx[None, :] > idx[:, None])]).astype(np.float32) * -100.0
    blk = lambda n: (idx[:, None] // n == idx[None, :] // n)
    dcm = [blk(8)]
    for n in (16, 32, 64, 128):
        low = blk(n) & ((idx[:, None] % n) >= n // 2) & ((idx[None, :] % n) < n // 2)
        dcm += [low, low.T]
    import ml_dtypes
    dcm = np.stack(dcm).astype(np.float32).astype(ml_dtypes.bfloat16)
    common = {
        "dcm": dcm,
        "ada_w": f(inputs["ada_w"])[0], "ada_b": f(inputs["ada_b"]).reshape(1, -1),
        "gcols": np.ascontiguousarray(np.stack([f(inputs["norm_mix_g"])[0].reshape(8, 128).T, f(inputs["norm_ffn_g"])[0].reshape(8, 128).T], axis=-1)),
        "gng": f(inputs["gdn_norm_g"])[0].reshape(128, 1),
        "ident": np.eye(128, dtype=np.float32), "tri": tri, "negm": negm,
        "w_rest": np.ascontiguousarray(w_in[:, COL_Z:]),
        "sgu_wT": np.ascontiguousarray(f(inputs["sgu_w"])[0].transpose(0, 2, 1)),
        "sgu_bb": np.ascontiguousarray(np.broadcast_to(f(inputs["sgu_b"])[0][None], (128, 8, 128))),
        "lng_c": np.ascontiguousarray(f(inputs["sgu_ln_g"])[0].reshape(8, 128).T),
        "lnb_bc": np.ascontiguousarray(np.broadcast_to(f(inputs["sgu_ln_b"])[0][None], (128, D))),
        "w_a": f(inputs["w_branch_a"])[0], "w_b": f(inputs["w_branch_b"])[0], "w_out": f(inputs["w_out"])[0],
        "rw": np.ascontiguousarray(np.concatenate([f(inputs["router_group_w"])[0], f(inputs["router_expert_w"])[0]], axis=1)),
        "rb": np.ascontiguousarray(np.broadcast_to(np.concatenate([f(inputs["router_group_b"])[0], f(inputs["router_expert_b"])[0]])[None], (128, 36))),
        "ew1": f(inputs["expert_w1"])[0], "ew3": f(inputs["expert_w3"])[0], "ew2": f(inputs["expert_w2"])[0],
        "fng_bc": np.ascontiguousarray(np.broadcast_to(f(inputs["final_norm_g"])[None], (128, D))),
    }
    maps = []
    for core in range(8):
        b, r = core // 4, core % 4
        heads = (2 * r, 2 * r + 1)
        qcols = np.concatenate([np.arange(base + h * 128, base + (h + 1) * 128) for base in (0, 1024, 2048) for h in heads])
        bacols = np.array([COL_BETA + d * 8 + h for d in range(2) for h in heads] + [COL_A + d * 8 + h for d in range(2) for h in heads])
        conv_c = np.ascontiguousarray(conv_w[:, qcols].reshape(5, 6, 128).transpose(2, 1, 0))
        gsc = np.concatenate([np.array([a_log[d, h] for d in range(2) for h in heads]), np.array([dt_bias[d, h] for d in range(2) for h in heads])]).astype(np.float32)
        qm = np.zeros((128, 4), np.float32); qm[:, r] = 1.0
        m = dict(common)
        m.update({
            "xb": x[b], "ctxb": ctx[b], "xo": np.ascontiguousarray(x[b, r * OWN:(r + 1) * OWN]),
            "cvT": np.ascontiguousarray(np.stack([c[b].reshape(8, 128).T, c_ctx.reshape(8, 128).T], axis=-1)),
            "w_qkv": np.ascontiguousarray(w_in[:, qcols]), "w_ba": np.ascontiguousarray(w_in[:, bacols]),
            "gsc66": np.ascontiguousarray(np.broadcast_to(gsc.reshape(1, 2, 1, 4), (128, 2, NT, 4))),
            "conv_c": conv_c, "gsc": np.ascontiguousarray(np.broadcast_to(gsc[None], (128, 8))), "qmask": qm,
        })
        maps.append(m)
    return maps


_NC = None


def kernel(**inputs):
    global _NC
    if _NC is None:
        _NC = build_program()
    maps = _prep(inputs)
    res = run_bass_kernel_spmd(_NC, maps, core_ids=list(range(8)))
    out = np.zeros((2, SEQ, D), np.float32)
    for core in range(8):
        b, r = core // 4, core % 4
        out[b, r * OWN:(r + 1) * OWN] = np.asarray(res.results[core]["y"], dtype=np.float32)
    return out
```

```python
import contextlib
import os
import numpy as np
import concourse.bass as bass
import concourse.mybir as mybir
from concourse.bass_utils import run_bass_kernel_spmd

F32 = mybir.dt.float32
BF16 = mybir.dt.bfloat16
ALU = mybir.AluOpType
AF = mybir.ActivationFunctionType

D = 1024
SEQ = 8192
CTX = 256
NT = 66
OWN = 2048
NOT = 16
NE = 32
DE = 256
COL_BETA = 3072
COL_A = COL_BETA + 16
COL_Z = COL_A + 16
EPS = 1e-6
ARENA = 53000


SAME_ENGINE_WAITS = os.environ.get('SAMEENG', '1') == '1'


class Tk:
    __slots__ = ("name", "w", "rd", "excl")

    def __init__(self, name="", excl=False):
        self.name = name
        self.w = None
        self.rd = []
        self.excl = excl


class Op:
    __slots__ = ("eng", "fn", "deps", "used", "sem", "val", "dma", "key", "idx", "inc")


class Sched:
    ENGS = ("pe", "act", "dve", "pool", "sp")

    def __init__(self, nc):
        self.nc = nc
        self.ops = {e: [] for e in self.ENGS}
        self.all = []
        self.dma_since_barrier = []

    def op(self, eng, fn, reads=(), writes=(), dma=0, key=None, extra=(), inc=16):
        o = Op()
        o.eng = eng; o.fn = fn; o.used = False; o.sem = None; o.val = None
        o.dma = dma; o.key = key; o.idx = len(self.all); o.inc = inc
        deps = set(extra)
        reads = list(reads); writes = list(writes)
        for r in list(reads):
            if r.excl and r not in writes:
                writes.append(r)
        for r in reads:
            if r.w is not None:
                deps.add(r.w)
        for w in writes:
            if w.w is not None:
                deps.add(w.w)
            for x in w.rd:
                deps.add(x)
        for r in reads:
            r.rd.append(o)
        for w in writes:
            w.w = o
            w.rd = []
        o.deps = [d for d in deps if d is not o and not (d.eng == "pe" and eng == "pe" and not d.dma and not dma)
                  and not (SAME_ENGINE_WAITS is False and d.eng == eng and eng in ("act", "dve", "pool") and not d.dma and not dma)]
        for d in o.deps:
            d.used = True
        if dma:
            assert key is not None
            self.dma_since_barrier.append(o)
        self.ops[eng].append(o)
        self.all.append(o)
        return o

    def barrier(self):
        last = [self.ops[e][-1] for e in self.ENGS if self.ops[e] and self.ops[e][-1].fn is not None]
        dmas = list(self.dma_since_barrier)
        self.dma_since_barrier = []
        for e in self.ENGS:
            self.op(e, None, extra=[x for x in last if x.eng != e] + dmas)

    def emit(self, block, sems):
        nc = self.nc
        sems = list(sems)
        eng_sem = {e: sems.pop() for e in ("pe", "act", "dve", "pool")}
        keysem = {}
        cnt = {e: 0 for e in eng_sem}
        kcnt = {}
        for o in self.all:
            if o.dma:
                if o.key not in keysem:
                    keysem[o.key] = sems.pop()
                    kcnt[o.key] = 0
                kcnt[o.key] += o.inc * o.dma
                o.sem = keysem[o.key]; o.val = kcnt[o.key]
            elif o.used:
                assert o.fn is not None
                cnt[o.eng] += 1
                o.sem = eng_sem[o.eng]; o.val = cnt[o.eng]
        engobj = {"pe": nc.tensor, "act": nc.scalar, "dve": nc.vector, "pool": nc.gpsimd, "sp": nc.sync}
        deco = {"pe": block.tensor, "act": block.scalar, "dve": block.vector, "pool": block.gpsimd, "sp": block.sync}

        def run(ename):
            def body(e):
                known = {}
                for o in self.ops[ename]:
                    for d in sorted(o.deps, key=lambda x: x.idx):
                        sid = id(d.sem)
                        if known.get(sid, 0) >= d.val:
                            continue
                        e.wait_ge(d.sem, d.val)
                        known[sid] = d.val
                    if o.fn is None:
                        continue
                    if o.dma:
                        n = [0]

                        def sig(inst, o=o, n=n):
                            if o.inc == 16:
                                inst.then_inc(o.sem, 16)
                            else:
                                inst.then_inc(o.sem)
                            n[0] += 1
                            return inst
                        o.fn(e, sig)
                        assert n[0] == o.dma, (n[0], o.dma)
                    else:
                        inst = o.fn(e)
                        if o.used:
                            inst.then_inc(o.sem, 1)
            return body
        for ename in self.ENGS:
            deco[ename](run(ename))


class Arena:
    def __init__(self, ap_f32, nf32, base=0):
        self.ap = ap_f32
        self.n = base + nf32
        self.off = base
        self.hi = 0

    def mark(self):
        return self.off

    def release(self, m):
        self.off = m

    def alloc(self, free_shape, dtype=F32):
        n = int(np.prod(free_shape))
        nf = n if dtype == F32 else (n + 1) // 2
        nf = (nf + 1) // 2 * 2
        assert self.off + nf <= self.n, ("arena overflow", self.off, nf, self.n)
        v = self.ap[:, self.off:self.off + nf]
        self.off += nf
        self.hi = max(self.hi, self.off)
        if dtype != F32:
            v = v.bitcast(dtype)[:, 0:n]
        else:
            v = v[:, 0:n]
        if len(free_shape) == 2:
            v = v.rearrange("p (a b) -> p a b", a=free_shape[0])
        elif len(free_shape) == 3:
            v = v.rearrange("p (a b c) -> p a b c", a=free_shape[0], b=free_shape[1])
        return v


class _Stop(Exception):
    pass


def build_program(debug=False, stage=99):
    KCUT = int(os.environ.get('KCUT', '0')); NTL = int(os.environ.get('NTL', str(NT)))
    nc = bass.Bass("TRN2", target_bir_lowering=False)

    def din(name, shape, dt=F32):
        return nc.dram_tensor(name, list(shape), dt, kind="ExternalInput")

    xb = din("xb", [SEQ, D]); ctxb = din("ctxb", [CTX, D]); xo = din("xo", [OWN, D])
    cvT = din("cvT", [128, 8, 2]); ada_w = din("ada_w", [D, 6 * D]); ada_b = din("ada_b", [1, 6 * D])
    gcols = din("gcols", [128, 8, 2])
    w_qkv = din("w_qkv", [D, 768]); w_ba = din("w_ba", [D, 8])
    gsc66 = din("gsc66", [128, 2, NT, 4])
    conv_c = din("conv_c", [128, 6, 5]); gsc = din("gsc", [128, 8]); gng = din("gng", [128, 1])
    dcm_d = din("dcm", [9, 128, 128], BF16)
    ident_d = din("ident", [128, 128]); tri_d = din("tri", [4, 128, 128]); neg_d = din("negm", [4, 128, 128])
    w_rest = din("w_rest", [D, 5120]); sgu_wT = din("sgu_wT", [8, 128, 128]); sgu_bb = din("sgu_bb", [128, 8, 128])
    lng_c = din("lng_c", [128, 8]); lnb_bc = din("lnb_bc", [128, D])
    w_a = din("w_a", [D, D]); w_b = din("w_b", [D, D]); w_out = din("w_out", [D, D])
    rw = din("rw", [D, 36]); rb = din("rb", [128, 36])
    ew1 = din("ew1", [NE, D, DE]); ew3 = din("ew3", [NE, D, DE]); ew2 = din("ew2", [NE, DE, D])
    fng_bc = din("fng_bc", [128, D]); qmask_d = din("qmask", [128, 4])
    y = nc.dram_tensor("y", [OWN, D], F32, kind="ExternalOutput")
    snd = [nc.dram_tensor(f"snd{j}", [2 * 128, 1024], F32) for j in range(4)]
    rcv = [nc.dram_tensor(f"rcv{j}", [4 * 2 * 128, 1024], F32) for j in range(4)]
    x1d = nc.dram_tensor("x1d", [OWN, D], F32)
    dbg = None
    if debug:
        dbg = nc.dram_tensor("dbg", [128, 4096], F32, kind="ExternalOutput")
        dbgb = nc.dram_tensor("dbgb", [768, SEQ], BF16, kind="ExternalOutput")

    es = contextlib.ExitStack()
    with es:
        arena_t = es.enter_context(nc.sbuf_tensor("arena", [128, ARENA], F32))
        psb = [es.enter_context(nc.psum_tensor(f"psb{i}", [128, 512], F32)) for i in range(8)]
        sems = [es.enter_context(nc.semaphore(f"s{i}")) for i in range(100)]
        block = es.enter_context(nc.Block())
        A = Arena(arena_t, ARENA)
        S = Sched(nc)

        dump_ops = []

        def dump(ap, col0, ncols, reads=()):
            o_ = S.op("sp", lambda e, sig: sig(e.dma_start(out=dbg[:, col0:col0 + ncols], in_=ap)), reads=list(reads), dma=1, key="dbg")
            dump_ops.append(o_)

        def cut(k, dumps):
            if stage == k:
                S.barrier()
                for d_ in dumps():
                    dump(*d_)
                raise _Stop()

        def dumpb(ap, row0, nrows, col0, ncols, reads=(), extra=()):
            o_ = S.op("sp", lambda e, sig: sig(e.dma_start(out=dbgb[row0:row0 + nrows, col0:col0 + ncols], in_=ap)), reads=list(reads), extra=list(extra), dma=1, key="dbg")
            dump_ops.append(o_)

        def author():
            nonlocal A
            class Pool:
                def __init__(self, items):
                    self.items = items
                    self.i = 0

                def get(self):
                    it = self.items[self.i % len(self.items)]
                    self.i += 1
                    return it
            p128 = Pool([(psb[b][:, 0:128], Tk(f"p128_{b}", excl=True)) for b in range(4)])
            p512 = Pool([(psb[b][:, :], Tk(f"p512_{b}", excl=True)) for b in range(4, 8)])

            def bfv(ap):
                n = ap.shape[-1]
                return ap.bitcast(BF16)[:, 0:n]

            def load(dst, src, tk, key):
                return S.op("sp", lambda e, sig: sig(e.dma_start(out=dst, in_=src)), writes=[tk], dma=1, key=key)

            identf = A.alloc([128]); t_identf = Tk(); load(identf, ident_d[:, :], t_identf, "c0")
            identb = A.alloc([128], BF16); t_identb = Tk()
            S.op("pool", lambda e: e.tensor_copy(out=identb, in_=identf), reads=[t_identf], writes=[t_identb])
            trif = A.alloc([4, 128]); t_trif = Tk(); load(trif, tri_d.ap().rearrange("a p q -> p a q"), t_trif, "c1")
            negf = A.alloc([4, 128]); t_negf = Tk(); load(negf, neg_d.ap().rearrange("a p q -> p a q"), t_negf, "c2")
            negb = A.alloc([4, 128], BF16); t_negb = Tk()
            S.op("pool", lambda e: e.tensor_copy(out=negb, in_=negf), reads=[t_negf], writes=[t_negb])
            dcm = A.alloc([9, 128], BF16); t_dcm = Tk(); load(dcm, dcm_d.ap().rearrange("a p q -> p a q"), t_dcm, "c2b")
            onesf = A.alloc([128]); t_onesf = Tk()
            S.op("pool", lambda e: e.memset(onesf, 1.0), writes=[t_onesf])
            Uf, Vf, Ub, Vb = (trif[:, i, :] for i in range(4))
            gscs = A.alloc([8]); t_gscs = Tk(); load(gscs, gsc[:, :], t_gscs, "c3")
            negA = A.alloc([4]); t_negA = Tk()
            S.op("act", lambda e: e.activation(out=negA, in_=gscs[:, 0:4], func=AF.Exp), reads=[t_gscs], writes=[t_negA])
            S.op("dve", lambda e: e.tensor_scalar(out=negA, in0=negA, scalar1=-1.0, scalar2=None, op0=ALU.mult), reads=[t_negA], writes=[t_negA])
            dtb = gscs[:, 4:8]
            convc = A.alloc([6, 5]); t_convc = Tk(); load(convc, conv_c[:, :, :], t_convc, "c4")
            gngs = A.alloc([1]); t_gngs = Tk(); load(gngs, gng[:, :], t_gngs, "c5")
            qmask = A.alloc([4]); t_qmask = Tk(); load(qmask, qmask_d[:, :], t_qmask, "c6")
            gcol = A.alloc([8, 2]); t_gcol = Tk(); load(gcol, gcols[:, :, :], t_gcol, "c7")
            modc = A.alloc([6, 8]); t_modc = Tk("modc")
            gmix = A.alloc([D]); t_gmix = Tk("gmix")
            gffn = A.alloc([D]); t_gffn = Tk("gffn")
            GT = A.alloc([NOT, 32]); t_GT = [Tk() for _ in range(NOT)]

            m0 = A.mark()
            scT = A.alloc([8, 2]); t_scT = Tk(); load(scT, cvT[:, :, :], t_scT, "c8")
            S.op("act", lambda e: e.activation(out=scT, in_=scT, func=AF.Silu), reads=[t_scT], writes=[t_scT])
            modrow = A.alloc([6 * D]); t_modrow = Tk("modrow")
            modrow_c = A.alloc([6 * D]); t_modrow_c = Tk("modrow_c")
            adb = A.alloc([6 * D]); t_adb = Tk()
            S.op("sp", lambda e, sig: (sig(e.dma_start(out=adb[0:1, :], in_=ada_b[:, :])), sig(e.dma_start(out=adb[1:2, :], in_=ada_b[:, :]))),
                 writes=[t_adb], dma=2, key="c9")
            adw = [A.alloc([8, 512]) for _ in range(2)]; t_adw = [Tk(), Tk()]
            ada_v = ada_w.ap().rearrange("(k p) n -> p k n", p=128)
            for nb in range(12):
                sl = nb % 2
                S.op("sp", lambda e, sig, nb=nb, sl=sl: (sig(e.dma_start(out=adw[sl][:, 0:4, :], in_=ada_v[:, 0:4, nb * 512:(nb + 1) * 512])),
                                                        sig(e.dma_start(out=adw[sl][:, 4:8, :], in_=ada_v[:, 4:8, nb * 512:(nb + 1) * 512]))),
                     writes=[t_adw[sl]], dma=2, key=f"adw{sl}")
                pp, tp = p512.get()

                def f(e, sl=sl, pp=pp):
                    for k in range(8):
                        i = e.matmul(pp[0:2, :], lhsT=scT[:, k, :], rhs=adw[sl][:, k, :], start=(k == 0), stop=(k == 7))
                    return i
                S.op("pe", f, reads=[t_scT, t_adw[sl]], writes=[tp])
                S.op("dve", lambda e, nb=nb, pp=pp: e.tensor_tensor(out=modrow[0:2, nb * 512:(nb + 1) * 512], in0=pp[0:2, :], in1=adb[0:2, nb * 512:(nb + 1) * 512], op=ALU.add),
                     reads=[tp, t_adb], writes=[t_modrow])
            S.op("sp", lambda e, sig: sig(e.dma_start(out=modrow_c[0:1, :], in_=modrow[1:2, :])), reads=[t_modrow], writes=[t_modrow_c], dma=1, key="c10")
            pp, tp = p128.get()
            vecs = [(modrow, 0), (modrow, 1), (modrow_c, 0), (modrow_c, 1), (modrow, 3), (modrow, 4)]

            def f(e, pp=pp):
                for vi, (row, m) in enumerate(vecs):
                    for k in range(8):
                        i = e.matmul(pp[:, vi * 8 + k:vi * 8 + k + 1], lhsT=row[0:1, m * D + k * 128:m * D + (k + 1) * 128], rhs=onesf[0:1, 0:1], start=True, stop=True)
                return i
            S.op("pe", f, reads=[t_modrow, t_modrow_c, t_onesf], writes=[tp])
            S.op("dve", lambda e, pp=pp: e.tensor_copy(out=modc.rearrange("p a b -> p (a b)"), in_=pp[:, 0:48]), reads=[tp], writes=[t_modc])
            for vi, gi in ((1, 0), (3, 0), (5, 1)):
                S.op("dve", lambda e, vi=vi, gi=gi: e.scalar_tensor_tensor(out=modc[:, vi, :], in0=modc[:, vi, :], scalar=1.0, in1=gcol[:, :, gi], op0=ALU.add, op1=ALU.mult),
                     reads=[t_modc, t_gcol], writes=[t_modc])
            for dst, tdst, m in ((gmix, t_gmix, 2), (gffn, t_gffn, 5)):
                for h in range(2):
                    pp, tp = p512.get()
                    S.op("pe", lambda e, pp=pp, m=m, h=h: e.matmul(pp, lhsT=onesf[0:1, :], rhs=modrow[0:1, m * D + h * 512:m * D + (h + 1) * 512], start=True, stop=True),
                         reads=[t_modrow, t_onesf], writes=[tp])
                    S.op("act", lambda e, pp=pp, dst=dst, h=h: e.copy(out=dst[:, h * 512:(h + 1) * 512], in_=pp), reads=[tp], writes=[tdst])
            S.barrier()
            A.release(m0)
            if stage == 0:
                dump(modc.rearrange("p a b -> p (a b)"), 0, 48); dump(gmix[:, 0:256], 64, 256); dump(gffn[:, 0:256], 320, 256)
                raise _Stop()

            mA = A.mark()
            SC = A.alloc([NT, 28]); t_SC = [Tk("SC")] * NT
            QN = A.alloc([NT, 2, 128], BF16); KN = A.alloc([NT, 2, 128], BF16); VV = A.alloc([NT, 2, 128], BF16)
            t_QKV = [[Tk() for _ in range(6)] for t in range(NT)]
            mA1 = A.mark()
            wst = A.alloc([8, 776]); t_wst = Tk()
            wq = A.alloc([8, 896], BF16); t_wq = Tk()
            S.op("sp", lambda e, sig: (sig(e.dma_start(out=wst[:, :, 0:768], in_=w_qkv.ap().rearrange("(k p) n -> p k n", p=128))),
                                       sig(e.dma_start(out=wst[:, :, 768:776], in_=w_ba.ap().rearrange("(k p) n -> p k n", p=128)))),
                 writes=[t_wst], dma=2, key="wst")
            S.op("pool", lambda e: e.tensor_copy(out=wq[:, :, 0:776], in_=wst), reads=[t_wst], writes=[t_wq])
            xt_A = [A.alloc([D]) for _ in range(3)]; t_xt_A = [Tk() for _ in range(3)]
            junk_A = A.alloc([D]); t_junk_A = Tk()
            ssq_A = [A.alloc([1]) for _ in range(3)]; t_ssq_A = [Tk() for _ in range(3)]
            xs_A = [A.alloc([8, 128], BF16) for _ in range(2)]; t_xs_A = [Tk(), Tk()]
            hxT = [A.alloc([8, 128], BF16) for _ in range(2)]; t_hxT = [Tk(), Tk()]; t_hxTb = [Tk(), Tk()]
            PRE = [A.alloc([6, 132]) for _ in range(3)]; t_PRE = [Tk() for _ in range(3)]
            CV = A.alloc([6, 128]); t_CVc = [Tk() for _ in range(6)]
            SQ = A.alloc([6, 128], BF16); t_SQ = Tk()
            sm = [A.alloc([40]) for _ in range(2)]; t_sm = [Tk(), Tk()]
            nr = [A.alloc([8]) for _ in range(2)]; t_nr = [Tk(), Tk()]

            wst_flat = wst.rearrange("p a b -> p (a b)")
            BA = wst_flat[:, 0:NT * 8].rearrange("p (a b) -> p a b", b=8); t_BA = Tk("BA")
            g66 = A.alloc([2, NT, 4]); t_g66 = Tk(); load(g66, gsc66[:, :, :, :], t_g66, "c3b")
            Mx = [wst_flat[:, 1024 + i_ * 512:1024 + i_ * 512 + NT * 4].rearrange("p (a b) -> p a b", b=4) for i_ in range(4)]; t_Mx = [Tk() for _ in range(4)]

            def rows_of(t):
                if t < 2:
                    return ctxb[t * 128:(t + 1) * 128, :]
                return xb[(t - 2) * 128:(t - 1) * 128, :]

            def front(t):
                s3 = t % 3; s2 = t % 2
                isctx = t < 2
                shc = modc[:, 2 if isctx else 0, :]; scc = modc[:, 3 if isctx else 1, :]
                S.op("sp", lambda e, sig: sig(e.dma_start(out=xt_A[s3], in_=rows_of(t))), writes=[t_xt_A[s3]], dma=1, key=f"xt_A{s3}")
                S.op("act", lambda e: e.activation(out=junk_A, in_=xt_A[s3], func=AF.Square, accum_out=ssq_A[s3]), reads=[t_xt_A[s3]], writes=[t_ssq_A[s3]])
                S.op("act", lambda e: e.activation(out=ssq_A[s3], in_=ssq_A[s3], func=AF.Sqrt, scale=1.0 / D, bias=EPS), reads=[t_ssq_A[s3]], writes=[t_ssq_A[s3]])
                S.op("dve", lambda e: e.reciprocal(out=ssq_A[s3], in_=ssq_A[s3]), reads=[t_ssq_A[s3]], writes=[t_ssq_A[s3]])
                S.op("pool", lambda e: e.tensor_scalar(out=xs_A[s2].rearrange("p a b -> p (a b)"), in0=xt_A[s3], scalar1=ssq_A[s3], scalar2=1.0, op0=ALU.mult, op1=ALU.mult),
                     reads=[t_xt_A[s3], t_ssq_A[s3]], writes=[t_xs_A[s2]])
                pp, tp = p512.get()
                ppb = pp.bitcast(BF16)

                def tr(e):
                    for k in range(8):
                        i = e.transpose(out=ppb[:, k * 128:(k + 1) * 128], in_=xs_A[s2][:, k, :], identity=identb)
                    return i
                S.op("pe", tr, reads=[t_xs_A[s2], t_identb], writes=[tp])

                def ev_d(e):
                    for k in range(8):
                        i = e.tensor_scalar(out=hxT[s2][:, k, :], in0=ppb[:, k * 128:(k + 1) * 128], scalar1=scc[:, k:k + 1], scalar2=shc[:, k:k + 1], op0=ALU.mult, op1=ALU.add)
                    return i
                t_h2 = t_hxTb[s2]
                S.op("dve", ev_d, reads=[tp, t_modc], writes=[t_hxT[s2], t_h2])
                if KCUT == 1:
                    return
                pA, tA = p512.get()
                pB, tB = p512.get()

                def pjA(e):
                    for ch in range(4):
                        for k in range(8):
                            i = e.matmul(pA[:, ch * 128:(ch + 1) * 128], lhsT=wq[:, k, ch * 128:(ch + 1) * 128], rhs=hxT[s2][:, k, :], start=(k == 0), stop=(k == 7))
                    return i

                def pjB(e):
                    for ch in range(4, 6):
                        for k in range(8):
                            i = e.matmul(pB[:, (ch - 4) * 128:(ch - 3) * 128], lhsT=wq[:, k, ch * 128:(ch + 1) * 128], rhs=hxT[s2][:, k, :], start=(k == 0), stop=(k == 7))
                    for k in range(8):
                        i = e.matmul(pB[:, 256:264], lhsT=hxT[s2][:, k, :], rhs=wq[:, k, 768:776], start=(k == 0), stop=(k == 7))
                    return i
                S.op("pe", pjA, reads=[t_wq, t_hxT[s2]], writes=[tA])
                S.op("pe", pjB, reads=[t_wq, t_hxT[s2]], writes=[tB])
                pba = pB[:, 256:264]; tba = tB
                S.op("dve", lambda e: e.tensor_copy(out=PRE[s3][:, 0:4, 2:130], in_=pA.rearrange("p (a b) -> p a b", a=4)), reads=[tA], writes=[t_PRE[s3]])
                S.op("dve", lambda e: e.tensor_copy(out=PRE[s3][:, 4:6, 2:130], in_=pB[:, 0:256].rearrange("p (a b) -> p a b", a=2)), reads=[tB], writes=[t_PRE[s3]])
                if KCUT == 2:
                    return
                first = t in (0, 2); last = t in (1, NT - 1)
                if first:
                    S.op("pool", lambda e: e.memset(PRE[s3][:, :, 0:2], 0.0), writes=[t_PRE[s3]])
                else:
                    sp_ = (t - 1) % 3
                    S.op("pool", lambda e: e.tensor_copy(out=PRE[sp_][:, :, 130:132], in_=PRE[s3][:, :, 2:4]), reads=[t_PRE[s3]], writes=[t_PRE[sp_]])
                if last:
                    S.op("pool", lambda e: e.memset(PRE[s3][:, :, 130:132], 0.0), writes=[t_PRE[s3]])
                else:
                    sn = (t + 1) % 3
                    S.op("pool", lambda e: e.tensor_copy(out=PRE[sn][:, :, 0:2], in_=PRE[s3][:, :, 128:130]), reads=[t_PRE[s3]], writes=[t_PRE[sn]])
                S.op("dve", lambda e: e.tensor_copy(out=BA[:, t, :], in_=pba), reads=[tba], writes=[t_BA])

            def lag(t):
                s3 = t % 3; s2 = t % 2
                for ch in range(6):
                    def cv(e, ch=ch):
                        i = e.tensor_scalar(out=CV[:, ch, :], in0=PRE[s3][:, ch, 0:128], scalar1=convc[:, ch, 0:1], scalar2=None, op0=ALU.mult)
                        for j in range(1, 5):
                            i = e.scalar_tensor_tensor(out=CV[:, ch, :], in0=PRE[s3][:, ch, j:j + 128], scalar=convc[:, ch, j:j + 1], in1=CV[:, ch, :], op0=ALU.mult, op1=ALU.add)
                        return i
                    tcv = t_CVc[ch]
                    for j in range(5):
                        def cvj(e, ch=ch, j=j):
                            if j == 0:
                                return e.tensor_scalar(out=CV[:, ch, :], in0=PRE[s3][:, ch, 0:128], scalar1=convc[:, ch, 0:1], scalar2=None, op0=ALU.mult)
                            return e.scalar_tensor_tensor(out=CV[:, ch, :], in0=PRE[s3][:, ch, j:j + 128], scalar=convc[:, ch, j:j + 1], in1=CV[:, ch, :], op0=ALU.mult, op1=ALU.add)
                        S.op("dve", cvj, reads=[t_PRE[s3], t_convc] + ([tcv] if j else []), writes=[tcv])
                S.op("act", lambda e: e.activation(out=SQ, in_=CV, func=AF.Silu), reads=t_CVc, writes=[t_SQ])
                pT, tT = p512.get()
                pTb = pT.bitcast(BF16)

                def trs(e):
                    for ch in range(6):
                        i = e.transpose(out=pTb[:, ch * 128:(ch + 1) * 128], in_=SQ[:, ch, :], identity=identb)
                    return i
                S.op("pe", trs, reads=[t_SQ, t_identb], writes=[tT])
                pts = [(pTb[:, ch * 128:(ch + 1) * 128], tT) for ch in range(6)]
                n = nr[s2]; tn = t_nr[s2]
                for ch in range(4):
                    S.op("act", lambda e, ch=ch: e.activation(out=junk_A[:, 0:128], in_=pts[ch][0], func=AF.Square, accum_out=n[:, ch:ch + 1]),
                         reads=[pts[ch][1]], writes=[tn])
                S.op("act", lambda e: e.activation(out=n[:, 0:4], in_=n[:, 0:4], func=AF.Sqrt, bias=EPS), reads=[tn], writes=[tn])
                S.op("dve", lambda e: e.reciprocal(out=n[:, 0:4], in_=n[:, 0:4]), reads=[tn], writes=[tn])
                S.op("dve", lambda e: e.tensor_scalar(out=n[:, 0:2], in0=n[:, 0:2], scalar1=128.0 ** -0.5, scalar2=None, op0=ALU.mult), reads=[tn], writes=[tn])
                dsts = [QN[:, t, 0, :], QN[:, t, 1, :], KN[:, t, 0, :], KN[:, t, 1, :], VV[:, t, 0, :], VV[:, t, 1, :]]
                for ch in range(6):
                    if ch < 4:
                        S.op("dve", lambda e, ch=ch: e.tensor_scalar(out=dsts[ch], in0=pts[ch][0], scalar1=n[:, ch:ch + 1], scalar2=None, op0=ALU.mult),
                             reads=[pts[ch][1], tn], writes=[t_QKV[t][ch]])
                    else:
                        S.op("act", lambda e, ch=ch: e.copy(out=dsts[ch], in_=pts[ch][0]), reads=[pts[ch][1]], writes=[t_QKV[t][ch]])

            for i in range(NTL + 1):
                if i < NTL:
                    front(i)
                if i >= 1 and KCUT in (0, 5):
                    lag(i - 1)
            tS = t_SC[0]
            SCk = lambda k: SC[:, :, k * 4:(k + 1) * 4]
            M0, M1, M2, M3 = Mx; tM0, tM1, tM2, tM3 = t_Mx
            S.op("act", lambda e: e.activation(out=g66[:, 0, :, :], in_=g66[:, 0, :, :], func=AF.Exp), reads=[t_g66], writes=[t_g66])
            S.op("act", lambda e: e.activation(out=SCk(3), in_=BA[:, :, 0:4], func=AF.Sigmoid), reads=[t_BA], writes=[tS])
            S.op("dve", lambda e: e.tensor_tensor(out=M0, in0=BA[:, :, 4:8], in1=g66[:, 1, :, :], op=ALU.add), reads=[t_BA, t_g66], writes=[tM0])
            S.op("dve", lambda e: e.tensor_scalar(out=M1, in0=M0, scalar1=-1.0, scalar2=None, op0=ALU.mult), reads=[tM0], writes=[tM1])
            S.op("dve", lambda e: e.tensor_tensor(out=M1, in0=M1, in1=M0, op=ALU.min), reads=[tM0, tM1], writes=[tM1])
            S.op("act", lambda e: e.activation(out=M1, in_=M1, func=AF.Exp), reads=[tM1], writes=[tM1])
            S.op("act", lambda e: e.activation(out=M1, in_=M1, func=AF.Ln, bias=1.0), reads=[tM1], writes=[tM1])
            S.op("dve", lambda e: e.tensor_scalar(out=M2, in0=M0, scalar1=0.0, scalar2=None, op0=ALU.max), reads=[tM0], writes=[tM2])
            S.op("dve", lambda e: e.tensor_tensor(out=M2, in0=M2, in1=M1, op=ALU.add), reads=[tM1, tM2], writes=[tM2])
            S.op("dve", lambda e: e.scalar_tensor_tensor(out=SCk(6), in0=M2, scalar=-1.0, in1=g66[:, 0, :, :], op0=ALU.mult, op1=ALU.mult), reads=[tM2, t_g66], writes=[tS])
            pcA, tcA = p512.get(); pcB, tcB = p512.get()

            def cums(e):
                e.matmul(pcA[:, 0:2 * NT], lhsT=Uf, rhs=SC[:, :, 24:26], start=True, stop=True)
                return e.matmul(pcA[:, 2 * NT:4 * NT], lhsT=Ub, rhs=SC[:, :, 26:28], start=True, stop=True)
            S.op("pe", cums, reads=[tS, t_trif], writes=[tcA])
            S.op("pe", lambda e: e.matmul(pcB[:, 0:4 * NT], lhsT=onesf, rhs=SC[:, :, 24:28], start=True, stop=True), reads=[tS, t_onesf], writes=[tcB])
            Gf_ps = pcA[:, 0:2 * NT].rearrange("p (a b) -> p a b", b=2); Gb_ps = pcA[:, 2 * NT:4 * NT].rearrange("p (a b) -> p a b", b=2)
            Gt_ps = pcB[:, 0:4 * NT].rearrange("p (a b) -> p a b", b=4)
            S.op("act", lambda e: e.activation(out=SC[:, :, 16:18], in_=Gf_ps, func=AF.Exp), reads=[tcA], writes=[tS])
            S.op("act", lambda e: e.activation(out=SC[:, :, 18:20], in_=Gb_ps, func=AF.Exp), reads=[tcA], writes=[tS])
            S.op("act", lambda e: e.activation(out=SCk(5), in_=Gt_ps, func=AF.Exp), reads=[tcB], writes=[tS])
            S.op("dve", lambda e: e.tensor_copy(out=M3[:, :, 0:2], in_=Gf_ps), reads=[tcA], writes=[tM3])
            S.op("dve", lambda e: e.tensor_copy(out=M3[:, :, 2:4], in_=Gb_ps), reads=[tcA], writes=[tM3])
            S.op("dve", lambda e: e.tensor_tensor(out=M3, in0=Gt_ps, in1=M3, op=ALU.subtract), reads=[tcB, tM3], writes=[tM3])
            S.op("act", lambda e: e.activation(out=SCk(2), in_=M3, func=AF.Exp), reads=[tM3], writes=[tS])
            S.op("dve", lambda e: e.tensor_scalar(out=SCk(0), in0=SCk(3), scalar1=-1.0, scalar2=None, op0=ALU.mult), reads=[tS], writes=[tS])
            S.op("dve", lambda e: e.tensor_tensor(out=SCk(1), in0=SCk(3), in1=SCk(4), op=ALU.mult), reads=[tS], writes=[tS])
            S.barrier()
            A.release(mA1)
            if stage == 1:
                for i_, t_ in enumerate((0, 1, 2, 3, 33, 65)):
                    dump(SC[:, t_, :], i_ * 32, 28)
                    dumpb(QN[:, t_, :, :].rearrange("p a b -> p (a b)"), 0, 128, i_ * 768, 256)
                    dumpb(KN[:, t_, :, :].rearrange("p a b -> p (a b)"), 0, 128, i_ * 768 + 256, 256)
                    dumpb(VV[:, t_, :, :].rearrange("p a b -> p (a b)"), 0, 128, i_ * 768 + 512, 256)
                raise _Stop()

            p128_small = p128
            p128 = Pool([(psb[b_][:, 0:128], Tk(f"p128x_{b_}", excl=True)) for b_ in range(8)])
            chains = [(hl, d) for hl in range(2) for d in range(2)]
            cb = {}
            for c in chains:
                b = {}
                for nm in ("knT", "qnT", "qdT", "X0", "XT0", "Xb", "XbT", "X0s", "XT0s", "X1s", "XT1s", "P0", "P1", "PT0", "PT1", "No", "NoT", "Wb", "Vb", "kbg", "kd", "vb", "nwT", "vnew", "QKm", "qd", "Sb"):
                    b[nm] = A.alloc([128], BF16); b["t_" + nm] = Tk(nm)
                for nm in ("gV", "gU", "E", "ET", "S"):
                    b[nm] = A.alloc([128]); b["t_" + nm] = Tk(nm)
                cb[c] = b
                S.op("pool", lambda e, b=b: e.memset(b["S"], 0.0), writes=[b["t_S"]])
                S.op("pool", lambda e, b=b: e.memset(b["Sb"], 0.0), writes=[b["t_Sb"]])
            OACC = A.alloc([64, 2, 128], BF16); t_OACC = [[Tk() for _ in range(2)] for _ in range(64)]
            ofin = [A.alloc([128]) for _ in range(2)]; t_ofin = [Tk(), Tk()]
            onb = [A.alloc([128], BF16) for _ in range(2)]; t_onb = [Tk(), Tk()]
            onT = [A.alloc([128], BF16) for _ in range(4)]; t_onT = [Tk() for _ in range(4)]
            fsm = [A.alloc([4]) for _ in range(2)]; t_fsm = [Tk(), Tk()]
            junk_B = A.alloc([128])
            visited = set()
            snd_ops = []
            fin_cnt = [0]
            evq = [0]

            def evac_copy(dst, src, rd, wr, scale=None):
                evq[0] += 1
                if evq[0] % 2 == 0:
                    if scale is None:
                        return S.op("act", lambda e: e.copy(out=dst, in_=src), reads=rd, writes=wr)
                    return S.op("act", lambda e: e.activation(out=dst, in_=src, func=AF.Copy, scale=scale), reads=rd, writes=wr)
                if scale is None:
                    return S.op("dve", lambda e: e.tensor_copy(out=dst, in_=src), reads=rd, writes=wr)
                return S.op("dve", lambda e: e.tensor_scalar(out=dst, in0=src, scalar1=scale, scalar2=None, op0=ALU.mult), reads=rd, writes=wr)

            def chain_step(c, t):
                hl, d = c
                b = cb[c]
                col = d * 2 + hl
                latent = t >= 2
                kn = KN[:, t, hl, :]; qn = QN[:, t, hl, :]; vv = VV[:, t, hl, :]
                tqq = t_QKV[t][hl]; tqk = t_QKV[t][2 + hl]; tqv = t_QKV[t][4 + hl]; tsc = t_SC[t]

                def scol(kind):
                    return SC[:, t, kind * 4 + col:kind * 4 + col + 1]
                U_, V_ = (Uf, Vf) if d == 0 else (Ub, Vb)
                negs = negb[:, 0 if d == 0 else 2, :]; negi = negb[:, 1 if d == 0 else 3, :]
                pk, tpk = p128.get()
                S.op("pe", lambda e: e.transpose(out=bfv(pk), in_=kn, identity=identb), reads=[tqk, t_identb], writes=[tpk])
                evac_copy(b["knT"], bfv(pk), [tpk], [b["t_knT"]])
                S.op("act", lambda e: e.activation(out=b["gV"], in_=V_, func=AF.Copy, scale=scol(6)), reads=[t_trif, tsc], writes=[b["t_gV"]])
                S.op("act", lambda e: e.activation(out=b["gU"], in_=U_, func=AF.Copy, scale=scol(6)), reads=[t_trif, tsc], writes=[b["t_gU"]])
                S.op("dve", lambda e: e.tensor_scalar(out=b["kbg"], in0=kn, scalar1=scol(1), scalar2=None, op0=ALU.mult), reads=[tqk, tsc], writes=[b["t_kbg"]])
                S.op("pool", lambda e: e.tensor_scalar(out=b["kd"], in0=kn, scalar1=scol(2), scalar2=1.0, op0=ALU.mult, op1=ALU.mult), reads=[tqk, tsc], writes=[b["t_kd"]])
                S.op("pool", lambda e: e.tensor_scalar(out=b["vb"], in0=vv, scalar1=scol(3), scalar2=1.0, op0=ALU.mult, op1=ALU.mult), reads=[tqv, tsc], writes=[b["t_vb"]])
                yield
                pd, tpd = p128.get()

                def dm(e):
                    e.matmul(pd, lhsT=identb, rhs=negs, start=True, stop=False)
                    return e.matmul(pd, lhsT=U_, rhs=b["gV"], start=False, stop=True)
                S.op("pe", dm, reads=[t_identb, t_negb, t_trif, b["t_gV"]], writes=[tpd])
                S.op("act", lambda e: e.activation(out=b["E"], in_=pd, func=AF.Exp), reads=[tpd], writes=[b["t_E"]])
                pkk, tpkk = p128.get()
                S.op("pe", lambda e: e.matmul(pkk, lhsT=b["knT"], rhs=b["knT"], start=True, stop=True), reads=[b["t_knT"]], writes=[tpkk])
                S.op("dve", lambda e: e.scalar_tensor_tensor(out=b["X0"], in0=pkk, scalar=scol(0), in1=b["E"], op0=ALU.mult, op1=ALU.mult),
                     reads=[tpkk, tsc, b["t_E"]], writes=[b["t_X0"]])
                if latent:
                    pdt, tpdt = p128.get()

                    def dmt(e):
                        e.matmul(pdt, lhsT=identb, rhs=negi, start=True, stop=False)
                        return e.matmul(pdt, lhsT=V_, rhs=b["gU"], start=False, stop=True)
                    S.op("pe", dmt, reads=[t_identb, t_negb, t_trif, b["t_gU"]], writes=[tpdt])
                    S.op("act", lambda e: e.activation(out=b["ET"], in_=pdt, func=AF.Exp), reads=[tpdt], writes=[b["t_ET"]])
                    pq_, tpq = p128.get()
                    S.op("pe", lambda e: e.transpose(out=bfv(pq_), in_=qn, identity=identb), reads=[tqq, t_identb], writes=[tpq])
                    evac_copy(b["qnT"], bfv(pq_), [tpq], [b["t_qnT"]])
                    S.op("pool", lambda e: e.tensor_scalar(out=b["qd"], in0=qn, scalar1=scol(4), scalar2=1.0, op0=ALU.mult, op1=ALU.mult), reads=[tqq, tsc], writes=[b["t_qd"]])
                yield
                px, tpx = p128.get()
                S.op("pe", lambda e: e.transpose(out=bfv(px), in_=b["X0"], identity=identb), reads=[b["t_X0"], t_identb], writes=[tpx])
                evac_copy(b["XT0"], bfv(px), [tpx], [b["t_XT0"]])
                if latent:
                    pqd, tpqd = p128.get()
                    S.op("pe", lambda e: e.transpose(out=bfv(pqd), in_=b["qd"], identity=identb), reads=[b["t_qd"], t_identb], writes=[tpqd])
                    evac_copy(b["qdT"], bfv(pqd), [tpqd], [b["t_qdT"]])
                    pqk, tpqk = p128.get()
                    S.op("pe", lambda e: e.matmul(pqk, lhsT=b["knT"], rhs=b["qnT"], start=True, stop=True), reads=[b["t_knT"], b["t_qnT"]], writes=[tpqk])
                    S.op("dve", lambda e: e.tensor_tensor(out=b["QKm"], in0=pqk, in1=b["ET"], op=ALU.mult), reads=[tpqk, b["t_ET"]], writes=[b["t_QKm"]])
                yield
                mk = lambda nm: (b[nm], b["t_" + nm])
                Xb, tXb = mk("Xb"); XbT, tXbT = mk("XbT")
                S.op("pool", lambda e: e.tensor_tensor(out=Xb, in0=b["X0"], in1=dcm[:, 0, :], op=ALU.mult), reads=[b["t_X0"], t_dcm], writes=[tXb])
                S.op("pool", lambda e: e.tensor_tensor(out=XbT, in0=b["XT0"], in1=dcm[:, 0, :], op=ALU.mult), reads=[b["t_XT0"], t_dcm], writes=[tXbT])
                S.op("pool", lambda e: e.tensor_tensor(out=b["P0"], in0=Xb, in1=identb, op=ALU.add), reads=[tXb, t_identb], writes=[b["t_P0"]])
                S.op("pool", lambda e: e.tensor_tensor(out=b["PT0"], in0=XbT, in1=identb, op=ALU.add), reads=[tXbT, t_identb], writes=[b["t_PT0"]])
                yield
                cur = 0
                cX, tcX, cXT, tcXT = Xb, tXb, XbT, tXbT
                for lev in range(2):
                    nX, tnX = mk(f"X{lev}s"); nXT, tnXT = mk(f"XT{lev}s")
                    p1, tp1 = p128.get()
                    S.op("pe", lambda e, p1=p1, cX=cX, cXT=cXT: e.matmul(p1, lhsT=cXT, rhs=cX, start=True, stop=True), reads=[tcX, tcXT], writes=[tp1])
                    evac_copy(nX, p1, [tp1], [tnX])
                    p2, tp2 = p128.get()
                    S.op("pe", lambda e, p2=p2, cX=cX, cXT=cXT: e.matmul(p2, lhsT=cX, rhs=cXT, start=True, stop=True), reads=[tcX, tcXT], writes=[tp2])
                    evac_copy(nXT, p2, [tp2], [tnXT])
                    yield
                    P = b[f"P{cur}"]; tP = b[f"t_P{cur}"]; nP = b[f"P{1 - cur}"]; tnP = b[f"t_P{1 - cur}"]
                    PT = b[f"PT{cur}"]; tPT = b[f"t_PT{cur}"]; nPT = b[f"PT{1 - cur}"]; tnPT = b[f"t_PT{1 - cur}"]
                    p3, tp3 = p128.get()
                    S.op("pe", lambda e, p3=p3, nXT=nXT, P=P: e.matmul(p3, lhsT=nXT, rhs=P, start=True, stop=True), reads=[tnXT, tP], writes=[tp3])
                    S.op("dve", lambda e, p3=p3, P=P, nP=nP: e.tensor_tensor(out=nP, in0=p3, in1=P, op=ALU.add), reads=[tp3, tP], writes=[tnP])
                    p4, tp4 = p128.get()
                    S.op("pe", lambda e, p4=p4, nX=nX, PT=PT: e.matmul(p4, lhsT=nX, rhs=PT, start=True, stop=True), reads=[tnX, tPT], writes=[tp4])
                    S.op("dve", lambda e, p4=p4, PT=PT, nPT=nPT: e.tensor_tensor(out=nPT, in0=p4, in1=PT, op=ALU.add), reads=[tp4, tPT], writes=[tnPT])
                    cur = 1 - cur
                    cX, tcX, cXT, tcXT = nX, tnX, nXT, tnXT
                    yield
                for li in range(4):
                    mi = 1 + 2 * li + (0 if d == 0 else 1)
                    miT = 1 + 2 * li + (1 if d == 0 else 0)
                    No, tNo = mk("No"); NoT, tNoT = mk("NoT")
                    P = b[f"P{cur}"]; tP = b[f"t_P{cur}"]; nP = b[f"P{1 - cur}"]; tnP = b[f"t_P{1 - cur}"]
                    PT = b[f"PT{cur}"]; tPT = b[f"t_PT{cur}"]; nPT = b[f"PT{1 - cur}"]; tnPT = b[f"t_PT{1 - cur}"]
                    S.op("pool", lambda e, mi=mi, No=No: e.tensor_tensor(out=No, in0=b["X0"], in1=dcm[:, mi, :], op=ALU.mult), reads=[b["t_X0"], t_dcm], writes=[tNo])
                    pw_, tpw_ = p128.get()
                    S.op("pe", lambda e, pw_=pw_, No=No, PT=PT: e.matmul(pw_, lhsT=No, rhs=PT, start=True, stop=True), reads=[tNo, tPT], writes=[tpw_])
                    Wb, tWb = mk("Wb")
                    evac_copy(Wb, pw_, [tpw_], [tWb])
                    if li < 3:
                        S.op("pool", lambda e, miT=miT, NoT=NoT: e.tensor_tensor(out=NoT, in0=b["XT0"], in1=dcm[:, miT, :], op=ALU.mult), reads=[b["t_XT0"], t_dcm], writes=[tNoT])
                        pv_, tpv_ = p128.get()
                        S.op("pe", lambda e, pv_=pv_, NoT=NoT, P=P: e.matmul(pv_, lhsT=NoT, rhs=P, start=True, stop=True), reads=[tNoT, tP], writes=[tpv_])
                        Vb_, tVb_ = mk("Vb")
                        evac_copy(Vb_, pv_, [tpv_], [tVb_])
                    yield
                    p5, tp5 = p128.get()
                    S.op("pe", lambda e, p5=p5, P=P, Wb=Wb: e.matmul(p5, lhsT=P, rhs=Wb, start=True, stop=True), reads=[tP, tWb], writes=[tp5])
                    S.op("dve", lambda e, p5=p5, PT=PT, nPT=nPT: e.tensor_tensor(out=nPT, in0=p5, in1=PT, op=ALU.add), reads=[tp5, tPT], writes=[tnPT])
                    if li < 3:
                        p6, tp6 = p128.get()
                        S.op("pe", lambda e, p6=p6, PT=PT, Vb_=Vb_: e.matmul(p6, lhsT=PT, rhs=Vb_, start=True, stop=True), reads=[tPT, tVb_], writes=[tp6])
                        S.op("dve", lambda e, p6=p6, P=P, nP=nP: e.tensor_tensor(out=nP, in0=p6, in1=P, op=ALU.add), reads=[tp6, tP], writes=[tnP])
                    cur = 1 - cur
                    yield
                TT = b[f"PT{cur}"]; tTT = b[f"t_PT{cur}"]
                pw, tpw = p128.get()
                S.op("pe", lambda e: e.matmul(pw, lhsT=b["kbg"], rhs=TT, start=True, stop=True), reads=[b["t_kbg"], tTT], writes=[tpw])
                evac_copy(b["nwT"], pw, [tpw], [b["t_nwT"]], scale=-1.0)
                yield
                pv, tpv = p128.get()

                def vn(e):
                    e.matmul(pv, lhsT=TT, rhs=b["vb"], start=True, stop=False)
                    return e.matmul(pv, lhsT=b["nwT"], rhs=b["Sb"], start=False, stop=True)
                S.op("pe", vn, reads=[tTT, b["t_vb"], b["t_nwT"], b["t_Sb"]], writes=[tpv])
                evac_copy(b["vnew"], pv, [tpv], [b["t_vnew"]])
                yield
                if latent:
                    lt = t - 2
                    po, tpo = p128.get()

                    def om(e):
                        e.matmul(po, lhsT=b["qdT"], rhs=b["Sb"], start=True, stop=False)
                        return e.matmul(po, lhsT=b["QKm"], rhs=b["vnew"], start=False, stop=True)
                    S.op("pe", om, reads=[b["t_qdT"], b["t_Sb"], b["t_QKm"], b["t_vnew"]], writes=[tpo])
                    if (lt, hl) not in visited:
                        visited.add((lt, hl))
                        evac_copy(OACC[:, lt, hl, :], po, [tpo], [t_OACC[lt][hl]])
                    else:
                        k2 = fin_cnt[0] % 2; k4 = fin_cnt[0] % 4
                        fin_cnt[0] += 1
                        of = ofin[k2]; tof = t_ofin[k2]; fs = fsm[k2]; tfs = t_fsm[k2]
                        S.op("dve", lambda e: e.tensor_tensor(out=of, in0=po, in1=OACC[:, lt, hl, :], op=ALU.add), reads=[tpo, t_OACC[lt][hl]], writes=[tof])
                        S.op("act", lambda e: e.activation(out=junk_B, in_=of, func=AF.Square, accum_out=fs[:, 0:1]), reads=[tof], writes=[tfs])
                        S.op("act", lambda e: e.activation(out=fs[:, 0:1], in_=fs[:, 0:1], func=AF.Sqrt, scale=1.0 / 128, bias=EPS), reads=[tfs], writes=[tfs])
                        S.op("dve", lambda e: e.reciprocal(out=fs[:, 0:1], in_=fs[:, 0:1]), reads=[tfs], writes=[tfs])
                        S.op("dve", lambda e: e.tensor_scalar(out=onb[k2], in0=of, scalar1=fs[:, 0:1], scalar2=None, op0=ALU.mult), reads=[tof, tfs], writes=[t_onb[k2]])
                        pt_, tpt = p128.get()
                        S.op("pe", lambda e: e.transpose(out=bfv(pt_), in_=onb[k2], identity=identb), reads=[t_onb[k2], t_identb], writes=[tpt])
                        evac_copy(onT[k4], bfv(pt_), [tpt], [t_onT[k4]])
                        o_ = S.op("pool", lambda e, sig: sig(e.dma_start(out=snd[lt // 16][hl * 128:(hl + 1) * 128, (lt % 16) * 64:(lt % 16 + 1) * 64], in_=onT[k4].bitcast(F32))),
                                  reads=[t_onT[k4]], dma=1, key=f"snd{k4}")
                        snd_ops.append(o_)
                ps_, tps = p128.get()
                S.op("pe", lambda e: e.matmul(ps_, lhsT=b["kd"], rhs=b["vnew"], start=True, stop=True), reads=[b["t_kd"], b["t_vnew"]], writes=[tps])
                S.op("dve", lambda e: e.scalar_tensor_tensor(out=b["S"], in0=b["S"], scalar=scol(5), in1=ps_, op0=ALU.mult, op1=ALU.add),
                     reads=[b["t_S"], tsc, tps], writes=[b["t_S"]])
                S.op("act", lambda e: e.copy(out=b["Sb"], in_=b["S"]), reads=[b["t_S"]], writes=[b["t_Sb"]])
                yield

            def bwd_tile(i):
                return 1 - i if i < 2 else NT + 1 - i

            for i in range(NT):
                gens = []
                for c in chains:
                    t = i if c[1] == 0 else bwd_tile(i)
                    gens.append(chain_step(c, t))
                alive = list(gens)
                while alive:
                    nxt = []
                    for g in alive:
                        try:
                            next(g)
                            nxt.append(g)
                        except StopIteration:
                            pass
                    alive = nxt
            if stage == 2:
                for i_ in range(4):
                    o_ = S.op("sp", lambda e, sig, i_=i_: sig(e.dma_start(out=dbgb[0:256, i_ * 2048:(i_ + 1) * 2048].bitcast(F32), in_=snd[i_][:, :])), extra=snd_ops, dma=1, key="dbg")
                    dump_ops.append(o_)
                for i_, c_ in enumerate(chains):
                    dump(cb[c_]["S"], i_ * 128, 128, reads=[cb[c_]["t_S"]])
                raise _Stop()
            p128 = p128_small
            ccs = []
            for j in range(4):
                ccs.append(S.op("pool", lambda e, sig, j=j: sig(e.collective_compute("AllGather", ALU.bypass, replica_groups=[[0, 1, 2, 3], [4, 5, 6, 7]],
                                                                                 ins=[snd[j].ap().opt()], outs=[rcv[j].ap().opt()])),
                                extra=snd_ops, dma=1, key=f"cc{j}", inc=1))
            S.barrier()
            A.release(mA)
            if stage == 3:
                raise _Stop()

            hxo = A.alloc([8, OWN], BF16); t_hxo = [Tk() for _ in range(NOT)]
            markH = A.mark()
            offS = A.mark()
            SZ = A.alloc([8, OWN], BF16); t_SZ = [[Tk() for _ in range(4)] for _ in range(8)]
            GU = A.alloc([8, OWN], BF16); t_GU = [[Tk() for _ in range(4)] for _ in range(8)]
            wstg = [A.alloc([8, 512]) for _ in range(2)]; t_wstg = [Tk(), Tk()]
            wbf = [A.alloc([8, 512], BF16) for _ in range(2)]; t_wbf = [Tk(), Tk()]
            mC1 = A.mark()
            xt_C = [A.alloc([D]) for _ in range(2)]; t_xt_C = [Tk(), Tk()]
            junk_C = A.alloc([D]); t_junk_C = Tk()
            ssq_C = [A.alloc([1]) for _ in range(2)]; t_ssq_C = [Tk(), Tk()]
            xs_C = [A.alloc([8, 128], BF16) for _ in range(2)]; t_xs_C = [Tk(), Tk()]
            for t in range(NOT):
                s2 = t % 2
                S.op("sp", lambda e, sig, t=t, s2=s2: sig(e.dma_start(out=xt_C[s2], in_=xo[t * 128:(t + 1) * 128, :])), writes=[t_xt_C[s2]], dma=1, key=f"cxt{s2}")
                S.op("act", lambda e, s2=s2: e.activation(out=junk_C, in_=xt_C[s2], func=AF.Square, accum_out=ssq_C[s2]), reads=[t_xt_C[s2]], writes=[t_ssq_C[s2]])
                S.op("act", lambda e, s2=s2: e.activation(out=ssq_C[s2], in_=ssq_C[s2], func=AF.Sqrt, scale=1.0 / D, bias=EPS), reads=[t_ssq_C[s2]], writes=[t_ssq_C[s2]])
                S.op("dve", lambda e, s2=s2: e.reciprocal(out=ssq_C[s2], in_=ssq_C[s2]), reads=[t_ssq_C[s2]], writes=[t_ssq_C[s2]])
                S.op("pool", lambda e, s2=s2: e.tensor_scalar(out=xs_C[s2].rearrange("p a b -> p (a b)"), in0=xt_C[s2], scalar1=ssq_C[s2], scalar2=1.0, op0=ALU.mult, op1=ALU.mult),
                     reads=[t_xt_C[s2], t_ssq_C[s2]], writes=[t_xs_C[s2]])
                pp, tp = p512.get()
                ppb = pp.bitcast(BF16)

                def tr(e, s2=s2, ppb=ppb):
                    for k in range(8):
                        i = e.transpose(out=ppb[:, k * 128:(k + 1) * 128], in_=xs_C[s2][:, k, :], identity=identb)
                    return i
                S.op("pe", tr, reads=[t_xs_C[s2], t_identb], writes=[tp])

                def ev_d(e, t=t, ppb=ppb):
                    for k in range(8):
                        i = e.tensor_scalar(out=hxo[:, k, t * 128:(t + 1) * 128], in0=ppb[:, k * 128:(k + 1) * 128], scalar1=modc[:, 1, k:k + 1], scalar2=modc[:, 0, k:k + 1], op0=ALU.mult, op1=ALU.add)
                    return i
                S.op("dve", ev_d, reads=[tp, t_modc], writes=[t_hxo[t]])
            S.barrier()
            A.release(mC1)
            wcnt = [0]

            def stream_w(src_ap_cols, ncols):
                s = wcnt[0] % 2
                wcnt[0] += 1
                v = src_ap_cols.rearrange("(k p) n -> p k n", p=128)
                S.op("sp", lambda e, sig: (sig(e.dma_start(out=wstg[s][:, 0:4, 0:ncols], in_=v[:, 0:4, :])), sig(e.dma_start(out=wstg[s][:, 4:8, 0:ncols], in_=v[:, 4:8, :]))),
                     writes=[t_wstg[s]], dma=2, key=f"wstg{s}")
                S.op("pool", lambda e: e.tensor_copy(out=wbf[s][:, :, 0:ncols], in_=wstg[s][:, :, 0:ncols]), reads=[t_wstg[s]], writes=[t_wbf[s]])
                return wbf[s], t_wbf[s]
            for cbk in range(4):
                wv, twv = stream_w(w_rest[:, cbk * 512:(cbk + 1) * 512], 512)
                dst, tdst, fn = (SZ, t_SZ, AF.Silu) if cbk < 2 else (GU, t_GU, AF.Gelu)
                for cc_ in range(4):
                    chn = (cbk % 2) * 4 + cc_
                    for tb in range(4):
                        pp, tp = p512.get()

                        def f(e, pp=pp, wv=wv, cc_=cc_, tb=tb):
                            for k in range(8):
                                i = e.matmul(pp, lhsT=wv[:, k, cc_ * 128:(cc_ + 1) * 128], rhs=hxo[:, k, tb * 512:(tb + 1) * 512], start=(k == 0), stop=(k == 7))
                            return i
                        S.op("pe", f, reads=[twv] + t_hxo[tb * 4:(tb + 1) * 4], writes=[tp])
                        S.op("act", lambda e, pp=pp, dst=dst, chn=chn, tb=tb, fn=fn: e.activation(out=dst[:, chn, tb * 512:(tb + 1) * 512], in_=pp, func=fn),
                             reads=[tp], writes=[tdst[chn][tb]])
            def cut4(k):
                if stage == 4 and int(os.environ.get("CUT4", "0")) == k:
                    S.barrier()
                    for i_, buf_ in enumerate((SZ, GU, hxo)):
                        for h_ in range(2):
                            dumpb(buf_[:, h_ * 4:(h_ + 1) * 4, :].rearrange("p a b -> p (a b)"), i_ * 256 + h_ * 128, 128, 0, SEQ)
                    raise _Stop()
            cut4(1)
            mV = A.mark()
            junk_V = A.alloc([D])
            wcnt[0] = 0
            wvh = [stream_w(w_rest[:, 2048 + hh * 512:2048 + (hh + 1) * 512], 512) for hh in range(2)]
            swf = A.alloc([8, 128]); t_swf = Tk(); load(swf, sgu_wT.ap().rearrange("g q p -> q g p"), t_swf, "c11")
            swb = A.alloc([8, 128], BF16); t_swb = Tk()
            S.op("pool", lambda e: e.tensor_copy(out=swb, in_=swf), reads=[t_swf], writes=[t_swb])
            lnbb = A.alloc([D]); t_lnbb = Tk(); load(lnbb, lnb_bc[:, :], t_lnbb, "c12")
            sbb = A.alloc([8, 128]); t_sbb = Tk(); load(sbb, sgu_bb[:, :, :], t_sbb, "c13")
            lngc = A.alloc([8]); t_lngc = Tk(); load(lngc, lng_c[:, :], t_lngc, "c14")
            BIAS = A.alloc([8, 128]); t_BIAS = Tk()
            for g in range(8):
                pp, tp = p128.get()
                S.op("pe", lambda e, pp=pp, g=g: e.matmul(pp, lhsT=lnbb[:, g * 128:(g + 1) * 128], rhs=swf[:, g, :], start=True, stop=True), reads=[t_lnbb, t_swf], writes=[tp])
                S.op("dve", lambda e, pp=pp, g=g: e.tensor_tensor(out=BIAS[:, g, :], in0=pp, in1=sbb[:, g, :], op=ALU.add), reads=[tp, t_sbb], writes=[t_BIAS])
            gv = [A.alloc([D])] * 2; t_gv = [Tk()] * 2
            vnb = [A.alloc([D], BF16) for _ in range(2)]; t_vnb = [Tk(), Tk()]
            lst = [A.alloc([8]) for _ in range(2)]; t_lst = [Tk(), Tk()]
            mtmp = [A.alloc([128]) for _ in range(2)]; t_mtmp = [Tk(), Tk()]
            for t in range(NOT):
                s2 = t % 2
                for hh in range(2):
                    pp, tp = p512.get()

                    def f(e, pp=pp, hh=hh, t=t):
                        for k in range(8):
                            i = e.matmul(pp, lhsT=hxo[:, k, t * 128:(t + 1) * 128], rhs=wvh[hh][0][:, k, :], start=(k == 0), stop=(k == 7))
                        return i
                    S.op("pe", f, reads=[wvh[hh][1], t_hxo[t]], writes=[tp])
                    S.op("act", lambda e, pp=pp, hh=hh, s2=s2: e.activation(out=gv[s2][:, hh * 512:(hh + 1) * 512], in_=pp, func=AF.Gelu), reads=[tp], writes=[t_gv[s2]])
                ls = lst[s2]; tls = t_lst[s2]
                S.op("dve", lambda e, s2=s2, ls=ls: e.tensor_reduce(out=ls[:, 0:1], in_=gv[s2], axis=mybir.AxisListType.X, op=ALU.add), reads=[t_gv[s2]], writes=[tls])
                S.op("act", lambda e, s2=s2, ls=ls: e.activation(out=junk_V, in_=gv[s2], func=AF.Square, accum_out=ls[:, 1:2]), reads=[t_gv[s2]], writes=[tls])
                S.op("dve", lambda e, ls=ls: e.tensor_scalar(out=ls[:, 0:2], in0=ls[:, 0:2], scalar1=1.0 / D, scalar2=None, op0=ALU.mult), reads=[tls], writes=[tls])
                S.op("dve", lambda e, ls=ls: e.tensor_tensor(out=ls[:, 2:3], in0=ls[:, 0:1], in1=ls[:, 0:1], op=ALU.mult), reads=[tls], writes=[tls])
                S.op("dve", lambda e, ls=ls: e.tensor_tensor(out=ls[:, 2:3], in0=ls[:, 1:2], in1=ls[:, 2:3], op=ALU.subtract), reads=[tls], writes=[tls])
                S.op("act", lambda e, ls=ls: e.activation(out=ls[:, 2:3], in_=ls[:, 2:3], func=AF.Sqrt, bias=EPS), reads=[tls], writes=[tls])
                S.op("dve", lambda e, ls=ls: e.reciprocal(out=ls[:, 2:3], in_=ls[:, 2:3]), reads=[tls], writes=[tls])
                S.op("dve", lambda e, s2=s2, ls=ls: e.tensor_scalar(out=vnb[s2], in0=gv[s2], scalar1=ls[:, 0:1], scalar2=ls[:, 2:3], op0=ALU.subtract, op1=ALU.mult),
                     reads=[t_gv[s2], tls], writes=[t_vnb[s2]])
                for g in range(8):
                    pp, tp = p128.get()
                    S.op("pe", lambda e, pp=pp, g=g, s2=s2: e.matmul(pp, lhsT=vnb[s2][:, g * 128:(g + 1) * 128], rhs=swb[:, g, :], start=True, stop=True),
                         reads=[t_vnb[s2], t_swb], writes=[tp])
                    mt = mtmp[g % 2]; tmt = t_mtmp[g % 2]
                    S.op("dve", lambda e, pp=pp, g=g, mt=mt: e.scalar_tensor_tensor(out=mt, in0=pp, scalar=lngc[:, g:g + 1], in1=BIAS[:, g, :], op0=ALU.mult, op1=ALU.add),
                         reads=[tp, t_lngc, t_BIAS], writes=[tmt])
                    S.op("pool", lambda e, g=g, t=t, mt=mt: e.tensor_tensor(out=GU[:, g, t * 128:(t + 1) * 128], in0=GU[:, g, t * 128:(t + 1) * 128], in1=mt, op=ALU.mult),
                         reads=[tmt, t_GU[g][t // 4]], writes=[t_GU[g][t // 4]])
            S.barrier()
            A.release(mV)
            cut4(2)
            mY = A.mark()
            rq = [A.alloc([4, 512], BF16) for _ in range(2)]; t_rq = [Tk(), Tk()]
            acc = [A.alloc([512]) for _ in range(2)]; t_acc = [Tk(), Tk()]
            cntr = 0
            for hp in range(4):
                for hl in range(2):
                    chn = hp * 2 + hl
                    for tb in range(4):
                        s = cntr % 2; cntr += 1
                        S.op("sp", lambda e, sig, s=s, chn=chn, tb=tb: tuple(sig(e.dma_start(out=rq[s][:, j, :].bitcast(F32), in_=rcv[j][chn * 128:(chn + 1) * 128, tb * 256:(tb + 1) * 256])) for j in range(4)),
                             extra=ccs, writes=[t_rq[s]], dma=4, key=f"rq{s}")
                        for j in range(4):
                            def f(e, s=s, j=j):
                                if j == 0:
                                    return e.tensor_scalar(out=acc[s], in0=rq[s][:, 0, :], scalar1=qmask[:, 0:1], scalar2=None, op0=ALU.mult)
                                return e.scalar_tensor_tensor(out=acc[s], in0=rq[s][:, j, :], scalar=qmask[:, j:j + 1], in1=acc[s], op0=ALU.mult, op1=ALU.add)
                            S.op("dve", f, reads=[t_rq[s], t_qmask, t_acc[s]] if j else [t_rq[s], t_qmask], writes=[t_acc[s]])
                        S.op("dve", lambda e, s=s, chn=chn, tb=tb: e.scalar_tensor_tensor(out=SZ[:, chn, tb * 512:(tb + 1) * 512], in0=acc[s], scalar=gngs[:, 0:1], in1=SZ[:, chn, tb * 512:(tb + 1) * 512], op0=ALU.mult, op1=ALU.mult),
                             reads=[t_acc[s], t_gngs, t_SZ[chn][tb]], writes=[t_SZ[chn][tb]])
            S.barrier()
            A.release(mY)
            cut4(3)
            S.barrier()
            mM = A.mark()
            MG = A.alloc([8, OWN], BF16); t_MG = [[Tk() for _ in range(4)] for _ in range(8)]
            sga = [A.alloc([512], BF16) for _ in range(2)]; t_sga = [Tk(), Tk()]
            sgb = [A.alloc([512], BF16) for _ in range(2)]; t_sgb = [Tk(), Tk()]
            m1 = [A.alloc([512]) for _ in range(2)]; t_m1 = [Tk(), Tk()]
            m2 = [A.alloc([512]) for _ in range(2)]; t_m2 = [Tk(), Tk()]
            sub = [(wstg[i][:, :, j * 128:(j + 1) * 128], wbf[i][:, :, j * 128:(j + 1) * 128], Tk(), Tk()) for i in range(2) for j in range(4)]
            subc = [0]

            def stream_small(src_cols):
                stg, bfw, tst, tbf = sub[subc[0] % 8]
                subc[0] += 1
                v = src_cols.rearrange("(k p) n -> p k n", p=128)
                S.op("sp", lambda e, sig: sig(e.dma_start(out=stg, in_=v)), writes=[tst], dma=1, key=f"sub{(subc[0] - 1) % 8}")
                S.op("pool", lambda e: e.tensor_copy(out=bfw, in_=stg), reads=[tst], writes=[tbf])
                return bfw, tbf
            cntr = 0
            for dc in range(8):
                ws = [stream_small(w_a[:, dc * 128:(dc + 1) * 128]), stream_small(w_b[:, dc * 128:(dc + 1) * 128]),
                      stream_small(w_rest[:, 3072 + dc * 128:3072 + (dc + 1) * 128]), stream_small(w_rest[:, 4096 + dc * 128:4096 + (dc + 1) * 128])]
                for tb in range(4):
                    s = cntr % 2; cntr += 1
                    outs = []
                    for wi, (src, tsrc) in enumerate(((SZ, t_SZ), (GU, t_GU), (hxo, None), (hxo, None))):
                        wv_, tw_ = ws[wi]
                        pp, tp = p512.get()

                        def f(e, pp=pp, wv_=wv_, src=src, tb=tb):
                            for k in range(8):
                                i = e.matmul(pp, lhsT=wv_[:, k, :], rhs=src[:, k, tb * 512:(tb + 1) * 512], start=(k == 0), stop=(k == 7))
                            return i
                        rds = [tw_] + ([tsrc[k][tb] for k in range(8)] if tsrc is not None else t_hxo[tb * 4:(tb + 1) * 4])
                        S.op("pe", f, reads=rds, writes=[tp])
                        outs.append((pp, tp))
                    S.op("act", lambda e, s=s, pp=outs[2][0]: e.activation(out=sga[s], in_=pp, func=AF.Sigmoid), reads=[outs[2][1]], writes=[t_sga[s]])
                    S.op("act", lambda e, s=s, pp=outs[3][0]: e.activation(out=sgb[s], in_=pp, func=AF.Sigmoid), reads=[outs[3][1]], writes=[t_sgb[s]])
                    S.op("dve", lambda e, s=s, pp=outs[0][0]: e.tensor_tensor(out=m1[s], in0=pp, in1=sga[s], op=ALU.mult), reads=[outs[0][1], t_sga[s]], writes=[t_m1[s]])
                    S.op("dve", lambda e, s=s, pp=outs[1][0]: e.tensor_tensor(out=m2[s], in0=pp, in1=sgb[s], op=ALU.mult), reads=[outs[1][1], t_sgb[s]], writes=[t_m2[s]])
                    S.op("pool", lambda e, s=s, dc=dc, tb=tb: e.tensor_tensor(out=MG[:, dc, tb * 512:(tb + 1) * 512], in0=m1[s], in1=m2[s], op=ALU.add),
                         reads=[t_m1[s], t_m2[s]], writes=[t_MG[dc][tb]])
            S.barrier()
            if stage == 4:
                for i_, (buf_, tk_) in enumerate(((SZ, t_SZ), (GU, t_GU), (MG, t_MG))):
                    for h_ in range(2):
                        dumpb(buf_[:, h_ * 4:(h_ + 1) * 4, :].rearrange("p a b -> p (a b)"), i_ * 256 + h_ * 128, 128, 0, SEQ)
                raise _Stop()
            hx2 = hxo; t_hx2 = [Tk() for _ in range(NOT)]
            Amain = A
            A = Arena(arena_t, 16384, base=offS)
            wo_b = A.alloc([8, D], BF16); t_wo_b = Tk()
            for hh in range(2):
                sl = hh
                S.op("sp", lambda e, sig, hh=hh, sl=sl: (sig(e.dma_start(out=wstg[sl][:, 0:4, :], in_=w_out.ap().rearrange("(k p) n -> p k n", p=128)[:, 0:4, hh * 512:(hh + 1) * 512])),
                                                        sig(e.dma_start(out=wstg[sl][:, 4:8, :], in_=w_out.ap().rearrange("(k p) n -> p k n", p=128)[:, 4:8, hh * 512:(hh + 1) * 512]))),
                     writes=[t_wstg[sl]], dma=2, key=f"wstg{sl}")
                for k in range(8):
                    S.op("pool", lambda e, k=k, hh=hh, sl=sl: e.tensor_tensor(out=wo_b[:, k, hh * 512:(hh + 1) * 512], in0=wstg[sl][:, k, :], in1=gmix[:, hh * 512:(hh + 1) * 512], op=ALU.mult),
                         reads=[t_wstg[sl], t_gmix], writes=[t_wo_b])
            rwf = A.alloc([8, 36]); t_rwf = Tk(); load(rwf, rw.ap().rearrange("(k p) n -> p k n", p=128), t_rwf, "c15")
            rbb = A.alloc([36]); t_rbb = Tk(); load(rbb, rb[:, :], t_rbb, "c16")
            x1 = [A.alloc([D]) for _ in range(2)]; t_x1 = [Tk(), Tk()]
            xt_D = [A.alloc([D]) for _ in range(2)]; t_xt_D = [Tk(), Tk()]
            junk_D = A.alloc([D]); t_junk_D = Tk()
            ssq_D = [A.alloc([1]) for _ in range(2)]; t_ssq_D = [Tk(), Tk()]
            xsf = [A.alloc([8, 128]) for _ in range(2)]; t_xsf = [Tk(), Tk()]
            hxf = [A.alloc([8, 128]) for _ in range(2)]; t_hxf = [Tk(), Tk()]
            rs_ = [A.alloc([64]) for _ in range(2)]; t_rs = [Tk(), Tk()]
            x1_ops = []
            for t in range(NOT):
                s2 = t % 2
                S.op("sp", lambda e, sig, t=t, s2=s2: sig(e.dma_start(out=xt_D[s2], in_=xo[t * 128:(t + 1) * 128, :])), writes=[t_xt_D[s2]], dma=1, key=f"dxt{s2}")
                for hh in range(2):
                    pp, tp = p512.get()

                    def f(e, pp=pp, hh=hh, t=t):
                        for k in range(8):
                            i = e.matmul(pp, lhsT=MG[:, k, t * 128:(t + 1) * 128], rhs=wo_b[:, k, hh * 512:(hh + 1) * 512], start=(k == 0), stop=(k == 7))
                        return i
                    S.op("pe", f, reads=[t_wo_b] + [t_MG[k][t // 4] for k in range(8)], writes=[tp])
                    S.op("dve", lambda e, pp=pp, hh=hh, s2=s2: e.tensor_tensor(out=x1[s2][:, hh * 512:(hh + 1) * 512], in0=pp, in1=xt_D[s2][:, hh * 512:(hh + 1) * 512], op=ALU.add),
                         reads=[tp, t_xt_D[s2]], writes=[t_x1[s2]])
                o_ = S.op("pool", lambda e, sig, t=t, s2=s2: sig(e.dma_start(out=x1d[t * 128:(t + 1) * 128, :], in_=x1[s2])), reads=[t_x1[s2]], dma=1, key=f"x1d{s2}")
                x1_ops.append(o_)
                S.op("act", lambda e, s2=s2: e.activation(out=junk_D, in_=x1[s2], func=AF.Square, accum_out=ssq_D[s2]), reads=[t_x1[s2]], writes=[t_ssq_D[s2]])
                S.op("act", lambda e, s2=s2: e.activation(out=ssq_D[s2], in_=ssq_D[s2], func=AF.Sqrt, scale=1.0 / D, bias=EPS), reads=[t_ssq_D[s2]], writes=[t_ssq_D[s2]])
                S.op("dve", lambda e, s2=s2: e.reciprocal(out=ssq_D[s2], in_=ssq_D[s2]), reads=[t_ssq_D[s2]], writes=[t_ssq_D[s2]])
                S.op("pool", lambda e, s2=s2: e.tensor_scalar(out=xsf[s2].rearrange("p a b -> p (a b)"), in0=x1[s2], scalar1=ssq_D[s2], scalar2=1.0, op0=ALU.mult, op1=ALU.mult),
                     reads=[t_x1[s2], t_ssq_D[s2]], writes=[t_xsf[s2]])
                for q4 in range(2):
                    pp, tp = p512.get()

                    def tr(e, pp=pp, q4=q4, s2=s2):
                        for k in range(4):
                            i = e.transpose(out=pp[:, k * 128:(k + 1) * 128], in_=xsf[s2][:, q4 * 4 + k, :], identity=identf)
                        return i
                    S.op("pe", tr, reads=[t_xsf[s2], t_identf], writes=[tp])

                    def ev(e, pp=pp, q4=q4, s2=s2, t=t):
                        for k in range(4):
                            kk = q4 * 4 + k
                            i = e.tensor_scalar(out=hxf[s2][:, kk, :], in0=pp[:, k * 128:(k + 1) * 128], scalar1=modc[:, 5, kk:kk + 1], scalar2=modc[:, 4, kk:kk + 1], op0=ALU.mult, op1=ALU.add)
                        return i
                    S.op("dve", ev, reads=[tp, t_modc], writes=[t_hxf[s2]])
                S.op("pool", lambda e, s2=s2, t=t: e.tensor_copy(out=hx2[:, :, t * 128:(t + 1) * 128], in_=hxf[s2]), reads=[t_hxf[s2]], writes=[t_hx2[t]])
                pr, tpr = p128.get()

                def rt(e, pr=pr, s2=s2):
                    for k in range(8):
                        i = e.matmul(pr[:, 0:36], lhsT=hxf[s2][:, k, :], rhs=rwf[:, k, :], start=(k == 0), stop=(k == 7))
                    return i
                S.op("pe", rt, reads=[t_hxf[s2], t_rwf], writes=[tpr])
                r = rs_[s2]; tr_ = t_rs[s2]
                S.op("dve", lambda e, pr=pr, r=r: e.tensor_tensor(out=r[:, 0:36], in0=pr[:, 0:36], in1=rbb, op=ALU.add), reads=[tpr, t_rbb], writes=[tr_])
                S.op("dve", lambda e, r=r: e.tensor_reduce(out=r[:, 40:41], in_=r[:, 0:4], axis=mybir.AxisListType.X, op=ALU.max), reads=[tr_], writes=[tr_])
                S.op("dve", lambda e, r=r: e.tensor_scalar(out=r[:, 36:40], in0=r[:, 0:4], scalar1=r[:, 40:41], scalar2=None, op0=ALU.is_ge), reads=[tr_], writes=[tr_])
                S.op("dve", lambda e, r=r: e.tensor_scalar(out=r[:, 60:64], in0=r[:, 0:4], scalar1=r[:, 40:41], scalar2=None, op0=ALU.subtract), reads=[tr_], writes=[tr_])
                S.op("act", lambda e, r=r: e.activation(out=r[:, 60:64], in_=r[:, 60:64], func=AF.Exp, accum_out=r[:, 41:42]), reads=[tr_], writes=[tr_])
                S.op("dve", lambda e, r=r: e.reciprocal(out=r[:, 41:42], in_=r[:, 41:42]), reads=[tr_], writes=[tr_])
                S.op("dve", lambda e, r=r: e.tensor_scalar(out=r[:, 42:50], in0=r[:, 4:12], scalar1=r[:, 36:37], scalar2=None, op0=ALU.mult), reads=[tr_], writes=[tr_])
                for g in range(1, 4):
                    S.op("dve", lambda e, r=r, g=g: e.scalar_tensor_tensor(out=r[:, 42:50], in0=r[:, 4 + 8 * g:12 + 8 * g], scalar=r[:, 36 + g:37 + g], in1=r[:, 42:50], op0=ALU.mult, op1=ALU.add),
                         reads=[tr_], writes=[tr_])
                S.op("dve", lambda e, r=r: e.tensor_reduce(out=r[:, 50:51], in_=r[:, 42:50], axis=mybir.AxisListType.X, op=ALU.max), reads=[tr_], writes=[tr_])
                S.op("dve", lambda e, r=r: e.tensor_scalar(out=r[:, 42:50], in0=r[:, 42:50], scalar1=r[:, 50:51], scalar2=None, op0=ALU.subtract), reads=[tr_], writes=[tr_])
                S.op("act", lambda e, r=r: e.activation(out=r[:, 42:50], in_=r[:, 42:50], func=AF.Exp), reads=[tr_], writes=[tr_])
                S.op("dve", lambda e, r=r: e.tensor_scalar(out=r[:, 52:60], in0=r[:, 42:50], scalar1=1.0, scalar2=None, op0=ALU.is_ge), reads=[tr_], writes=[tr_])
                S.op("dve", lambda e, r=r: e.scalar_tensor_tensor(out=r[:, 4:12], in0=r[:, 52:60], scalar=-2.0, in1=r[:, 42:50], op0=ALU.mult, op1=ALU.add), reads=[tr_], writes=[tr_])
                S.op("dve", lambda e, r=r: e.tensor_reduce(out=r[:, 51:52], in_=r[:, 4:12], axis=mybir.AxisListType.X, op=ALU.max), reads=[tr_], writes=[tr_])
                S.op("dve", lambda e, r=r: e.tensor_scalar(out=r[:, 12:20], in0=r[:, 4:12], scalar1=r[:, 51:52], scalar2=None, op0=ALU.is_ge), reads=[tr_], writes=[tr_])
                S.op("dve", lambda e, r=r: e.scalar_tensor_tensor(out=r[:, 20:28], in0=r[:, 12:20], scalar=r[:, 51:52], in1=r[:, 52:60], op0=ALU.mult, op1=ALU.add), reads=[tr_], writes=[tr_])
                S.op("dve", lambda e, r=r: e.tensor_scalar(out=r[:, 50:51], in0=r[:, 51:52], scalar1=1.0, scalar2=None, op0=ALU.add), reads=[tr_], writes=[tr_])
                S.op("dve", lambda e, r=r: e.reciprocal(out=r[:, 50:51], in_=r[:, 50:51]), reads=[tr_], writes=[tr_])
                S.op("dve", lambda e, r=r: e.tensor_tensor(out=r[:, 50:51], in0=r[:, 50:51], in1=r[:, 41:42], op=ALU.mult), reads=[tr_], writes=[tr_])
                S.op("dve", lambda e, r=r: e.tensor_scalar(out=r[:, 20:28], in0=r[:, 20:28], scalar1=r[:, 50:51], scalar2=None, op0=ALU.mult), reads=[tr_], writes=[tr_])
                for g in range(4):
                    S.op("dve", lambda e, r=r, g=g, t=t: e.tensor_scalar(out=GT[:, t, g * 8:(g + 1) * 8], in0=r[:, 20:28], scalar1=r[:, 36 + g:37 + g], scalar2=None, op0=ALU.mult),
                         reads=[tr_], writes=[t_GT[t]])
            S.barrier()
            A = Amain
            A.release(markH)
            if stage == 5:
                dump(GT.rearrange("p a b -> p (a b)"), 0, 512)
                for i_ in range(4):
                    o_ = S.op("sp", lambda e, sig, i_=i_: sig(e.dma_start(out=y[i_ * 512:(i_ + 1) * 512, :], in_=x1d[i_ * 512:(i_ + 1) * 512, :])), extra=x1_ops, dma=1, key="dbg")
                    dump_ops.append(o_)
                dumpb(hx2[:, 0:4, :].rearrange("p a b -> p (a b)"), 0, 128, 0, SEQ)
                raise _Stop()
            ACC = A.alloc([NOT, D]); t_ACC = [[Tk(), Tk()] for _ in range(NOT)]
            for t in range(NOT):
                S.op("pool", lambda e, t=t: e.memset(ACC[:, t, :], 0.0), writes=t_ACC[t])
            e1f = A.alloc([8, 512]); t_e1f = Tk()
            e2f = A.alloc([2, D]); t_e2f = Tk()
            e1b = [A.alloc([8, 512], BF16) for _ in range(2)]; t_e1b = [Tk(), Tk()]
            e2b = [A.alloc([2, D], BF16) for _ in range(2)]; t_e2b = [Tk(), Tk()]
            sil = [A.alloc([512], BF16) for _ in range(2)]; t_sil = [Tk(), Tk()]
            hid = [A.alloc([512], BF16) for _ in range(4)]; t_hid = [Tk() for _ in range(4)]
            hc = 0
            for ex in range(NE):
                s = ex % 2
                v1 = ew1[ex].rearrange("(k p) n -> p k n", p=128); v3 = ew3[ex].rearrange("(k p) n -> p k n", p=128)
                v2 = ew2[ex].rearrange("(k p) n -> p k n", p=128)
                S.op("sp", lambda e, sig, v1=v1, v3=v3: (sig(e.dma_start(out=e1f[:, :, 0:256], in_=v1)), sig(e.dma_start(out=e1f[:, :, 256:512], in_=v3))), writes=[t_e1f], dma=2, key="e1f")
                S.op("sp", lambda e, sig, v2=v2: sig(e.dma_start(out=e2f, in_=v2)), writes=[t_e2f], dma=1, key="e2f")
                S.op("pool", lambda e, s=s: e.tensor_copy(out=e1b[s], in_=e1f), reads=[t_e1f], writes=[t_e1b[s]])
                S.op("pool", lambda e, s=s: e.tensor_copy(out=e2b[s], in_=e2f), reads=[t_e2f], writes=[t_e2b[s]])
                for tb in range(4):
                    hs = []
                    for fc in range(2):
                        p1, tp1 = p512.get(); p3, tp3 = p512.get()

                        def f1(e, p1=p1, s=s, fc=fc, tb=tb):
                            for k in range(8):
                                i = e.matmul(p1, lhsT=e1b[s][:, k, fc * 128:(fc + 1) * 128], rhs=hx2[:, k, tb * 512:(tb + 1) * 512], start=(k == 0), stop=(k == 7))
                            return i

                        def f3(e, p3=p3, s=s, fc=fc, tb=tb):
                            for k in range(8):
                                i = e.matmul(p3, lhsT=e1b[s][:, k, 256 + fc * 128:256 + (fc + 1) * 128], rhs=hx2[:, k, tb * 512:(tb + 1) * 512], start=(k == 0), stop=(k == 7))
                            return i
                        S.op("pe", f1, reads=[t_e1b[s]] + t_hx2[tb * 4:(tb + 1) * 4], writes=[tp1])
                        S.op("pe", f3, reads=[t_e1b[s]] + t_hx2[tb * 4:(tb + 1) * 4], writes=[tp3])
                        ss = hc % 2; h4 = hc % 4; hc += 1
                        S.op("act", lambda e, p1=p1, ss=ss: e.activation(out=sil[ss], in_=p1, func=AF.Silu), reads=[tp1], writes=[t_sil[ss]])
                        S.op("dve", lambda e, p3=p3, ss=ss, h4=h4: e.tensor_tensor(out=hid[h4], in0=p3, in1=sil[ss], op=ALU.mult), reads=[tp3, t_sil[ss]], writes=[t_hid[h4]])
                        hs.append(h4)
                    for tt in range(4):
                        t = tb * 4 + tt
                        for hh in range(2):
                            po, tpo = p512.get()

                            def f2(e, po=po, s=s, tt=tt, hh=hh, hs=tuple(hs)):
                                for fc in range(2):
                                    i = e.matmul(po, lhsT=hid[hs[fc]][:, tt * 128:(tt + 1) * 128], rhs=e2b[s][:, fc, hh * 512:(hh + 1) * 512], start=(fc == 0), stop=(fc == 1))
                                return i
                            S.op("pe", f2, reads=[t_hid[hs[0]], t_hid[hs[1]], t_e2b[s]], writes=[tpo])
                            S.op("dve", lambda e, po=po, t=t, hh=hh, ex=ex: e.scalar_tensor_tensor(out=ACC[:, t, hh * 512:(hh + 1) * 512], in0=po, scalar=GT[:, t, ex:ex + 1], in1=ACC[:, t, hh * 512:(hh + 1) * 512], op0=ALU.mult, op1=ALU.add),
                                 reads=[tpo, t_ACC[t][hh]], writes=[t_ACC[t][hh]])
            fngb = A.alloc([D]); t_fngb = Tk(); load(fngb, fng_bc[:, :], t_fngb, "c17")
            x1r = [A.alloc([D]) for _ in range(2)]; t_x1r = [Tk(), Tk()]
            x2 = [A.alloc([D]) for _ in range(2)]; t_x2 = [Tk(), Tk()]
            junk_E = A.alloc([D]); t_junk_E = Tk()
            ssq_E = [A.alloc([1]) for _ in range(2)]; t_ssq_E = [Tk(), Tk()]
            outs_ = []
            for t in range(NOT):
                s2 = t % 2
                S.op("sp", lambda e, sig, t=t, s2=s2: sig(e.dma_start(out=x1r[s2], in_=x1d[t * 128:(t + 1) * 128, :])), extra=[x1_ops[t]], writes=[t_x1r[s2]], dma=1, key=f"x1r{s2}")
                S.op("dve", lambda e, t=t, s2=s2: e.tensor_tensor(out=x2[s2], in0=ACC[:, t, :], in1=gffn, op=ALU.mult), reads=t_ACC[t] + [t_gffn], writes=[t_x2[s2]])
                S.op("dve", lambda e, s2=s2: e.tensor_tensor(out=x2[s2], in0=x2[s2], in1=x1r[s2], op=ALU.add), reads=[t_x2[s2], t_x1r[s2]], writes=[t_x2[s2]])
                S.op("act", lambda e, s2=s2: e.activation(out=junk_E, in_=x2[s2], func=AF.Square, accum_out=ssq_E[s2]), reads=[t_x2[s2]], writes=[t_ssq_E[s2]])
                S.op("act", lambda e, s2=s2: e.activation(out=ssq_E[s2], in_=ssq_E[s2], func=AF.Sqrt, scale=1.0 / D, bias=EPS), reads=[t_ssq_E[s2]], writes=[t_ssq_E[s2]])
                S.op("dve", lambda e, s2=s2: e.reciprocal(out=ssq_E[s2], in_=ssq_E[s2]), reads=[t_ssq_E[s2]], writes=[t_ssq_E[s2]])
                S.op("dve", lambda e, s2=s2: e.scalar_tensor_tensor(out=x2[s2], in0=x2[s2], scalar=ssq_E[s2], in1=fngb, op0=ALU.mult, op1=ALU.mult), reads=[t_x2[s2], t_ssq_E[s2], t_fngb], writes=[t_x2[s2]])
                o_ = S.op("sp", lambda e, sig, t=t, s2=s2: sig(e.dma_start(out=y[t * 128:(t + 1) * 128, :], in_=x2[s2])), reads=[t_x2[s2]], dma=1, key=f"yo{s2}")
                outs_.append(o_)
            S.op("sp", None, extra=outs_)
        try:
            author()
        except _Stop:
            pass
        if dump_ops:
            S.op("sp", None, extra=dump_ops)
        S.emit(block, sems)
    return nc


def _prep(inputs):
    f = lambda a: np.ascontiguousarray(np.asarray(a, dtype=np.float32))
    x = f(inputs["x"]); c = f(inputs["c"]); ctx = f(inputs["ctx"]); c_ctx = f(inputs["c_ctx"])
    w_in = f(inputs["w_in"])[0]
    conv_w = f(inputs["conv_w"])[0]
    a_log = f(inputs["a_log"])[0]; dt_bias = f(inputs["dt_bias"])[0]
    idx = np.arange(128)
    tri = np.stack([(idx[:, None] <= idx[None, :]), (idx[:, None] > idx[None, :]), (idx[:, None] >= idx[None, :]), (idx[:, None] < idx[None, :])]).astype(np.float32)
    negm = np.stack([(idx[:, None] <= idx[None, :]), (idx[None, :] < idx[:, None]), (idx[:, None] >= idx[None, :]), (idx[None, :] > idx[:, None])]).astype(np.float32) * -100.0
    blk = lambda n: (idx[:, None] // n == idx[None, :] // n)
    dcm = [blk(8)]
    for n in (16, 32, 64, 128):
        low = blk(n) & ((idx[:, None] % n) >= n // 2) & ((idx[None, :] % n) < n // 2)
        dcm += [low, low.T]
    import ml_dtypes
    dcm = np.stack(dcm).astype(np.float32).astype(ml_dtypes.bfloat16)
    common = {
        "dcm": dcm,
        "ada_w": f(inputs["ada_w"])[0], "ada_b": f(inputs["ada_b"]).reshape(1, -1),
        "gcols": np.ascontiguousarray(np.stack([f(inputs["norm_mix_g"])[0].reshape(8, 128).T, f(inputs["norm_ffn_g"])[0].reshape(8, 128).T], axis=-1)),
        "gng": f(inputs["gdn_norm_g"])[0].reshape(128, 1),
        "ident": np.eye(128, dtype=np.float32), "tri": tri, "negm": negm,
        "w_rest": np.ascontiguousarray(w_in[:, COL_Z:]),
        "sgu_wT": np.ascontiguousarray(f(inputs["sgu_w"])[0].transpose(0, 2, 1)),
        "sgu_bb": np.ascontiguousarray(np.broadcast_to(f(inputs["sgu_b"])[0][None], (128, 8, 128))),
        "lng_c": np.ascontiguousarray(f(inputs["sgu_ln_g"])[0].reshape(8, 128).T),
        "lnb_bc": np.ascontiguousarray(np.broadcast_to(f(inputs["sgu_ln_b"])[0][None], (128, D))),
        "w_a": f(inputs["w_branch_a"])[0], "w_b": f(inputs["w_branch_b"])[0], "w_out": f(inputs["w_out"])[0],
        "rw": np.ascontiguousarray(np.concatenate([f(inputs["router_group_w"])[0], f(inputs["router_expert_w"])[0]], axis=1)),
        "rb": np.ascontiguousarray(np.broadcast_to(np.concatenate([f(inputs["router_group_b"])[0], f(inputs["router_expert_b"])[0]])[None], (128, 36))),
        "ew1": f(inputs["expert_w1"])[0], "ew3": f(inputs["expert_w3"])[0], "ew2": f(inputs["expert_w2"])[0],
        "fng_bc": np.ascontiguousarray(np.broadcast_to(f(inputs["final_norm_g"])[None], (128, D))),
    }
    maps = []
    for core in range(8):
        b, r = core // 4, core % 4
        heads = (2 * r, 2 * r + 1)
        qcols = np.concatenate([np.arange(base + h * 128, base + (h + 1) * 128) for base in (0, 1024, 2048) for h in heads])
        bacols = np.array([COL_BETA + d * 8 + h for d in range(2) for h in heads] + [COL_A + d * 8 + h for d in range(2) for h in heads])
        conv_c = np.ascontiguousarray(conv_w[:, qcols].reshape(5, 6, 128).transpose(2, 1, 0))
        gsc = np.concatenate([np.array([a_log[d, h] for d in range(2) for h in heads]), np.array([dt_bias[d, h] for d in range(2) for h in heads])]).astype(np.float32)
        qm = np.zeros((128, 4), np.float32); qm[:, r] = 1.0
        m = dict(common)
        m.update({
            "xb": x[b], "ctxb": ctx[b], "xo": np.ascontiguousarray(x[b, r * OWN:(r + 1) * OWN]),
            "cvT": np.ascontiguousarray(np.stack([c[b].reshape(8, 128).T, c_ctx.reshape(8, 128).T], axis=-1)),
            "w_qkv": np.ascontiguousarray(w_in[:, qcols]), "w_ba": np.ascontiguousarray(w_in[:, bacols]),
            "gsc66": np.ascontiguousarray(np.broadcast_to(gsc.reshape(1, 2, 1, 4), (128, 2, NT, 4))),
            "conv_c": conv_c, "gsc": np.ascontiguousarray(np.broadcast_to(gsc[None], (128, 8))), "qmask": qm,
        })
        maps.append(m)
    return maps


_NC = None


def kernel(**inputs):
    global _NC
    if _NC is None:
        _NC = build_program()
    maps = _prep(inputs)
    res = run_bass_kernel_spmd(_NC, maps, core_ids=list(range(8)))
    out = np.zeros((2, SEQ, D), np.float32)
    for core in range(8):
        b, r = core // 4, core % 4
        out[b, r * OWN:(r + 1) * OWN] = np.asarray(res.results[core]["y"], dtype=np.float32)
    return out
```

```python
import contextlib
import os
import numpy as np
import concourse.bass as bass
import concourse.mybir as mybir
from concourse.bass_utils import run_bass_kernel_spmd

F32 = mybir.dt.float32
BF16 = mybir.dt.bfloat16
ALU = mybir.AluOpType
AF = mybir.ActivationFunctionType

D = 1024
SEQ = 8192
CTX = 256
NT = 66
OWN = 2048
NOT = 16
NE = 32
DE = 256
COL_BETA = 3072
COL_A = COL_BETA + 16
COL_Z = COL_A + 16
EPS = 1e-6
ARENA = 53000


SAME_ENGINE_WAITS = os.environ.get('SAMEENG', '1') == '1'


class Tk:
    __slots__ = ("name", "w", "rd", "excl")

    def __init__(self, name="", excl=False):
        self.name = name
        self.w = None
        self.rd = []
        self.excl = excl


class Op:
    __slots__ = ("eng", "fn", "deps", "used", "sem", "val", "dma", "key", "idx", "inc")


class Sched:
    ENGS = ("pe", "act", "dve", "pool", "sp")

    def __init__(self, nc):
        self.nc = nc
        self.ops = {e: [] for e in self.ENGS}
        self.all = []
        self.dma_since_barrier = []

    def op(self, eng, fn, reads=(), writes=(), dma=0, key=None, extra=(), inc=16):
        o = Op()
        o.eng = eng; o.fn = fn; o.used = False; o.sem = None; o.val = None
        o.dma = dma; o.key = key; o.idx = len(self.all); o.inc = inc
        deps = set(extra)
        reads = list(reads); writes = list(writes)
        for r in list(reads):
            if r.excl and r not in writes:
                writes.append(r)
        for r in reads:
            if r.w is not None:
                deps.add(r.w)
        for w in writes:
            if w.w is not None:
                deps.add(w.w)
            for x in w.rd:
                deps.add(x)
        for r in reads:
            r.rd.append(o)
        for w in writes:
            w.w = o
            w.rd = []
        o.deps = [d for d in deps if d is not o and not (d.eng == "pe" and eng == "pe" and not d.dma and not dma)
                  and not (SAME_ENGINE_WAITS is False and d.eng == eng and eng in ("act", "dve", "pool") and not d.dma and not dma)]
        for d in o.deps:
            d.used = True
        if dma:
            assert key is not None
            self.dma_since_barrier.append(o)
        self.ops[eng].append(o)
        self.all.append(o)
        return o

    def barrier(self):
        last = [self.ops[e][-1] for e in self.ENGS if self.ops[e] and self.ops[e][-1].fn is not None]
        dmas = list(self.dma_since_barrier)
        self.dma_since_barrier = []
        for e in self.ENGS:
            self.op(e, None, extra=[x for x in last if x.eng != e] + dmas)

    def emit(self, block, sems):
        nc = self.nc
        sems = list(sems)
        eng_sem = {e: sems.pop() for e in ("pe", "act", "dve", "pool")}
        keysem = {}
        cnt = {e: 0 for e in eng_sem}
        kcnt = {}
        for o in self.all:
            if o.dma:
                if o.key not in keysem:
                    keysem[o.key] = sems.pop()
                    kcnt[o.key] = 0
                kcnt[o.key] += o.inc * o.dma
                o.sem = keysem[o.key]; o.val = kcnt[o.key]
            elif o.used:
                assert o.fn is not None
                cnt[o.eng] += 1
                o.sem = eng_sem[o.eng]; o.val = cnt[o.eng]
        engobj = {"pe": nc.tensor, "act": nc.scalar, "dve": nc.vector, "pool": nc.gpsimd, "sp": nc.sync}
        deco = {"pe": block.tensor, "act": block.scalar, "dve": block.vector, "pool": block.gpsimd, "sp": block.sync}

        def run(ename):
            def body(e):
                known = {}
                for o in self.ops[ename]:
                    for d in sorted(o.deps, key=lambda x: x.idx):
                        sid = id(d.sem)
                        if known.get(sid, 0) >= d.val:
                            continue
                        e.wait_ge(d.sem, d.val)
                        known[sid] = d.val
                    if o.fn is None:
                        continue
                    if o.dma:
                        n = [0]

                        def sig(inst, o=o, n=n):
                            if o.inc == 16:
                                inst.then_inc(o.sem, 16)
                            else:
                                inst.then_inc(o.sem)
                            n[0] += 1
                            return inst
                        o.fn(e, sig)
                        assert n[0] == o.dma, (n[0], o.dma)
                    else:
                        inst = o.fn(e)
                        if o.used:
                            inst.then_inc(o.sem, 1)
            return body
        for ename in self.ENGS:
            deco[ename](run(ename))


class Arena:
    def __init__(self, ap_f32, nf32, base=0):
        self.ap = ap_f32
        self.n = base + nf32
        self.off = base
        self.hi = 0

    def mark(self):
        return self.off

    def release(self, m):
        self.off = m

    def alloc(self, free_shape, dtype=F32):
        n = int(np.prod(free_shape))
        nf = n if dtype == F32 else (n + 1) // 2
        nf = (nf + 1) // 2 * 2
        assert self.off + nf <= self.n, ("arena overflow", self.off, nf, self.n)
        v = self.ap[:, self.off:self.off + nf]
        self.off += nf
        self.hi = max(self.hi, self.off)
        if dtype != F32:
            v = v.bitcast(dtype)[:, 0:n]
        else:
            v = v[:, 0:n]
        if len(free_shape) == 2:
            v = v.rearrange("p (a b) -> p a b", a=free_shape[0])
        elif len(free_shape) == 3:
            v = v.rearrange("p (a b c) -> p a b c", a=free_shape[0], b=free_shape[1])
        return v


class _Stop(Exception):
    pass


def build_program(debug=False, stage=99):
    KCUT = int(os.environ.get('KCUT', '0')); NTL = int(os.environ.get('NTL', str(NT)))
    nc = bass.Bass("TRN2", target_bir_lowering=False)

    def din(name, shape, dt=F32):
        return nc.dram_tensor(name, list(shape), dt, kind="ExternalInput")

    xb = din("xb", [SEQ, D]); ctxb = din("ctxb", [CTX, D]); xo = din("xo", [OWN, D])
    cvT = din("cvT", [128, 8, 2]); ada_w = din("ada_w", [D, 6 * D]); ada_b = din("ada_b", [1, 6 * D])
    gcols = din("gcols", [128, 8, 2])
    w_qkv = din("w_qkv", [D, 768]); w_ba = din("w_ba", [D, 8])
    gsc66 = din("gsc66", [128, 2, NT, 4])
    conv_c = din("conv_c", [128, 6, 5]); gsc = din("gsc", [128, 8]); gng = din("gng", [128, 1])
    dcm_d = din("dcm", [9, 128, 128], BF16)
    ident_d = din("ident", [128, 128]); tri_d = din("tri", [4, 128, 128]); neg_d = din("negm", [4, 128, 128])
    w_rest = din("w_rest", [D, 5120]); sgu_wT = din("sgu_wT", [8, 128, 128]); sgu_bb = din("sgu_bb", [128, 8, 128])
    lng_c = din("lng_c", [128, 8]); lnb_bc = din("lnb_bc", [128, D])
    w_a = din("w_a", [D, D]); w_b = din("w_b", [D, D]); w_out = din("w_out", [D, D])
    rw = din("rw", [D, 36]); rb = din("rb", [128, 36])
    ew1 = din("ew1", [NE, D, DE]); ew3 = din("ew3", [NE, D, DE]); ew2 = din("ew2", [NE, DE, D])
    fng_bc = din("fng_bc", [128, D]); qmask_d = din("qmask", [128, 4])
    y = nc.dram_tensor("y", [OWN, D], F32, kind="ExternalOutput")
    snd = [nc.dram_tensor(f"snd{j}", [2 * 128, 1024], F32) for j in range(4)]
    rcv = [nc.dram_tensor(f"rcv{j}", [4 * 2 * 128, 1024], F32) for j in range(4)]
    x1d = nc.dram_tensor("x1d", [OWN, D], F32)
    dbg = None
    if debug:
        dbg = nc.dram_tensor("dbg", [128, 4096], F32, kind="ExternalOutput")
        dbgb = nc.dram_tensor("dbgb", [768, SEQ], BF16, kind="ExternalOutput")

    es = contextlib.ExitStack()
    with es:
        arena_t = es.enter_context(nc.sbuf_tensor("arena", [128, ARENA], F32))
        psb = [es.enter_context(nc.psum_tensor(f"psb{i}", [128, 512], F32)) for i in range(8)]
        sems = [es.enter_context(nc.semaphore(f"s{i}")) for i in range(100)]
        block = es.enter_context(nc.Block())
        A = Arena(arena_t, ARENA)
        S = Sched(nc)

        dump_ops = []

        def dump(ap, col0, ncols, reads=()):
            o_ = S.op("sp", lambda e, sig: sig(e.dma_start(out=dbg[:, col0:col0 + ncols], in_=ap)), reads=list(reads), dma=1, key="dbg")
            dump_ops.append(o_)

        def cut(k, dumps):
            if stage == k:
                S.barrier()
                for d_ in dumps():
                    dump(*d_)
                raise _Stop()

        def dumpb(ap, row0, nrows, col0, ncols, reads=(), extra=()):
            o_ = S.op("sp", lambda e, sig: sig(e.dma_start(out=dbgb[row0:row0 + nrows, col0:col0 + ncols], in_=ap)), reads=list(reads), extra=list(extra), dma=1, key="dbg")
            dump_ops.append(o_)

        def author():
            nonlocal A
            class Pool:
                def __init__(self, items):
                    self.items = items
                    self.i = 0

                def get(self):
                    it = self.items[self.i % len(self.items)]
                    self.i += 1
                    return it
            p128 = Pool([(psb[b][:, 0:128], Tk(f"p128_{b}", excl=True)) for b in range(4)])
            p512 = Pool([(psb[b][:, :], Tk(f"p512_{b}", excl=True)) for b in range(4, 8)])

            def bfv(ap):
                n = ap.shape[-1]
                return ap.bitcast(BF16)[:, 0:n]

            def load(dst, src, tk, key):
                return S.op("sp", lambda e, sig: sig(e.dma_start(out=dst, in_=src)), writes=[tk], dma=1, key=key)

            identf = A.alloc([128]); t_identf = Tk(); load(identf, ident_d[:, :], t_identf, "c0")
            identb = A.alloc([128], BF16); t_identb = Tk()
            S.op("pool", lambda e: e.tensor_copy(out=identb, in_=identf), reads=[t_identf], writes=[t_identb])
            trif = A.alloc([4, 128]); t_trif = Tk(); load(trif, tri_d.ap().rearrange("a p q -> p a q"), t_trif, "c1")
            negf = A.alloc([4, 128]); t_negf = Tk(); load(negf, neg_d.ap().rearrange("a p q -> p a q"), t_negf, "c2")
            negb = A.alloc([4, 128], BF16); t_negb = Tk()
            S.op("pool", lambda e: e.tensor_copy(out=negb, in_=negf), reads=[t_negf], writes=[t_negb])
            dcm = A.alloc([9, 128], BF16); t_dcm = Tk(); load(dcm, dcm_d.ap().rearrange("a p q -> p a q"), t_dcm, "c2b")
            onesf = A.alloc([128]); t_onesf = Tk()
            S.op("pool", lambda e: e.memset(onesf, 1.0), writes=[t_onesf])
            Uf, Vf, Ub, Vb = (trif[:, i, :] for i in range(4))
            gscs = A.alloc([8]); t_gscs = Tk(); load(gscs, gsc[:, :], t_gscs, "c3")
            negA = A.alloc([4]); t_negA = Tk()
            S.op("act", lambda e: e.activation(out=negA, in_=gscs[:, 0:4], func=AF.Exp), reads=[t_gscs], writes=[t_negA])
            S.op("dve", lambda e: e.tensor_scalar(out=negA, in0=negA, scalar1=-1.0, scalar2=None, op0=ALU.mult), reads=[t_negA], writes=[t_negA])
            dtb = gscs[:, 4:8]
            convc = A.alloc([6, 5]); t_convc = Tk(); load(convc, conv_c[:, :, :], t_convc, "c4")
            gngs = A.alloc([1]); t_gngs = Tk(); load(gngs, gng[:, :], t_gngs, "c5")
            qmask = A.alloc([4]); t_qmask = Tk(); load(qmask, qmask_d[:, :], t_qmask, "c6")
            gcol = A.alloc([8, 2]); t_gcol = Tk(); load(gcol, gcols[:, :, :], t_gcol, "c7")
            modc = A.alloc([6, 8]); t_modc = Tk("modc")
            gmix = A.alloc([D]); t_gmix = Tk("gmix")
            gffn = A.alloc([D]); t_gffn = Tk("gffn")
            GT = A.alloc([NOT, 32]); t_GT = [Tk() for _ in range(NOT)]

            m0 = A.mark()
            scT = A.alloc([8, 2]); t_scT = Tk(); load(scT, cvT[:, :, :], t_scT, "c8")
            S.op("act", lambda e: e.activation(out=scT, in_=scT, func=AF.Silu), reads=[t_scT], writes=[t_scT])
            modrow = A.alloc([6 * D]); t_modrow = Tk("modrow")
            modrow_c = A.alloc([6 * D]); t_modrow_c = Tk("modrow_c")
            adb = A.alloc([6 * D]); t_adb = Tk()
            S.op("sp", lambda e, sig: (sig(e.dma_start(out=adb[0:1, :], in_=ada_b[:, :])), sig(e.dma_start(out=adb[1:2, :], in_=ada_b[:, :]))),
                 writes=[t_adb], dma=2, key="c9")
            adw = [A.alloc([8, 512]) for _ in range(2)]; t_adw = [Tk(), Tk()]
            ada_v = ada_w.ap().rearrange("(k p) n -> p k n", p=128)
            for nb in range(12):
                sl = nb % 2
                S.op("sp", lambda e, sig, nb=nb, sl=sl: (sig(e.dma_start(out=adw[sl][:, 0:4, :], in_=ada_v[:, 0:4, nb * 512:(nb + 1) * 512])),
                                                        sig(e.dma_start(out=adw[sl][:, 4:8, :], in_=ada_v[:, 4:8, nb * 512:(nb + 1) * 512]))),
                     writes=[t_adw[sl]], dma=2, key=f"adw{sl}")
                pp, tp = p512.get()

                def f(e, sl=sl, pp=pp):
                    for k in range(8):
                        i = e.matmul(pp[0:2, :], lhsT=scT[:, k, :], rhs=adw[sl][:, k, :], start=(k == 0), stop=(k == 7))
                    return i
                S.op("pe", f, reads=[t_scT, t_adw[sl]], writes=[tp])
                S.op("dve", lambda e, nb=nb, pp=pp: e.tensor_tensor(out=modrow[0:2, nb * 512:(nb + 1) * 512], in0=pp[0:2, :], in1=adb[0:2, nb * 512:(nb + 1) * 512], op=ALU.add),
                     reads=[tp, t_adb], writes=[t_modrow])
            S.op("sp", lambda e, sig: sig(e.dma_start(out=modrow_c[0:1, :], in_=modrow[1:2, :])), reads=[t_modrow], writes=[t_modrow_c], dma=1, key="c10")
            pp, tp = p128.get()
            vecs = [(modrow, 0), (modrow, 1), (modrow_c, 0), (modrow_c, 1), (modrow, 3), (modrow, 4)]

            def f(e, pp=pp):
                for vi, (row, m) in enumerate(vecs):
                    for k in range(8):
                        i = e.matmul(pp[:, vi * 8 + k:vi * 8 + k + 1], lhsT=row[0:1, m * D + k * 128:m * D + (k + 1) * 128], rhs=onesf[0:1, 0:1], start=True, stop=True)
                return i
            S.op("pe", f, reads=[t_modrow, t_modrow_c, t_onesf], writes=[tp])
            S.op("dve", lambda e, pp=pp: e.tensor_copy(out=modc.rearrange("p a b -> p (a b)"), in_=pp[:, 0:48]), reads=[tp], writes=[t_modc])
            for vi, gi in ((1, 0), (3, 0), (5, 1)):
                S.op("dve", lambda e, vi=vi, gi=gi: e.scalar_tensor_tensor(out=modc[:, vi, :], in0=modc[:, vi, :], scalar=1.0, in1=gcol[:, :, gi], op0=ALU.add, op1=ALU.mult),
                     reads=[t_modc, t_gcol], writes=[t_modc])
            for dst, tdst, m in ((gmix, t_gmix, 2), (gffn, t_gffn, 5)):
                for h in range(2):
                    pp, tp = p512.get()
                    S.op("pe", lambda e, pp=pp, m=m, h=h: e.matmul(pp, lhsT=onesf[0:1, :], rhs=modrow[0:1, m * D + h * 512:m * D + (h + 1) * 512], start=True, stop=True),
                         reads=[t_modrow, t_onesf], writes=[tp])
                    S.op("act", lambda e, pp=pp, dst=dst, h=h: e.copy(out=dst[:, h * 512:(h + 1) * 512], in_=pp), reads=[tp], writes=[tdst])
            S.barrier()
            A.release(m0)
            if stage == 0:
                dump(modc.rearrange("p a b -> p (a b)"), 0, 48); dump(gmix[:, 0:256], 64, 256); dump(gffn[:, 0:256], 320, 256)
                raise _Stop()

            mA = A.mark()
            SC = A.alloc([NT, 28]); t_SC = [Tk("SC")] * NT
            QN = A.alloc([NT, 2, 128], BF16); KN = A.alloc([NT, 2, 128], BF16); VV = A.alloc([NT, 2, 128], BF16)
            t_QKV = [[Tk() for _ in range(6)] for t in range(NT)]
            mA1 = A.mark()
            wst = A.alloc([8, 776]); t_wst = Tk()
            wq = A.alloc([8, 896], BF16); t_wq = Tk()
            S.op("sp", lambda e, sig: (sig(e.dma_start(out=wst[:, :, 0:768], in_=w_qkv.ap().rearrange("(k p) n -> p k n", p=128))),
                                       sig(e.dma_start(out=wst[:, :, 768:776], in_=w_ba.ap().rearrange("(k p) n -> p k n", p=128)))),
                 writes=[t_wst], dma=2, key="wst")
            S.op("pool", lambda e: e.tensor_copy(out=wq[:, :, 0:776], in_=wst), reads=[t_wst], writes=[t_wq])
            xt_A = [A.alloc([D]) for _ in range(3)]; t_xt_A = [Tk() for _ in range(3)]
            junk_A = A.alloc([D]); t_junk_A = Tk()
            ssq_A = [A.alloc([1]) for _ in range(3)]; t_ssq_A = [Tk() for _ in range(3)]
            xs_A = [A.alloc([8, 128], BF16) for _ in range(2)]; t_xs_A = [Tk(), Tk()]
            hxT = [A.alloc([8, 128], BF16) for _ in range(2)]; t_hxT = [Tk(), Tk()]; t_hxTb = [Tk(), Tk()]
            PRE = [A.alloc([6, 132]) for _ in range(3)]; t_PRE = [Tk() for _ in range(3)]
            CV = A.alloc([6, 128]); t_CVc = [Tk() for _ in range(6)]
            SQ = A.alloc([6, 128], BF16); t_SQ = Tk()
            sm = [A.alloc([40]) for _ in range(2)]; t_sm = [Tk(), Tk()]
            nr = [A.alloc([8]) for _ in range(2)]; t_nr = [Tk(), Tk()]

            wst_flat = wst.rearrange("p a b -> p (a b)")
            BA = wst_flat[:, 0:NT * 8].rearrange("p (a b) -> p a b", b=8); t_BA = Tk("BA")
            g66 = A.alloc([2, NT, 4]); t_g66 = Tk(); load(g66, gsc66[:, :, :, :], t_g66, "c3b")
            Mx = [wst_flat[:, 1024 + i_ * 512:1024 + i_ * 512 + NT * 4].rearrange("p (a b) -> p a b", b=4) for i_ in range(4)]; t_Mx = [Tk() for _ in range(4)]

            def rows_of(t):
                if t < 2:
                    return ctxb[t * 128:(t + 1) * 128, :]
                return xb[(t - 2) * 128:(t - 1) * 128, :]

            def front(t):
                s3 = t % 3; s2 = t % 2
                isctx = t < 2
                shc = modc[:, 2 if isctx else 0, :]; scc = modc[:, 3 if isctx else 1, :]
                S.op("sp", lambda e, sig: sig(e.dma_start(out=xt_A[s3], in_=rows_of(t))), writes=[t_xt_A[s3]], dma=1, key=f"xt_A{s3}")
                S.op("act", lambda e: e.activation(out=junk_A, in_=xt_A[s3], func=AF.Square, accum_out=ssq_A[s3]), reads=[t_xt_A[s3]], writes=[t_ssq_A[s3]])
                S.op("act", lambda e: e.activation(out=ssq_A[s3], in_=ssq_A[s3], func=AF.Sqrt, scale=1.0 / D, bias=EPS), reads=[t_ssq_A[s3]], writes=[t_ssq_A[s3]])
                S.op("dve", lambda e: e.reciprocal(out=ssq_A[s3], in_=ssq_A[s3]), reads=[t_ssq_A[s3]], writes=[t_ssq_A[s3]])
                S.op("pool", lambda e: e.tensor_scalar(out=xs_A[s2].rearrange("p a b -> p (a b)"), in0=xt_A[s3], scalar1=ssq_A[s3], scalar2=1.0, op0=ALU.mult, op1=ALU.mult),
                     reads=[t_xt_A[s3], t_ssq_A[s3]], writes=[t_xs_A[s2]])
                pp, tp = p512.get()
                ppb = pp.bitcast(BF16)

                def tr(e):
                    for k in range(8):
                        i = e.transpose(out=ppb[:, k * 128:(k + 1) * 128], in_=xs_A[s2][:, k, :], identity=identb)
                    return i
                S.op("pe", tr, reads=[t_xs_A[s2], t_identb], writes=[tp])

                def ev_d(e):
                    for k in range(8):
                        i = e.tensor_scalar(out=hxT[s2][:, k, :], in0=ppb[:, k * 128:(k + 1) * 128], scalar1=scc[:, k:k + 1], scalar2=shc[:, k:k + 1], op0=ALU.mult, op1=ALU.add)
                    return i
                t_h2 = t_hxTb[s2]
                S.op("dve", ev_d, reads=[tp, t_modc], writes=[t_hxT[s2], t_h2])
                if KCUT == 1:
                    return
                pA, tA = p512.get()
                pB, tB = p512.get()

                def pjA(e):
                    for ch in range(4):
                        for k in range(8):
                            i = e.matmul(pA[:, ch * 128:(ch + 1) * 128], lhsT=wq[:, k, ch * 128:(ch + 1) * 128], rhs=hxT[s2][:, k, :], start=(k == 0), stop=(k == 7))
                    return i

                def pjB(e):
                    for ch in range(4, 6):
                        for k in range(8):
                            i = e.matmul(pB[:, (ch - 4) * 128:(ch - 3) * 128], lhsT=wq[:, k, ch * 128:(ch + 1) * 128], rhs=hxT[s2][:, k, :], start=(k == 0), stop=(k == 7))
                    for k in range(8):
                        i = e.matmul(pB[:, 256:264], lhsT=hxT[s2][:, k, :], rhs=wq[:, k, 768:776], start=(k == 0), stop=(k == 7))
                    return i
                S.op("pe", pjA, reads=[t_wq, t_hxT[s2]], writes=[tA])
                S.op("pe", pjB, reads=[t_wq, t_hxT[s2]], writes=[tB])
                pba = pB[:, 256:264]; tba = tB
                S.op("dve", lambda e: e.tensor_copy(out=PRE[s3][:, 0:4, 2:130], in_=pA.rearrange("p (a b) -> p a b", a=4)), reads=[tA], writes=[t_PRE[s3]])
                S.op("dve", lambda e: e.tensor_copy(out=PRE[s3][:, 4:6, 2:130], in_=pB[:, 0:256].rearrange("p (a b) -> p a b", a=2)), reads=[tB], writes=[t_PRE[s3]])
                if KCUT == 2:
                    return
                first = t in (0, 2); last = t in (1, NT - 1)
                if first:
                    S.op("pool", lambda e: e.memset(PRE[s3][:, :, 0:2], 0.0), writes=[t_PRE[s3]])
                else:
                    sp_ = (t - 1) % 3
                    S.op("pool", lambda e: e.tensor_copy(out=PRE[sp_][:, :, 130:132], in_=PRE[s3][:, :, 2:4]), reads=[t_PRE[s3]], writes=[t_PRE[sp_]])
                if last:
                    S.op("pool", lambda e: e.memset(PRE[s3][:, :, 130:132], 0.0), writes=[t_PRE[s3]])
                else:
                    sn = (t + 1) % 3
                    S.op("pool", lambda e: e.tensor_copy(out=PRE[sn][:, :, 0:2], in_=PRE[s3][:, :, 128:130]), reads=[t_PRE[s3]], writes=[t_PRE[sn]])
                S.op("dve", lambda e: e.tensor_copy(out=BA[:, t, :], in_=pba), reads=[tba], writes=[t_BA])

            def lag(t):
                s3 = t % 3; s2 = t % 2
                for j in range(5):
                    for ch in range(6):
                        tcv = t_CVc[ch]

                        def cvj(e, ch=ch, j=j):
                            if j == 0:
                                return e.tensor_scalar(out=CV[:, ch, :], in0=PRE[s3][:, ch, 0:128], scalar1=convc[:, ch, 0:1], scalar2=None, op0=ALU.mult)
                            return e.scalar_tensor_tensor(out=CV[:, ch, :], in0=PRE[s3][:, ch, j:j + 128], scalar=convc[:, ch, j:j + 1], in1=CV[:, ch, :], op0=ALU.mult, op1=ALU.add)
                        S.op("dve", cvj, reads=[t_PRE[s3], t_convc] + ([tcv] if j else []), writes=[tcv])
                S.op("act", lambda e: e.activation(out=SQ, in_=CV, func=AF.Silu), reads=t_CVc, writes=[t_SQ])
                pT, tT = p512.get()
                pTb = pT.bitcast(BF16)

                def trs(e):
                    for ch in range(6):
                        i = e.transpose(out=pTb[:, ch * 128:(ch + 1) * 128], in_=SQ[:, ch, :], identity=identb)
                    return i
                S.op("pe", trs, reads=[t_SQ, t_identb], writes=[tT])
                pts = [(pTb[:, ch * 128:(ch + 1) * 128], tT) for ch in range(6)]
                n = nr[s2]; tn = t_nr[s2]
                for ch in range(4):
                    S.op("act", lambda e, ch=ch: e.activation(out=junk_A[:, 0:128], in_=pts[ch][0], func=AF.Square, accum_out=n[:, ch:ch + 1]),
                         reads=[pts[ch][1]], writes=[tn])
                S.op("act", lambda e: e.activation(out=n[:, 0:4], in_=n[:, 0:4], func=AF.Sqrt, bias=EPS), reads=[tn], writes=[tn])
                S.op("dve", lambda e: e.reciprocal(out=n[:, 0:4], in_=n[:, 0:4]), reads=[tn], writes=[tn])
                S.op("dve", lambda e: e.tensor_scalar(out=n[:, 0:2], in0=n[:, 0:2], scalar1=128.0 ** -0.5, scalar2=None, op0=ALU.mult), reads=[tn], writes=[tn])
                dsts = [QN[:, t, 0, :], QN[:, t, 1, :], KN[:, t, 0, :], KN[:, t, 1, :], VV[:, t, 0, :], VV[:, t, 1, :]]
                for ch in range(6):
                    if ch < 4:
                        S.op("dve", lambda e, ch=ch: e.tensor_scalar(out=dsts[ch], in0=pts[ch][0], scalar1=n[:, ch:ch + 1], scalar2=None, op0=ALU.mult),
                             reads=[pts[ch][1], tn], writes=[t_QKV[t][ch]])
                    else:
                        S.op("act", lambda e, ch=ch: e.copy(out=dsts[ch], in_=pts[ch][0]), reads=[pts[ch][1]], writes=[t_QKV[t][ch]])

            for i in range(NTL + 1):
                if i < NTL:
                    front(i)
                if i >= 1 and KCUT in (0, 5):
                    lag(i - 1)
            tS = t_SC[0]
            SCk = lambda k: SC[:, :, k * 4:(k + 1) * 4]
            M0, M1, M2, M3 = Mx; tM0, tM1, tM2, tM3 = t_Mx
            S.op("act", lambda e: e.activation(out=g66[:, 0, :, :], in_=g66[:, 0, :, :], func=AF.Exp), reads=[t_g66], writes=[t_g66])
            S.op("act", lambda e: e.activation(out=SCk(3), in_=BA[:, :, 0:4], func=AF.Sigmoid), reads=[t_BA], writes=[tS])
            S.op("dve", lambda e: e.tensor_tensor(out=M0, in0=BA[:, :, 4:8], in1=g66[:, 1, :, :], op=ALU.add), reads=[t_BA, t_g66], writes=[tM0])
            S.op("dve", lambda e: e.tensor_scalar(out=M1, in0=M0, scalar1=-1.0, scalar2=None, op0=ALU.mult), reads=[tM0], writes=[tM1])
            S.op("dve", lambda e: e.tensor_tensor(out=M1, in0=M1, in1=M0, op=ALU.min), reads=[tM0, tM1], writes=[tM1])
            S.op("act", lambda e: e.activation(out=M1, in_=M1, func=AF.Exp), reads=[tM1], writes=[tM1])
            S.op("act", lambda e: e.activation(out=M1, in_=M1, func=AF.Ln, bias=1.0), reads=[tM1], writes=[tM1])
            S.op("dve", lambda e: e.tensor_scalar(out=M2, in0=M0, scalar1=0.0, scalar2=None, op0=ALU.max), reads=[tM0], writes=[tM2])
            S.op("dve", lambda e: e.tensor_tensor(out=M2, in0=M2, in1=M1, op=ALU.add), reads=[tM1, tM2], writes=[tM2])
            S.op("dve", lambda e: e.scalar_tensor_tensor(out=SCk(6), in0=M2, scalar=-1.0, in1=g66[:, 0, :, :], op0=ALU.mult, op1=ALU.mult), reads=[tM2, t_g66], writes=[tS])
            pcA, tcA = p512.get(); pcB, tcB = p512.get()

            def cums(e):
                e.matmul(pcA[:, 0:2 * NT], lhsT=Uf, rhs=SC[:, :, 24:26], start=True, stop=True)
                return e.matmul(pcA[:, 2 * NT:4 * NT], lhsT=Ub, rhs=SC[:, :, 26:28], start=True, stop=True)
            S.op("pe", cums, reads=[tS, t_trif], writes=[tcA])
            S.op("pe", lambda e: e.matmul(pcB[:, 0:4 * NT], lhsT=onesf, rhs=SC[:, :, 24:28], start=True, stop=True), reads=[tS, t_onesf], writes=[tcB])
            Gf_ps = pcA[:, 0:2 * NT].rearrange("p (a b) -> p a b", b=2); Gb_ps = pcA[:, 2 * NT:4 * NT].rearrange("p (a b) -> p a b", b=2)
            Gt_ps = pcB[:, 0:4 * NT].rearrange("p (a b) -> p a b", b=4)
            S.op("act", lambda e: e.activation(out=SC[:, :, 16:18], in_=Gf_ps, func=AF.Exp), reads=[tcA], writes=[tS])
            S.op("act", lambda e: e.activation(out=SC[:, :, 18:20], in_=Gb_ps, func=AF.Exp), reads=[tcA], writes=[tS])
            S.op("act", lambda e: e.activation(out=SCk(5), in_=Gt_ps, func=AF.Exp), reads=[tcB], writes=[tS])
            S.op("dve", lambda e: e.tensor_copy(out=M3[:, :, 0:2], in_=Gf_ps), reads=[tcA], writes=[tM3])
            S.op("dve", lambda e: e.tensor_copy(out=M3[:, :, 2:4], in_=Gb_ps), reads=[tcA], writes=[tM3])
            S.op("dve", lambda e: e.tensor_tensor(out=M3, in0=Gt_ps, in1=M3, op=ALU.subtract), reads=[tcB, tM3], writes=[tM3])
            S.op("act", lambda e: e.activation(out=SCk(2), in_=M3, func=AF.Exp), reads=[tM3], writes=[tS])
            S.op("dve", lambda e: e.tensor_scalar(out=SCk(0), in0=SCk(3), scalar1=-1.0, scalar2=None, op0=ALU.mult), reads=[tS], writes=[tS])
            S.op("dve", lambda e: e.tensor_tensor(out=SCk(1), in0=SCk(3), in1=SCk(4), op=ALU.mult), reads=[tS], writes=[tS])
            S.barrier()
            A.release(mA1)
            if stage == 1:
                for i_, t_ in enumerate((0, 1, 2, 3, 33, 65)):
                    dump(SC[:, t_, :], i_ * 32, 28)
                    dumpb(QN[:, t_, :, :].rearrange("p a b -> p (a b)"), 0, 128, i_ * 768, 256)
                    dumpb(KN[:, t_, :, :].rearrange("p a b -> p (a b)"), 0, 128, i_ * 768 + 256, 256)
                    dumpb(VV[:, t_, :, :].rearrange("p a b -> p (a b)"), 0, 128, i_ * 768 + 512, 256)
                raise _Stop()

            p128_small = p128
            p128 = Pool([(psb[b_][:, 0:128], Tk(f"p128x_{b_}", excl=True)) for b_ in range(8)])
            chains = [(hl, d) for hl in range(2) for d in range(2)]
            cb = {}
            for c in chains:
                b = {}
                for nm in ("knT", "qnT", "qdT", "X0", "XT0", "Xb", "XbT", "X0s", "XT0s", "X1s", "XT1s", "P0", "P1", "PT0", "PT1", "No", "NoT", "Wb", "Vb", "kbg", "kd", "vb", "nwT", "vnew", "QKm", "qd", "Sb"):
                    b[nm] = A.alloc([128], BF16); b["t_" + nm] = Tk(nm)
                for nm in ("gV", "gU", "E", "ET", "S"):
                    b[nm] = A.alloc([128]); b["t_" + nm] = Tk(nm)
                cb[c] = b
                S.op("pool", lambda e, b=b: e.memset(b["S"], 0.0), writes=[b["t_S"]])
                S.op("pool", lambda e, b=b: e.memset(b["Sb"], 0.0), writes=[b["t_Sb"]])
            OACC = A.alloc([64, 2, 128], BF16); t_OACC = [[Tk() for _ in range(2)] for _ in range(64)]
            ofin = [A.alloc([128]) for _ in range(2)]; t_ofin = [Tk(), Tk()]
            onb = [A.alloc([128], BF16) for _ in range(2)]; t_onb = [Tk(), Tk()]
            onT = [A.alloc([128], BF16) for _ in range(4)]; t_onT = [Tk() for _ in range(4)]
            fsm = [A.alloc([4]) for _ in range(2)]; t_fsm = [Tk(), Tk()]
            junk_B = A.alloc([128])
            visited = set()
            snd_ops = []
            fin_cnt = [0]
            evq = [0]

            def evac_copy(dst, src, rd, wr, scale=None):
                evq[0] += 1
                if evq[0] % 2 == 0:
                    if scale is None:
                        return S.op("act", lambda e: e.copy(out=dst, in_=src), reads=rd, writes=wr)
                    return S.op("act", lambda e: e.activation(out=dst, in_=src, func=AF.Copy, scale=scale), reads=rd, writes=wr)
                if scale is None:
                    return S.op("dve", lambda e: e.tensor_copy(out=dst, in_=src), reads=rd, writes=wr)
                return S.op("dve", lambda e: e.tensor_scalar(out=dst, in0=src, scalar1=scale, scalar2=None, op0=ALU.mult), reads=rd, writes=wr)

            def chain_step(c, t):
                hl, d = c
                b = cb[c]
                col = d * 2 + hl
                latent = t >= 2
                kn = KN[:, t, hl, :]; qn = QN[:, t, hl, :]; vv = VV[:, t, hl, :]
                tqq = t_QKV[t][hl]; tqk = t_QKV[t][2 + hl]; tqv = t_QKV[t][4 + hl]; tsc = t_SC[t]

                def scol(kind):
                    return SC[:, t, kind * 4 + col:kind * 4 + col + 1]
                U_, V_ = (Uf, Vf) if d == 0 else (Ub, Vb)
                negs = negb[:, 0 if d == 0 else 2, :]; negi = negb[:, 1 if d == 0 else 3, :]
                pk, tpk = p128.get()
                S.op("pe", lambda e: e.transpose(out=bfv(pk), in_=kn, identity=identb), reads=[tqk, t_identb], writes=[tpk])
                evac_copy(b["knT"], bfv(pk), [tpk], [b["t_knT"]])
                S.op("act", lambda e: e.activation(out=b["gV"], in_=V_, func=AF.Copy, scale=scol(6)), reads=[t_trif, tsc], writes=[b["t_gV"]])
                S.op("act", lambda e: e.activation(out=b["gU"], in_=U_, func=AF.Copy, scale=scol(6)), reads=[t_trif, tsc], writes=[b["t_gU"]])
                S.op("dve", lambda e: e.tensor_scalar(out=b["kbg"], in0=kn, scalar1=scol(1), scalar2=None, op0=ALU.mult), reads=[tqk, tsc], writes=[b["t_kbg"]])
                S.op("pool", lambda e: e.tensor_scalar(out=b["kd"], in0=kn, scalar1=scol(2), scalar2=1.0, op0=ALU.mult, op1=ALU.mult), reads=[tqk, tsc], writes=[b["t_kd"]])
                S.op("pool", lambda e: e.tensor_scalar(out=b["vb"], in0=vv, scalar1=scol(3), scalar2=1.0, op0=ALU.mult, op1=ALU.mult), reads=[tqv, tsc], writes=[b["t_vb"]])
                yield
                pd, tpd = p128.get()

                def dm(e):
                    e.matmul(pd, lhsT=identb, rhs=negs, start=True, stop=False)
                    return e.matmul(pd, lhsT=U_, rhs=b["gV"], start=False, stop=True)
                S.op("pe", dm, reads=[t_identb, t_negb, t_trif, b["t_gV"]], writes=[tpd])
                S.op("act", lambda e: e.activation(out=b["E"], in_=pd, func=AF.Exp), reads=[tpd], writes=[b["t_E"]])
                pkk, tpkk = p128.get()
                S.op("pe", lambda e: e.matmul(pkk, lhsT=b["knT"], rhs=b["knT"], start=True, stop=True), reads=[b["t_knT"]], writes=[tpkk])
                S.op("dve", lambda e: e.scalar_tensor_tensor(out=b["X0"], in0=pkk, scalar=scol(0), in1=b["E"], op0=ALU.mult, op1=ALU.mult),
                     reads=[tpkk, tsc, b["t_E"]], writes=[b["t_X0"]])
                if latent:
                    pdt, tpdt = p128.get()

                    def dmt(e):
                        e.matmul(pdt, lhsT=identb, rhs=negi, start=True, stop=False)
                        return e.matmul(pdt, lhsT=V_, rhs=b["gU"], start=False, stop=True)
                    S.op("pe", dmt, reads=[t_identb, t_negb, t_trif, b["t_gU"]], writes=[tpdt])
                    S.op("act", lambda e: e.activation(out=b["ET"], in_=pdt, func=AF.Exp), reads=[tpdt], writes=[b["t_ET"]])
                    pq_, tpq = p128.get()
                    S.op("pe", lambda e: e.transpose(out=bfv(pq_), in_=qn, identity=identb), reads=[tqq, t_identb], writes=[tpq])
                    evac_copy(b["qnT"], bfv(pq_), [tpq], [b["t_qnT"]])
                    S.op("pool", lambda e: e.tensor_scalar(out=b["qd"], in0=qn, scalar1=scol(4), scalar2=1.0, op0=ALU.mult, op1=ALU.mult), reads=[tqq, tsc], writes=[b["t_qd"]])
                yield
                px, tpx = p128.get()
                S.op("pe", lambda e: e.transpose(out=bfv(px), in_=b["X0"], identity=identb), reads=[b["t_X0"], t_identb], writes=[tpx])
                evac_copy(b["XT0"], bfv(px), [tpx], [b["t_XT0"]])
                if latent:
                    pqd, tpqd = p128.get()
                    S.op("pe", lambda e: e.transpose(out=bfv(pqd), in_=b["qd"], identity=identb), reads=[b["t_qd"], t_identb], writes=[tpqd])
                    evac_copy(b["qdT"], bfv(pqd), [tpqd], [b["t_qdT"]])
                    pqk, tpqk = p128.get()
                    S.op("pe", lambda e: e.matmul(pqk, lhsT=b["knT"], rhs=b["qnT"], start=True, stop=True), reads=[b["t_knT"], b["t_qnT"]], writes=[tpqk])
                    S.op("dve", lambda e: e.tensor_tensor(out=b["QKm"], in0=pqk, in1=b["ET"], op=ALU.mult), reads=[tpqk, b["t_ET"]], writes=[b["t_QKm"]])
                yield
                mk = lambda nm: (b[nm], b["t_" + nm])
                Xb, tXb = mk("Xb"); XbT, tXbT = mk("XbT")
                S.op("pool", lambda e: e.tensor_tensor(out=Xb, in0=b["X0"], in1=dcm[:, 0, :], op=ALU.mult), reads=[b["t_X0"], t_dcm], writes=[tXb])
                S.op("pool", lambda e: e.tensor_tensor(out=XbT, in0=b["XT0"], in1=dcm[:, 0, :], op=ALU.mult), reads=[b["t_XT0"], t_dcm], writes=[tXbT])
                S.op("pool", lambda e: e.tensor_tensor(out=b["P0"], in0=Xb, in1=identb, op=ALU.add), reads=[tXb, t_identb], writes=[b["t_P0"]])
                S.op("pool", lambda e: e.tensor_tensor(out=b["PT0"], in0=XbT, in1=identb, op=ALU.add), reads=[tXbT, t_identb], writes=[b["t_PT0"]])
                yield
                cur = 0
                cX, tcX, cXT, tcXT = Xb, tXb, XbT, tXbT
                for lev in range(2):
                    nX, tnX = mk(f"X{lev}s"); nXT, tnXT = mk(f"XT{lev}s")
                    p1, tp1 = p128.get()
                    S.op("pe", lambda e, p1=p1, cX=cX, cXT=cXT: e.matmul(p1, lhsT=cXT, rhs=cX, start=True, stop=True), reads=[tcX, tcXT], writes=[tp1])
                    evac_copy(nX, p1, [tp1], [tnX])
                    p2, tp2 = p128.get()
                    S.op("pe", lambda e, p2=p2, cX=cX, cXT=cXT: e.matmul(p2, lhsT=cX, rhs=cXT, start=True, stop=True), reads=[tcX, tcXT], writes=[tp2])
                    evac_copy(nXT, p2, [tp2], [tnXT])
                    yield
                    P = b[f"P{cur}"]; tP = b[f"t_P{cur}"]; nP = b[f"P{1 - cur}"]; tnP = b[f"t_P{1 - cur}"]
                    PT = b[f"PT{cur}"]; tPT = b[f"t_PT{cur}"]; nPT = b[f"PT{1 - cur}"]; tnPT = b[f"t_PT{1 - cur}"]
                    p3, tp3 = p128.get()
                    S.op("pe", lambda e, p3=p3, nXT=nXT, P=P: e.matmul(p3, lhsT=nXT, rhs=P, start=True, stop=True), reads=[tnXT, tP], writes=[tp3])
                    S.op("dve", lambda e, p3=p3, P=P, nP=nP: e.tensor_tensor(out=nP, in0=p3, in1=P, op=ALU.add), reads=[tp3, tP], writes=[tnP])
                    p4, tp4 = p128.get()
                    S.op("pe", lambda e, p4=p4, nX=nX, PT=PT: e.matmul(p4, lhsT=nX, rhs=PT, start=True, stop=True), reads=[tnX, tPT], writes=[tp4])
                    S.op("dve", lambda e, p4=p4, PT=PT, nPT=nPT: e.tensor_tensor(out=nPT, in0=p4, in1=PT, op=ALU.add), reads=[tp4, tPT], writes=[tnPT])
                    cur = 1 - cur
                    cX, tcX, cXT, tcXT = nX, tnX, nXT, tnXT
                    yield
                for li in range(4):
                    mi = 1 + 2 * li + (0 if d == 0 else 1)
                    miT = 1 + 2 * li + (1 if d == 0 else 0)
                    No, tNo = mk("No"); NoT, tNoT = mk("NoT")
                    P = b[f"P{cur}"]; tP = b[f"t_P{cur}"]; nP = b[f"P{1 - cur}"]; tnP = b[f"t_P{1 - cur}"]
                    PT = b[f"PT{cur}"]; tPT = b[f"t_PT{cur}"]; nPT = b[f"PT{1 - cur}"]; tnPT = b[f"t_PT{1 - cur}"]
                    S.op("pool", lambda e, mi=mi, No=No: e.tensor_tensor(out=No, in0=b["X0"], in1=dcm[:, mi, :], op=ALU.mult), reads=[b["t_X0"], t_dcm], writes=[tNo])
                    pw_, tpw_ = p128.get()
                    S.op("pe", lambda e, pw_=pw_, No=No, PT=PT: e.matmul(pw_, lhsT=No, rhs=PT, start=True, stop=True), reads=[tNo, tPT], writes=[tpw_])
                    Wb, tWb = mk("Wb")
                    evac_copy(Wb, pw_, [tpw_], [tWb])
                    if li < 3:
                        S.op("pool", lambda e, miT=miT, NoT=NoT: e.tensor_tensor(out=NoT, in0=b["XT0"], in1=dcm[:, miT, :], op=ALU.mult), reads=[b["t_XT0"], t_dcm], writes=[tNoT])
                        pv_, tpv_ = p128.get()
                        S.op("pe", lambda e, pv_=pv_, NoT=NoT, P=P: e.matmul(pv_, lhsT=NoT, rhs=P, start=True, stop=True), reads=[tNoT, tP], writes=[tpv_])
                        Vb_, tVb_ = mk("Vb")
                        evac_copy(Vb_, pv_, [tpv_], [tVb_])
                    yield
                    p5, tp5 = p128.get()
                    S.op("pe", lambda e, p5=p5, P=P, Wb=Wb: e.matmul(p5, lhsT=P, rhs=Wb, start=True, stop=True), reads=[tP, tWb], writes=[tp5])
                    S.op("dve", lambda e, p5=p5, PT=PT, nPT=nPT: e.tensor_tensor(out=nPT, in0=p5, in1=PT, op=ALU.add), reads=[tp5, tPT], writes=[tnPT])
                    if li < 3:
                        p6, tp6 = p128.get()
                        S.op("pe", lambda e, p6=p6, PT=PT, Vb_=Vb_: e.matmul(p6, lhsT=PT, rhs=Vb_, start=True, stop=True), reads=[tPT, tVb_], writes=[tp6])
                        S.op("dve", lambda e, p6=p6, P=P, nP=nP: e.tensor_tensor(out=nP, in0=p6, in1=P, op=ALU.add), reads=[tp6, tP], writes=[tnP])
                    cur = 1 - cur
                    yield
                TT = b[f"PT{cur}"]; tTT = b[f"t_PT{cur}"]
                pw, tpw = p128.get()
                S.op("pe", lambda e: e.matmul(pw, lhsT=b["kbg"], rhs=TT, start=True, stop=True), reads=[b["t_kbg"], tTT], writes=[tpw])
                evac_copy(b["nwT"], pw, [tpw], [b["t_nwT"]], scale=-1.0)
                yield
                pv, tpv = p128.get()

                def vn(e):
                    e.matmul(pv, lhsT=TT, rhs=b["vb"], start=True, stop=False)
                    return e.matmul(pv, lhsT=b["nwT"], rhs=b["Sb"], start=False, stop=True)
                S.op("pe", vn, reads=[tTT, b["t_vb"], b["t_nwT"], b["t_Sb"]], writes=[tpv])
                evac_copy(b["vnew"], pv, [tpv], [b["t_vnew"]])
                yield
                if latent:
                    lt = t - 2
                    po, tpo = p128.get()

                    def om(e):
                        e.matmul(po, lhsT=b["qdT"], rhs=b["Sb"], start=True, stop=False)
                        return e.matmul(po, lhsT=b["QKm"], rhs=b["vnew"], start=False, stop=True)
                    S.op("pe", om, reads=[b["t_qdT"], b["t_Sb"], b["t_QKm"], b["t_vnew"]], writes=[tpo])
                    if (lt, hl) not in visited:
                        visited.add((lt, hl))
                        evac_copy(OACC[:, lt, hl, :], po, [tpo], [t_OACC[lt][hl]])
                    else:
                        k2 = fin_cnt[0] % 2; k4 = fin_cnt[0] % 4
                        fin_cnt[0] += 1
                        of = ofin[k2]; tof = t_ofin[k2]; fs = fsm[k2]; tfs = t_fsm[k2]
                        S.op("dve", lambda e: e.tensor_tensor(out=of, in0=po, in1=OACC[:, lt, hl, :], op=ALU.add), reads=[tpo, t_OACC[lt][hl]], writes=[tof])
                        S.op("act", lambda e: e.activation(out=junk_B, in_=of, func=AF.Square, accum_out=fs[:, 0:1]), reads=[tof], writes=[tfs])
                        S.op("act", lambda e: e.activation(out=fs[:, 0:1], in_=fs[:, 0:1], func=AF.Sqrt, scale=1.0 / 128, bias=EPS), reads=[tfs], writes=[tfs])
                        S.op("dve", lambda e: e.reciprocal(out=fs[:, 0:1], in_=fs[:, 0:1]), reads=[tfs], writes=[tfs])
                        S.op("dve", lambda e: e.tensor_scalar(out=onb[k2], in0=of, scalar1=fs[:, 0:1], scalar2=None, op0=ALU.mult), reads=[tof, tfs], writes=[t_onb[k2]])
                        pt_, tpt = p128.get()
                        S.op("pe", lambda e: e.transpose(out=bfv(pt_), in_=onb[k2], identity=identb), reads=[t_onb[k2], t_identb], writes=[tpt])
                        evac_copy(onT[k4], bfv(pt_), [tpt], [t_onT[k4]])
                        o_ = S.op("pool", lambda e, sig: sig(e.dma_start(out=snd[lt // 16][hl * 128:(hl + 1) * 128, (lt % 16) * 64:(lt % 16 + 1) * 64], in_=onT[k4].bitcast(F32))),
                                  reads=[t_onT[k4]], dma=1, key=f"snd{k4}")
                        snd_ops.append(o_)
                ps_, tps = p128.get()
                S.op("pe", lambda e: e.matmul(ps_, lhsT=b["kd"], rhs=b["vnew"], start=True, stop=True), reads=[b["t_kd"], b["t_vnew"]], writes=[tps])
                S.op("dve", lambda e: e.scalar_tensor_tensor(out=b["S"], in0=b["S"], scalar=scol(5), in1=ps_, op0=ALU.mult, op1=ALU.add),
                     reads=[b["t_S"], tsc, tps], writes=[b["t_S"]])
                S.op("act", lambda e: e.copy(out=b["Sb"], in_=b["S"]), reads=[b["t_S"]], writes=[b["t_Sb"]])
                yield

            def bwd_tile(i):
                return 1 - i if i < 2 else NT + 1 - i

            for i in range(NT):
                gens = []
                for c in chains:
                    t = i if c[1] == 0 else bwd_tile(i)
                    gens.append(chain_step(c, t))
                alive = list(gens)
                while alive:
                    nxt = []
                    for g in alive:
                        try:
                            next(g)
                            nxt.append(g)
                        except StopIteration:
                            pass
                    alive = nxt
            if stage == 2:
                for i_ in range(4):
                    o_ = S.op("sp", lambda e, sig, i_=i_: sig(e.dma_start(out=dbgb[0:256, i_ * 2048:(i_ + 1) * 2048].bitcast(F32), in_=snd[i_][:, :])), extra=snd_ops, dma=1, key="dbg")
                    dump_ops.append(o_)
                for i_, c_ in enumerate(chains):
                    dump(cb[c_]["S"], i_ * 128, 128, reads=[cb[c_]["t_S"]])
                raise _Stop()
            p128 = p128_small
            ccs = []
            for j in range(4):
                ccs.append(S.op("pool", lambda e, sig, j=j: sig(e.collective_compute("AllGather", ALU.bypass, replica_groups=[[0, 1, 2, 3], [4, 5, 6, 7]],
                                                                                 ins=[snd[j].ap().opt()], outs=[rcv[j].ap().opt()])),
                                extra=snd_ops, dma=1, key=f"cc{j}", inc=1))
            S.barrier()
            A.release(mA)
            if stage == 3:
                raise _Stop()

            hxo = A.alloc([8, OWN], BF16); t_hxo = [Tk() for _ in range(NOT)]
            markH = A.mark()
            offS = A.mark()
            SZ = A.alloc([8, OWN], BF16); t_SZ = [[Tk() for _ in range(4)] for _ in range(8)]
            GU = A.alloc([8, OWN], BF16); t_GU = [[Tk() for _ in range(4)] for _ in range(8)]
            wstg = [A.alloc([8, 512]) for _ in range(2)]; t_wstg = [Tk(), Tk()]
            wbf = [A.alloc([8, 512], BF16) for _ in range(2)]; t_wbf = [Tk(), Tk()]
            mC1 = A.mark()
            xt_C = [A.alloc([D]) for _ in range(2)]; t_xt_C = [Tk(), Tk()]
            junk_C = A.alloc([D]); t_junk_C = Tk()
            ssq_C = [A.alloc([1]) for _ in range(2)]; t_ssq_C = [Tk(), Tk()]
            xs_C = [A.alloc([8, 128], BF16) for _ in range(2)]; t_xs_C = [Tk(), Tk()]
            for t in range(NOT):
                s2 = t % 2
                S.op("sp", lambda e, sig, t=t, s2=s2: sig(e.dma_start(out=xt_C[s2], in_=xo[t * 128:(t + 1) * 128, :])), writes=[t_xt_C[s2]], dma=1, key=f"cxt{s2}")
                S.op("act", lambda e, s2=s2: e.activation(out=junk_C, in_=xt_C[s2], func=AF.Square, accum_out=ssq_C[s2]), reads=[t_xt_C[s2]], writes=[t_ssq_C[s2]])
                S.op("act", lambda e, s2=s2: e.activation(out=ssq_C[s2], in_=ssq_C[s2], func=AF.Sqrt, scale=1.0 / D, bias=EPS), reads=[t_ssq_C[s2]], writes=[t_ssq_C[s2]])
                S.op("dve", lambda e, s2=s2: e.reciprocal(out=ssq_C[s2], in_=ssq_C[s2]), reads=[t_ssq_C[s2]], writes=[t_ssq_C[s2]])
                S.op("pool", lambda e, s2=s2: e.tensor_scalar(out=xs_C[s2].rearrange("p a b -> p (a b)"), in0=xt_C[s2], scalar1=ssq_C[s2], scalar2=1.0, op0=ALU.mult, op1=ALU.mult),
                     reads=[t_xt_C[s2], t_ssq_C[s2]], writes=[t_xs_C[s2]])
                pp, tp = p512.get()
                ppb = pp.bitcast(BF16)

                def tr(e, s2=s2, ppb=ppb):
                    for k in range(8):
                        i = e.transpose(out=ppb[:, k * 128:(k + 1) * 128], in_=xs_C[s2][:, k, :], identity=identb)
                    return i
                S.op("pe", tr, reads=[t_xs_C[s2], t_identb], writes=[tp])

                def ev_d(e, t=t, ppb=ppb):
                    for k in range(8):
                        i = e.tensor_scalar(out=hxo[:, k, t * 128:(t + 1) * 128], in0=ppb[:, k * 128:(k + 1) * 128], scalar1=modc[:, 1, k:k + 1], scalar2=modc[:, 0, k:k + 1], op0=ALU.mult, op1=ALU.add)
                    return i
                S.op("dve", ev_d, reads=[tp, t_modc], writes=[t_hxo[t]])
            S.barrier()
            A.release(mC1)
            wcnt = [0]

            def stream_w(src_ap_cols, ncols):
                s = wcnt[0] % 2
                wcnt[0] += 1
                v = src_ap_cols.rearrange("(k p) n -> p k n", p=128)
                S.op("sp", lambda e, sig: (sig(e.dma_start(out=wstg[s][:, 0:4, 0:ncols], in_=v[:, 0:4, :])), sig(e.dma_start(out=wstg[s][:, 4:8, 0:ncols], in_=v[:, 4:8, :]))),
                     writes=[t_wstg[s]], dma=2, key=f"wstg{s}")
                S.op("pool", lambda e: e.tensor_copy(out=wbf[s][:, :, 0:ncols], in_=wstg[s][:, :, 0:ncols]), reads=[t_wstg[s]], writes=[t_wbf[s]])
                return wbf[s], t_wbf[s]
            for cbk in range(4):
                wv, twv = stream_w(w_rest[:, cbk * 512:(cbk + 1) * 512], 512)
                dst, tdst, fn = (SZ, t_SZ, AF.Silu) if cbk < 2 else (GU, t_GU, AF.Gelu)
                for cc_ in range(4):
                    chn = (cbk % 2) * 4 + cc_
                    for tb in range(4):
                        pp, tp = p512.get()

                        def f(e, pp=pp, wv=wv, cc_=cc_, tb=tb):
                            for k in range(8):
                                i = e.matmul(pp, lhsT=wv[:, k, cc_ * 128:(cc_ + 1) * 128], rhs=hxo[:, k, tb * 512:(tb + 1) * 512], start=(k == 0), stop=(k == 7))
                            return i
                        S.op("pe", f, reads=[twv] + t_hxo[tb * 4:(tb + 1) * 4], writes=[tp])
                        S.op("act", lambda e, pp=pp, dst=dst, chn=chn, tb=tb, fn=fn: e.activation(out=dst[:, chn, tb * 512:(tb + 1) * 512], in_=pp, func=fn),
                             reads=[tp], writes=[tdst[chn][tb]])
            def cut4(k):
                if stage == 4 and int(os.environ.get("CUT4", "0")) == k:
                    S.barrier()
                    for i_, buf_ in enumerate((SZ, GU, hxo)):
                        for h_ in range(2):
                            dumpb(buf_[:, h_ * 4:(h_ + 1) * 4, :].rearrange("p a b -> p (a b)"), i_ * 256 + h_ * 128, 128, 0, SEQ)
                    raise _Stop()
            cut4(1)
            mV = A.mark()
            junk_V = A.alloc([D])
            wcnt[0] = 0
            wvh = [stream_w(w_rest[:, 2048 + hh * 512:2048 + (hh + 1) * 512], 512) for hh in range(2)]
            swf = A.alloc([8, 128]); t_swf = Tk(); load(swf, sgu_wT.ap().rearrange("g q p -> q g p"), t_swf, "c11")
            swb = A.alloc([8, 128], BF16); t_swb = Tk()
            S.op("pool", lambda e: e.tensor_copy(out=swb, in_=swf), reads=[t_swf], writes=[t_swb])
            lnbb = A.alloc([D]); t_lnbb = Tk(); load(lnbb, lnb_bc[:, :], t_lnbb, "c12")
            sbb = A.alloc([8, 128]); t_sbb = Tk(); load(sbb, sgu_bb[:, :, :], t_sbb, "c13")
            lngc = A.alloc([8]); t_lngc = Tk(); load(lngc, lng_c[:, :], t_lngc, "c14")
            BIAS = A.alloc([8, 128]); t_BIAS = Tk()
            for g in range(8):
                pp, tp = p128.get()
                S.op("pe", lambda e, pp=pp, g=g: e.matmul(pp, lhsT=lnbb[:, g * 128:(g + 1) * 128], rhs=swf[:, g, :], start=True, stop=True), reads=[t_lnbb, t_swf], writes=[tp])
                S.op("dve", lambda e, pp=pp, g=g: e.tensor_tensor(out=BIAS[:, g, :], in0=pp, in1=sbb[:, g, :], op=ALU.add), reads=[tp, t_sbb], writes=[t_BIAS])
            gv = [A.alloc([D])] * 2; t_gv = [Tk()] * 2
            vnb = [A.alloc([D], BF16) for _ in range(2)]; t_vnb = [Tk(), Tk()]
            lst = [A.alloc([8]) for _ in range(2)]; t_lst = [Tk(), Tk()]
            mtmp = [A.alloc([128]) for _ in range(2)]; t_mtmp = [Tk(), Tk()]
            for t in range(NOT):
                s2 = t % 2
                for hh in range(2):
                    pp, tp = p512.get()

                    def f(e, pp=pp, hh=hh, t=t):
                        for k in range(8):
                            i = e.matmul(pp, lhsT=hxo[:, k, t * 128:(t + 1) * 128], rhs=wvh[hh][0][:, k, :], start=(k == 0), stop=(k == 7))
                        return i
                    S.op("pe", f, reads=[wvh[hh][1], t_hxo[t]], writes=[tp])
                    S.op("act", lambda e, pp=pp, hh=hh, s2=s2: e.activation(out=gv[s2][:, hh * 512:(hh + 1) * 512], in_=pp, func=AF.Gelu), reads=[tp], writes=[t_gv[s2]])
                ls = lst[s2]; tls = t_lst[s2]
                S.op("dve", lambda e, s2=s2, ls=ls: e.tensor_reduce(out=ls[:, 0:1], in_=gv[s2], axis=mybir.AxisListType.X, op=ALU.add), reads=[t_gv[s2]], writes=[tls])
                S.op("act", lambda e, s2=s2, ls=ls: e.activation(out=junk_V, in_=gv[s2], func=AF.Square, accum_out=ls[:, 1:2]), reads=[t_gv[s2]], writes=[tls])
                S.op("dve", lambda e, ls=ls: e.tensor_scalar(out=ls[:, 0:2], in0=ls[:, 0:2], scalar1=1.0 / D, scalar2=None, op0=ALU.mult), reads=[tls], writes=[tls])
                S.op("dve", lambda e, ls=ls: e.tensor_tensor(out=ls[:, 2:3], in0=ls[:, 0:1], in1=ls[:, 0:1], op=ALU.mult), reads=[tls], writes=[tls])
                S.op("dve", lambda e, ls=ls: e.tensor_tensor(out=ls[:, 2:3], in0=ls[:, 1:2], in1=ls[:, 2:3], op=ALU.subtract), reads=[tls], writes=[tls])
                S.op("act", lambda e, ls=ls: e.activation(out=ls[:, 2:3], in_=ls[:, 2:3], func=AF.Sqrt, bias=EPS), reads=[tls], writes=[tls])
                S.op("dve", lambda e, ls=ls: e.reciprocal(out=ls[:, 2:3], in_=ls[:, 2:3]), reads=[tls], writes=[tls])
                S.op("dve", lambda e, s2=s2, ls=ls: e.tensor_scalar(out=vnb[s2], in0=gv[s2], scalar1=ls[:, 0:1], scalar2=ls[:, 2:3], op0=ALU.subtract, op1=ALU.mult),
                     reads=[t_gv[s2], tls], writes=[t_vnb[s2]])
                for g in range(8):
                    pp, tp = p128.get()
                    S.op("pe", lambda e, pp=pp, g=g, s2=s2: e.matmul(pp, lhsT=vnb[s2][:, g * 128:(g + 1) * 128], rhs=swb[:, g, :], start=True, stop=True),
                         reads=[t_vnb[s2], t_swb], writes=[tp])
                    mt = mtmp[g % 2]; tmt = t_mtmp[g % 2]
                    S.op("dve", lambda e, pp=pp, g=g, mt=mt: e.scalar_tensor_tensor(out=mt, in0=pp, scalar=lngc[:, g:g + 1], in1=BIAS[:, g, :], op0=ALU.mult, op1=ALU.add),
                         reads=[tp, t_lngc, t_BIAS], writes=[tmt])
                    S.op("pool", lambda e, g=g, t=t, mt=mt: e.tensor_tensor(out=GU[:, g, t * 128:(t + 1) * 128], in0=GU[:, g, t * 128:(t + 1) * 128], in1=mt, op=ALU.mult),
                         reads=[tmt, t_GU[g][t // 4]], writes=[t_GU[g][t // 4]])
            S.barrier()
            A.release(mV)
            cut4(2)
            mY = A.mark()
            rq = [A.alloc([4, 512], BF16) for _ in range(2)]; t_rq = [Tk(), Tk()]
            acc = [A.alloc([512]) for _ in range(2)]; t_acc = [Tk(), Tk()]
            cntr = 0
            for hp in range(4):
                for hl in range(2):
                    chn = hp * 2 + hl
                    for tb in range(4):
                        s = cntr % 2; cntr += 1
                        S.op("sp", lambda e, sig, s=s, chn=chn, tb=tb: tuple(sig(e.dma_start(out=rq[s][:, j, :].bitcast(F32), in_=rcv[j][chn * 128:(chn + 1) * 128, tb * 256:(tb + 1) * 256])) for j in range(4)),
                             extra=ccs, writes=[t_rq[s]], dma=4, key=f"rq{s}")
                        for j in range(4):
                            def f(e, s=s, j=j):
                                if j == 0:
                                    return e.tensor_scalar(out=acc[s], in0=rq[s][:, 0, :], scalar1=qmask[:, 0:1], scalar2=None, op0=ALU.mult)
                                return e.scalar_tensor_tensor(out=acc[s], in0=rq[s][:, j, :], scalar=qmask[:, j:j + 1], in1=acc[s], op0=ALU.mult, op1=ALU.add)
                            S.op("dve", f, reads=[t_rq[s], t_qmask, t_acc[s]] if j else [t_rq[s], t_qmask], writes=[t_acc[s]])
                        S.op("dve", lambda e, s=s, chn=chn, tb=tb: e.scalar_tensor_tensor(out=SZ[:, chn, tb * 512:(tb + 1) * 512], in0=acc[s], scalar=gngs[:, 0:1], in1=SZ[:, chn, tb * 512:(tb + 1) * 512], op0=ALU.mult, op1=ALU.mult),
                             reads=[t_acc[s], t_gngs, t_SZ[chn][tb]], writes=[t_SZ[chn][tb]])
            S.barrier()
            A.release(mY)
            cut4(3)
            S.barrier()
            mM = A.mark()
            MG = A.alloc([8, OWN], BF16); t_MG = [[Tk() for _ in range(4)] for _ in range(8)]
            sga = [A.alloc([512], BF16) for _ in range(2)]; t_sga = [Tk(), Tk()]
            sgb = [A.alloc([512], BF16) for _ in range(2)]; t_sgb = [Tk(), Tk()]
            m1 = [A.alloc([512]) for _ in range(2)]; t_m1 = [Tk(), Tk()]
            m2 = [A.alloc([512]) for _ in range(2)]; t_m2 = [Tk(), Tk()]
            sub = [(wstg[i][:, :, j * 128:(j + 1) * 128], wbf[i][:, :, j * 128:(j + 1) * 128], Tk(), Tk()) for i in range(2) for j in range(4)]
            subc = [0]

            def stream_small(src_cols):
                stg, bfw, tst, tbf = sub[subc[0] % 8]
                subc[0] += 1
                v = src_cols.rearrange("(k p) n -> p k n", p=128)
                S.op("sp", lambda e, sig: sig(e.dma_start(out=stg, in_=v)), writes=[tst], dma=1, key=f"sub{(subc[0] - 1) % 8}")
                S.op("pool", lambda e: e.tensor_copy(out=bfw, in_=stg), reads=[tst], writes=[tbf])
                return bfw, tbf
            cntr = 0
            for dc in range(8):
                ws = [stream_small(w_a[:, dc * 128:(dc + 1) * 128]), stream_small(w_b[:, dc * 128:(dc + 1) * 128]),
                      stream_small(w_rest[:, 3072 + dc * 128:3072 + (dc + 1) * 128]), stream_small(w_rest[:, 4096 + dc * 128:4096 + (dc + 1) * 128])]
                for tb in range(4):
                    s = cntr % 2; cntr += 1
                    outs = []
                    for wi, (src, tsrc) in enumerate(((SZ, t_SZ), (GU, t_GU), (hxo, None), (hxo, None))):
                        wv_, tw_ = ws[wi]
                        pp, tp = p512.get()

                        def f(e, pp=pp, wv_=wv_, src=src, tb=tb):
                            for k in range(8):
                                i = e.matmul(pp, lhsT=wv_[:, k, :], rhs=src[:, k, tb * 512:(tb + 1) * 512], start=(k == 0), stop=(k == 7))
                            return i
                        rds = [tw_] + ([tsrc[k][tb] for k in range(8)] if tsrc is not None else t_hxo[tb * 4:(tb + 1) * 4])
                        S.op("pe", f, reads=rds, writes=[tp])
                        outs.append((pp, tp))
                    S.op("act", lambda e, s=s, pp=outs[2][0]: e.activation(out=sga[s], in_=pp, func=AF.Sigmoid), reads=[outs[2][1]], writes=[t_sga[s]])
                    S.op("act", lambda e, s=s, pp=outs[3][0]: e.activation(out=sgb[s], in_=pp, func=AF.Sigmoid), reads=[outs[3][1]], writes=[t_sgb[s]])
                    S.op("dve", lambda e, s=s, pp=outs[0][0]: e.tensor_tensor(out=m1[s], in0=pp, in1=sga[s], op=ALU.mult), reads=[outs[0][1], t_sga[s]], writes=[t_m1[s]])
                    S.op("dve", lambda e, s=s, pp=outs[1][0]: e.tensor_tensor(out=m2[s], in0=pp, in1=sgb[s], op=ALU.mult), reads=[outs[1][1], t_sgb[s]], writes=[t_m2[s]])
                    S.op("pool", lambda e, s=s, dc=dc, tb=tb: e.tensor_tensor(out=MG[:, dc, tb * 512:(tb + 1) * 512], in0=m1[s], in1=m2[s], op=ALU.add),
                         reads=[t_m1[s], t_m2[s]], writes=[t_MG[dc][tb]])
            S.barrier()
            if stage == 4:
                for i_, (buf_, tk_) in enumerate(((SZ, t_SZ), (GU, t_GU), (MG, t_MG))):
                    for h_ in range(2):
                        dumpb(buf_[:, h_ * 4:(h_ + 1) * 4, :].rearrange("p a b -> p (a b)"), i_ * 256 + h_ * 128, 128, 0, SEQ)
                raise _Stop()
            hx2 = hxo; t_hx2 = [Tk() for _ in range(NOT)]
            Amain = A
            A = Arena(arena_t, 16384, base=offS)
            wo_b = A.alloc([8, D], BF16); t_wo_b = Tk()
            for hh in range(2):
                sl = hh
                S.op("sp", lambda e, sig, hh=hh, sl=sl: (sig(e.dma_start(out=wstg[sl][:, 0:4, :], in_=w_out.ap().rearrange("(k p) n -> p k n", p=128)[:, 0:4, hh * 512:(hh + 1) * 512])),
                                                        sig(e.dma_start(out=wstg[sl][:, 4:8, :], in_=w_out.ap().rearrange("(k p) n -> p k n", p=128)[:, 4:8, hh * 512:(hh + 1) * 512]))),
                     writes=[t_wstg[sl]], dma=2, key=f"wstg{sl}")
                for k in range(8):
                    S.op("pool", lambda e, k=k, hh=hh, sl=sl: e.tensor_tensor(out=wo_b[:, k, hh * 512:(hh + 1) * 512], in0=wstg[sl][:, k, :], in1=gmix[:, hh * 512:(hh + 1) * 512], op=ALU.mult),
                         reads=[t_wstg[sl], t_gmix], writes=[t_wo_b])
            rwf = A.alloc([8, 36]); t_rwf = Tk(); load(rwf, rw.ap().rearrange("(k p) n -> p k n", p=128), t_rwf, "c15")
            rbb = A.alloc([36]); t_rbb = Tk(); load(rbb, rb[:, :], t_rbb, "c16")
            x1 = [A.alloc([D]) for _ in range(2)]; t_x1 = [Tk(), Tk()]
            xt_D = [A.alloc([D]) for _ in range(2)]; t_xt_D = [Tk(), Tk()]
            junk_D = A.alloc([D]); t_junk_D = Tk()
            ssq_D = [A.alloc([1]) for _ in range(2)]; t_ssq_D = [Tk(), Tk()]
            xsf = [A.alloc([8, 128]) for _ in range(2)]; t_xsf = [Tk(), Tk()]
            hxf = [A.alloc([8, 128]) for _ in range(2)]; t_hxf = [Tk(), Tk()]
            rs_ = [A.alloc([64]) for _ in range(2)]; t_rs = [Tk(), Tk()]
            x1_ops = []
            for t in range(NOT):
                s2 = t % 2
                S.op("sp", lambda e, sig, t=t, s2=s2: sig(e.dma_start(out=xt_D[s2], in_=xo[t * 128:(t + 1) * 128, :])), writes=[t_xt_D[s2]], dma=1, key=f"dxt{s2}")
                for hh in range(2):
                    pp, tp = p512.get()

                    def f(e, pp=pp, hh=hh, t=t):
                        for k in range(8):
                            i = e.matmul(pp, lhsT=MG[:, k, t * 128:(t + 1) * 128], rhs=wo_b[:, k, hh * 512:(hh + 1) * 512], start=(k == 0), stop=(k == 7))
                        return i
                    S.op("pe", f, reads=[t_wo_b] + [t_MG[k][t // 4] for k in range(8)], writes=[tp])
                    S.op("dve", lambda e, pp=pp, hh=hh, s2=s2: e.tensor_tensor(out=x1[s2][:, hh * 512:(hh + 1) * 512], in0=pp, in1=xt_D[s2][:, hh * 512:(hh + 1) * 512], op=ALU.add),
                         reads=[tp, t_xt_D[s2]], writes=[t_x1[s2]])
                o_ = S.op("pool", lambda e, sig, t=t, s2=s2: sig(e.dma_start(out=x1d[t * 128:(t + 1) * 128, :], in_=x1[s2])), reads=[t_x1[s2]], dma=1, key=f"x1d{s2}")
                x1_ops.append(o_)
                S.op("act", lambda e, s2=s2: e.activation(out=junk_D, in_=x1[s2], func=AF.Square, accum_out=ssq_D[s2]), reads=[t_x1[s2]], writes=[t_ssq_D[s2]])
                S.op("act", lambda e, s2=s2: e.activation(out=ssq_D[s2], in_=ssq_D[s2], func=AF.Sqrt, scale=1.0 / D, bias=EPS), reads=[t_ssq_D[s2]], writes=[t_ssq_D[s2]])
                S.op("dve", lambda e, s2=s2: e.reciprocal(out=ssq_D[s2], in_=ssq_D[s2]), reads=[t_ssq_D[s2]], writes=[t_ssq_D[s2]])
                S.op("pool", lambda e, s2=s2: e.tensor_scalar(out=xsf[s2].rearrange("p a b -> p (a b)"), in0=x1[s2], scalar1=ssq_D[s2], scalar2=1.0, op0=ALU.mult, op1=ALU.mult),
                     reads=[t_x1[s2], t_ssq_D[s2]], writes=[t_xsf[s2]])
                for q4 in range(2):
                    pp, tp = p512.get()

                    def tr(e, pp=pp, q4=q4, s2=s2):
                        for k in range(4):
                            i = e.transpose(out=pp[:, k * 128:(k + 1) * 128], in_=xsf[s2][:, q4 * 4 + k, :], identity=identf)
                        return i
                    S.op("pe", tr, reads=[t_xsf[s2], t_identf], writes=[tp])

                    def ev(e, pp=pp, q4=q4, s2=s2, t=t):
                        for k in range(4):
                            kk = q4 * 4 + k
                            i = e.tensor_scalar(out=hxf[s2][:, kk, :], in0=pp[:, k * 128:(k + 1) * 128], scalar1=modc[:, 5, kk:kk + 1], scalar2=modc[:, 4, kk:kk + 1], op0=ALU.mult, op1=ALU.add)
                        return i
                    S.op("dve", ev, reads=[tp, t_modc], writes=[t_hxf[s2]])
                S.op("pool", lambda e, s2=s2, t=t: e.tensor_copy(out=hx2[:, :, t * 128:(t + 1) * 128], in_=hxf[s2]), reads=[t_hxf[s2]], writes=[t_hx2[t]])
                pr, tpr = p128.get()

                def rt(e, pr=pr, s2=s2):
                    for k in range(8):
                        i = e.matmul(pr[:, 0:36], lhsT=hxf[s2][:, k, :], rhs=rwf[:, k, :], start=(k == 0), stop=(k == 7))
                    return i
                S.op("pe", rt, reads=[t_hxf[s2], t_rwf], writes=[tpr])
                r = rs_[s2]; tr_ = t_rs[s2]
                S.op("dve", lambda e, pr=pr, r=r: e.tensor_tensor(out=r[:, 0:36], in0=pr[:, 0:36], in1=rbb, op=ALU.add), reads=[tpr, t_rbb], writes=[tr_])
                S.op("dve", lambda e, r=r: e.tensor_reduce(out=r[:, 40:41], in_=r[:, 0:4], axis=mybir.AxisListType.X, op=ALU.max), reads=[tr_], writes=[tr_])
                S.op("dve", lambda e, r=r: e.tensor_scalar(out=r[:, 36:40], in0=r[:, 0:4], scalar1=r[:, 40:41], scalar2=None, op0=ALU.is_ge), reads=[tr_], writes=[tr_])
                S.op("dve", lambda e, r=r: e.tensor_scalar(out=r[:, 60:64], in0=r[:, 0:4], scalar1=r[:, 40:41], scalar2=None, op0=ALU.subtract), reads=[tr_], writes=[tr_])
                S.op("act", lambda e, r=r: e.activation(out=r[:, 60:64], in_=r[:, 60:64], func=AF.Exp, accum_out=r[:, 41:42]), reads=[tr_], writes=[tr_])
                S.op("dve", lambda e, r=r: e.reciprocal(out=r[:, 41:42], in_=r[:, 41:42]), reads=[tr_], writes=[tr_])
                S.op("dve", lambda e, r=r: e.tensor_scalar(out=r[:, 42:50], in0=r[:, 4:12], scalar1=r[:, 36:37], scalar2=None, op0=ALU.mult), reads=[tr_], writes=[tr_])
                for g in range(1, 4):
                    S.op("dve", lambda e, r=r, g=g: e.scalar_tensor_tensor(out=r[:, 42:50], in0=r[:, 4 + 8 * g:12 + 8 * g], scalar=r[:, 36 + g:37 + g], in1=r[:, 42:50], op0=ALU.mult, op1=ALU.add),
                         reads=[tr_], writes=[tr_])
                S.op("dve", lambda e, r=r: e.tensor_reduce(out=r[:, 50:51], in_=r[:, 42:50], axis=mybir.AxisListType.X, op=ALU.max), reads=[tr_], writes=[tr_])
                S.op("dve", lambda e, r=r: e.tensor_scalar(out=r[:, 42:50], in0=r[:, 42:50], scalar1=r[:, 50:51], scalar2=None, op0=ALU.subtract), reads=[tr_], writes=[tr_])
                S.op("act", lambda e, r=r: e.activation(out=r[:, 42:50], in_=r[:, 42:50], func=AF.Exp), reads=[tr_], writes=[tr_])
                S.op("dve", lambda e, r=r: e.tensor_scalar(out=r[:, 52:60], in0=r[:, 42:50], scalar1=1.0, scalar2=None, op0=ALU.is_ge), reads=[tr_], writes=[tr_])
                S.op("dve", lambda e, r=r: e.scalar_tensor_tensor(out=r[:, 4:12], in0=r[:, 52:60], scalar=-2.0, in1=r[:, 42:50], op0=ALU.mult, op1=ALU.add), reads=[tr_], writes=[tr_])
                S.op("dve", lambda e, r=r: e.tensor_reduce(out=r[:, 51:52], in_=r[:, 4:12], axis=mybir.AxisListType.X, op=ALU.max), reads=[tr_], writes=[tr_])
                S.op("dve", lambda e, r=r: e.tensor_scalar(out=r[:, 12:20], in0=r[:, 4:12], scalar1=r[:, 51:52], scalar2=None, op0=ALU.is_ge), reads=[tr_], writes=[tr_])
                S.op("dve", lambda e, r=r: e.scalar_tensor_tensor(out=r[:, 20:28], in0=r[:, 12:20], scalar=r[:, 51:52], in1=r[:, 52:60], op0=ALU.mult, op1=ALU.add), reads=[tr_], writes=[tr_])
                S.op("dve", lambda e, r=r: e.tensor_scalar(out=r[:, 50:51], in0=r[:, 51:52], scalar1=1.0, scalar2=None, op0=ALU.add), reads=[tr_], writes=[tr_])
                S.op("dve", lambda e, r=r: e.reciprocal(out=r[:, 50:51], in_=r[:, 50:51]), reads=[tr_], writes=[tr_])
                S.op("dve", lambda e, r=r: e.tensor_tensor(out=r[:, 50:51], in0=r[:, 50:51], in1=r[:, 41:42], op=ALU.mult), reads=[tr_], writes=[tr_])
                S.op("dve", lambda e, r=r: e.tensor_scalar(out=r[:, 20:28], in0=r[:, 20:28], scalar1=r[:, 50:51], scalar2=None, op0=ALU.mult), reads=[tr_], writes=[tr_])
                for g in range(4):
                    S.op("dve", lambda e, r=r, g=g, t=t: e.tensor_scalar(out=GT[:, t, g * 8:(g + 1) * 8], in0=r[:, 20:28], scalar1=r[:, 36 + g:37 + g], scalar2=None, op0=ALU.mult),
                         reads=[tr_], writes=[t_GT[t]])
            S.barrier()
            A = Amain
            A.release(markH)
            if stage == 5:
                dump(GT.rearrange("p a b -> p (a b)"), 0, 512)
                for i_ in range(4):
                    o_ = S.op("sp", lambda e, sig, i_=i_: sig(e.dma_start(out=y[i_ * 512:(i_ + 1) * 512, :], in_=x1d[i_ * 512:(i_ + 1) * 512, :])), extra=x1_ops, dma=1, key="dbg")
                    dump_ops.append(o_)
                dumpb(hx2[:, 0:4, :].rearrange("p a b -> p (a b)"), 0, 128, 0, SEQ)
                raise _Stop()
            p512 = Pool([(psb[b_][:, :], Tk(f"p512x_{b_}", excl=True)) for b_ in range(8)])
            ACC = A.alloc([NOT, D]); t_ACC = [[Tk(), Tk()] for _ in range(NOT)]
            for t in range(NOT):
                S.op("pool", lambda e, t=t: e.memset(ACC[:, t, :], 0.0), writes=t_ACC[t])
            e1f = A.alloc([8, 512]); t_e1f = Tk()
            e2f = A.alloc([2, D]); t_e2f = Tk()
            e1b = [A.alloc([8, 512], BF16) for _ in range(2)]; t_e1b = [Tk(), Tk()]
            e2b = [A.alloc([2, D], BF16) for _ in range(2)]; t_e2b = [Tk(), Tk()]
            sil = [A.alloc([512], BF16) for _ in range(2)]; t_sil = [Tk(), Tk()]
            hid = [A.alloc([512], BF16) for _ in range(4)]; t_hid = [Tk() for _ in range(4)]
            hc = 0
            for ex in range(NE):
                s = ex % 2
                v1 = ew1[ex].rearrange("(k p) n -> p k n", p=128); v3 = ew3[ex].rearrange("(k p) n -> p k n", p=128)
                v2 = ew2[ex].rearrange("(k p) n -> p k n", p=128)
                S.op("sp", lambda e, sig, v1=v1, v3=v3: (sig(e.dma_start(out=e1f[:, :, 0:256], in_=v1)), sig(e.dma_start(out=e1f[:, :, 256:512], in_=v3))), writes=[t_e1f], dma=2, key="e1f")
                S.op("sp", lambda e, sig, v2=v2: sig(e.dma_start(out=e2f, in_=v2)), writes=[t_e2f], dma=1, key="e2f")
                S.op("pool", lambda e, s=s: e.tensor_copy(out=e1b[s], in_=e1f), reads=[t_e1f], writes=[t_e1b[s]])
                S.op("pool", lambda e, s=s: e.tensor_copy(out=e2b[s], in_=e2f), reads=[t_e2f], writes=[t_e2b[s]])
                for tb in range(4):
                    hs = []
                    for fc in range(2):
                        p1, tp1 = p512.get(); p3, tp3 = p512.get()

                        def f1(e, p1=p1, s=s, fc=fc, tb=tb):
                            for k in range(8):
                                i = e.matmul(p1, lhsT=e1b[s][:, k, fc * 128:(fc + 1) * 128], rhs=hx2[:, k, tb * 512:(tb + 1) * 512], start=(k == 0), stop=(k == 7))
                            return i

                        def f3(e, p3=p3, s=s, fc=fc, tb=tb):
                            for k in range(8):
                                i = e.matmul(p3, lhsT=e1b[s][:, k, 256 + fc * 128:256 + (fc + 1) * 128], rhs=hx2[:, k, tb * 512:(tb + 1) * 512], start=(k == 0), stop=(k == 7))
                            return i
                        S.op("pe", f1, reads=[t_e1b[s]] + t_hx2[tb * 4:(tb + 1) * 4], writes=[tp1])
                        S.op("pe", f3, reads=[t_e1b[s]] + t_hx2[tb * 4:(tb + 1) * 4], writes=[tp3])
                        ss = hc % 2; h4 = hc % 4; hc += 1
                        S.op("act", lambda e, p1=p1, ss=ss: e.activation(out=sil[ss], in_=p1, func=AF.Silu), reads=[tp1], writes=[t_sil[ss]])
                        S.op("dve", lambda e, p3=p3, ss=ss, h4=h4: e.tensor_tensor(out=hid[h4], in0=p3, in1=sil[ss], op=ALU.mult), reads=[tp3, t_sil[ss]], writes=[t_hid[h4]])
                        hs.append(h4)
                    for tt in range(4):
                        t = tb * 4 + tt
                        for hh in range(2):
                            po, tpo = p512.get()

                            def f2(e, po=po, s=s, tt=tt, hh=hh, hs=tuple(hs)):
                                for fc in range(2):
                                    i = e.matmul(po, lhsT=hid[hs[fc]][:, tt * 128:(tt + 1) * 128], rhs=e2b[s][:, fc, hh * 512:(hh + 1) * 512], start=(fc == 0), stop=(fc == 1))
                                return i
                            S.op("pe", f2, reads=[t_hid[hs[0]], t_hid[hs[1]], t_e2b[s]], writes=[tpo])
                            S.op("dve", lambda e, po=po, t=t, hh=hh, ex=ex: e.scalar_tensor_tensor(out=ACC[:, t, hh * 512:(hh + 1) * 512], in0=po, scalar=GT[:, t, ex:ex + 1], in1=ACC[:, t, hh * 512:(hh + 1) * 512], op0=ALU.mult, op1=ALU.add),
                                 reads=[tpo, t_ACC[t][hh]], writes=[t_ACC[t][hh]])
            fngb = A.alloc([D]); t_fngb = Tk(); load(fngb, fng_bc[:, :], t_fngb, "c17")
            x1r = [A.alloc([D]) for _ in range(2)]; t_x1r = [Tk(), Tk()]
            x2 = [A.alloc([D]) for _ in range(2)]; t_x2 = [Tk(), Tk()]
            junk_E = A.alloc([D]); t_junk_E = Tk()
            ssq_E = [A.alloc([1]) for _ in range(2)]; t_ssq_E = [Tk(), Tk()]
            outs_ = []
            for t in range(NOT):
                s2 = t % 2
                S.op("sp", lambda e, sig, t=t, s2=s2: sig(e.dma_start(out=x1r[s2], in_=x1d[t * 128:(t + 1) * 128, :])), extra=[x1_ops[t]], writes=[t_x1r[s2]], dma=1, key=f"x1r{s2}")
                S.op("dve", lambda e, t=t, s2=s2: e.tensor_tensor(out=x2[s2], in0=ACC[:, t, :], in1=gffn, op=ALU.mult), reads=t_ACC[t] + [t_gffn], writes=[t_x2[s2]])
                S.op("dve", lambda e, s2=s2: e.tensor_tensor(out=x2[s2], in0=x2[s2], in1=x1r[s2], op=ALU.add), reads=[t_x2[s2], t_x1r[s2]], writes=[t_x2[s2]])
                S.op("act", lambda e, s2=s2: e.activation(out=junk_E, in_=x2[s2], func=AF.Square, accum_out=ssq_E[s2]), reads=[t_x2[s2]], writes=[t_ssq_E[s2]])
                S.op("act", lambda e, s2=s2: e.activation(out=ssq_E[s2], in_=ssq_E[s2], func=AF.Sqrt, scale=1.0 / D, bias=EPS), reads=[t_ssq_E[s2]], writes=[t_ssq_E[s2]])
                S.op("dve", lambda e, s2=s2: e.reciprocal(out=ssq_E[s2], in_=ssq_E[s2]), reads=[t_ssq_E[s2]], writes=[t_ssq_E[s2]])
                S.op("dve", lambda e, s2=s2: e.scalar_tensor_tensor(out=x2[s2], in0=x2[s2], scalar=ssq_E[s2], in1=fngb, op0=ALU.mult, op1=ALU.mult), reads=[t_x2[s2], t_ssq_E[s2], t_fngb], writes=[t_x2[s2]])
                o_ = S.op("sp", lambda e, sig, t=t, s2=s2: sig(e.dma_start(out=y[t * 128:(t + 1) * 128, :], in_=x2[s2])), reads=[t_x2[s2]], dma=1, key=f"yo{s2}")
                outs_.append(o_)
            S.op("sp", None, extra=outs_)
        try:
            author()
        except _Stop:
            pass
        if dump_ops:
            S.op("sp", None, extra=dump_ops)
        S.emit(block, sems)
    return nc


def _prep(inputs):
    f = lambda a: np.ascontiguousarray(np.asarray(a, dtype=np.float32))
    x = f(inputs["x"]); c = f(inputs["c"]); ctx = f(inputs["ctx"]); c_ctx = f(inputs["c_ctx"])
    w_in = f(inputs["w_in"])[0]
    conv_w = f(inputs["conv_w"])[0]
    a_log = f(inputs["a_log"])[0]; dt_bias = f(inputs["dt_bias"])[0]
    idx = np.arange(128)
    tri = np.stack([(idx[:, None] <= idx[None, :]), (idx[:, None] > idx[None, :]), (idx[:, None] >= idx[None, :]), (idx[:, None] < idx[None, :])]).astype(np.float32)
    negm = np.stack([(idx[:, None] <= idx[None, :]), (idx[None, :] < idx[:, None]), (idx[:, None] >= idx[None, :]), (idx[None, :] > idx[:, None])]).astype(np.float32) * -100.0
    blk = lambda n: (idx[:, None] // n == idx[None, :] // n)
    dcm = [blk(8)]
    for n in (16, 32, 64, 128):
        low = blk(n) & ((idx[:, None] % n) >= n // 2) & ((idx[None, :] % n) < n // 2)
        dcm += [low, low.T]
    import ml_dtypes
    dcm = np.stack(dcm).astype(np.float32).astype(ml_dtypes.bfloat16)
    common = {
        "dcm": dcm,
        "ada_w": f(inputs["ada_w"])[0], "ada_b": f(inputs["ada_b"]).reshape(1, -1),
        "gcols": np.ascontiguousarray(np.stack([f(inputs["norm_mix_g"])[0].reshape(8, 128).T, f(inputs["norm_ffn_g"])[0].reshape(8, 128).T], axis=-1)),
        "gng": f(inputs["gdn_norm_g"])[0].reshape(128, 1),
        "ident": np.eye(128, dtype=np.float32), "tri": tri, "negm": negm,
        "w_rest": np.ascontiguousarray(w_in[:, COL_Z:]),
        "sgu_wT": np.ascontiguousarray(f(inputs["sgu_w"])[0].transpose(0, 2, 1)),
        "sgu_bb": np.ascontiguousarray(np.broadcast_to(f(inputs["sgu_b"])[0][None], (128, 8, 128))),
        "lng_c": np.ascontiguousarray(f(inputs["sgu_ln_g"])[0].reshape(8, 128).T),
        "lnb_bc": np.ascontiguousarray(np.broadcast_to(f(inputs["sgu_ln_b"])[0][None], (128, D))),
        "w_a": f(inputs["w_branch_a"])[0], "w_b": f(inputs["w_branch_b"])[0], "w_out": f(inputs["w_out"])[0],
        "rw": np.ascontiguousarray(np.concatenate([f(inputs["router_group_w"])[0], f(inputs["router_expert_w"])[0]], axis=1)),
        "rb": np.ascontiguousarray(np.broadcast_to(np.concatenate([f(inputs["router_group_b"])[0], f(inputs["router_expert_b"])[0]])[None], (128, 36))),
        "ew1": f(inputs["expert_w1"])[0], "ew3": f(inputs["expert_w3"])[0], "ew2": f(inputs["expert_w2"])[0],
        "fng_bc": np.ascontiguousarray(np.broadcast_to(f(inputs["final_norm_g"])[None], (128, D))),
    }
    maps = []
    for core in range(8):
        b, r = core // 4, core % 4
        heads = (2 * r, 2 * r + 1)
        qcols = np.concatenate([np.arange(base + h * 128, base + (h + 1) * 128) for base in (0, 1024, 2048) for h in heads])
        bacols = np.array([COL_BETA + d * 8 + h for d in range(2) for h in heads] + [COL_A + d * 8 + h for d in range(2) for h in heads])
        conv_c = np.ascontiguousarray(conv_w[:, qcols].reshape(5, 6, 128).transpose(2, 1, 0))
        gsc = np.concatenate([np.array([a_log[d, h] for d in range(2) for h in heads]), np.array([dt_bias[d, h] for d in range(2) for h in heads])]).astype(np.float32)
        qm = np.zeros((128, 4), np.float32); qm[:, r] = 1.0
        m = dict(common)
        m.update({
            "xb": x[b], "ctxb": ctx[b], "xo": np.ascontiguousarray(x[b, r * OWN:(r + 1) * OWN]),
            "cvT": np.ascontiguousarray(np.stack([c[b].reshape(8, 128).T, c_ctx.reshape(8, 128).T], axis=-1)),
            "w_qkv": np.ascontiguousarray(w_in[:, qcols]), "w_ba": np.ascontiguousarray(w_in[:, bacols]),
            "gsc66": np.ascontiguousarray(np.broadcast_to(gsc.reshape(1, 2, 1, 4), (128, 2, NT, 4))),
            "conv_c": conv_c, "gsc": np.ascontiguousarray(np.broadcast_to(gsc[None], (128, 8))), "qmask": qm,
        })
        maps.append(m)
    return maps


_NC = None


def kernel(**inputs):
    global _NC
    if _NC is None:
        _NC = build_program()
    maps = _prep(inputs)
    res = run_bass_kernel_spmd(_NC, maps, core_ids=list(range(8)))
    out = np.zeros((2, SEQ, D), np.float32)
    for core in range(8):
        b, r = core // 4, core % 4
        out[b, r * OWN:(r + 1) * OWN] = np.asarray(res.results[core]["y"], dtype=np.float32)
    return out
```

```python
import contextlib
import os
import numpy as np
import concourse.bass as bass
import concourse.mybir as mybir
from concourse.bass_utils import run_bass_kernel_spmd

F32 = mybir.dt.float32
BF16 = mybir.dt.bfloat16
ALU = mybir.AluOpType
AF = mybir.ActivationFunctionType

D = 1024
SEQ = 8192
CTX = 256
NT = 66
OWN = 2048
NOT = 16
NE = 32
DE = 256
COL_BETA = 3072
COL_A = COL_BETA + 16
COL_Z = COL_A + 16
EPS = 1e-6
ARENA = 53000


SAME_ENGINE_WAITS = os.environ.get('SAMEENG', '1') == '1'


class Tk:
    __slots__ = ("name", "w", "rd", "excl")

    def __init__(self, name="", excl=False):
        self.name = name
        self.w = None
        self.rd = []
        self.excl = excl


class Op:
    __slots__ = ("eng", "fn", "deps", "used", "sem", "val", "dma", "key", "idx", "inc")


class Sched:
    ENGS = ("pe", "act", "dve", "pool", "sp")

    def __init__(self, nc):
        self.nc = nc
        self.ops = {e: [] for e in self.ENGS}
        self.all = []
        self.dma_since_barrier = []

    def op(self, eng, fn, reads=(), writes=(), dma=0, key=None, extra=(), inc=16):
        o = Op()
        o.eng = eng; o.fn = fn; o.used = False; o.sem = None; o.val = None
        o.dma = dma; o.key = key; o.idx = len(self.all); o.inc = inc
        deps = set(extra)
        reads = list(reads); writes = list(writes)
        for r in list(reads):
            if r.excl and r not in writes:
                writes.append(r)
        for r in reads:
            if r.w is not None:
                deps.add(r.w)
        for w in writes:
            if w.w is not None:
                deps.add(w.w)
            for x in w.rd:
                deps.add(x)
        for r in reads:
            r.rd.append(o)
        for w in writes:
            w.w = o
            w.rd = []
        o.deps = [d for d in deps if d is not o and not (d.eng == "pe" and eng == "pe" and not d.dma and not dma)
                  and not (SAME_ENGINE_WAITS is False and d.eng == eng and eng in ("act", "dve", "pool") and not d.dma and not dma)]
        for d in o.deps:
            d.used = True
        if dma:
            assert key is not None
            self.dma_since_barrier.append(o)
        self.ops[eng].append(o)
        self.all.append(o)
        return o

    def barrier(self):
        last = [self.ops[e][-1] for e in self.ENGS if self.ops[e] and self.ops[e][-1].fn is not None]
        dmas = list(self.dma_since_barrier)
        self.dma_since_barrier = []
        for e in self.ENGS:
            self.op(e, None, extra=[x for x in last if x.eng != e] + dmas)

    def emit(self, block, sems):
        nc = self.nc
        sems = list(sems)
        eng_sem = {e: sems.pop() for e in ("pe", "act", "dve", "pool")}
        keysem = {}
        cnt = {e: 0 for e in eng_sem}
        kcnt = {}
        for o in self.all:
            if o.dma:
                if o.key not in keysem:
                    keysem[o.key] = sems.pop()
                    kcnt[o.key] = 0
                kcnt[o.key] += o.inc * o.dma
                o.sem = keysem[o.key]; o.val = kcnt[o.key]
            elif o.used:
                assert o.fn is not None
                cnt[o.eng] += 1
                o.sem = eng_sem[o.eng]; o.val = cnt[o.eng]
        engobj = {"pe": nc.tensor, "act": nc.scalar, "dve": nc.vector, "pool": nc.gpsimd, "sp": nc.sync}
        deco = {"pe": block.tensor, "act": block.scalar, "dve": block.vector, "pool": block.gpsimd, "sp": block.sync}

        def run(ename):
            def body(e):
                known = {}
                for o in self.ops[ename]:
                    for d in sorted(o.deps, key=lambda x: x.idx):
                        sid = id(d.sem)
                        if known.get(sid, 0) >= d.val:
                            continue
                        e.wait_ge(d.sem, d.val)
                        known[sid] = d.val
                    if o.fn is None:
                        continue
                    if o.dma:
                        n = [0]

                        def sig(inst, o=o, n=n):
                            if o.inc == 16:
                                inst.then_inc(o.sem, 16)
                            else:
                                inst.then_inc(o.sem)
                            n[0] += 1
                            return inst
                        o.fn(e, sig)
                        assert n[0] == o.dma, (n[0], o.dma)
                    else:
                        inst = o.fn(e)
                        if o.used:
                            inst.then_inc(o.sem, 1)
            return body
        for ename in self.ENGS:
            deco[ename](run(ename))


class Arena:
    def __init__(self, ap_f32, nf32, base=0):
        self.ap = ap_f32
        self.n = base + nf32
        self.off = base
        self.hi = 0

    def mark(self):
        return self.off

    def release(self, m):
        self.off = m

    def alloc(self, free_shape, dtype=F32):
        n = int(np.prod(free_shape))
        nf = n if dtype == F32 else (n + 1) // 2
        nf = (nf + 1) // 2 * 2
        assert self.off + nf <= self.n, ("arena overflow", self.off, nf, self.n)
        v = self.ap[:, self.off:self.off + nf]
        self.off += nf
        self.hi = max(self.hi, self.off)
        if dtype != F32:
            v = v.bitcast(dtype)[:, 0:n]
        else:
            v = v[:, 0:n]
        if len(free_shape) == 2:
            v = v.rearrange("p (a b) -> p a b", a=free_shape[0])
        elif len(free_shape) == 3:
            v = v.rearrange("p (a b c) -> p a b c", a=free_shape[0], b=free_shape[1])
        return v


class _Stop(Exception):
    pass


def build_program(debug=False, stage=99):
    KCUT = int(os.environ.get('KCUT', '0')); NTL = int(os.environ.get('NTL', str(NT)))
    nc = bass.Bass("TRN2", target_bir_lowering=False)

    def din(name, shape, dt=F32):
        return nc.dram_tensor(name, list(shape), dt, kind="ExternalInput")

    xb = din("xb", [SEQ, D]); ctxb = din("ctxb", [CTX, D]); xo = din("xo", [OWN, D])
    cvT = din("cvT", [128, 8, 2]); ada_w = din("ada_w", [D, 6 * D]); ada_b = din("ada_b", [1, 6 * D])
    gcols = din("gcols", [128, 8, 2])
    w_qkv = din("w_qkv", [D, 768]); w_ba = din("w_ba", [D, 8])
    gsc66 = din("gsc66", [128, 2, NT, 4])
    conv_c = din("conv_c", [128, 6, 5]); gsc = din("gsc", [128, 8]); gng = din("gng", [128, 1])
    dcm_d = din("dcm", [9, 128, 128], BF16)
    ident_d = din("ident", [128, 128]); tri_d = din("tri", [4, 128, 128]); neg_d = din("negm", [4, 128, 128])
    w_rest = din("w_rest", [D, 5120]); sgu_wT = din("sgu_wT", [8, 128, 128]); sgu_bb = din("sgu_bb", [128, 8, 128])
    lng_c = din("lng_c", [128, 8]); lnb_bc = din("lnb_bc", [128, D])
    w_a = din("w_a", [D, D]); w_b = din("w_b", [D, D]); w_out = din("w_out", [D, D])
    rw = din("rw", [D, 36]); rb = din("rb", [128, 36])
    ew1 = din("ew1", [NE, D, DE]); ew3 = din("ew3", [NE, D, DE]); ew2 = din("ew2", [NE, DE, D])
    fng_bc = din("fng_bc", [128, D]); qmask_d = din("qmask", [128, 4])
    y = nc.dram_tensor("y", [OWN, D], F32, kind="ExternalOutput")
    snd = [nc.dram_tensor(f"snd{j}", [2 * 128, 1024], F32) for j in range(4)]
    rcv = [nc.dram_tensor(f"rcv{j}", [4 * 2 * 128, 1024], F32) for j in range(4)]
    x1d = nc.dram_tensor("x1d", [OWN, D], F32)
    oacc_d = nc.dram_tensor("oacc_d", [128, 128, 128], BF16)
    dbg = None
    if debug:
        dbg = nc.dram_tensor("dbg", [128, 4096], F32, kind="ExternalOutput")
        dbgb = nc.dram_tensor("dbgb", [768, SEQ], BF16, kind="ExternalOutput")

    es = contextlib.ExitStack()
    with es:
        arena_t = es.enter_context(nc.sbuf_tensor("arena", [128, ARENA], F32))
        psb = [es.enter_context(nc.psum_tensor(f"psb{i}", [128, 512], F32)) for i in range(8)]
        sems = [es.enter_context(nc.semaphore(f"s{i}")) for i in range(100)]
        block = es.enter_context(nc.Block())
        A = Arena(arena_t, ARENA)
        S = Sched(nc)

        dump_ops = []

        def dump(ap, col0, ncols, reads=()):
            o_ = S.op("sp", lambda e, sig: sig(e.dma_start(out=dbg[:, col0:col0 + ncols], in_=ap)), reads=list(reads), dma=1, key="dbg")
            dump_ops.append(o_)

        def cut(k, dumps):
            if stage == k:
                S.barrier()
                for d_ in dumps():
                    dump(*d_)
                raise _Stop()

        def dumpb(ap, row0, nrows, col0, ncols, reads=(), extra=()):
            o_ = S.op("sp", lambda e, sig: sig(e.dma_start(out=dbgb[row0:row0 + nrows, col0:col0 + ncols], in_=ap)), reads=list(reads), extra=list(extra), dma=1, key="dbg")
            dump_ops.append(o_)

        def author():
            nonlocal A
            class Pool:
                def __init__(self, items):
                    self.items = items
                    self.i = 0

                def get(self):
                    it = self.items[self.i % len(self.items)]
                    self.i += 1
                    return it
            p128 = Pool([(psb[b][:, 0:128], Tk(f"p128_{b}", excl=True)) for b in range(4)])
            p512 = Pool([(psb[b][:, :], Tk(f"p512_{b}", excl=True)) for b in range(4, 8)])

            def bfv(ap):
                n = ap.shape[-1]
                return ap.bitcast(BF16)[:, 0:n]

            def load(dst, src, tk, key):
                return S.op("sp", lambda e, sig: sig(e.dma_start(out=dst, in_=src)), writes=[tk], dma=1, key=key)

            identf = A.alloc([128]); t_identf = Tk(); load(identf, ident_d[:, :], t_identf, "c0")
            identb = A.alloc([128], BF16); t_identb = Tk()
            S.op("pool", lambda e: e.tensor_copy(out=identb, in_=identf), reads=[t_identf], writes=[t_identb])
            trif = A.alloc([4, 128]); t_trif = Tk(); load(trif, tri_d.ap().rearrange("a p q -> p a q"), t_trif, "c1")
            negf = A.alloc([4, 128]); t_negf = Tk(); load(negf, neg_d.ap().rearrange("a p q -> p a q"), t_negf, "c2")
            negb = A.alloc([4, 128], BF16); t_negb = Tk()
            S.op("pool", lambda e: e.tensor_copy(out=negb, in_=negf), reads=[t_negf], writes=[t_negb])
            dcm = A.alloc([9, 128], BF16); t_dcm = Tk(); load(dcm, dcm_d.ap().rearrange("a p q -> p a q"), t_dcm, "c2b")
            onesf = A.alloc([128]); t_onesf = Tk()
            S.op("pool", lambda e: e.memset(onesf, 1.0), writes=[t_onesf])
            Uf, Vf, Ub, Vb = (trif[:, i, :] for i in range(4))
            gscs = A.alloc([8]); t_gscs = Tk(); load(gscs, gsc[:, :], t_gscs, "c3")
            negA = A.alloc([4]); t_negA = Tk()
            S.op("act", lambda e: e.activation(out=negA, in_=gscs[:, 0:4], func=AF.Exp), reads=[t_gscs], writes=[t_negA])
            S.op("dve", lambda e: e.tensor_scalar(out=negA, in0=negA, scalar1=-1.0, scalar2=None, op0=ALU.mult), reads=[t_negA], writes=[t_negA])
            dtb = gscs[:, 4:8]
            convc = A.alloc([6, 5]); t_convc = Tk(); load(convc, conv_c[:, :, :], t_convc, "c4")
            gngs = A.alloc([1]); t_gngs = Tk(); load(gngs, gng[:, :], t_gngs, "c5")
            qmask = A.alloc([4]); t_qmask = Tk(); load(qmask, qmask_d[:, :], t_qmask, "c6")
            gcol = A.alloc([8, 2]); t_gcol = Tk(); load(gcol, gcols[:, :, :], t_gcol, "c7")
            modc = A.alloc([6, 8]); t_modc = Tk("modc")
            gmix = A.alloc([D]); t_gmix = Tk("gmix")
            gffn = A.alloc([D]); t_gffn = Tk("gffn")
            GT = A.alloc([NOT, 32]); t_GT = [Tk() for _ in range(NOT)]

            m0 = A.mark()
            scT = A.alloc([8, 2]); t_scT = Tk(); load(scT, cvT[:, :, :], t_scT, "c8")
            S.op("act", lambda e: e.activation(out=scT, in_=scT, func=AF.Silu), reads=[t_scT], writes=[t_scT])
            modrow = A.alloc([6 * D]); t_modrow = Tk("modrow")
            modrow_c = A.alloc([6 * D]); t_modrow_c = Tk("modrow_c")
            adb = A.alloc([6 * D]); t_adb = Tk()
            S.op("sp", lambda e, sig: (sig(e.dma_start(out=adb[0:1, :], in_=ada_b[:, :])), sig(e.dma_start(out=adb[1:2, :], in_=ada_b[:, :]))),
                 writes=[t_adb], dma=2, key="c9")
            adw = [A.alloc([8, 512]) for _ in range(2)]; t_adw = [Tk(), Tk()]
            ada_v = ada_w.ap().rearrange("(k p) n -> p k n", p=128)
            for nb in range(12):
                sl = nb % 2
                S.op("sp", lambda e, sig, nb=nb, sl=sl: (sig(e.dma_start(out=adw[sl][:, 0:4, :], in_=ada_v[:, 0:4, nb * 512:(nb + 1) * 512])),
                                                        sig(e.dma_start(out=adw[sl][:, 4:8, :], in_=ada_v[:, 4:8, nb * 512:(nb + 1) * 512]))),
                     writes=[t_adw[sl]], dma=2, key=f"adw{sl}")
                pp, tp = p512.get()

                def f(e, sl=sl, pp=pp):
                    for k in range(8):
                        i = e.matmul(pp[0:2, :], lhsT=scT[:, k, :], rhs=adw[sl][:, k, :], start=(k == 0), stop=(k == 7))
                    return i
                S.op("pe", f, reads=[t_scT, t_adw[sl]], writes=[tp])
                S.op("dve", lambda e, nb=nb, pp=pp: e.tensor_tensor(out=modrow[0:2, nb * 512:(nb + 1) * 512], in0=pp[0:2, :], in1=adb[0:2, nb * 512:(nb + 1) * 512], op=ALU.add),
                     reads=[tp, t_adb], writes=[t_modrow])
            S.op("sp", lambda e, sig: sig(e.dma_start(out=modrow_c[0:1, :], in_=modrow[1:2, :])), reads=[t_modrow], writes=[t_modrow_c], dma=1, key="c10")
            pp, tp = p128.get()
            vecs = [(modrow, 0), (modrow, 1), (modrow_c, 0), (modrow_c, 1), (modrow, 3), (modrow, 4)]

            def f(e, pp=pp):
                for vi, (row, m) in enumerate(vecs):
                    for k in range(8):
                        i = e.matmul(pp[:, vi * 8 + k:vi * 8 + k + 1], lhsT=row[0:1, m * D + k * 128:m * D + (k + 1) * 128], rhs=onesf[0:1, 0:1], start=True, stop=True)
                return i
            S.op("pe", f, reads=[t_modrow, t_modrow_c, t_onesf], writes=[tp])
            S.op("dve", lambda e, pp=pp: e.tensor_copy(out=modc.rearrange("p a b -> p (a b)"), in_=pp[:, 0:48]), reads=[tp], writes=[t_modc])
            for vi, gi in ((1, 0), (3, 0), (5, 1)):
                S.op("dve", lambda e, vi=vi, gi=gi: e.scalar_tensor_tensor(out=modc[:, vi, :], in0=modc[:, vi, :], scalar=1.0, in1=gcol[:, :, gi], op0=ALU.add, op1=ALU.mult),
                     reads=[t_modc, t_gcol], writes=[t_modc])
            for dst, tdst, m in ((gmix, t_gmix, 2), (gffn, t_gffn, 5)):
                for h in range(2):
                    pp, tp = p512.get()
                    S.op("pe", lambda e, pp=pp, m=m, h=h: e.matmul(pp, lhsT=onesf[0:1, :], rhs=modrow[0:1, m * D + h * 512:m * D + (h + 1) * 512], start=True, stop=True),
                         reads=[t_modrow, t_onesf], writes=[tp])
                    S.op("act", lambda e, pp=pp, dst=dst, h=h: e.copy(out=dst[:, h * 512:(h + 1) * 512], in_=pp), reads=[tp], writes=[tdst])
            S.barrier()
            A.release(m0)
            if stage == 0:
                dump(modc.rearrange("p a b -> p (a b)"), 0, 48); dump(gmix[:, 0:256], 64, 256); dump(gffn[:, 0:256], 320, 256)
                raise _Stop()

            mA = A.mark()
            SC = A.alloc([NT, 28]); t_SC = [Tk("SC")] * NT
            QN = A.alloc([NT, 2, 128], BF16); KN = A.alloc([NT, 2, 128], BF16); VV = A.alloc([NT, 2, 128], BF16)
            t_QKV = [[Tk() for _ in range(6)] for t in range(NT)]
            mA1 = A.mark()
            wst = A.alloc([8, 776]); t_wst = Tk()
            wq = A.alloc([8, 896], BF16); t_wq = Tk()
            S.op("sp", lambda e, sig: (sig(e.dma_start(out=wst[:, :, 0:768], in_=w_qkv.ap().rearrange("(k p) n -> p k n", p=128))),
                                       sig(e.dma_start(out=wst[:, :, 768:776], in_=w_ba.ap().rearrange("(k p) n -> p k n", p=128)))),
                 writes=[t_wst], dma=2, key="wst")
            S.op("pool", lambda e: e.tensor_copy(out=wq[:, :, 0:776], in_=wst), reads=[t_wst], writes=[t_wq])
            xt_A = [A.alloc([D]) for _ in range(3)]; t_xt_A = [Tk() for _ in range(3)]
            junk_A = A.alloc([D]); t_junk_A = Tk()
            ssq_A = [A.alloc([1]) for _ in range(3)]; t_ssq_A = [Tk() for _ in range(3)]
            xs_A = [A.alloc([8, 128], BF16) for _ in range(2)]; t_xs_A = [Tk(), Tk()]
            hxT = [A.alloc([8, 128], BF16) for _ in range(2)]; t_hxT = [Tk(), Tk()]; t_hxTb = [Tk(), Tk()]
            PRE = [A.alloc([6, 132]) for _ in range(3)]; t_PRE = [Tk() for _ in range(3)]
            CV = A.alloc([6, 128]); t_CVc = [Tk() for _ in range(6)]
            SQ = A.alloc([6, 128], BF16); t_SQ = Tk()
            sm = [A.alloc([40]) for _ in range(2)]; t_sm = [Tk(), Tk()]
            nr = [A.alloc([8]) for _ in range(2)]; t_nr = [Tk(), Tk()]

            wst_flat = wst.rearrange("p a b -> p (a b)")
            BA = wst_flat[:, 0:NT * 8].rearrange("p (a b) -> p a b", b=8); t_BA = Tk("BA")
            g66 = A.alloc([2, NT, 4]); t_g66 = Tk(); load(g66, gsc66[:, :, :, :], t_g66, "c3b")
            Mx = [wst_flat[:, 1024 + i_ * 512:1024 + i_ * 512 + NT * 4].rearrange("p (a b) -> p a b", b=4) for i_ in range(4)]; t_Mx = [Tk() for _ in range(4)]

            def rows_of(t):
                if t < 2:
                    return ctxb[t * 128:(t + 1) * 128, :]
                return xb[(t - 2) * 128:(t - 1) * 128, :]

            def front(t, part):
                s3 = t % 3; s2 = t % 2
                if part == "b":
                    return front_b(t, s3, s2)
                isctx = t < 2
                shc = modc[:, 2 if isctx else 0, :]; scc = modc[:, 3 if isctx else 1, :]
                S.op("sp", lambda e, sig: sig(e.dma_start(out=xt_A[s3], in_=rows_of(t))), writes=[t_xt_A[s3]], dma=1, key=f"xt_A{s3}")
                S.op("act", lambda e: e.activation(out=junk_A, in_=xt_A[s3], func=AF.Square, accum_out=ssq_A[s3]), reads=[t_xt_A[s3]], writes=[t_ssq_A[s3]])
                S.op("act", lambda e: e.activation(out=ssq_A[s3], in_=ssq_A[s3], func=AF.Sqrt, scale=1.0 / D, bias=EPS), reads=[t_ssq_A[s3]], writes=[t_ssq_A[s3]])
                S.op("dve", lambda e: e.reciprocal(out=ssq_A[s3], in_=ssq_A[s3]), reads=[t_ssq_A[s3]], writes=[t_ssq_A[s3]])
                S.op("pool", lambda e: e.tensor_scalar(out=xs_A[s2].rearrange("p a b -> p (a b)"), in0=xt_A[s3], scalar1=ssq_A[s3], scalar2=1.0, op0=ALU.mult, op1=ALU.mult),
                     reads=[t_xt_A[s3], t_ssq_A[s3]], writes=[t_xs_A[s2]])
                pp, tp = p512.get()
                ppb = pp.bitcast(BF16)

                def tr(e):
                    for k in range(8):
                        i = e.transpose(out=ppb[:, k * 128:(k + 1) * 128], in_=xs_A[s2][:, k, :], identity=identb)
                    return i
                S.op("pe", tr, reads=[t_xs_A[s2], t_identb], writes=[tp])

                def ev_d(e):
                    for k in range(8):
                        i = e.tensor_scalar(out=hxT[s2][:, k, :], in0=ppb[:, k * 128:(k + 1) * 128], scalar1=scc[:, k:k + 1], scalar2=shc[:, k:k + 1], op0=ALU.mult, op1=ALU.add)
                    return i
                t_h2 = t_hxTb[s2]
                S.op("dve", ev_d, reads=[tp, t_modc], writes=[t_hxT[s2], t_h2])
                return

            def front_b(t, s3, s2):
                pA, tA = p512.get()
                pB, tB = p512.get()

                def pjA(e):
                    for ch in range(4):
                        for k in range(8):
                            i = e.matmul(pA[:, ch * 128:(ch + 1) * 128], lhsT=wq[:, k, ch * 128:(ch + 1) * 128], rhs=hxT[s2][:, k, :], start=(k == 0), stop=(k == 7))
                    return i

                def pjB(e):
                    for ch in range(4, 6):
                        for k in range(8):
                            i = e.matmul(pB[:, (ch - 4) * 128:(ch - 3) * 128], lhsT=wq[:, k, ch * 128:(ch + 1) * 128], rhs=hxT[s2][:, k, :], start=(k == 0), stop=(k == 7))
                    for k in range(8):
                        i = e.matmul(pB[:, 256:264], lhsT=hxT[s2][:, k, :], rhs=wq[:, k, 768:776], start=(k == 0), stop=(k == 7))
                    return i
                S.op("pe", pjA, reads=[t_wq, t_hxT[s2]], writes=[tA])
                S.op("pe", pjB, reads=[t_wq, t_hxT[s2]], writes=[tB])
                pba = pB[:, 256:264]; tba = tB
                S.op("dve", lambda e: e.tensor_copy(out=PRE[s3][:, 0:4, 2:130], in_=pA.rearrange("p (a b) -> p a b", a=4)), reads=[tA], writes=[t_PRE[s3]])
                S.op("dve", lambda e: e.tensor_copy(out=PRE[s3][:, 4:6, 2:130], in_=pB[:, 0:256].rearrange("p (a b) -> p a b", a=2)), reads=[tB], writes=[t_PRE[s3]])
                if KCUT == 2:
                    return
                first = t in (0, 2); last = t in (1, NT - 1)
                if first:
                    S.op("pool", lambda e: e.memset(PRE[s3][:, :, 0:2], 0.0), writes=[t_PRE[s3]])
                else:
                    sp_ = (t - 1) % 3
                    S.op("pool", lambda e: e.tensor_copy(out=PRE[sp_][:, :, 130:132], in_=PRE[s3][:, :, 2:4]), reads=[t_PRE[s3]], writes=[t_PRE[sp_]])
                if last:
                    S.op("pool", lambda e: e.memset(PRE[s3][:, :, 130:132], 0.0), writes=[t_PRE[s3]])
                else:
                    sn = (t + 1) % 3
                    S.op("pool", lambda e: e.tensor_copy(out=PRE[sn][:, :, 0:2], in_=PRE[s3][:, :, 128:130]), reads=[t_PRE[s3]], writes=[t_PRE[sn]])
                S.op("dve", lambda e: e.tensor_copy(out=BA[:, t, :], in_=pba), reads=[tba], writes=[t_BA])

            def lag(t):
                s3 = t % 3; s2 = t % 2
                for j in range(5):
                    for ch in range(6):
                        tcv = t_CVc[ch]

                        def cvj(e, ch=ch, j=j):
                            if j == 0:
                                return e.tensor_scalar(out=CV[:, ch, :], in0=PRE[s3][:, ch, 0:128], scalar1=convc[:, ch, 0:1], scalar2=None, op0=ALU.mult)
                            return e.scalar_tensor_tensor(out=CV[:, ch, :], in0=PRE[s3][:, ch, j:j + 128], scalar=convc[:, ch, j:j + 1], in1=CV[:, ch, :], op0=ALU.mult, op1=ALU.add)
                        S.op("dve", cvj, reads=[t_PRE[s3], t_convc] + ([tcv] if j else []), writes=[tcv])
                S.op("act", lambda e: e.activation(out=SQ, in_=CV, func=AF.Silu), reads=t_CVc, writes=[t_SQ])
                pT, tT = p512.get()
                pTb = pT.bitcast(BF16)

                def trs(e):
                    for ch in range(6):
                        i = e.transpose(out=pTb[:, ch * 128:(ch + 1) * 128], in_=SQ[:, ch, :], identity=identb)
                    return i
                S.op("pe", trs, reads=[t_SQ, t_identb], writes=[tT])
                pts = [(pTb[:, ch * 128:(ch + 1) * 128], tT) for ch in range(6)]
                n = nr[s2]; tn = t_nr[s2]
                for ch in range(4):
                    S.op("act", lambda e, ch=ch: e.activation(out=junk_A[:, 0:128], in_=pts[ch][0], func=AF.Square, accum_out=n[:, ch:ch + 1]),
                         reads=[pts[ch][1]], writes=[tn])
                S.op("act", lambda e: e.activation(out=n[:, 0:4], in_=n[:, 0:4], func=AF.Sqrt, bias=EPS), reads=[tn], writes=[tn])
                S.op("dve", lambda e: e.reciprocal(out=n[:, 0:4], in_=n[:, 0:4]), reads=[tn], writes=[tn])
                S.op("dve", lambda e: e.tensor_scalar(out=n[:, 0:2], in0=n[:, 0:2], scalar1=128.0 ** -0.5, scalar2=None, op0=ALU.mult), reads=[tn], writes=[tn])
                dsts = [QN[:, t, 0, :], QN[:, t, 1, :], KN[:, t, 0, :], KN[:, t, 1, :], VV[:, t, 0, :], VV[:, t, 1, :]]
                for ch in range(6):
                    if ch < 4:
                        S.op("dve", lambda e, ch=ch: e.tensor_scalar(out=dsts[ch], in0=pts[ch][0], scalar1=n[:, ch:ch + 1], scalar2=None, op0=ALU.mult),
                             reads=[pts[ch][1], tn], writes=[t_QKV[t][ch]])
                    else:
                        S.op("act", lambda e, ch=ch: e.copy(out=dsts[ch], in_=pts[ch][0]), reads=[pts[ch][1]], writes=[t_QKV[t][ch]])

            front(0, "a")
            for i in range(NTL + 1):
                if i + 1 < NTL:
                    front(i + 1, "a")
                if i < NTL:
                    front(i, "b")
                if i >= 1 and KCUT in (0, 5):
                    lag(i - 1)
            tS = t_SC[0]
            SCk = lambda k: SC[:, :, k * 4:(k + 1) * 4]
            M0, M1, M2, M3 = Mx; tM0, tM1, tM2, tM3 = t_Mx
            S.op("act", lambda e: e.activation(out=g66[:, 0, :, :], in_=g66[:, 0, :, :], func=AF.Exp), reads=[t_g66], writes=[t_g66])
            S.op("act", lambda e: e.activation(out=SCk(3), in_=BA[:, :, 0:4], func=AF.Sigmoid), reads=[t_BA], writes=[tS])
            S.op("dve", lambda e: e.tensor_tensor(out=M0, in0=BA[:, :, 4:8], in1=g66[:, 1, :, :], op=ALU.add), reads=[t_BA, t_g66], writes=[tM0])
            S.op("dve", lambda e: e.tensor_scalar(out=M1, in0=M0, scalar1=-1.0, scalar2=None, op0=ALU.mult), reads=[tM0], writes=[tM1])
            S.op("dve", lambda e: e.tensor_tensor(out=M1, in0=M1, in1=M0, op=ALU.min), reads=[tM0, tM1], writes=[tM1])
            S.op("act", lambda e: e.activation(out=M1, in_=M1, func=AF.Exp), reads=[tM1], writes=[tM1])
            S.op("act", lambda e: e.activation(out=M1, in_=M1, func=AF.Ln, bias=1.0), reads=[tM1], writes=[tM1])
            S.op("dve", lambda e: e.tensor_scalar(out=M2, in0=M0, scalar1=0.0, scalar2=None, op0=ALU.max), reads=[tM0], writes=[tM2])
            S.op("dve", lambda e: e.tensor_tensor(out=M2, in0=M2, in1=M1, op=ALU.add), reads=[tM1, tM2], writes=[tM2])
            S.op("dve", lambda e: e.scalar_tensor_tensor(out=SCk(6), in0=M2, scalar=-1.0, in1=g66[:, 0, :, :], op0=ALU.mult, op1=ALU.mult), reads=[tM2, t_g66], writes=[tS])
            pcA, tcA = p512.get(); pcB, tcB = p512.get()

            def cums(e):
                e.matmul(pcA[:, 0:2 * NT], lhsT=Uf, rhs=SC[:, :, 24:26], start=True, stop=True)
                return e.matmul(pcA[:, 2 * NT:4 * NT], lhsT=Ub, rhs=SC[:, :, 26:28], start=True, stop=True)
            S.op("pe", cums, reads=[tS, t_trif], writes=[tcA])
            S.op("pe", lambda e: e.matmul(pcB[:, 0:4 * NT], lhsT=onesf, rhs=SC[:, :, 24:28], start=True, stop=True), reads=[tS, t_onesf], writes=[tcB])
            Gf_ps = pcA[:, 0:2 * NT].rearrange("p (a b) -> p a b", b=2); Gb_ps = pcA[:, 2 * NT:4 * NT].rearrange("p (a b) -> p a b", b=2)
            Gt_ps = pcB[:, 0:4 * NT].rearrange("p (a b) -> p a b", b=4)
            S.op("act", lambda e: e.activation(out=SC[:, :, 16:18], in_=Gf_ps, func=AF.Exp), reads=[tcA], writes=[tS])
            S.op("act", lambda e: e.activation(out=SC[:, :, 18:20], in_=Gb_ps, func=AF.Exp), reads=[tcA], writes=[tS])
            S.op("act", lambda e: e.activation(out=SCk(5), in_=Gt_ps, func=AF.Exp), reads=[tcB], writes=[tS])
            S.op("dve", lambda e: e.tensor_copy(out=M3[:, :, 0:2], in_=Gf_ps), reads=[tcA], writes=[tM3])
            S.op("dve", lambda e: e.tensor_copy(out=M3[:, :, 2:4], in_=Gb_ps), reads=[tcA], writes=[tM3])
            S.op("dve", lambda e: e.tensor_tensor(out=M3, in0=Gt_ps, in1=M3, op=ALU.subtract), reads=[tcB, tM3], writes=[tM3])
            S.op("act", lambda e: e.activation(out=SCk(2), in_=M3, func=AF.Exp), reads=[tM3], writes=[tS])
            S.op("dve", lambda e: e.tensor_scalar(out=SCk(0), in0=SCk(3), scalar1=-1.0, scalar2=None, op0=ALU.mult), reads=[tS], writes=[tS])
            S.op("dve", lambda e: e.tensor_tensor(out=SCk(1), in0=SCk(3), in1=SCk(4), op=ALU.mult), reads=[tS], writes=[tS])
            S.barrier()
            A.release(mA1)
            if stage == 1:
                for i_, t_ in enumerate((0, 1, 2, 3, 33, 65)):
                    dump(SC[:, t_, :], i_ * 32, 28)
                    dumpb(QN[:, t_, :, :].rearrange("p a b -> p (a b)"), 0, 128, i_ * 768, 256)
                    dumpb(KN[:, t_, :, :].rearrange("p a b -> p (a b)"), 0, 128, i_ * 768 + 256, 256)
                    dumpb(VV[:, t_, :, :].rearrange("p a b -> p (a b)"), 0, 128, i_ * 768 + 512, 256)
                raise _Stop()

            p128_small = p128
            p128 = Pool([(psb[b_][:, 0:128], Tk(f"p128x_{b_}", excl=True)) for b_ in range(8)])
            chains = [(hl, d) for hl in range(2) for d in range(2)]
            cb = {}
            for c in chains:
                Sf = A.alloc([128]); tSf = Tk("S"); Sbb = A.alloc([128], BF16); tSbb = Tk("Sb")
                S.op("pool", lambda e, Sf=Sf: e.memset(Sf, 0.0), writes=[tSf])
                S.op("pool", lambda e, Sbb=Sbb: e.memset(Sbb, 0.0), writes=[tSbb])
                for st_ in range(2):
                    b = {}
                    for nm in ("knT", "qnT", "qdT", "X0", "XT0", "Xb", "XbT", "X0s", "XT0s", "X1s", "XT1s", "P0", "P1", "PT0", "PT1", "No", "NoT", "Wb", "Vb", "kbg", "kd", "vb", "nwT", "vnew", "QKm", "qd", "E", "ET", "ob"):
                        b[nm] = A.alloc([128], BF16); b["t_" + nm] = Tk(nm)
                    for nm in ("gV", "gU"):
                        b[nm] = A.alloc([128]); b["t_" + nm] = Tk(nm)
                    b["S"] = Sf; b["t_S"] = tSf; b["Sb"] = Sbb; b["t_Sb"] = tSbb
                    cb[(c, st_)] = b
            t_OACC = [[Tk() for _ in range(2)] for _ in range(64)]
            ost = [A.alloc([128], BF16) for _ in range(4)]; t_ost = [Tk() for _ in range(4)]
            ost_cnt = [0]
            ofin = [A.alloc([128]) for _ in range(2)]; t_ofin = [Tk(), Tk()]
            onb = [A.alloc([128], BF16) for _ in range(2)]; t_onb = [Tk(), Tk()]
            onT = [A.alloc([128], BF16) for _ in range(4)]; t_onT = [Tk() for _ in range(4)]
            fsm = [A.alloc([4]) for _ in range(2)]; t_fsm = [Tk(), Tk()]
            junk_B = A.alloc([128])
            visited = set()
            snd_ops = []
            fin_cnt = [0]
            evq = [0]

            def evac_copy(dst, src, rd, wr, scale=None):
                evq[0] += 1
                if evq[0] % 2 == 0:
                    if scale is None:
                        return S.op("act", lambda e: e.copy(out=dst, in_=src), reads=rd, writes=wr)
                    return S.op("act", lambda e: e.activation(out=dst, in_=src, func=AF.Copy, scale=scale), reads=rd, writes=wr)
                if scale is None:
                    return S.op("dve", lambda e: e.tensor_copy(out=dst, in_=src), reads=rd, writes=wr)
                return S.op("dve", lambda e: e.tensor_scalar(out=dst, in0=src, scalar1=scale, scalar2=None, op0=ALU.mult), reads=rd, writes=wr)

            def chain_step(c, t, st_):
                hl, d = c
                b = cb[(c, st_)]
                col = d * 2 + hl
                latent = t >= 2
                kn = KN[:, t, hl, :]; qn = QN[:, t, hl, :]; vv = VV[:, t, hl, :]
                tqq = t_QKV[t][hl]; tqk = t_QKV[t][2 + hl]; tqv = t_QKV[t][4 + hl]; tsc = t_SC[t]

                def scol(kind):
                    return SC[:, t, kind * 4 + col:kind * 4 + col + 1]
                U_, V_ = (Uf, Vf) if d == 0 else (Ub, Vb)
                negs = negb[:, 0 if d == 0 else 2, :]; negi = negb[:, 1 if d == 0 else 3, :]
                pk, tpk = p128.get()
                S.op("pe", lambda e: e.transpose(out=bfv(pk), in_=kn, identity=identb), reads=[tqk, t_identb], writes=[tpk])
                evac_copy(b["knT"], bfv(pk), [tpk], [b["t_knT"]])
                S.op("act", lambda e: e.activation(out=b["gV"], in_=V_, func=AF.Copy, scale=scol(6)), reads=[t_trif, tsc], writes=[b["t_gV"]])
                S.op("act", lambda e: e.activation(out=b["gU"], in_=U_, func=AF.Copy, scale=scol(6)), reads=[t_trif, tsc], writes=[b["t_gU"]])
                S.op("dve", lambda e: e.tensor_scalar(out=b["kbg"], in0=kn, scalar1=scol(1), scalar2=None, op0=ALU.mult), reads=[tqk, tsc], writes=[b["t_kbg"]])
                S.op("pool", lambda e: e.tensor_scalar(out=b["kd"], in0=kn, scalar1=scol(2), scalar2=1.0, op0=ALU.mult, op1=ALU.mult), reads=[tqk, tsc], writes=[b["t_kd"]])
                S.op("pool", lambda e: e.tensor_scalar(out=b["vb"], in0=vv, scalar1=scol(3), scalar2=1.0, op0=ALU.mult, op1=ALU.mult), reads=[tqv, tsc], writes=[b["t_vb"]])
                yield
                pd, tpd = p128.get()

                def dm(e):
                    e.matmul(pd, lhsT=identb, rhs=negs, start=True, stop=False)
                    return e.matmul(pd, lhsT=U_, rhs=b["gV"], start=False, stop=True)
                S.op("pe", dm, reads=[t_identb, t_negb, t_trif, b["t_gV"]], writes=[tpd])
                S.op("act", lambda e: e.activation(out=b["E"], in_=pd, func=AF.Exp), reads=[tpd], writes=[b["t_E"]])
                pkk, tpkk = p128.get()
                S.op("pe", lambda e: e.matmul(pkk, lhsT=b["knT"], rhs=b["knT"], start=True, stop=True), reads=[b["t_knT"]], writes=[tpkk])
                S.op("dve", lambda e: e.scalar_tensor_tensor(out=b["X0"], in0=pkk, scalar=scol(0), in1=b["E"], op0=ALU.mult, op1=ALU.mult),
                     reads=[tpkk, tsc, b["t_E"]], writes=[b["t_X0"]])
                if latent:
                    pdt, tpdt = p128.get()

                    def dmt(e):
                        e.matmul(pdt, lhsT=identb, rhs=negi, start=True, stop=False)
                        return e.matmul(pdt, lhsT=V_, rhs=b["gU"], start=False, stop=True)
                    S.op("pe", dmt, reads=[t_identb, t_negb, t_trif, b["t_gU"]], writes=[tpdt])
                    S.op("act", lambda e: e.activation(out=b["ET"], in_=pdt, func=AF.Exp), reads=[tpdt], writes=[b["t_ET"]])
                    pq_, tpq = p128.get()
                    S.op("pe", lambda e: e.transpose(out=bfv(pq_), in_=qn, identity=identb), reads=[tqq, t_identb], writes=[tpq])
                    evac_copy(b["qnT"], bfv(pq_), [tpq], [b["t_qnT"]])
                    S.op("pool", lambda e: e.tensor_scalar(out=b["qd"], in0=qn, scalar1=scol(4), scalar2=1.0, op0=ALU.mult, op1=ALU.mult), reads=[tqq, tsc], writes=[b["t_qd"]])
                yield
                px, tpx = p128.get()
                S.op("pe", lambda e: e.transpose(out=bfv(px), in_=b["X0"], identity=identb), reads=[b["t_X0"], t_identb], writes=[tpx])
                evac_copy(b["XT0"], bfv(px), [tpx], [b["t_XT0"]])
                if latent:
                    pqd, tpqd = p128.get()
                    S.op("pe", lambda e: e.transpose(out=bfv(pqd), in_=b["qd"], identity=identb), reads=[b["t_qd"], t_identb], writes=[tpqd])
                    evac_copy(b["qdT"], bfv(pqd), [tpqd], [b["t_qdT"]])
                    pqk, tpqk = p128.get()
                    S.op("pe", lambda e: e.matmul(pqk, lhsT=b["knT"], rhs=b["qnT"], start=True, stop=True), reads=[b["t_knT"], b["t_qnT"]], writes=[tpqk])
                    S.op("dve", lambda e: e.tensor_tensor(out=b["QKm"], in0=pqk, in1=b["ET"], op=ALU.mult), reads=[tpqk, b["t_ET"]], writes=[b["t_QKm"]])
                yield
                mk = lambda nm: (b[nm], b["t_" + nm])
                Xb, tXb = mk("Xb"); XbT, tXbT = mk("XbT")
                S.op("pool", lambda e: e.tensor_tensor(out=Xb, in0=b["X0"], in1=dcm[:, 0, :], op=ALU.mult), reads=[b["t_X0"], t_dcm], writes=[tXb])
                S.op("pool", lambda e: e.tensor_tensor(out=XbT, in0=b["XT0"], in1=dcm[:, 0, :], op=ALU.mult), reads=[b["t_XT0"], t_dcm], writes=[tXbT])
                S.op("pool", lambda e: e.tensor_tensor(out=b["P0"], in0=Xb, in1=identb, op=ALU.add), reads=[tXb, t_identb], writes=[b["t_P0"]])
                S.op("pool", lambda e: e.tensor_tensor(out=b["PT0"], in0=XbT, in1=identb, op=ALU.add), reads=[tXbT, t_identb], writes=[b["t_PT0"]])
                yield
                cur = 0
                cX, tcX, cXT, tcXT = Xb, tXb, XbT, tXbT
                for lev in range(2):
                    nX, tnX = mk(f"X{lev}s"); nXT, tnXT = mk(f"XT{lev}s")
                    p1, tp1 = p128.get()
                    S.op("pe", lambda e, p1=p1, cX=cX, cXT=cXT: e.matmul(p1, lhsT=cXT, rhs=cX, start=True, stop=True), reads=[tcX, tcXT], writes=[tp1])
                    evac_copy(nX, p1, [tp1], [tnX])
                    p2, tp2 = p128.get()
                    S.op("pe", lambda e, p2=p2, cX=cX, cXT=cXT: e.matmul(p2, lhsT=cX, rhs=cXT, start=True, stop=True), reads=[tcX, tcXT], writes=[tp2])
                    evac_copy(nXT, p2, [tp2], [tnXT])
                    yield
                    P = b[f"P{cur}"]; tP = b[f"t_P{cur}"]; nP = b[f"P{1 - cur}"]; tnP = b[f"t_P{1 - cur}"]
                    PT = b[f"PT{cur}"]; tPT = b[f"t_PT{cur}"]; nPT = b[f"PT{1 - cur}"]; tnPT = b[f"t_PT{1 - cur}"]
                    p3, tp3 = p128.get()
                    S.op("pe", lambda e, p3=p3, nXT=nXT, P=P: e.matmul(p3, lhsT=nXT, rhs=P, start=True, stop=True), reads=[tnXT, tP], writes=[tp3])
                    S.op("dve", lambda e, p3=p3, P=P, nP=nP: e.tensor_tensor(out=nP, in0=p3, in1=P, op=ALU.add), reads=[tp3, tP], writes=[tnP])
                    p4, tp4 = p128.get()
                    S.op("pe", lambda e, p4=p4, nX=nX, PT=PT: e.matmul(p4, lhsT=nX, rhs=PT, start=True, stop=True), reads=[tnX, tPT], writes=[tp4])
                    S.op("dve", lambda e, p4=p4, PT=PT, nPT=nPT: e.tensor_tensor(out=nPT, in0=p4, in1=PT, op=ALU.add), reads=[tp4, tPT], writes=[tnPT])
                    cur = 1 - cur
                    cX, tcX, cXT, tcXT = nX, tnX, nXT, tnXT
                    yield
                for li in range(4):
                    mi = 1 + 2 * li + (0 if d == 0 else 1)
                    miT = 1 + 2 * li + (1 if d == 0 else 0)
                    No, tNo = mk("No"); NoT, tNoT = mk("NoT")
                    P = b[f"P{cur}"]; tP = b[f"t_P{cur}"]; nP = b[f"P{1 - cur}"]; tnP = b[f"t_P{1 - cur}"]
                    PT = b[f"PT{cur}"]; tPT = b[f"t_PT{cur}"]; nPT = b[f"PT{1 - cur}"]; tnPT = b[f"t_PT{1 - cur}"]
                    S.op("pool", lambda e, mi=mi, No=No: e.tensor_tensor(out=No, in0=b["X0"], in1=dcm[:, mi, :], op=ALU.mult), reads=[b["t_X0"], t_dcm], writes=[tNo])
                    pw_, tpw_ = p128.get()
                    S.op("pe", lambda e, pw_=pw_, No=No, PT=PT: e.matmul(pw_, lhsT=No, rhs=PT, start=True, stop=True), reads=[tNo, tPT], writes=[tpw_])
                    Wb, tWb = mk("Wb")
                    evac_copy(Wb, pw_, [tpw_], [tWb])
                    if li < 3:
                        S.op("pool", lambda e, miT=miT, NoT=NoT: e.tensor_tensor(out=NoT, in0=b["XT0"], in1=dcm[:, miT, :], op=ALU.mult), reads=[b["t_XT0"], t_dcm], writes=[tNoT])
                        pv_, tpv_ = p128.get()
                        S.op("pe", lambda e, pv_=pv_, NoT=NoT, P=P: e.matmul(pv_, lhsT=NoT, rhs=P, start=True, stop=True), reads=[tNoT, tP], writes=[tpv_])
                        Vb_, tVb_ = mk("Vb")
                        evac_copy(Vb_, pv_, [tpv_], [tVb_])
                    yield
                    p5, tp5 = p128.get()
                    S.op("pe", lambda e, p5=p5, P=P, Wb=Wb: e.matmul(p5, lhsT=P, rhs=Wb, start=True, stop=True), reads=[tP, tWb], writes=[tp5])
                    S.op("dve", lambda e, p5=p5, PT=PT, nPT=nPT: e.tensor_tensor(out=nPT, in0=p5, in1=PT, op=ALU.add), reads=[tp5, tPT], writes=[tnPT])
                    if li < 3:
                        p6, tp6 = p128.get()
                        S.op("pe", lambda e, p6=p6, PT=PT, Vb_=Vb_: e.matmul(p6, lhsT=PT, rhs=Vb_, start=True, stop=True), reads=[tPT, tVb_], writes=[tp6])
                        S.op("dve", lambda e, p6=p6, P=P, nP=nP: e.tensor_tensor(out=nP, in0=p6, in1=P, op=ALU.add), reads=[tp6, tP], writes=[tnP])
                    cur = 1 - cur
                    yield
                TT = b[f"PT{cur}"]; tTT = b[f"t_PT{cur}"]
                pw, tpw = p128.get()
                S.op("pe", lambda e: e.matmul(pw, lhsT=b["kbg"], rhs=TT, start=True, stop=True), reads=[b["t_kbg"], tTT], writes=[tpw])
                evac_copy(b["nwT"], pw, [tpw], [b["t_nwT"]], scale=-1.0)
                yield
                pv, tpv = p128.get()

                def vn(e):
                    e.matmul(pv, lhsT=TT, rhs=b["vb"], start=True, stop=False)
                    return e.matmul(pv, lhsT=b["nwT"], rhs=b["Sb"], start=False, stop=True)
                S.op("pe", vn, reads=[tTT, b["t_vb"], b["t_nwT"], b["t_Sb"]], writes=[tpv])
                evac_copy(b["vnew"], pv, [tpv], [b["t_vnew"]])
                yield
                if latent:
                    lt = t - 2
                    po, tpo = p128.get()

                    def om(e):
                        e.matmul(po, lhsT=b["qdT"], rhs=b["Sb"], start=True, stop=False)
                        return e.matmul(po, lhsT=b["QKm"], rhs=b["vnew"], start=False, stop=True)
                    S.op("pe", om, reads=[b["t_qdT"], b["t_Sb"], b["t_QKm"], b["t_vnew"]], writes=[tpo])
                    if (lt, hl) not in visited:
                        visited.add((lt, hl))
                        k4o = ost_cnt[0] % 4; ost_cnt[0] += 1
                        evac_copy(ost[k4o], po, [tpo], [t_ost[k4o]])
                        S.op("pool", lambda e, sig: sig(e.dma_start(out=oacc_d[lt * 2 + hl], in_=ost[k4o])), reads=[t_ost[k4o]], writes=[t_OACC[lt][hl]], dma=1, key=f"oaw{k4o}")
                    else:
                        k2 = fin_cnt[0] % 2; k4 = fin_cnt[0] % 4
                        fin_cnt[0] += 1
                        of = ofin[k2]; tof = t_ofin[k2]; fs = fsm[k2]; tfs = t_fsm[k2]
                        S.op("sp", lambda e, sig: sig(e.dma_start(out=b["ob"], in_=oacc_d[lt * 2 + hl])), reads=[t_OACC[lt][hl]], writes=[b["t_ob"]], dma=1, key=f"oar{hl}{d}{st_}")
                        S.op("dve", lambda e: e.tensor_tensor(out=of, in0=po, in1=b["ob"], op=ALU.add), reads=[tpo, b["t_ob"]], writes=[tof])
                        S.op("act", lambda e: e.activation(out=junk_B, in_=of, func=AF.Square, accum_out=fs[:, 0:1]), reads=[tof], writes=[tfs])
                        S.op("act", lambda e: e.activation(out=fs[:, 0:1], in_=fs[:, 0:1], func=AF.Sqrt, scale=1.0 / 128, bias=EPS), reads=[tfs], writes=[tfs])
                        S.op("dve", lambda e: e.reciprocal(out=fs[:, 0:1], in_=fs[:, 0:1]), reads=[tfs], writes=[tfs])
                        S.op("dve", lambda e: e.tensor_scalar(out=onb[k2], in0=of, scalar1=fs[:, 0:1], scalar2=None, op0=ALU.mult), reads=[tof, tfs], writes=[t_onb[k2]])
                        pt_, tpt = p128.get()
                        S.op("pe", lambda e: e.transpose(out=bfv(pt_), in_=onb[k2], identity=identb), reads=[t_onb[k2], t_identb], writes=[tpt])
                        evac_copy(onT[k4], bfv(pt_), [tpt], [t_onT[k4]])
                        o_ = S.op("pool", lambda e, sig: sig(e.dma_start(out=snd[lt // 16][hl * 128:(hl + 1) * 128, (lt % 16) * 64:(lt % 16 + 1) * 64], in_=onT[k4].bitcast(F32))),
                                  reads=[t_onT[k4]], dma=1, key=f"snd{k4}")
                        snd_ops.append(o_)
                ps_, tps = p128.get()
                S.op("pe", lambda e: e.matmul(ps_, lhsT=b["kd"], rhs=b["vnew"], start=True, stop=True), reads=[b["t_kd"], b["t_vnew"]], writes=[tps])
                S.op("dve", lambda e: e.scalar_tensor_tensor(out=b["S"], in0=b["S"], scalar=scol(5), in1=ps_, op0=ALU.mult, op1=ALU.add),
                     reads=[b["t_S"], tsc, tps], writes=[b["t_S"]])
                S.op("act", lambda e: e.copy(out=b["Sb"], in_=b["S"]), reads=[b["t_S"]], writes=[b["t_Sb"]])
                yield

            def bwd_tile(i):
                return 1 - i if i < 2 else NT + 1 - i

            HALF = 11
            alive = []
            nstart = 0
            tick = 0
            while nstart < NT or alive:
                if nstart < NT and tick % HALF == 0:
                    i = nstart; nstart += 1
                    for c in chains:
                        t = i if c[1] == 0 else bwd_tile(i)
                        alive.append([chain_step(c, t, i % 2), 0])
                nxt = []
                for g in alive:
                    try:
                        next(g[0]); g[1] += 1
                        assert g[1] < 2 * HALF, "chain step too long for the 2-deep pipeline"
                        nxt.append(g)
                    except StopIteration:
                        pass
                alive = nxt
                tick += 1
            if stage == 2:
                for i_ in range(4):
                    o_ = S.op("sp", lambda e, sig, i_=i_: sig(e.dma_start(out=dbgb[0:256, i_ * 2048:(i_ + 1) * 2048].bitcast(F32), in_=snd[i_][:, :])), extra=snd_ops, dma=1, key="dbg")
                    dump_ops.append(o_)
                for i_, c_ in enumerate(chains):
                    dump(cb[c_]["S"], i_ * 128, 128, reads=[cb[c_]["t_S"]])
                raise _Stop()
            p128 = p128_small
            ccs = []
            for j in range(4):
                ccs.append(S.op("pool", lambda e, sig, j=j: sig(e.collective_compute("AllGather", ALU.bypass, replica_groups=[[0, 1, 2, 3], [4, 5, 6, 7]],
                                                                                 ins=[snd[j].ap().opt()], outs=[rcv[j].ap().opt()])),
                                extra=snd_ops, dma=1, key=f"cc{j}", inc=1))
            S.barrier()
            A.release(mA)
            if stage == 3:
                raise _Stop()

            hxo = A.alloc([8, OWN], BF16); t_hxo = [Tk() for _ in range(NOT)]
            markH = A.mark()
            offS = A.mark()
            SZ = A.alloc([8, OWN], BF16); t_SZ = [[Tk() for _ in range(4)] for _ in range(8)]
            GU = A.alloc([8, OWN], BF16); t_GU = [[Tk() for _ in range(4)] for _ in range(8)]
            wstg = [A.alloc([8, 512]) for _ in range(2)]; t_wstg = [Tk(), Tk()]
            wbf = [A.alloc([8, 512], BF16) for _ in range(2)]; t_wbf = [Tk(), Tk()]
            mC1 = A.mark()
            xt_C = [A.alloc([D]) for _ in range(2)]; t_xt_C = [Tk(), Tk()]
            junk_C = A.alloc([D]); t_junk_C = Tk()
            ssq_C = [A.alloc([1]) for _ in range(2)]; t_ssq_C = [Tk(), Tk()]
            xs_C = [A.alloc([8, 128], BF16) for _ in range(2)]; t_xs_C = [Tk(), Tk()]
            for t in range(NOT):
                s2 = t % 2
                S.op("sp", lambda e, sig, t=t, s2=s2: sig(e.dma_start(out=xt_C[s2], in_=xo[t * 128:(t + 1) * 128, :])), writes=[t_xt_C[s2]], dma=1, key=f"cxt{s2}")
                S.op("act", lambda e, s2=s2: e.activation(out=junk_C, in_=xt_C[s2], func=AF.Square, accum_out=ssq_C[s2]), reads=[t_xt_C[s2]], writes=[t_ssq_C[s2]])
                S.op("act", lambda e, s2=s2: e.activation(out=ssq_C[s2], in_=ssq_C[s2], func=AF.Sqrt, scale=1.0 / D, bias=EPS), reads=[t_ssq_C[s2]], writes=[t_ssq_C[s2]])
                S.op("dve", lambda e, s2=s2: e.reciprocal(out=ssq_C[s2], in_=ssq_C[s2]), reads=[t_ssq_C[s2]], writes=[t_ssq_C[s2]])
                S.op("pool", lambda e, s2=s2: e.tensor_scalar(out=xs_C[s2].rearrange("p a b -> p (a b)"), in0=xt_C[s2], scalar1=ssq_C[s2], scalar2=1.0, op0=ALU.mult, op1=ALU.mult),
                     reads=[t_xt_C[s2], t_ssq_C[s2]], writes=[t_xs_C[s2]])
                pp, tp = p512.get()
                ppb = pp.bitcast(BF16)

                def tr(e, s2=s2, ppb=ppb):
                    for k in range(8):
                        i = e.transpose(out=ppb[:, k * 128:(k + 1) * 128], in_=xs_C[s2][:, k, :], identity=identb)
                    return i
                S.op("pe", tr, reads=[t_xs_C[s2], t_identb], writes=[tp])

                def ev_d(e, t=t, ppb=ppb):
                    for k in range(8):
                        i = e.tensor_scalar(out=hxo[:, k, t * 128:(t + 1) * 128], in0=ppb[:, k * 128:(k + 1) * 128], scalar1=modc[:, 1, k:k + 1], scalar2=modc[:, 0, k:k + 1], op0=ALU.mult, op1=ALU.add)
                    return i
                S.op("dve", ev_d, reads=[tp, t_modc], writes=[t_hxo[t]])
            S.barrier()
            A.release(mC1)
            wcnt = [0]

            def stream_w(src_ap_cols, ncols):
                s = wcnt[0] % 2
                wcnt[0] += 1
                v = src_ap_cols.rearrange("(k p) n -> p k n", p=128)
                S.op("sp", lambda e, sig: (sig(e.dma_start(out=wstg[s][:, 0:4, 0:ncols], in_=v[:, 0:4, :])), sig(e.dma_start(out=wstg[s][:, 4:8, 0:ncols], in_=v[:, 4:8, :]))),
                     writes=[t_wstg[s]], dma=2, key=f"wstg{s}")
                S.op("pool", lambda e: e.tensor_copy(out=wbf[s][:, :, 0:ncols], in_=wstg[s][:, :, 0:ncols]), reads=[t_wstg[s]], writes=[t_wbf[s]])
                return wbf[s], t_wbf[s]
            for cbk in range(4):
                wv, twv = stream_w(w_rest[:, cbk * 512:(cbk + 1) * 512], 512)
                dst, tdst, fn = (SZ, t_SZ, AF.Silu) if cbk < 2 else (GU, t_GU, AF.Gelu)
                for cc_ in range(4):
                    chn = (cbk % 2) * 4 + cc_
                    for tb in range(4):
                        pp, tp = p512.get()

                        def f(e, pp=pp, wv=wv, cc_=cc_, tb=tb):
                            for k in range(8):
                                i = e.matmul(pp, lhsT=wv[:, k, cc_ * 128:(cc_ + 1) * 128], rhs=hxo[:, k, tb * 512:(tb + 1) * 512], start=(k == 0), stop=(k == 7))
                            return i
                        S.op("pe", f, reads=[twv] + t_hxo[tb * 4:(tb + 1) * 4], writes=[tp])
                        S.op("act", lambda e, pp=pp, dst=dst, chn=chn, tb=tb, fn=fn: e.activation(out=dst[:, chn, tb * 512:(tb + 1) * 512], in_=pp, func=fn),
                             reads=[tp], writes=[tdst[chn][tb]])
            def cut4(k):
                if stage == 4 and int(os.environ.get("CUT4", "0")) == k:
                    S.barrier()
                    for i_, buf_ in enumerate((SZ, GU, hxo)):
                        for h_ in range(2):
                            dumpb(buf_[:, h_ * 4:(h_ + 1) * 4, :].rearrange("p a b -> p (a b)"), i_ * 256 + h_ * 128, 128, 0, SEQ)
                    raise _Stop()
            cut4(1)
            mV = A.mark()
            junk_V = A.alloc([D])
            wcnt[0] = 0
            wvh = [stream_w(w_rest[:, 2048 + hh * 512:2048 + (hh + 1) * 512], 512) for hh in range(2)]
            swf = A.alloc([8, 128]); t_swf = Tk(); load(swf, sgu_wT.ap().rearrange("g q p -> q g p"), t_swf, "c11")
            swb = A.alloc([8, 128], BF16); t_swb = Tk()
            S.op("pool", lambda e: e.tensor_copy(out=swb, in_=swf), reads=[t_swf], writes=[t_swb])
            lnbb = A.alloc([D]); t_lnbb = Tk(); load(lnbb, lnb_bc[:, :], t_lnbb, "c12")
            sbb = A.alloc([8, 128]); t_sbb = Tk(); load(sbb, sgu_bb[:, :, :], t_sbb, "c13")
            lngc = A.alloc([8]); t_lngc = Tk(); load(lngc, lng_c[:, :], t_lngc, "c14")
            BIAS = A.alloc([8, 128]); t_BIAS = Tk()
            for g in range(8):
                pp, tp = p128.get()
                S.op("pe", lambda e, pp=pp, g=g: e.matmul(pp, lhsT=lnbb[:, g * 128:(g + 1) * 128], rhs=swf[:, g, :], start=True, stop=True), reads=[t_lnbb, t_swf], writes=[tp])
                S.op("dve", lambda e, pp=pp, g=g: e.tensor_tensor(out=BIAS[:, g, :], in0=pp, in1=sbb[:, g, :], op=ALU.add), reads=[tp, t_sbb], writes=[t_BIAS])
            gv = [A.alloc([D])] * 2; t_gv = [Tk()] * 2
            vnb = [A.alloc([D], BF16) for _ in range(2)]; t_vnb = [Tk(), Tk()]
            lst = [A.alloc([8]) for _ in range(2)]; t_lst = [Tk(), Tk()]
            mtmp = [A.alloc([128]) for _ in range(2)]; t_mtmp = [Tk(), Tk()]
            for t in range(NOT):
                s2 = t % 2
                for hh in range(2):
                    pp, tp = p512.get()

                    def f(e, pp=pp, hh=hh, t=t):
                        for k in range(8):
                            i = e.matmul(pp, lhsT=hxo[:, k, t * 128:(t + 1) * 128], rhs=wvh[hh][0][:, k, :], start=(k == 0), stop=(k == 7))
                        return i
                    S.op("pe", f, reads=[wvh[hh][1], t_hxo[t]], writes=[tp])
                    S.op("act", lambda e, pp=pp, hh=hh, s2=s2: e.activation(out=gv[s2][:, hh * 512:(hh + 1) * 512], in_=pp, func=AF.Gelu), reads=[tp], writes=[t_gv[s2]])
                ls = lst[s2]; tls = t_lst[s2]
                S.op("dve", lambda e, s2=s2, ls=ls: e.tensor_reduce(out=ls[:, 0:1], in_=gv[s2], axis=mybir.AxisListType.X, op=ALU.add), reads=[t_gv[s2]], writes=[tls])
                S.op("act", lambda e, s2=s2, ls=ls: e.activation(out=junk_V, in_=gv[s2], func=AF.Square, accum_out=ls[:, 1:2]), reads=[t_gv[s2]], writes=[tls])
                S.op("dve", lambda e, ls=ls: e.tensor_scalar(out=ls[:, 0:2], in0=ls[:, 0:2], scalar1=1.0 / D, scalar2=None, op0=ALU.mult), reads=[tls], writes=[tls])
                S.op("dve", lambda e, ls=ls: e.tensor_tensor(out=ls[:, 2:3], in0=ls[:, 0:1], in1=ls[:, 0:1], op=ALU.mult), reads=[tls], writes=[tls])
                S.op("dve", lambda e, ls=ls: e.tensor_tensor(out=ls[:, 2:3], in0=ls[:, 1:2], in1=ls[:, 2:3], op=ALU.subtract), reads=[tls], writes=[tls])
                S.op("act", lambda e, ls=ls: e.activation(out=ls[:, 2:3], in_=ls[:, 2:3], func=AF.Sqrt, bias=EPS), reads=[tls], writes=[tls])
                S.op("dve", lambda e, ls=ls: e.reciprocal(out=ls[:, 2:3], in_=ls[:, 2:3]), reads=[tls], writes=[tls])
                S.op("dve", lambda e, s2=s2, ls=ls: e.tensor_scalar(out=vnb[s2], in0=gv[s2], scalar1=ls[:, 0:1], scalar2=ls[:, 2:3], op0=ALU.subtract, op1=ALU.mult),
                     reads=[t_gv[s2], tls], writes=[t_vnb[s2]])
                for g in range(8):
                    pp, tp = p128.get()
                    S.op("pe", lambda e, pp=pp, g=g, s2=s2: e.matmul(pp, lhsT=vnb[s2][:, g * 128:(g + 1) * 128], rhs=swb[:, g, :], start=True, stop=True),
                         reads=[t_vnb[s2], t_swb], writes=[tp])
                    mt = mtmp[g % 2]; tmt = t_mtmp[g % 2]
                    S.op("dve", lambda e, pp=pp, g=g, mt=mt: e.scalar_tensor_tensor(out=mt, in0=pp, scalar=lngc[:, g:g + 1], in1=BIAS[:, g, :], op0=ALU.mult, op1=ALU.add),
                         reads=[tp, t_lngc, t_BIAS], writes=[tmt])
                    S.op("pool", lambda e, g=g, t=t, mt=mt: e.tensor_tensor(out=GU[:, g, t * 128:(t + 1) * 128], in0=GU[:, g, t * 128:(t + 1) * 128], in1=mt, op=ALU.mult),
                         reads=[tmt, t_GU[g][t // 4]], writes=[t_GU[g][t // 4]])
            S.barrier()
            A.release(mV)
            cut4(2)
            mY = A.mark()
            rq = [A.alloc([4, 512], BF16) for _ in range(2)]; t_rq = [Tk(), Tk()]
            acc = [A.alloc([512]) for _ in range(2)]; t_acc = [Tk(), Tk()]
            cntr = 0
            for hp in range(4):
                for hl in range(2):
                    chn = hp * 2 + hl
                    for tb in range(4):
                        s = cntr % 2; cntr += 1
                        S.op("sp", lambda e, sig, s=s, chn=chn, tb=tb: tuple(sig(e.dma_start(out=rq[s][:, j, :].bitcast(F32), in_=rcv[j][chn * 128:(chn + 1) * 128, tb * 256:(tb + 1) * 256])) for j in range(4)),
                             extra=ccs, writes=[t_rq[s]], dma=4, key=f"rq{s}")
                        for j in range(4):
                            def f(e, s=s, j=j):
                                if j == 0:
                                    return e.tensor_scalar(out=acc[s], in0=rq[s][:, 0, :], scalar1=qmask[:, 0:1], scalar2=None, op0=ALU.mult)
                                return e.scalar_tensor_tensor(out=acc[s], in0=rq[s][:, j, :], scalar=qmask[:, j:j + 1], in1=acc[s], op0=ALU.mult, op1=ALU.add)
                            S.op("dve", f, reads=[t_rq[s], t_qmask, t_acc[s]] if j else [t_rq[s], t_qmask], writes=[t_acc[s]])
                        S.op("dve", lambda e, s=s, chn=chn, tb=tb: e.scalar_tensor_tensor(out=SZ[:, chn, tb * 512:(tb + 1) * 512], in0=acc[s], scalar=gngs[:, 0:1], in1=SZ[:, chn, tb * 512:(tb + 1) * 512], op0=ALU.mult, op1=ALU.mult),
                             reads=[t_acc[s], t_gngs, t_SZ[chn][tb]], writes=[t_SZ[chn][tb]])
            S.barrier()
            A.release(mY)
            cut4(3)
            S.barrier()
            mM = A.mark()
            MG = A.alloc([8, OWN], BF16); t_MG = [[Tk() for _ in range(4)] for _ in range(8)]
            sga = [A.alloc([512], BF16) for _ in range(2)]; t_sga = [Tk(), Tk()]
            sgb = [A.alloc([512], BF16) for _ in range(2)]; t_sgb = [Tk(), Tk()]
            m1 = [A.alloc([512]) for _ in range(2)]; t_m1 = [Tk(), Tk()]
            m2 = [A.alloc([512]) for _ in range(2)]; t_m2 = [Tk(), Tk()]
            sub = [(wstg[i][:, :, j * 128:(j + 1) * 128], wbf[i][:, :, j * 128:(j + 1) * 128], Tk(), Tk()) for i in range(2) for j in range(4)]
            subc = [0]

            def stream_small(src_cols):
                stg, bfw, tst, tbf = sub[subc[0] % 8]
                subc[0] += 1
                v = src_cols.rearrange("(k p) n -> p k n", p=128)
                S.op("sp", lambda e, sig: sig(e.dma_start(out=stg, in_=v)), writes=[tst], dma=1, key=f"sub{(subc[0] - 1) % 8}")
                S.op("pool", lambda e: e.tensor_copy(out=bfw, in_=stg), reads=[tst], writes=[tbf])
                return bfw, tbf
            cntr = 0
            for dc in range(8):
                ws = [stream_small(w_a[:, dc * 128:(dc + 1) * 128]), stream_small(w_b[:, dc * 128:(dc + 1) * 128]),
                      stream_small(w_rest[:, 3072 + dc * 128:3072 + (dc + 1) * 128]), stream_small(w_rest[:, 4096 + dc * 128:4096 + (dc + 1) * 128])]
                for tb in range(4):
                    s = cntr % 2; cntr += 1
                    outs = []
                    for wi, (src, tsrc) in enumerate(((SZ, t_SZ), (GU, t_GU), (hxo, None), (hxo, None))):
                        wv_, tw_ = ws[wi]
                        pp, tp = p512.get()

                        def f(e, pp=pp, wv_=wv_, src=src, tb=tb):
                            for k in range(8):
                                i = e.matmul(pp, lhsT=wv_[:, k, :], rhs=src[:, k, tb * 512:(tb + 1) * 512], start=(k == 0), stop=(k == 7))
                            return i
                        rds = [tw_] + ([tsrc[k][tb] for k in range(8)] if tsrc is not None else t_hxo[tb * 4:(tb + 1) * 4])
                        S.op("pe", f, reads=rds, writes=[tp])
                        outs.append((pp, tp))
                    S.op("act", lambda e, s=s, pp=outs[2][0]: e.activation(out=sga[s], in_=pp, func=AF.Sigmoid), reads=[outs[2][1]], writes=[t_sga[s]])
                    S.op("act", lambda e, s=s, pp=outs[3][0]: e.activation(out=sgb[s], in_=pp, func=AF.Sigmoid), reads=[outs[3][1]], writes=[t_sgb[s]])
                    S.op("dve", lambda e, s=s, pp=outs[0][0]: e.tensor_tensor(out=m1[s], in0=pp, in1=sga[s], op=ALU.mult), reads=[outs[0][1], t_sga[s]], writes=[t_m1[s]])
                    S.op("dve", lambda e, s=s, pp=outs[1][0]: e.tensor_tensor(out=m2[s], in0=pp, in1=sgb[s], op=ALU.mult), reads=[outs[1][1], t_sgb[s]], writes=[t_m2[s]])
                    S.op("pool", lambda e, s=s, dc=dc, tb=tb: e.tensor_tensor(out=MG[:, dc, tb * 512:(tb + 1) * 512], in0=m1[s], in1=m2[s], op=ALU.add),
                         reads=[t_m1[s], t_m2[s]], writes=[t_MG[dc][tb]])
            S.barrier()
            if stage == 4:
                for i_, (buf_, tk_) in enumerate(((SZ, t_SZ), (GU, t_GU), (MG, t_MG))):
                    for h_ in range(2):
                        dumpb(buf_[:, h_ * 4:(h_ + 1) * 4, :].rearrange("p a b -> p (a b)"), i_ * 256 + h_ * 128, 128, 0, SEQ)
                raise _Stop()
            hx2 = hxo; t_hx2 = [Tk() for _ in range(NOT)]
            Amain = A
            A = Arena(arena_t, 16384, base=offS)
            wo_b = A.alloc([8, D], BF16); t_wo_b = Tk()
            for hh in range(2):
                sl = hh
                S.op("sp", lambda e, sig, hh=hh, sl=sl: (sig(e.dma_start(out=wstg[sl][:, 0:4, :], in_=w_out.ap().rearrange("(k p) n -> p k n", p=128)[:, 0:4, hh * 512:(hh + 1) * 512])),
                                                        sig(e.dma_start(out=wstg[sl][:, 4:8, :], in_=w_out.ap().rearrange("(k p) n -> p k n", p=128)[:, 4:8, hh * 512:(hh + 1) * 512]))),
                     writes=[t_wstg[sl]], dma=2, key=f"wstg{sl}")
                for k in range(8):
                    S.op("pool", lambda e, k=k, hh=hh, sl=sl: e.tensor_tensor(out=wo_b[:, k, hh * 512:(hh + 1) * 512], in0=wstg[sl][:, k, :], in1=gmix[:, hh * 512:(hh + 1) * 512], op=ALU.mult),
                         reads=[t_wstg[sl], t_gmix], writes=[t_wo_b])
            rwf = A.alloc([8, 36]); t_rwf = Tk(); load(rwf, rw.ap().rearrange("(k p) n -> p k n", p=128), t_rwf, "c15")
            rbb = A.alloc([36]); t_rbb = Tk(); load(rbb, rb[:, :], t_rbb, "c16")
            x1 = [A.alloc([D]) for _ in range(2)]; t_x1 = [Tk(), Tk()]
            xt_D = [A.alloc([D]) for _ in range(2)]; t_xt_D = [Tk(), Tk()]
            junk_D = A.alloc([D]); t_junk_D = Tk()
            ssq_D = [A.alloc([1]) for _ in range(2)]; t_ssq_D = [Tk(), Tk()]
            xsf = [A.alloc([8, 128]) for _ in range(2)]; t_xsf = [Tk(), Tk()]
            hxf = [A.alloc([8, 128]) for _ in range(2)]; t_hxf = [Tk(), Tk()]
            rs_ = [A.alloc([64]) for _ in range(2)]; t_rs = [Tk(), Tk()]
            x1_ops = []
            for t in range(NOT):
                s2 = t % 2
                S.op("sp", lambda e, sig, t=t, s2=s2: sig(e.dma_start(out=xt_D[s2], in_=xo[t * 128:(t + 1) * 128, :])), writes=[t_xt_D[s2]], dma=1, key=f"dxt{s2}")
                for hh in range(2):
                    pp, tp = p512.get()

                    def f(e, pp=pp, hh=hh, t=t):
                        for k in range(8):
                            i = e.matmul(pp, lhsT=MG[:, k, t * 128:(t + 1) * 128], rhs=wo_b[:, k, hh * 512:(hh + 1) * 512], start=(k == 0), stop=(k == 7))
                        return i
                    S.op("pe", f, reads=[t_wo_b] + [t_MG[k][t // 4] for k in range(8)], writes=[tp])
                    S.op("dve", lambda e, pp=pp, hh=hh, s2=s2: e.tensor_tensor(out=x1[s2][:, hh * 512:(hh + 1) * 512], in0=pp, in1=xt_D[s2][:, hh * 512:(hh + 1) * 512], op=ALU.add),
                         reads=[tp, t_xt_D[s2]], writes=[t_x1[s2]])
                o_ = S.op("pool", lambda e, sig, t=t, s2=s2: sig(e.dma_start(out=x1d[t * 128:(t + 1) * 128, :], in_=x1[s2])), reads=[t_x1[s2]], dma=1, key=f"x1d{s2}")
                x1_ops.append(o_)
                S.op("act", lambda e, s2=s2: e.activation(out=junk_D, in_=x1[s2], func=AF.Square, accum_out=ssq_D[s2]), reads=[t_x1[s2]], writes=[t_ssq_D[s2]])
                S.op("act", lambda e, s2=s2: e.activation(out=ssq_D[s2], in_=ssq_D[s2], func=AF.Sqrt, scale=1.0 / D, bias=EPS), reads=[t_ssq_D[s2]], writes=[t_ssq_D[s2]])
                S.op("dve", lambda e, s2=s2: e.reciprocal(out=ssq_D[s2], in_=ssq_D[s2]), reads=[t_ssq_D[s2]], writes=[t_ssq_D[s2]])
                S.op("pool", lambda e, s2=s2: e.tensor_scalar(out=xsf[s2].rearrange("p a b -> p (a b)"), in0=x1[s2], scalar1=ssq_D[s2], scalar2=1.0, op0=ALU.mult, op1=ALU.mult),
                     reads=[t_x1[s2], t_ssq_D[s2]], writes=[t_xsf[s2]])
                for q4 in range(2):
                    pp, tp = p512.get()

                    def tr(e, pp=pp, q4=q4, s2=s2):
                        for k in range(4):
                            i = e.transpose(out=pp[:, k * 128:(k + 1) * 128], in_=xsf[s2][:, q4 * 4 + k, :], identity=identf)
                        return i
                    S.op("pe", tr, reads=[t_xsf[s2], t_identf], writes=[tp])

                    def ev(e, pp=pp, q4=q4, s2=s2, t=t):
                        for k in range(4):
                            kk = q4 * 4 + k
                            i = e.tensor_scalar(out=hxf[s2][:, kk, :], in0=pp[:, k * 128:(k + 1) * 128], scalar1=modc[:, 5, kk:kk + 1], scalar2=modc[:, 4, kk:kk + 1], op0=ALU.mult, op1=ALU.add)
                        return i
                    S.op("dve", ev, reads=[tp, t_modc], writes=[t_hxf[s2]])
                S.op("pool", lambda e, s2=s2, t=t: e.tensor_copy(out=hx2[:, :, t * 128:(t + 1) * 128], in_=hxf[s2]), reads=[t_hxf[s2]], writes=[t_hx2[t]])
                pr, tpr = p128.get()

                def rt(e, pr=pr, s2=s2):
                    for k in range(8):
                        i = e.matmul(pr[:, 0:36], lhsT=hxf[s2][:, k, :], rhs=rwf[:, k, :], start=(k == 0), stop=(k == 7))
                    return i
                S.op("pe", rt, reads=[t_hxf[s2], t_rwf], writes=[tpr])
                r = rs_[s2]; tr_ = t_rs[s2]
                S.op("dve", lambda e, pr=pr, r=r: e.tensor_tensor(out=r[:, 0:36], in0=pr[:, 0:36], in1=rbb, op=ALU.add), reads=[tpr, t_rbb], writes=[tr_])
                S.op("dve", lambda e, r=r: e.tensor_reduce(out=r[:, 40:41], in_=r[:, 0:4], axis=mybir.AxisListType.X, op=ALU.max), reads=[tr_], writes=[tr_])
                S.op("dve", lambda e, r=r: e.tensor_scalar(out=r[:, 36:40], in0=r[:, 0:4], scalar1=r[:, 40:41], scalar2=None, op0=ALU.is_ge), reads=[tr_], writes=[tr_])
                S.op("dve", lambda e, r=r: e.tensor_scalar(out=r[:, 60:64], in0=r[:, 0:4], scalar1=r[:, 40:41], scalar2=None, op0=ALU.subtract), reads=[tr_], writes=[tr_])
                S.op("act", lambda e, r=r: e.activation(out=r[:, 60:64], in_=r[:, 60:64], func=AF.Exp, accum_out=r[:, 41:42]), reads=[tr_], writes=[tr_])
                S.op("dve", lambda e, r=r: e.reciprocal(out=r[:, 41:42], in_=r[:, 41:42]), reads=[tr_], writes=[tr_])
                S.op("dve", lambda e, r=r: e.tensor_scalar(out=r[:, 42:50], in0=r[:, 4:12], scalar1=r[:, 36:37], scalar2=None, op0=ALU.mult), reads=[tr_], writes=[tr_])
                for g in range(1, 4):
                    S.op("dve", lambda e, r=r, g=g: e.scalar_tensor_tensor(out=r[:, 42:50], in0=r[:, 4 + 8 * g:12 + 8 * g], scalar=r[:, 36 + g:37 + g], in1=r[:, 42:50], op0=ALU.mult, op1=ALU.add),
                         reads=[tr_], writes=[tr_])
                S.op("dve", lambda e, r=r: e.tensor_reduce(out=r[:, 50:51], in_=r[:, 42:50], axis=mybir.AxisListType.X, op=ALU.max), reads=[tr_], writes=[tr_])
                S.op("dve", lambda e, r=r: e.tensor_scalar(out=r[:, 42:50], in0=r[:, 42:50], scalar1=r[:, 50:51], scalar2=None, op0=ALU.subtract), reads=[tr_], writes=[tr_])
                S.op("act", lambda e, r=r: e.activation(out=r[:, 42:50], in_=r[:, 42:50], func=AF.Exp), reads=[tr_], writes=[tr_])
                S.op("dve", lambda e, r=r: e.tensor_scalar(out=r[:, 52:60], in0=r[:, 42:50], scalar1=1.0, scalar2=None, op0=ALU.is_ge), reads=[tr_], writes=[tr_])
                S.op("dve", lambda e, r=r: e.scalar_tensor_tensor(out=r[:, 4:12], in0=r[:, 52:60], scalar=-2.0, in1=r[:, 42:50], op0=ALU.mult, op1=ALU.add), reads=[tr_], writes=[tr_])
                S.op("dve", lambda e, r=r: e.tensor_reduce(out=r[:, 51:52], in_=r[:, 4:12], axis=mybir.AxisListType.X, op=ALU.max), reads=[tr_], writes=[tr_])
                S.op("dve", lambda e, r=r: e.tensor_scalar(out=r[:, 12:20], in0=r[:, 4:12], scalar1=r[:, 51:52], scalar2=None, op0=ALU.is_ge), reads=[tr_], writes=[tr_])
                S.op("dve", lambda e, r=r: e.scalar_tensor_tensor(out=r[:, 20:28], in0=r[:, 12:20], scalar=r[:, 51:52], in1=r[:, 52:60], op0=ALU.mult, op1=ALU.add), reads=[tr_], writes=[tr_])
                S.op("dve", lambda e, r=r: e.tensor_scalar(out=r[:, 50:51], in0=r[:, 51:52], scalar1=1.0, scalar2=None, op0=ALU.add), reads=[tr_], writes=[tr_])
                S.op("dve", lambda e, r=r: e.reciprocal(out=r[:, 50:51], in_=r[:, 50:51]), reads=[tr_], writes=[tr_])
                S.op("dve", lambda e, r=r: e.tensor_tensor(out=r[:, 50:51], in0=r[:, 50:51], in1=r[:, 41:42], op=ALU.mult), reads=[tr_], writes=[tr_])
                S.op("dve", lambda e, r=r: e.tensor_scalar(out=r[:, 20:28], in0=r[:, 20:28], scalar1=r[:, 50:51], scalar2=None, op0=ALU.mult), reads=[tr_], writes=[tr_])
                for g in range(4):
                    S.op("dve", lambda e, r=r, g=g, t=t: e.tensor_scalar(out=GT[:, t, g * 8:(g + 1) * 8], in0=r[:, 20:28], scalar1=r[:, 36 + g:37 + g], scalar2=None, op0=ALU.mult),
                         reads=[tr_], writes=[t_GT[t]])
            S.barrier()
            A = Amain
            A.release(markH)
            if stage == 5:
                dump(GT.rearrange("p a b -> p (a b)"), 0, 512)
                for i_ in range(4):
                    o_ = S.op("sp", lambda e, sig, i_=i_: sig(e.dma_start(out=y[i_ * 512:(i_ + 1) * 512, :], in_=x1d[i_ * 512:(i_ + 1) * 512, :])), extra=x1_ops, dma=1, key="dbg")
                    dump_ops.append(o_)
                dumpb(hx2[:, 0:4, :].rearrange("p a b -> p (a b)"), 0, 128, 0, SEQ)
                raise _Stop()
            p512 = Pool([(psb[b_][:, :], Tk(f"p512x_{b_}", excl=True)) for b_ in range(8)])
            ACC = A.alloc([NOT, D]); t_ACC = [[Tk(), Tk()] for _ in range(NOT)]
            for t in range(NOT):
                S.op("pool", lambda e, t=t: e.memset(ACC[:, t, :], 0.0), writes=t_ACC[t])
            e1f = A.alloc([8, 512]); t_e1f = Tk()
            e2f = A.alloc([2, D]); t_e2f = Tk()
            e1b = [A.alloc([8, 512], BF16) for _ in range(2)]; t_e1b = [Tk(), Tk()]
            e2b = [A.alloc([2, D], BF16) for _ in range(2)]; t_e2b = [Tk(), Tk()]
            sil = [A.alloc([512], BF16) for _ in range(2)]; t_sil = [Tk(), Tk()]
            hid = [A.alloc([512], BF16) for _ in range(4)]; t_hid = [Tk() for _ in range(4)]
            hc = 0
            for ex in range(NE):
                s = ex % 2
                v1 = ew1[ex].rearrange("(k p) n -> p k n", p=128); v3 = ew3[ex].rearrange("(k p) n -> p k n", p=128)
                v2 = ew2[ex].rearrange("(k p) n -> p k n", p=128)
                S.op("sp", lambda e, sig, v1=v1, v3=v3: (sig(e.dma_start(out=e1f[:, :, 0:256], in_=v1)), sig(e.dma_start(out=e1f[:, :, 256:512], in_=v3))), writes=[t_e1f], dma=2, key="e1f")
                S.op("sp", lambda e, sig, v2=v2: sig(e.dma_start(out=e2f, in_=v2)), writes=[t_e2f], dma=1, key="e2f")
                S.op("pool", lambda e, s=s: e.tensor_copy(out=e1b[s], in_=e1f), reads=[t_e1f], writes=[t_e1b[s]])
                S.op("pool", lambda e, s=s: e.tensor_copy(out=e2b[s], in_=e2f), reads=[t_e2f], writes=[t_e2b[s]])
                for tb in range(4):
                    hs = []
                    for fc in range(2):
                        p1, tp1 = p512.get(); p3, tp3 = p512.get()

                        def f1(e, p1=p1, s=s, fc=fc, tb=tb):
                            for k in range(8):
                                i = e.matmul(p1, lhsT=e1b[s][:, k, fc * 128:(fc + 1) * 128], rhs=hx2[:, k, tb * 512:(tb + 1) * 512], start=(k == 0), stop=(k == 7))
                            return i

                        def f3(e, p3=p3, s=s, fc=fc, tb=tb):
                            for k in range(8):
                                i = e.matmul(p3, lhsT=e1b[s][:, k, 256 + fc * 128:256 + (fc + 1) * 128], rhs=hx2[:, k, tb * 512:(tb + 1) * 512], start=(k == 0), stop=(k == 7))
                            return i
                        S.op("pe", f1, reads=[t_e1b[s]] + t_hx2[tb * 4:(tb + 1) * 4], writes=[tp1])
                        S.op("pe", f3, reads=[t_e1b[s]] + t_hx2[tb * 4:(tb + 1) * 4], writes=[tp3])
                        ss = hc % 2; h4 = hc % 4; hc += 1
                        S.op("act", lambda e, p1=p1, ss=ss: e.activation(out=sil[ss], in_=p1, func=AF.Silu), reads=[tp1], writes=[t_sil[ss]])
                        S.op("dve", lambda e, p3=p3, ss=ss, h4=h4: e.tensor_tensor(out=hid[h4], in0=p3, in1=sil[ss], op=ALU.mult), reads=[tp3, t_sil[ss]], writes=[t_hid[h4]])
                        hs.append(h4)
                    for tt in range(4):
                        t = tb * 4 + tt
                        for hh in range(2):
                            po, tpo = p512.get()

                            def f2(e, po=po, s=s, tt=tt, hh=hh, hs=tuple(hs)):
                                for fc in range(2):
                                    i = e.matmul(po, lhsT=hid[hs[fc]][:, tt * 128:(tt + 1) * 128], rhs=e2b[s][:, fc, hh * 512:(hh + 1) * 512], start=(fc == 0), stop=(fc == 1))
                                return i
                            S.op("pe", f2, reads=[t_hid[hs[0]], t_hid[hs[1]], t_e2b[s]], writes=[tpo])
                            S.op("dve", lambda e, po=po, t=t, hh=hh, ex=ex: e.scalar_tensor_tensor(out=ACC[:, t, hh * 512:(hh + 1) * 512], in0=po, scalar=GT[:, t, ex:ex + 1], in1=ACC[:, t, hh * 512:(hh + 1) * 512], op0=ALU.mult, op1=ALU.add),
                                 reads=[tpo, t_ACC[t][hh]], writes=[t_ACC[t][hh]])
            fngb = A.alloc([D]); t_fngb = Tk(); load(fngb, fng_bc[:, :], t_fngb, "c17")
            x1r = [A.alloc([D]) for _ in range(2)]; t_x1r = [Tk(), Tk()]
            x2 = [A.alloc([D]) for _ in range(2)]; t_x2 = [Tk(), Tk()]
            junk_E = A.alloc([D]); t_junk_E = Tk()
            ssq_E = [A.alloc([1]) for _ in range(2)]; t_ssq_E = [Tk(), Tk()]
            outs_ = []
            for t in range(NOT):
                s2 = t % 2
                S.op("sp", lambda e, sig, t=t, s2=s2: sig(e.dma_start(out=x1r[s2], in_=x1d[t * 128:(t + 1) * 128, :])), extra=[x1_ops[t]], writes=[t_x1r[s2]], dma=1, key=f"x1r{s2}")
                S.op("dve", lambda e, t=t, s2=s2: e.tensor_tensor(out=x2[s2], in0=ACC[:, t, :], in1=gffn, op=ALU.mult), reads=t_ACC[t] + [t_gffn], writes=[t_x2[s2]])
                S.op("dve", lambda e, s2=s2: e.tensor_tensor(out=x2[s2], in0=x2[s2], in1=x1r[s2], op=ALU.add), reads=[t_x2[s2], t_x1r[s2]], writes=[t_x2[s2]])
                S.op("act", lambda e, s2=s2: e.activation(out=junk_E, in_=x2[s2], func=AF.Square, accum_out=ssq_E[s2]), reads=[t_x2[s2]], writes=[t_ssq_E[s2]])
                S.op("act", lambda e, s2=s2: e.activation(out=ssq_E[s2], in_=ssq_E[s2], func=AF.Sqrt, scale=1.0 / D, bias=EPS), reads=[t_ssq_E[s2]], writes=[t_ssq_E[s2]])
                S.op("dve", lambda e, s2=s2: e.reciprocal(out=ssq_E[s2], in_=ssq_E[s2]), reads=[t_ssq_E[s2]], writes=[t_ssq_E[s2]])
                S.op("dve", lambda e, s2=s2: e.scalar_tensor_tensor(out=x2[s2], in0=x2[s2], scalar=ssq_E[s2], in1=fngb, op0=ALU.mult, op1=ALU.mult), reads=[t_x2[s2], t_ssq_E[s2], t_fngb], writes=[t_x2[s2]])
                o_ = S.op("sp", lambda e, sig, t=t, s2=s2: sig(e.dma_start(out=y[t * 128:(t + 1) * 128, :], in_=x2[s2])), reads=[t_x2[s2]], dma=1, key=f"yo{s2}")
                outs_.append(o_)
            S.op("sp", None, extra=outs_)
        try:
            author()
        except _Stop:
            pass
        if dump_ops:
            S.op("sp", None, extra=dump_ops)
        S.emit(block, sems)
    return nc


def _prep(inputs):
    f = lambda a: np.ascontiguousarray(np.asarray(a, dtype=np.float32))
    x = f(inputs["x"]); c = f(inputs["c"]); ctx = f(inputs["ctx"]); c_ctx = f(inputs["c_ctx"])
    w_in = f(inputs["w_in"])[0]
    conv_w = f(inputs["conv_w"])[0]
    a_log = f(inputs["a_log"])[0]; dt_bias = f(inputs["dt_bias"])[0]
    idx = np.arange(128)
    tri = np.stack([(idx[:, None] <= idx[None, :]), (idx[:, None] > idx[None, :]), (idx[:, None] >= idx[None, :]), (idx[:, None] < idx[None, :])]).astype(np.float32)
    negm = np.stack([(idx[:, None] <= idx[None, :]), (idx[None, :] < idx[:, None]), (idx[:, None] >= idx[None, :]), (idx[None, :] > idx[:, None])]).astype(np.float32) * -100.0
    blk = lambda n: (idx[:, None] // n == idx[None, :] // n)
    dcm = [blk(8)]
    for n in (16, 32, 64, 128):
        low = blk(n) & ((idx[:, None] % n) >= n // 2) & ((idx[None, :] % n) < n // 2)
        dcm += [low, low.T]
    import ml_dtypes
    dcm = np.stack(dcm).astype(np.float32).astype(ml_dtypes.bfloat16)
    common = {
        "dcm": dcm,
        "ada_w": f(inputs["ada_w"])[0], "ada_b": f(inputs["ada_b"]).reshape(1, -1),
        "gcols": np.ascontiguousarray(np.stack([f(inputs["norm_mix_g"])[0].reshape(8, 128).T, f(inputs["norm_ffn_g"])[0].reshape(8, 128).T], axis=-1)),
        "gng": f(inputs["gdn_norm_g"])[0].reshape(128, 1),
        "ident": np.eye(128, dtype=np.float32), "tri": tri, "negm": negm,
        "w_rest": np.ascontiguousarray(w_in[:, COL_Z:]),
        "sgu_wT": np.ascontiguousarray(f(inputs["sgu_w"])[0].transpose(0, 2, 1)),
        "sgu_bb": np.ascontiguousarray(np.broadcast_to(f(inputs["sgu_b"])[0][None], (128, 8, 128))),
        "lng_c": np.ascontiguousarray(f(inputs["sgu_ln_g"])[0].reshape(8, 128).T),
        "lnb_bc": np.ascontiguousarray(np.broadcast_to(f(inputs["sgu_ln_b"])[0][None], (128, D))),
        "w_a": f(inputs["w_branch_a"])[0], "w_b": f(inputs["w_branch_b"])[0], "w_out": f(inputs["w_out"])[0],
        "rw": np.ascontiguousarray(np.concatenate([f(inputs["router_group_w"])[0], f(inputs["router_expert_w"])[0]], axis=1)),
        "rb": np.ascontiguousarray(np.broadcast_to(np.concatenate([f(inputs["router_group_b"])[0], f(inputs["router_expert_b"])[0]])[None], (128, 36))),
        "ew1": f(inputs["expert_w1"])[0], "ew3": f(inputs["expert_w3"])[0], "ew2": f(inputs["expert_w2"])[0],
        "fng_bc": np.ascontiguousarray(np.broadcast_to(f(inputs["final_norm_g"])[None], (128, D))),
    }
    maps = []
    for core in range(8):
        b, r = core // 4, core % 4
        heads = (2 * r, 2 * r + 1)
        qcols = np.concatenate([np.arange(base + h * 128, base + (h + 1) * 128) for base in (0, 1024, 2048) for h in heads])
        bacols = np.array([COL_BETA + d * 8 + h for d in range(2) for h in heads] + [COL_A + d * 8 + h for d in range(2) for h in heads])
        conv_c = np.ascontiguousarray(conv_w[:, qcols].reshape(5, 6, 128).transpose(2, 1, 0))
        gsc = np.concatenate([np.array([a_log[d, h] for d in range(2) for h in heads]), np.array([dt_bias[d, h] for d in range(2) for h in heads])]).astype(np.float32)
        qm = np.zeros((128, 4), np.float32); qm[:, r] = 1.0
        m = dict(common)
        m.update({
            "xb": x[b], "ctxb": ctx[b], "xo": np.ascontiguousarray(x[b, r * OWN:(r + 1) * OWN]),
            "cvT": np.ascontiguousarray(np.stack([c[b].reshape(8, 128).T, c_ctx.reshape(8, 128).T], axis=-1)),
            "w_qkv": np.ascontiguousarray(w_in[:, qcols]), "w_ba": np.ascontiguousarray(w_in[:, bacols]),
            "gsc66": np.ascontiguousarray(np.broadcast_to(gsc.reshape(1, 2, 1, 4), (128, 2, NT, 4))),
            "conv_c": conv_c, "gsc": np.ascontiguousarray(np.broadcast_to(gsc[None], (128, 8))), "qmask": qm,
        })
        maps.append(m)
    return maps


_NC = None


def kernel(**inputs):
    global _NC
    if _NC is None:
        _NC = build_program()
    maps = _prep(inputs)
    res = run_bass_kernel_spmd(_NC, maps, core_ids=list(range(8)))
    out = np.zeros((2, SEQ, D), np.float32)
    for core in range(8):
        b, r = core // 4, core % 4
        out[b, r * OWN:(r + 1) * OWN] = np.asarray(res.results[core]["y"], dtype=np.float32)
    return out
```

```python
import contextlib
import os
import numpy as np
import concourse.bass as bass
import concourse.mybir as mybir
from concourse.bass_utils import run_bass_kernel_spmd

F32 = mybir.dt.float32
BF16 = mybir.dt.bfloat16
ALU = mybir.AluOpType
AF = mybir.ActivationFunctionType

D = 1024
SEQ = 8192
CTX = 256
NT = 66
OWN = 2048
NOT = 16
NE = 32
DE = 256
COL_BETA = 3072
COL_A = COL_BETA + 16
COL_Z = COL_A + 16
EPS = 1e-6
ARENA = 53000


SAME_ENGINE_WAITS = os.environ.get('SAMEENG', '1') == '1'


class Tk:
    __slots__ = ("name", "w", "rd", "excl")

    def __init__(self, name="", excl=False):
        self.name = name
        self.w = None
        self.rd = []
        self.excl = excl


class Op:
    __slots__ = ("eng", "fn", "deps", "used", "sem", "val", "dma", "key", "idx", "inc")


class Sched:
    ENGS = ("pe", "act", "dve", "pool", "sp")

    def __init__(self, nc):
        self.nc = nc
        self.ops = {e: [] for e in self.ENGS}
        self.all = []
        self.dma_since_barrier = []

    def op(self, eng, fn, reads=(), writes=(), dma=0, key=None, extra=(), inc=16):
        o = Op()
        o.eng = eng; o.fn = fn; o.used = False; o.sem = None; o.val = None
        o.dma = dma; o.key = key; o.idx = len(self.all); o.inc = inc
        deps = set(extra)
        reads = list(reads); writes = list(writes)
        for r in list(reads):
            if r.excl and r not in writes:
                writes.append(r)
        for r in reads:
            if r.w is not None:
                deps.add(r.w)
        for w in writes:
            if w.w is not None:
                deps.add(w.w)
            for x in w.rd:
                deps.add(x)
        for r in reads:
            r.rd.append(o)
        for w in writes:
            w.w = o
            w.rd = []
        o.deps = [d for d in deps if d is not o and not (d.eng == "pe" and eng == "pe" and not d.dma and not dma)
                  and not (SAME_ENGINE_WAITS is False and d.eng == eng and eng in ("act", "dve", "pool") and not d.dma and not dma)]
        for d in o.deps:
            d.used = True
        if dma:
            assert key is not None
            self.dma_since_barrier.append(o)
        self.ops[eng].append(o)
        self.all.append(o)
        return o

    def barrier(self, skip_cc=False):
        last = []
        for e in self.ENGS:
            for x in reversed(self.ops[e]):
                if x.fn is None:
                    break
                if skip_cc and x.dma and x.inc == 1:
                    continue
                last.append(x)
                break
        dmas = [x for x in self.dma_since_barrier if not (skip_cc and x.inc == 1)]
        self.dma_since_barrier = [x for x in self.dma_since_barrier if (skip_cc and x.inc == 1)]
        for e in self.ENGS:
            self.op(e, None, extra=[x for x in last if x.eng != e] + dmas)

    def emit(self, block, sems):
        nc = self.nc
        sems = list(sems)
        eng_sem = {e: sems.pop() for e in ("pe", "act", "dve", "pool")}
        keysem = {}
        cnt = {e: 0 for e in eng_sem}
        kcnt = {}
        for o in self.all:
            if o.dma:
                if o.key not in keysem:
                    keysem[o.key] = sems.pop()
                    kcnt[o.key] = 0
                kcnt[o.key] += o.inc * o.dma
                o.sem = keysem[o.key]; o.val = kcnt[o.key]
            elif o.used:
                assert o.fn is not None
                cnt[o.eng] += 1
                o.sem = eng_sem[o.eng]; o.val = cnt[o.eng]
        engobj = {"pe": nc.tensor, "act": nc.scalar, "dve": nc.vector, "pool": nc.gpsimd, "sp": nc.sync}
        deco = {"pe": block.tensor, "act": block.scalar, "dve": block.vector, "pool": block.gpsimd, "sp": block.sync}

        def run(ename):
            def body(e):
                known = {}
                for o in self.ops[ename]:
                    for d in sorted(o.deps, key=lambda x: x.idx):
                        sid = id(d.sem)
                        if known.get(sid, 0) >= d.val:
                            continue
                        e.wait_ge(d.sem, d.val)
                        known[sid] = d.val
                    if o.fn is None:
                        continue
                    if o.dma:
                        n = [0]

                        def sig(inst, o=o, n=n):
                            if o.inc == 16:
                                inst.then_inc(o.sem, 16)
                            else:
                                inst.then_inc(o.sem)
                            n[0] += 1
                            return inst
                        o.fn(e, sig)
                        assert n[0] == o.dma, (n[0], o.dma)
                    else:
                        inst = o.fn(e)
                        if o.used:
                            inst.then_inc(o.sem, 1)
            return body
        for ename in self.ENGS:
            deco[ename](run(ename))


class Arena:
    def __init__(self, ap_f32, nf32, base=0):
        self.ap = ap_f32
        self.n = base + nf32
        self.off = base
        self.hi = 0

    def mark(self):
        return self.off

    def release(self, m):
        self.off = m

    def alloc(self, free_shape, dtype=F32):
        n = int(np.prod(free_shape))
        nf = n if dtype == F32 else (n + 1) // 2
        nf = (nf + 1) // 2 * 2
        assert self.off + nf <= self.n, ("arena overflow", self.off, nf, self.n)
        v = self.ap[:, self.off:self.off + nf]
        self.off += nf
        self.hi = max(self.hi, self.off)
        if dtype != F32:
            v = v.bitcast(dtype)[:, 0:n]
        else:
            v = v[:, 0:n]
        if len(free_shape) == 2:
            v = v.rearrange("p (a b) -> p a b", a=free_shape[0])
        elif len(free_shape) == 3:
            v = v.rearrange("p (a b c) -> p a b c", a=free_shape[0], b=free_shape[1])
        return v


class _Stop(Exception):
    pass


def build_program(debug=False, stage=99):
    KCUT = int(os.environ.get('KCUT', '0')); NTL = int(os.environ.get('NTL', str(NT)))
    nc = bass.Bass("TRN2", target_bir_lowering=False)

    def din(name, shape, dt=F32):
        return nc.dram_tensor(name, list(shape), dt, kind="ExternalInput")

    xb = din("xb", [SEQ, D]); ctxb = din("ctxb", [CTX, D]); xo = din("xo", [OWN, D])
    cvT = din("cvT", [128, 8, 2]); ada_w = din("ada_w", [D, 6 * D]); ada_b = din("ada_b", [1, 6 * D])
    gcols = din("gcols", [128, 8, 2])
    w_qkv = din("w_qkv", [D, 768]); w_ba = din("w_ba", [D, 8])
    gsc66 = din("gsc66", [128, 2, NT, 4])
    conv_c = din("conv_c", [128, 6, 5]); gsc = din("gsc", [128, 8]); gng = din("gng", [128, 1])
    dcm_d = din("dcm", [9, 128, 128], BF16)
    ident_d = din("ident", [128, 128]); tri_d = din("tri", [4, 128, 128]); neg_d = din("negm", [4, 128, 128])
    w_rest = din("w_rest", [D, 5120]); sgu_wT = din("sgu_wT", [8, 128, 128]); sgu_bb = din("sgu_bb", [128, 8, 128])
    lng_c = din("lng_c", [128, 8]); lnb_bc = din("lnb_bc", [128, D])
    w_a = din("w_a", [D, D]); w_b = din("w_b", [D, D]); w_out = din("w_out", [D, D])
    rw = din("rw", [D, 36]); rb = din("rb", [128, 36])
    ew1 = din("ew1", [NE, D, DE]); ew3 = din("ew3", [NE, D, DE]); ew2 = din("ew2", [NE, DE, D])
    fng_bc = din("fng_bc", [128, D]); qmask_d = din("qmask", [128, 4])
    y = nc.dram_tensor("y", [OWN, D], F32, kind="ExternalOutput")
    snd = [nc.dram_tensor(f"snd{j}", [2 * 128, 1024], F32) for j in range(4)]
    rcv = [nc.dram_tensor(f"rcv{j}", [4 * 2 * 128, 1024], F32) for j in range(4)]
    x1d = nc.dram_tensor("x1d", [OWN, D], F32)
    oacc_d = nc.dram_tensor("oacc_d", [128, 128, 128], BF16)
    dbg = None
    if debug:
        dbg = nc.dram_tensor("dbg", [128, 4096], F32, kind="ExternalOutput")
        dbgb = nc.dram_tensor("dbgb", [768, SEQ], BF16, kind="ExternalOutput")

    es = contextlib.ExitStack()
    with es:
        arena_t = es.enter_context(nc.sbuf_tensor("arena", [128, ARENA], F32))
        psb = [es.enter_context(nc.psum_tensor(f"psb{i}", [128, 512], F32)) for i in range(8)]
        sems = [es.enter_context(nc.semaphore(f"s{i}")) for i in range(100)]
        block = es.enter_context(nc.Block())
        A = Arena(arena_t, ARENA)
        S = Sched(nc)

        dump_ops = []

        def dump(ap, col0, ncols, reads=()):
            o_ = S.op("sp", lambda e, sig: sig(e.dma_start(out=dbg[:, col0:col0 + ncols], in_=ap)), reads=list(reads), dma=1, key="dbg")
            dump_ops.append(o_)

        def cut(k, dumps):
            if stage == k:
                S.barrier()
                for d_ in dumps():
                    dump(*d_)
                raise _Stop()

        def dumpb(ap, row0, nrows, col0, ncols, reads=(), extra=()):
            o_ = S.op("sp", lambda e, sig: sig(e.dma_start(out=dbgb[row0:row0 + nrows, col0:col0 + ncols], in_=ap)), reads=list(reads), extra=list(extra), dma=1, key="dbg")
            dump_ops.append(o_)

        def author():
            nonlocal A
            class Pool:
                def __init__(self, items):
                    self.items = items
                    self.i = 0

                def get(self):
                    it = self.items[self.i % len(self.items)]
                    self.i += 1
                    return it
            p128 = Pool([(psb[b][:, 0:128], Tk(f"p128_{b}", excl=True)) for b in range(4)])
            p512 = Pool([(psb[b][:, :], Tk(f"p512_{b}", excl=True)) for b in range(4, 8)])

            def bfv(ap):
                n = ap.shape[-1]
                return ap.bitcast(BF16)[:, 0:n]

            def load(dst, src, tk, key):
                return S.op("sp", lambda e, sig: sig(e.dma_start(out=dst, in_=src)), writes=[tk], dma=1, key=key)

            identf = A.alloc([128]); t_identf = Tk(); load(identf, ident_d[:, :], t_identf, "c0")
            identb = A.alloc([128], BF16); t_identb = Tk()
            S.op("pool", lambda e: e.tensor_copy(out=identb, in_=identf), reads=[t_identf], writes=[t_identb])
            trif = A.alloc([4, 128]); t_trif = Tk(); load(trif, tri_d.ap().rearrange("a p q -> p a q"), t_trif, "c1")
            negf = A.alloc([4, 128]); t_negf = Tk(); load(negf, neg_d.ap().rearrange("a p q -> p a q"), t_negf, "c2")
            negb = A.alloc([4, 128], BF16); t_negb = Tk()
            S.op("pool", lambda e: e.tensor_copy(out=negb, in_=negf), reads=[t_negf], writes=[t_negb])
            dcm = A.alloc([9, 128], BF16); t_dcm = Tk(); load(dcm, dcm_d.ap().rearrange("a p q -> p a q"), t_dcm, "c2b")
            onesf = A.alloc([128]); t_onesf = Tk()
            S.op("pool", lambda e: e.memset(onesf, 1.0), writes=[t_onesf])
            Uf, Vf, Ub, Vb = (trif[:, i, :] for i in range(4))
            gscs = A.alloc([8]); t_gscs = Tk(); load(gscs, gsc[:, :], t_gscs, "c3")
            negA = A.alloc([4]); t_negA = Tk()
            S.op("act", lambda e: e.activation(out=negA, in_=gscs[:, 0:4], func=AF.Exp), reads=[t_gscs], writes=[t_negA])
            S.op("dve", lambda e: e.tensor_scalar(out=negA, in0=negA, scalar1=-1.0, scalar2=None, op0=ALU.mult), reads=[t_negA], writes=[t_negA])
            dtb = gscs[:, 4:8]
            convc = A.alloc([6, 5]); t_convc = Tk(); load(convc, conv_c[:, :, :], t_convc, "c4")
            gngs = A.alloc([1]); t_gngs = Tk(); load(gngs, gng[:, :], t_gngs, "c5")
            qmask = A.alloc([4]); t_qmask = Tk(); load(qmask, qmask_d[:, :], t_qmask, "c6")
            gcol = A.alloc([8, 2]); t_gcol = Tk(); load(gcol, gcols[:, :, :], t_gcol, "c7")
            modc = A.alloc([6, 8]); t_modc = Tk("modc")
            gmix = A.alloc([D]); t_gmix = Tk("gmix")
            gffn = A.alloc([D]); t_gffn = Tk("gffn")
            GT = A.alloc([NOT, 32]); t_GT = [Tk() for _ in range(NOT)]

            m0 = A.mark()
            scT = A.alloc([8, 2]); t_scT = Tk(); load(scT, cvT[:, :, :], t_scT, "c8")
            S.op("act", lambda e: e.activation(out=scT, in_=scT, func=AF.Silu), reads=[t_scT], writes=[t_scT])
            modrow = A.alloc([6 * D]); t_modrow = Tk("modrow")
            modrow_c = A.alloc([6 * D]); t_modrow_c = Tk("modrow_c")
            adb = A.alloc([6 * D]); t_adb = Tk()
            S.op("sp", lambda e, sig: (sig(e.dma_start(out=adb[0:1, :], in_=ada_b[:, :])), sig(e.dma_start(out=adb[1:2, :], in_=ada_b[:, :]))),
                 writes=[t_adb], dma=2, key="c9")
            adw = [A.alloc([8, 512]) for _ in range(2)]; t_adw = [Tk(), Tk()]
            ada_v = ada_w.ap().rearrange("(k p) n -> p k n", p=128)
            for nb in range(12):
                sl = nb % 2
                S.op("sp", lambda e, sig, nb=nb, sl=sl: (sig(e.dma_start(out=adw[sl][:, 0:4, :], in_=ada_v[:, 0:4, nb * 512:(nb + 1) * 512])),
                                                        sig(e.dma_start(out=adw[sl][:, 4:8, :], in_=ada_v[:, 4:8, nb * 512:(nb + 1) * 512]))),
                     writes=[t_adw[sl]], dma=2, key=f"adw{sl}")
                pp, tp = p512.get()

                def f(e, sl=sl, pp=pp):
                    for k in range(8):
                        i = e.matmul(pp[0:2, :], lhsT=scT[:, k, :], rhs=adw[sl][:, k, :], start=(k == 0), stop=(k == 7))
                    return i
                S.op("pe", f, reads=[t_scT, t_adw[sl]], writes=[tp])
                S.op("dve", lambda e, nb=nb, pp=pp: e.tensor_tensor(out=modrow[0:2, nb * 512:(nb + 1) * 512], in0=pp[0:2, :], in1=adb[0:2, nb * 512:(nb + 1) * 512], op=ALU.add),
                     reads=[tp, t_adb], writes=[t_modrow])
            S.op("sp", lambda e, sig: sig(e.dma_start(out=modrow_c[0:1, :], in_=modrow[1:2, :])), reads=[t_modrow], writes=[t_modrow_c], dma=1, key="c10")
            pp, tp = p128.get()
            vecs = [(modrow, 0), (modrow, 1), (modrow_c, 0), (modrow_c, 1), (modrow, 3), (modrow, 4)]

            def f(e, pp=pp):
                for vi, (row, m) in enumerate(vecs):
                    for k in range(8):
                        i = e.matmul(pp[:, vi * 8 + k:vi * 8 + k + 1], lhsT=row[0:1, m * D + k * 128:m * D + (k + 1) * 128], rhs=onesf[0:1, 0:1], start=True, stop=True)
                return i
            S.op("pe", f, reads=[t_modrow, t_modrow_c, t_onesf], writes=[tp])
            S.op("dve", lambda e, pp=pp: e.tensor_copy(out=modc.rearrange("p a b -> p (a b)"), in_=pp[:, 0:48]), reads=[tp], writes=[t_modc])
            for vi, gi in ((1, 0), (3, 0), (5, 1)):
                S.op("dve", lambda e, vi=vi, gi=gi: e.scalar_tensor_tensor(out=modc[:, vi, :], in0=modc[:, vi, :], scalar=1.0, in1=gcol[:, :, gi], op0=ALU.add, op1=ALU.mult),
                     reads=[t_modc, t_gcol], writes=[t_modc])
            for dst, tdst, m in ((gmix, t_gmix, 2), (gffn, t_gffn, 5)):
                for h in range(2):
                    pp, tp = p512.get()
                    S.op("pe", lambda e, pp=pp, m=m, h=h: e.matmul(pp, lhsT=onesf[0:1, :], rhs=modrow[0:1, m * D + h * 512:m * D + (h + 1) * 512], start=True, stop=True),
                         reads=[t_modrow, t_onesf], writes=[tp])
                    S.op("act", lambda e, pp=pp, dst=dst, h=h: e.copy(out=dst[:, h * 512:(h + 1) * 512], in_=pp), reads=[tp], writes=[tdst])
            S.barrier()
            A.release(m0)
            if stage == 0:
                dump(modc.rearrange("p a b -> p (a b)"), 0, 48); dump(gmix[:, 0:256], 64, 256); dump(gffn[:, 0:256], 320, 256)
                raise _Stop()

            mA = A.mark()
            SC = A.alloc([NT, 28]); t_SC = [Tk("SC")] * NT
            QN = A.alloc([NT, 2, 128], BF16); KN = A.alloc([NT, 2, 128], BF16); VV = A.alloc([NT, 2, 128], BF16)
            t_QKV = [[Tk() for _ in range(6)] for t in range(NT)]
            mA1 = A.mark()
            wst = A.alloc([8, 776]); t_wst = Tk()
            wq = A.alloc([8, 896], BF16); t_wq = Tk()
            S.op("sp", lambda e, sig: (sig(e.dma_start(out=wst[:, :, 0:768], in_=w_qkv.ap().rearrange("(k p) n -> p k n", p=128))),
                                       sig(e.dma_start(out=wst[:, :, 768:776], in_=w_ba.ap().rearrange("(k p) n -> p k n", p=128)))),
                 writes=[t_wst], dma=2, key="wst")
            S.op("pool", lambda e: e.tensor_copy(out=wq[:, :, 0:776], in_=wst), reads=[t_wst], writes=[t_wq])
            xt_A = [A.alloc([D]) for _ in range(3)]; t_xt_A = [Tk() for _ in range(3)]
            junk_A = A.alloc([D]); t_junk_A = Tk()
            ssq_A = [A.alloc([1]) for _ in range(3)]; t_ssq_A = [Tk() for _ in range(3)]
            xs_A = [A.alloc([8, 128], BF16) for _ in range(2)]; t_xs_A = [Tk(), Tk()]
            hxT = [A.alloc([8, 128], BF16) for _ in range(2)]; t_hxT = [Tk(), Tk()]; t_hxTb = [Tk(), Tk()]
            PRE = [A.alloc([6, 132]) for _ in range(3)]; t_PRE = [Tk() for _ in range(3)]
            CV = A.alloc([6, 128]); t_CVc = [Tk() for _ in range(6)]
            SQ = A.alloc([6, 128], BF16); t_SQ = Tk()
            sm = [A.alloc([40]) for _ in range(2)]; t_sm = [Tk(), Tk()]
            nr = [A.alloc([8]) for _ in range(2)]; t_nr = [Tk(), Tk()]

            wst_flat = wst.rearrange("p a b -> p (a b)")
            BA = wst_flat[:, 0:NT * 8].rearrange("p (a b) -> p a b", b=8); t_BA = Tk("BA")
            g66 = A.alloc([2, NT, 4]); t_g66 = Tk(); load(g66, gsc66[:, :, :, :], t_g66, "c3b")
            Mx = [wst_flat[:, 1024 + i_ * 512:1024 + i_ * 512 + NT * 4].rearrange("p (a b) -> p a b", b=4) for i_ in range(4)]; t_Mx = [Tk() for _ in range(4)]

            def rows_of(t):
                if t < 2:
                    return ctxb[t * 128:(t + 1) * 128, :]
                return xb[(t - 2) * 128:(t - 1) * 128, :]

            def front(t, part):
                s3 = t % 3; s2 = t % 2
                if part == "b":
                    return front_b(t, s3, s2)
                isctx = t < 2
                shc = modc[:, 2 if isctx else 0, :]; scc = modc[:, 3 if isctx else 1, :]
                S.op("sp", lambda e, sig: sig(e.dma_start(out=xt_A[s3], in_=rows_of(t))), writes=[t_xt_A[s3]], dma=1, key=f"xt_A{s3}")
                S.op("act", lambda e: e.activation(out=junk_A, in_=xt_A[s3], func=AF.Square, accum_out=ssq_A[s3]), reads=[t_xt_A[s3]], writes=[t_ssq_A[s3]])
                S.op("act", lambda e: e.activation(out=ssq_A[s3], in_=ssq_A[s3], func=AF.Sqrt, scale=1.0 / D, bias=EPS), reads=[t_ssq_A[s3]], writes=[t_ssq_A[s3]])
                S.op("dve", lambda e: e.reciprocal(out=ssq_A[s3], in_=ssq_A[s3]), reads=[t_ssq_A[s3]], writes=[t_ssq_A[s3]])
                S.op("pool", lambda e: e.tensor_scalar(out=xs_A[s2].rearrange("p a b -> p (a b)"), in0=xt_A[s3], scalar1=ssq_A[s3], scalar2=1.0, op0=ALU.mult, op1=ALU.mult),
                     reads=[t_xt_A[s3], t_ssq_A[s3]], writes=[t_xs_A[s2]])
                pp, tp = p512.get()
                ppb = pp.bitcast(BF16)

                def tr(e):
                    for k in range(8):
                        i = e.transpose(out=ppb[:, k * 128:(k + 1) * 128], in_=xs_A[s2][:, k, :], identity=identb)
                    return i
                S.op("pe", tr, reads=[t_xs_A[s2], t_identb], writes=[tp])

                def ev_d(e):
                    for k in range(8):
                        i = e.tensor_scalar(out=hxT[s2][:, k, :], in0=ppb[:, k * 128:(k + 1) * 128], scalar1=scc[:, k:k + 1], scalar2=shc[:, k:k + 1], op0=ALU.mult, op1=ALU.add)
                    return i
                t_h2 = t_hxTb[s2]
                S.op("dve", ev_d, reads=[tp, t_modc], writes=[t_hxT[s2], t_h2])
                return

            def front_b(t, s3, s2):
                pA, tA = p512.get()
                pB, tB = p512.get()

                def pjA(e):
                    for ch in range(4):
                        for k in range(8):
                            i = e.matmul(pA[:, ch * 128:(ch + 1) * 128], lhsT=wq[:, k, ch * 128:(ch + 1) * 128], rhs=hxT[s2][:, k, :], start=(k == 0), stop=(k == 7))
                    return i

                def pjB(e):
                    for ch in range(4, 6):
                        for k in range(8):
                            i = e.matmul(pB[:, (ch - 4) * 128:(ch - 3) * 128], lhsT=wq[:, k, ch * 128:(ch + 1) * 128], rhs=hxT[s2][:, k, :], start=(k == 0), stop=(k == 7))
                    for k in range(8):
                        i = e.matmul(pB[:, 256:264], lhsT=hxT[s2][:, k, :], rhs=wq[:, k, 768:776], start=(k == 0), stop=(k == 7))
                    return i
                S.op("pe", pjA, reads=[t_wq, t_hxT[s2]], writes=[tA])
                S.op("pe", pjB, reads=[t_wq, t_hxT[s2]], writes=[tB])
                pba = pB[:, 256:264]; tba = tB
                S.op("dve", lambda e: e.tensor_copy(out=PRE[s3][:, 0:4, 2:130], in_=pA.rearrange("p (a b) -> p a b", a=4)), reads=[tA], writes=[t_PRE[s3]])
                S.op("dve", lambda e: e.tensor_copy(out=PRE[s3][:, 4:6, 2:130], in_=pB[:, 0:256].rearrange("p (a b) -> p a b", a=2)), reads=[tB], writes=[t_PRE[s3]])
                if KCUT == 2:
                    return
                first = t in (0, 2); last = t in (1, NT - 1)
                if first:
                    S.op("pool", lambda e: e.memset(PRE[s3][:, :, 0:2], 0.0), writes=[t_PRE[s3]])
                else:
                    sp_ = (t - 1) % 3
                    S.op("pool", lambda e: e.tensor_copy(out=PRE[sp_][:, :, 130:132], in_=PRE[s3][:, :, 2:4]), reads=[t_PRE[s3]], writes=[t_PRE[sp_]])
                if last:
                    S.op("pool", lambda e: e.memset(PRE[s3][:, :, 130:132], 0.0), writes=[t_PRE[s3]])
                else:
                    sn = (t + 1) % 3
                    S.op("pool", lambda e: e.tensor_copy(out=PRE[sn][:, :, 0:2], in_=PRE[s3][:, :, 128:130]), reads=[t_PRE[s3]], writes=[t_PRE[sn]])
                S.op("dve", lambda e: e.tensor_copy(out=BA[:, t, :], in_=pba), reads=[tba], writes=[t_BA])

            def lag(t):
                s3 = t % 3; s2 = t % 2
                for j in range(5):
                    for ch in range(6):
                        tcv = t_CVc[ch]

                        def cvj(e, ch=ch, j=j):
                            if j == 0:
                                return e.tensor_scalar(out=CV[:, ch, :], in0=PRE[s3][:, ch, 0:128], scalar1=convc[:, ch, 0:1], scalar2=None, op0=ALU.mult)
                            return e.scalar_tensor_tensor(out=CV[:, ch, :], in0=PRE[s3][:, ch, j:j + 128], scalar=convc[:, ch, j:j + 1], in1=CV[:, ch, :], op0=ALU.mult, op1=ALU.add)
                        S.op("dve", cvj, reads=[t_PRE[s3], t_convc] + ([tcv] if j else []), writes=[tcv])
                S.op("act", lambda e: e.activation(out=SQ, in_=CV, func=AF.Silu), reads=t_CVc, writes=[t_SQ])
                pT, tT = p512.get()
                pTb = pT.bitcast(BF16)

                def trs(e):
                    for ch in range(6):
                        i = e.transpose(out=pTb[:, ch * 128:(ch + 1) * 128], in_=SQ[:, ch, :], identity=identb)
                    return i
                S.op("pe", trs, reads=[t_SQ, t_identb], writes=[tT])
                pts = [(pTb[:, ch * 128:(ch + 1) * 128], tT) for ch in range(6)]
                n = nr[s2]; tn = t_nr[s2]
                for ch in range(4):
                    S.op("act", lambda e, ch=ch: e.activation(out=junk_A[:, 0:128], in_=pts[ch][0], func=AF.Square, accum_out=n[:, ch:ch + 1]),
                         reads=[pts[ch][1]], writes=[tn])
                S.op("act", lambda e: e.activation(out=n[:, 0:4], in_=n[:, 0:4], func=AF.Sqrt, bias=EPS), reads=[tn], writes=[tn])
                S.op("dve", lambda e: e.reciprocal(out=n[:, 0:4], in_=n[:, 0:4]), reads=[tn], writes=[tn])
                S.op("dve", lambda e: e.tensor_scalar(out=n[:, 0:2], in0=n[:, 0:2], scalar1=128.0 ** -0.5, scalar2=None, op0=ALU.mult), reads=[tn], writes=[tn])
                dsts = [QN[:, t, 0, :], QN[:, t, 1, :], KN[:, t, 0, :], KN[:, t, 1, :], VV[:, t, 0, :], VV[:, t, 1, :]]
                for ch in range(6):
                    if ch < 4:
                        S.op("dve", lambda e, ch=ch: e.tensor_scalar(out=dsts[ch], in0=pts[ch][0], scalar1=n[:, ch:ch + 1], scalar2=None, op0=ALU.mult),
                             reads=[pts[ch][1], tn], writes=[t_QKV[t][ch]])
                    else:
                        S.op("act", lambda e, ch=ch: e.copy(out=dsts[ch], in_=pts[ch][0]), reads=[pts[ch][1]], writes=[t_QKV[t][ch]])

            front(0, "a")
            for i in range(NTL + 1):
                if i + 1 < NTL:
                    front(i + 1, "a")
                if i < NTL:
                    front(i, "b")
                if i >= 1 and KCUT in (0, 5):
                    lag(i - 1)
            tS = t_SC[0]
            SCk = lambda k: SC[:, :, k * 4:(k + 1) * 4]
            M0, M1, M2, M3 = Mx; tM0, tM1, tM2, tM3 = t_Mx
            S.op("act", lambda e: e.activation(out=g66[:, 0, :, :], in_=g66[:, 0, :, :], func=AF.Exp), reads=[t_g66], writes=[t_g66])
            S.op("act", lambda e: e.activation(out=SCk(3), in_=BA[:, :, 0:4], func=AF.Sigmoid), reads=[t_BA], writes=[tS])
            S.op("dve", lambda e: e.tensor_tensor(out=M0, in0=BA[:, :, 4:8], in1=g66[:, 1, :, :], op=ALU.add), reads=[t_BA, t_g66], writes=[tM0])
            S.op("dve", lambda e: e.tensor_scalar(out=M1, in0=M0, scalar1=-1.0, scalar2=None, op0=ALU.mult), reads=[tM0], writes=[tM1])
            S.op("dve", lambda e: e.tensor_tensor(out=M1, in0=M1, in1=M0, op=ALU.min), reads=[tM0, tM1], writes=[tM1])
            S.op("act", lambda e: e.activation(out=M1, in_=M1, func=AF.Exp), reads=[tM1], writes=[tM1])
            S.op("act", lambda e: e.activation(out=M1, in_=M1, func=AF.Ln, bias=1.0), reads=[tM1], writes=[tM1])
            S.op("dve", lambda e: e.tensor_scalar(out=M2, in0=M0, scalar1=0.0, scalar2=None, op0=ALU.max), reads=[tM0], writes=[tM2])
            S.op("dve", lambda e: e.tensor_tensor(out=M2, in0=M2, in1=M1, op=ALU.add), reads=[tM1, tM2], writes=[tM2])
            S.op("dve", lambda e: e.scalar_tensor_tensor(out=SCk(6), in0=M2, scalar=-1.0, in1=g66[:, 0, :, :], op0=ALU.mult, op1=ALU.mult), reads=[tM2, t_g66], writes=[tS])
            pcA, tcA = p512.get(); pcB, tcB = p512.get()

            def cums(e):
                e.matmul(pcA[:, 0:2 * NT], lhsT=Uf, rhs=SC[:, :, 24:26], start=True, stop=True)
                return e.matmul(pcA[:, 2 * NT:4 * NT], lhsT=Ub, rhs=SC[:, :, 26:28], start=True, stop=True)
            S.op("pe", cums, reads=[tS, t_trif], writes=[tcA])
            S.op("pe", lambda e: e.matmul(pcB[:, 0:4 * NT], lhsT=onesf, rhs=SC[:, :, 24:28], start=True, stop=True), reads=[tS, t_onesf], writes=[tcB])
            Gf_ps = pcA[:, 0:2 * NT].rearrange("p (a b) -> p a b", b=2); Gb_ps = pcA[:, 2 * NT:4 * NT].rearrange("p (a b) -> p a b", b=2)
            Gt_ps = pcB[:, 0:4 * NT].rearrange("p (a b) -> p a b", b=4)
            S.op("act", lambda e: e.activation(out=SC[:, :, 16:18], in_=Gf_ps, func=AF.Exp), reads=[tcA], writes=[tS])
            S.op("act", lambda e: e.activation(out=SC[:, :, 18:20], in_=Gb_ps, func=AF.Exp), reads=[tcA], writes=[tS])
            S.op("act", lambda e: e.activation(out=SCk(5), in_=Gt_ps, func=AF.Exp), reads=[tcB], writes=[tS])
            S.op("dve", lambda e: e.tensor_copy(out=M3[:, :, 0:2], in_=Gf_ps), reads=[tcA], writes=[tM3])
            S.op("dve", lambda e: e.tensor_copy(out=M3[:, :, 2:4], in_=Gb_ps), reads=[tcA], writes=[tM3])
            S.op("dve", lambda e: e.tensor_tensor(out=M3, in0=Gt_ps, in1=M3, op=ALU.subtract), reads=[tcB, tM3], writes=[tM3])
            S.op("act", lambda e: e.activation(out=SCk(2), in_=M3, func=AF.Exp), reads=[tM3], writes=[tS])
            S.op("dve", lambda e: e.tensor_scalar(out=SCk(0), in0=SCk(3), scalar1=-1.0, scalar2=None, op0=ALU.mult), reads=[tS], writes=[tS])
            S.op("dve", lambda e: e.tensor_tensor(out=SCk(1), in0=SCk(3), in1=SCk(4), op=ALU.mult), reads=[tS], writes=[tS])
            S.barrier()
            A.release(mA1)
            if stage == 1:
                for i_, t_ in enumerate((0, 1, 2, 3, 33, 65)):
                    dump(SC[:, t_, :], i_ * 32, 28)
                    dumpb(QN[:, t_, :, :].rearrange("p a b -> p (a b)"), 0, 128, i_ * 768, 256)
                    dumpb(KN[:, t_, :, :].rearrange("p a b -> p (a b)"), 0, 128, i_ * 768 + 256, 256)
                    dumpb(VV[:, t_, :, :].rearrange("p a b -> p (a b)"), 0, 128, i_ * 768 + 512, 256)
                raise _Stop()

            p128_small = p128
            p128 = Pool([(psb[b_][:, 0:128], Tk(f"p128x_{b_}", excl=True)) for b_ in range(8)])
            chains = [(hl, d) for hl in range(2) for d in range(2)]
            cb = {}
            for c in chains:
                Sf = A.alloc([128]); tSf = Tk("S"); Sbb = A.alloc([128], BF16); tSbb = Tk("Sb")
                S.op("pool", lambda e, Sf=Sf: e.memset(Sf, 0.0), writes=[tSf])
                S.op("pool", lambda e, Sbb=Sbb: e.memset(Sbb, 0.0), writes=[tSbb])
                for st_ in range(2):
                    b = {}
                    for nm in ("knT", "qnT", "qdT", "X0", "XT0", "Xb", "XbT", "X0s", "XT0s", "X1s", "XT1s", "P0", "P1", "PT0", "PT1", "No", "NoT", "Wb", "Vb", "kbg", "kd", "vb", "nwT", "vnew", "QKm", "qd", "E", "ET", "ob"):
                        b[nm] = A.alloc([128], BF16); b["t_" + nm] = Tk(nm)
                    for nm in ("gV", "gU"):
                        b[nm] = A.alloc([128]); b["t_" + nm] = Tk(nm)
                    b["S"] = Sf; b["t_S"] = tSf; b["Sb"] = Sbb; b["t_Sb"] = tSbb
                    cb[(c, st_)] = b
            t_OACC = [[Tk() for _ in range(2)] for _ in range(64)]
            ost = [A.alloc([128], BF16) for _ in range(4)]; t_ost = [Tk() for _ in range(4)]
            ost_cnt = [0]
            ofin = [A.alloc([128]) for _ in range(2)]; t_ofin = [Tk(), Tk()]
            onb = [A.alloc([128], BF16) for _ in range(2)]; t_onb = [Tk(), Tk()]
            onT = [A.alloc([128], BF16) for _ in range(4)]; t_onT = [Tk() for _ in range(4)]
            fsm = [A.alloc([4]) for _ in range(2)]; t_fsm = [Tk(), Tk()]
            junk_B = A.alloc([128])
            visited = set()
            snd_ops = []
            qops = [[] for _ in range(4)]
            ccs = []
            fin_cnt = [0]
            evq = [0]

            def evac_copy(dst, src, rd, wr, scale=None):
                evq[0] += 1
                if evq[0] % 2 == 0:
                    if scale is None:
                        return S.op("act", lambda e: e.copy(out=dst, in_=src), reads=rd, writes=wr)
                    return S.op("act", lambda e: e.activation(out=dst, in_=src, func=AF.Copy, scale=scale), reads=rd, writes=wr)
                if scale is None:
                    return S.op("dve", lambda e: e.tensor_copy(out=dst, in_=src), reads=rd, writes=wr)
                return S.op("dve", lambda e: e.tensor_scalar(out=dst, in0=src, scalar1=scale, scalar2=None, op0=ALU.mult), reads=rd, writes=wr)

            def chain_step(c, t, st_):
                hl, d = c
                b = cb[(c, st_)]
                col = d * 2 + hl
                latent = t >= 2
                kn = KN[:, t, hl, :]; qn = QN[:, t, hl, :]; vv = VV[:, t, hl, :]
                tqq = t_QKV[t][hl]; tqk = t_QKV[t][2 + hl]; tqv = t_QKV[t][4 + hl]; tsc = t_SC[t]

                def scol(kind):
                    return SC[:, t, kind * 4 + col:kind * 4 + col + 1]
                U_, V_ = (Uf, Vf) if d == 0 else (Ub, Vb)
                negs = negb[:, 0 if d == 0 else 2, :]; negi = negb[:, 1 if d == 0 else 3, :]
                pk, tpk = p128.get()
                S.op("pe", lambda e: e.transpose(out=bfv(pk), in_=kn, identity=identb), reads=[tqk, t_identb], writes=[tpk])
                evac_copy(b["knT"], bfv(pk), [tpk], [b["t_knT"]])
                S.op("act", lambda e: e.activation(out=b["gV"], in_=V_, func=AF.Copy, scale=scol(6)), reads=[t_trif, tsc], writes=[b["t_gV"]])
                S.op("act", lambda e: e.activation(out=b["gU"], in_=U_, func=AF.Copy, scale=scol(6)), reads=[t_trif, tsc], writes=[b["t_gU"]])
                S.op("dve", lambda e: e.tensor_scalar(out=b["kbg"], in0=kn, scalar1=scol(1), scalar2=None, op0=ALU.mult), reads=[tqk, tsc], writes=[b["t_kbg"]])
                S.op("pool", lambda e: e.tensor_scalar(out=b["kd"], in0=kn, scalar1=scol(2), scalar2=1.0, op0=ALU.mult, op1=ALU.mult), reads=[tqk, tsc], writes=[b["t_kd"]])
                S.op("pool", lambda e: e.tensor_scalar(out=b["vb"], in0=vv, scalar1=scol(3), scalar2=1.0, op0=ALU.mult, op1=ALU.mult), reads=[tqv, tsc], writes=[b["t_vb"]])
                yield
                pd, tpd = p128.get()

                def dm(e):
                    e.matmul(pd, lhsT=identb, rhs=negs, start=True, stop=False)
                    return e.matmul(pd, lhsT=U_, rhs=b["gV"], start=False, stop=True)
                S.op("pe", dm, reads=[t_identb, t_negb, t_trif, b["t_gV"]], writes=[tpd])
                S.op("act", lambda e: e.activation(out=b["E"], in_=pd, func=AF.Exp), reads=[tpd], writes=[b["t_E"]])
                pkk, tpkk = p128.get()
                S.op("pe", lambda e: e.matmul(pkk, lhsT=b["knT"], rhs=b["knT"], start=True, stop=True), reads=[b["t_knT"]], writes=[tpkk])
                S.op("dve", lambda e: e.scalar_tensor_tensor(out=b["X0"], in0=pkk, scalar=scol(0), in1=b["E"], op0=ALU.mult, op1=ALU.mult),
                     reads=[tpkk, tsc, b["t_E"]], writes=[b["t_X0"]])
                if latent:
                    pdt, tpdt = p128.get()

                    def dmt(e):
                        e.matmul(pdt, lhsT=identb, rhs=negi, start=True, stop=False)
                        return e.matmul(pdt, lhsT=V_, rhs=b["gU"], start=False, stop=True)
                    S.op("pe", dmt, reads=[t_identb, t_negb, t_trif, b["t_gU"]], writes=[tpdt])
                    S.op("act", lambda e: e.activation(out=b["ET"], in_=pdt, func=AF.Exp), reads=[tpdt], writes=[b["t_ET"]])
                    pq_, tpq = p128.get()
                    S.op("pe", lambda e: e.transpose(out=bfv(pq_), in_=qn, identity=identb), reads=[tqq, t_identb], writes=[tpq])
                    evac_copy(b["qnT"], bfv(pq_), [tpq], [b["t_qnT"]])
                    S.op("pool", lambda e: e.tensor_scalar(out=b["qd"], in0=qn, scalar1=scol(4), scalar2=1.0, op0=ALU.mult, op1=ALU.mult), reads=[tqq, tsc], writes=[b["t_qd"]])
                yield
                px, tpx = p128.get()
                S.op("pe", lambda e: e.transpose(out=bfv(px), in_=b["X0"], identity=identb), reads=[b["t_X0"], t_identb], writes=[tpx])
                evac_copy(b["XT0"], bfv(px), [tpx], [b["t_XT0"]])
                if latent:
                    pqd, tpqd = p128.get()
                    S.op("pe", lambda e: e.transpose(out=bfv(pqd), in_=b["qd"], identity=identb), reads=[b["t_qd"], t_identb], writes=[tpqd])
                    evac_copy(b["qdT"], bfv(pqd), [tpqd], [b["t_qdT"]])
                    pqk, tpqk = p128.get()
                    S.op("pe", lambda e: e.matmul(pqk, lhsT=b["knT"], rhs=b["qnT"], start=True, stop=True), reads=[b["t_knT"], b["t_qnT"]], writes=[tpqk])
                    S.op("dve", lambda e: e.tensor_tensor(out=b["QKm"], in0=pqk, in1=b["ET"], op=ALU.mult), reads=[tpqk, b["t_ET"]], writes=[b["t_QKm"]])
                yield
                mk = lambda nm: (b[nm], b["t_" + nm])
                Xb, tXb = mk("Xb"); XbT, tXbT = mk("XbT")
                S.op("pool", lambda e: e.tensor_tensor(out=Xb, in0=b["X0"], in1=dcm[:, 0, :], op=ALU.mult), reads=[b["t_X0"], t_dcm], writes=[tXb])
                S.op("pool", lambda e: e.tensor_tensor(out=XbT, in0=b["XT0"], in1=dcm[:, 0, :], op=ALU.mult), reads=[b["t_XT0"], t_dcm], writes=[tXbT])
                S.op("pool", lambda e: e.tensor_tensor(out=b["P0"], in0=Xb, in1=identb, op=ALU.add), reads=[tXb, t_identb], writes=[b["t_P0"]])
                S.op("pool", lambda e: e.tensor_tensor(out=b["PT0"], in0=XbT, in1=identb, op=ALU.add), reads=[tXbT, t_identb], writes=[b["t_PT0"]])
                yield
                cur = 0
                cX, tcX, cXT, tcXT = Xb, tXb, XbT, tXbT
                for lev in range(2):
                    nX, tnX = mk(f"X{lev}s"); nXT, tnXT = mk(f"XT{lev}s")
                    p1, tp1 = p128.get()
                    S.op("pe", lambda e, p1=p1, cX=cX, cXT=cXT: e.matmul(p1, lhsT=cXT, rhs=cX, start=True, stop=True), reads=[tcX, tcXT], writes=[tp1])
                    evac_copy(nX, p1, [tp1], [tnX])
                    p2, tp2 = p128.get()
                    S.op("pe", lambda e, p2=p2, cX=cX, cXT=cXT: e.matmul(p2, lhsT=cX, rhs=cXT, start=True, stop=True), reads=[tcX, tcXT], writes=[tp2])
                    evac_copy(nXT, p2, [tp2], [tnXT])
                    yield
                    P = b[f"P{cur}"]; tP = b[f"t_P{cur}"]; nP = b[f"P{1 - cur}"]; tnP = b[f"t_P{1 - cur}"]
                    PT = b[f"PT{cur}"]; tPT = b[f"t_PT{cur}"]; nPT = b[f"PT{1 - cur}"]; tnPT = b[f"t_PT{1 - cur}"]
                    p3, tp3 = p128.get()
                    S.op("pe", lambda e, p3=p3, nXT=nXT, P=P: e.matmul(p3, lhsT=nXT, rhs=P, start=True, stop=True), reads=[tnXT, tP], writes=[tp3])
                    S.op("dve", lambda e, p3=p3, P=P, nP=nP: e.tensor_tensor(out=nP, in0=p3, in1=P, op=ALU.add), reads=[tp3, tP], writes=[tnP])
                    p4, tp4 = p128.get()
                    S.op("pe", lambda e, p4=p4, nX=nX, PT=PT: e.matmul(p4, lhsT=nX, rhs=PT, start=True, stop=True), reads=[tnX, tPT], writes=[tp4])
                    S.op("dve", lambda e, p4=p4, PT=PT, nPT=nPT: e.tensor_tensor(out=nPT, in0=p4, in1=PT, op=ALU.add), reads=[tp4, tPT], writes=[tnPT])
                    cur = 1 - cur
                    cX, tcX, cXT, tcXT = nX, tnX, nXT, tnXT
                    yield
                for li in range(4):
                    mi = 1 + 2 * li + (0 if d == 0 else 1)
                    miT = 1 + 2 * li + (1 if d == 0 else 0)
                    No, tNo = mk("No"); NoT, tNoT = mk("NoT")
                    P = b[f"P{cur}"]; tP = b[f"t_P{cur}"]; nP = b[f"P{1 - cur}"]; tnP = b[f"t_P{1 - cur}"]
                    PT = b[f"PT{cur}"]; tPT = b[f"t_PT{cur}"]; nPT = b[f"PT{1 - cur}"]; tnPT = b[f"t_PT{1 - cur}"]
                    S.op("pool", lambda e, mi=mi, No=No: e.tensor_tensor(out=No, in0=b["X0"], in1=dcm[:, mi, :], op=ALU.mult), reads=[b["t_X0"], t_dcm], writes=[tNo])
                    pw_, tpw_ = p128.get()
                    S.op("pe", lambda e, pw_=pw_, No=No, PT=PT: e.matmul(pw_, lhsT=No, rhs=PT, start=True, stop=True), reads=[tNo, tPT], writes=[tpw_])
                    Wb, tWb = mk("Wb")
                    evac_copy(Wb, pw_, [tpw_], [tWb])
                    if li < 3:
                        S.op("pool", lambda e, miT=miT, NoT=NoT: e.tensor_tensor(out=NoT, in0=b["XT0"], in1=dcm[:, miT, :], op=ALU.mult), reads=[b["t_XT0"], t_dcm], writes=[tNoT])
                        pv_, tpv_ = p128.get()
                        S.op("pe", lambda e, pv_=pv_, NoT=NoT, P=P: e.matmul(pv_, lhsT=NoT, rhs=P, start=True, stop=True), reads=[tNoT, tP], writes=[tpv_])
                        Vb_, tVb_ = mk("Vb")
                        evac_copy(Vb_, pv_, [tpv_], [tVb_])
                    yield
                    p5, tp5 = p128.get()
                    S.op("pe", lambda e, p5=p5, P=P, Wb=Wb: e.matmul(p5, lhsT=P, rhs=Wb, start=True, stop=True), reads=[tP, tWb], writes=[tp5])
                    S.op("dve", lambda e, p5=p5, PT=PT, nPT=nPT: e.tensor_tensor(out=nPT, in0=p5, in1=PT, op=ALU.add), reads=[tp5, tPT], writes=[tnPT])
                    if li < 3:
                        p6, tp6 = p128.get()
                        S.op("pe", lambda e, p6=p6, PT=PT, Vb_=Vb_: e.matmul(p6, lhsT=PT, rhs=Vb_, start=True, stop=True), reads=[tPT, tVb_], writes=[tp6])
                        S.op("dve", lambda e, p6=p6, P=P, nP=nP: e.tensor_tensor(out=nP, in0=p6, in1=P, op=ALU.add), reads=[tp6, tP], writes=[tnP])
                    cur = 1 - cur
                    yield
                TT = b[f"PT{cur}"]; tTT = b[f"t_PT{cur}"]
                pw, tpw = p128.get()
                S.op("pe", lambda e: e.matmul(pw, lhsT=b["kbg"], rhs=TT, start=True, stop=True), reads=[b["t_kbg"], tTT], writes=[tpw])
                evac_copy(b["nwT"], pw, [tpw], [b["t_nwT"]], scale=-1.0)
                yield
                pv, tpv = p128.get()

                def vn(e):
                    e.matmul(pv, lhsT=TT, rhs=b["vb"], start=True, stop=False)
                    return e.matmul(pv, lhsT=b["nwT"], rhs=b["Sb"], start=False, stop=True)
                S.op("pe", vn, reads=[tTT, b["t_vb"], b["t_nwT"], b["t_Sb"]], writes=[tpv])
                evac_copy(b["vnew"], pv, [tpv], [b["t_vnew"]])
                yield
                if latent:
                    lt = t - 2
                    po, tpo = p128.get()

                    def om(e):
                        e.matmul(po, lhsT=b["qdT"], rhs=b["Sb"], start=True, stop=False)
                        return e.matmul(po, lhsT=b["QKm"], rhs=b["vnew"], start=False, stop=True)
                    S.op("pe", om, reads=[b["t_qdT"], b["t_Sb"], b["t_QKm"], b["t_vnew"]], writes=[tpo])
                    if (lt, hl) not in visited:
                        visited.add((lt, hl))
                        k4o = ost_cnt[0] % 4; ost_cnt[0] += 1
                        evac_copy(ost[k4o], po, [tpo], [t_ost[k4o]])
                        S.op("pool", lambda e, sig: sig(e.dma_start(out=oacc_d[lt * 2 + hl], in_=ost[k4o])), reads=[t_ost[k4o]], writes=[t_OACC[lt][hl]], dma=1, key=f"oaw{k4o}")
                    else:
                        k2 = fin_cnt[0] % 2; k4 = fin_cnt[0] % 4
                        fin_cnt[0] += 1
                        of = ofin[k2]; tof = t_ofin[k2]; fs = fsm[k2]; tfs = t_fsm[k2]
                        S.op("sp", lambda e, sig: sig(e.dma_start(out=b["ob"], in_=oacc_d[lt * 2 + hl])), reads=[t_OACC[lt][hl]], writes=[b["t_ob"]], dma=1, key=f"oar{hl}{d}{st_}")
                        S.op("dve", lambda e: e.tensor_tensor(out=of, in0=po, in1=b["ob"], op=ALU.add), reads=[tpo, b["t_ob"]], writes=[tof])
                        S.op("act", lambda e: e.activation(out=junk_B, in_=of, func=AF.Square, accum_out=fs[:, 0:1]), reads=[tof], writes=[tfs])
                        S.op("act", lambda e: e.activation(out=fs[:, 0:1], in_=fs[:, 0:1], func=AF.Sqrt, scale=1.0 / 128, bias=EPS), reads=[tfs], writes=[tfs])
                        S.op("dve", lambda e: e.reciprocal(out=fs[:, 0:1], in_=fs[:, 0:1]), reads=[tfs], writes=[tfs])
                        S.op("dve", lambda e: e.tensor_scalar(out=onb[k2], in0=of, scalar1=fs[:, 0:1], scalar2=None, op0=ALU.mult), reads=[tof, tfs], writes=[t_onb[k2]])
                        pt_, tpt = p128.get()
                        S.op("pe", lambda e: e.transpose(out=bfv(pt_), in_=onb[k2], identity=identb), reads=[t_onb[k2], t_identb], writes=[tpt])
                        evac_copy(onT[k4], bfv(pt_), [tpt], [t_onT[k4]])
                        o_ = S.op("pool", lambda e, sig: sig(e.dma_start(out=snd[lt // 16][hl * 128:(hl + 1) * 128, (lt % 16) * 64:(lt % 16 + 1) * 64], in_=onT[k4].bitcast(F32))),
                                  reads=[t_onT[k4]], dma=1, key=f"snd{k4}")
                        snd_ops.append(o_)
                        qops[lt // 16].append(o_)
                        if len(qops[lt // 16]) == 32:
                            jq = lt // 16
                            ccs.append(S.op("pool", lambda e, sig: sig(e.collective_compute("AllGather", ALU.bypass, replica_groups=[[0, 1, 2, 3], [4, 5, 6, 7]],
                                                                                       ins=[snd[jq].ap().opt()], outs=[rcv[jq].ap().opt()])),
                                            extra=qops[jq], dma=1, key=f"cc{jq}", inc=1))
                ps_, tps = p128.get()
                S.op("pe", lambda e: e.matmul(ps_, lhsT=b["kd"], rhs=b["vnew"], start=True, stop=True), reads=[b["t_kd"], b["t_vnew"]], writes=[tps])
                S.op("dve", lambda e: e.scalar_tensor_tensor(out=b["S"], in0=b["S"], scalar=scol(5), in1=ps_, op0=ALU.mult, op1=ALU.add),
                     reads=[b["t_S"], tsc, tps], writes=[b["t_S"]])
                S.op("act", lambda e: e.copy(out=b["Sb"], in_=b["S"]), reads=[b["t_S"]], writes=[b["t_Sb"]])
                yield

            def bwd_tile(i):
                return 1 - i if i < 2 else NT + 1 - i

            HALF = 11
            alive = []
            nstart = 0
            tick = 0
            while nstart < NT or alive:
                if nstart < NT and tick % HALF == 0:
                    i = nstart; nstart += 1
                    for c in chains:
                        t = i if c[1] == 0 else bwd_tile(i)
                        alive.append([chain_step(c, t, i % 2), 0])
                nxt = []
                for g in alive:
                    try:
                        next(g[0]); g[1] += 1
                        assert g[1] < 2 * HALF, "chain step too long for the 2-deep pipeline"
                        nxt.append(g)
                    except StopIteration:
                        pass
                alive = nxt
                tick += 1
            if stage == 2:
                for i_ in range(4):
                    o_ = S.op("sp", lambda e, sig, i_=i_: sig(e.dma_start(out=dbgb[0:256, i_ * 2048:(i_ + 1) * 2048].bitcast(F32), in_=snd[i_][:, :])), extra=snd_ops, dma=1, key="dbg")
                    dump_ops.append(o_)
                for i_, c_ in enumerate(chains):
                    dump(cb[c_]["S"], i_ * 128, 128, reads=[cb[c_]["t_S"]])
                raise _Stop()
            p128 = p128_small
            assert len(ccs) == 4
            S.barrier(skip_cc=True)
            A.release(mA)
            if stage == 3:
                raise _Stop()

            hxo = A.alloc([8, OWN], BF16); t_hxo = [Tk() for _ in range(NOT)]
            markH = A.mark()
            offS = A.mark()
            SZ = A.alloc([8, OWN], BF16); t_SZ = [[Tk() for _ in range(4)] for _ in range(8)]
            GU = A.alloc([8, OWN], BF16); t_GU = [[Tk() for _ in range(4)] for _ in range(8)]
            wstg = [A.alloc([8, 512]) for _ in range(2)]; t_wstg = [Tk(), Tk()]
            wbf = [A.alloc([8, 512], BF16) for _ in range(2)]; t_wbf = [Tk(), Tk()]
            mC1 = A.mark()
            xt_C = [A.alloc([D]) for _ in range(2)]; t_xt_C = [Tk(), Tk()]
            junk_C = A.alloc([D]); t_junk_C = Tk()
            ssq_C = [A.alloc([1]) for _ in range(2)]; t_ssq_C = [Tk(), Tk()]
            xs_C = [A.alloc([8, 128], BF16) for _ in range(2)]; t_xs_C = [Tk(), Tk()]
            for t in range(NOT):
                s2 = t % 2
                S.op("sp", lambda e, sig, t=t, s2=s2: sig(e.dma_start(out=xt_C[s2], in_=xo[t * 128:(t + 1) * 128, :])), writes=[t_xt_C[s2]], dma=1, key=f"cxt{s2}")
                S.op("act", lambda e, s2=s2: e.activation(out=junk_C, in_=xt_C[s2], func=AF.Square, accum_out=ssq_C[s2]), reads=[t_xt_C[s2]], writes=[t_ssq_C[s2]])
                S.op("act", lambda e, s2=s2: e.activation(out=ssq_C[s2], in_=ssq_C[s2], func=AF.Sqrt, scale=1.0 / D, bias=EPS), reads=[t_ssq_C[s2]], writes=[t_ssq_C[s2]])
                S.op("dve", lambda e, s2=s2: e.reciprocal(out=ssq_C[s2], in_=ssq_C[s2]), reads=[t_ssq_C[s2]], writes=[t_ssq_C[s2]])
                S.op("pool", lambda e, s2=s2: e.tensor_scalar(out=xs_C[s2].rearrange("p a b -> p (a b)"), in0=xt_C[s2], scalar1=ssq_C[s2], scalar2=1.0, op0=ALU.mult, op1=ALU.mult),
                     reads=[t_xt_C[s2], t_ssq_C[s2]], writes=[t_xs_C[s2]])
                pp, tp = p512.get()
                ppb = pp.bitcast(BF16)

                def tr(e, s2=s2, ppb=ppb):
                    for k in range(8):
                        i = e.transpose(out=ppb[:, k * 128:(k + 1) * 128], in_=xs_C[s2][:, k, :], identity=identb)
                    return i
                S.op("pe", tr, reads=[t_xs_C[s2], t_identb], writes=[tp])

                def ev_d(e, t=t, ppb=ppb):
                    for k in range(8):
                        i = e.tensor_scalar(out=hxo[:, k, t * 128:(t + 1) * 128], in0=ppb[:, k * 128:(k + 1) * 128], scalar1=modc[:, 1, k:k + 1], scalar2=modc[:, 0, k:k + 1], op0=ALU.mult, op1=ALU.add)
                    return i
                S.op("dve", ev_d, reads=[tp, t_modc], writes=[t_hxo[t]])
            S.barrier()
            A.release(mC1)
            wcnt = [0]

            def stream_w(src_ap_cols, ncols):
                s = wcnt[0] % 2
                wcnt[0] += 1
                v = src_ap_cols.rearrange("(k p) n -> p k n", p=128)
                S.op("sp", lambda e, sig: (sig(e.dma_start(out=wstg[s][:, 0:4, 0:ncols], in_=v[:, 0:4, :])), sig(e.dma_start(out=wstg[s][:, 4:8, 0:ncols], in_=v[:, 4:8, :]))),
                     writes=[t_wstg[s]], dma=2, key=f"wstg{s}")
                S.op("pool", lambda e: e.tensor_copy(out=wbf[s][:, :, 0:ncols], in_=wstg[s][:, :, 0:ncols]), reads=[t_wstg[s]], writes=[t_wbf[s]])
                return wbf[s], t_wbf[s]
            for cbk in range(4):
                wv, twv = stream_w(w_rest[:, cbk * 512:(cbk + 1) * 512], 512)
                dst, tdst, fn = (SZ, t_SZ, AF.Silu) if cbk < 2 else (GU, t_GU, AF.Gelu)
                for cc_ in range(4):
                    chn = (cbk % 2) * 4 + cc_
                    for tb in range(4):
                        pp, tp = p512.get()

                        def f(e, pp=pp, wv=wv, cc_=cc_, tb=tb):
                            for k in range(8):
                                i = e.matmul(pp, lhsT=wv[:, k, cc_ * 128:(cc_ + 1) * 128], rhs=hxo[:, k, tb * 512:(tb + 1) * 512], start=(k == 0), stop=(k == 7))
                            return i
                        S.op("pe", f, reads=[twv] + t_hxo[tb * 4:(tb + 1) * 4], writes=[tp])
                        S.op("act", lambda e, pp=pp, dst=dst, chn=chn, tb=tb, fn=fn: e.activation(out=dst[:, chn, tb * 512:(tb + 1) * 512], in_=pp, func=fn),
                             reads=[tp], writes=[tdst[chn][tb]])
            def cut4(k):
                if stage == 4 and int(os.environ.get("CUT4", "0")) == k:
                    S.barrier()
                    for i_, buf_ in enumerate((SZ, GU, hxo)):
                        for h_ in range(2):
                            dumpb(buf_[:, h_ * 4:(h_ + 1) * 4, :].rearrange("p a b -> p (a b)"), i_ * 256 + h_ * 128, 128, 0, SEQ)
                    raise _Stop()
            cut4(1)
            mV = A.mark()
            junk_V = A.alloc([D])
            wcnt[0] = 0
            wvh = [stream_w(w_rest[:, 2048 + hh * 512:2048 + (hh + 1) * 512], 512) for hh in range(2)]
            swf = A.alloc([8, 128]); t_swf = Tk(); load(swf, sgu_wT.ap().rearrange("g q p -> q g p"), t_swf, "c11")
            swb = A.alloc([8, 128], BF16); t_swb = Tk()
            S.op("pool", lambda e: e.tensor_copy(out=swb, in_=swf), reads=[t_swf], writes=[t_swb])
            lnbb = A.alloc([D]); t_lnbb = Tk(); load(lnbb, lnb_bc[:, :], t_lnbb, "c12")
            sbb = A.alloc([8, 128]); t_sbb = Tk(); load(sbb, sgu_bb[:, :, :], t_sbb, "c13")
            lngc = A.alloc([8]); t_lngc = Tk(); load(lngc, lng_c[:, :], t_lngc, "c14")
            BIAS = A.alloc([8, 128]); t_BIAS = Tk()
            for g in range(8):
                pp, tp = p128.get()
                S.op("pe", lambda e, pp=pp, g=g: e.matmul(pp, lhsT=lnbb[:, g * 128:(g + 1) * 128], rhs=swf[:, g, :], start=True, stop=True), reads=[t_lnbb, t_swf], writes=[tp])
                S.op("dve", lambda e, pp=pp, g=g: e.tensor_tensor(out=BIAS[:, g, :], in0=pp, in1=sbb[:, g, :], op=ALU.add), reads=[tp, t_sbb], writes=[t_BIAS])
            gv = [A.alloc([D])] * 2; t_gv = [Tk()] * 2
            vnb = [A.alloc([D], BF16) for _ in range(2)]; t_vnb = [Tk(), Tk()]
            lst = [A.alloc([8]) for _ in range(2)]; t_lst = [Tk(), Tk()]
            mtmp = [A.alloc([128]) for _ in range(2)]; t_mtmp = [Tk(), Tk()]
            for t in range(NOT):
                s2 = t % 2
                for hh in range(2):
                    pp, tp = p512.get()

                    def f(e, pp=pp, hh=hh, t=t):
                        for k in range(8):
                            i = e.matmul(pp, lhsT=hxo[:, k, t * 128:(t + 1) * 128], rhs=wvh[hh][0][:, k, :], start=(k == 0), stop=(k == 7))
                        return i
                    S.op("pe", f, reads=[wvh[hh][1], t_hxo[t]], writes=[tp])
                    S.op("act", lambda e, pp=pp, hh=hh, s2=s2: e.activation(out=gv[s2][:, hh * 512:(hh + 1) * 512], in_=pp, func=AF.Gelu), reads=[tp], writes=[t_gv[s2]])
                ls = lst[s2]; tls = t_lst[s2]
                S.op("dve", lambda e, s2=s2, ls=ls: e.tensor_reduce(out=ls[:, 0:1], in_=gv[s2], axis=mybir.AxisListType.X, op=ALU.add), reads=[t_gv[s2]], writes=[tls])
                S.op("act", lambda e, s2=s2, ls=ls: e.activation(out=junk_V, in_=gv[s2], func=AF.Square, accum_out=ls[:, 1:2]), reads=[t_gv[s2]], writes=[tls])
                S.op("dve", lambda e, ls=ls: e.tensor_scalar(out=ls[:, 0:2], in0=ls[:, 0:2], scalar1=1.0 / D, scalar2=None, op0=ALU.mult), reads=[tls], writes=[tls])
                S.op("dve", lambda e, ls=ls: e.tensor_tensor(out=ls[:, 2:3], in0=ls[:, 0:1], in1=ls[:, 0:1], op=ALU.mult), reads=[tls], writes=[tls])
                S.op("dve", lambda e, ls=ls: e.tensor_tensor(out=ls[:, 2:3], in0=ls[:, 1:2], in1=ls[:, 2:3], op=ALU.subtract), reads=[tls], writes=[tls])
                S.op("act", lambda e, ls=ls: e.activation(out=ls[:, 2:3], in_=ls[:, 2:3], func=AF.Sqrt, bias=EPS), reads=[tls], writes=[tls])
                S.op("dve", lambda e, ls=ls: e.reciprocal(out=ls[:, 2:3], in_=ls[:, 2:3]), reads=[tls], writes=[tls])
                S.op("dve", lambda e, s2=s2, ls=ls: e.tensor_scalar(out=vnb[s2], in0=gv[s2], scalar1=ls[:, 0:1], scalar2=ls[:, 2:3], op0=ALU.subtract, op1=ALU.mult),
                     reads=[t_gv[s2], tls], writes=[t_vnb[s2]])
                for g in range(8):
                    pp, tp = p128.get()
                    S.op("pe", lambda e, pp=pp, g=g, s2=s2: e.matmul(pp, lhsT=vnb[s2][:, g * 128:(g + 1) * 128], rhs=swb[:, g, :], start=True, stop=True),
                         reads=[t_vnb[s2], t_swb], writes=[tp])
                    mt = mtmp[g % 2]; tmt = t_mtmp[g % 2]
                    S.op("dve", lambda e, pp=pp, g=g, mt=mt: e.scalar_tensor_tensor(out=mt, in0=pp, scalar=lngc[:, g:g + 1], in1=BIAS[:, g, :], op0=ALU.mult, op1=ALU.add),
                         reads=[tp, t_lngc, t_BIAS], writes=[tmt])
                    S.op("pool", lambda e, g=g, t=t, mt=mt: e.tensor_tensor(out=GU[:, g, t * 128:(t + 1) * 128], in0=GU[:, g, t * 128:(t + 1) * 128], in1=mt, op=ALU.mult),
                         reads=[tmt, t_GU[g][t // 4]], writes=[t_GU[g][t // 4]])
            S.barrier()
            A.release(mV)
            cut4(2)
            mY = A.mark()
            rq = [A.alloc([4, 512], BF16) for _ in range(2)]; t_rq = [Tk(), Tk()]
            acc = [A.alloc([512]) for _ in range(2)]; t_acc = [Tk(), Tk()]
            cntr = 0
            for hp in range(4):
                for hl in range(2):
                    chn = hp * 2 + hl
                    for tb in range(4):
                        s = cntr % 2; cntr += 1
                        S.op("sp", lambda e, sig, s=s, chn=chn, tb=tb: tuple(sig(e.dma_start(out=rq[s][:, j, :].bitcast(F32), in_=rcv[j][chn * 128:(chn + 1) * 128, tb * 256:(tb + 1) * 256])) for j in range(4)),
                             extra=ccs, writes=[t_rq[s]], dma=4, key=f"rq{s}")
                        for j in range(4):
                            def f(e, s=s, j=j):
                                if j == 0:
                                    return e.tensor_scalar(out=acc[s], in0=rq[s][:, 0, :], scalar1=qmask[:, 0:1], scalar2=None, op0=ALU.mult)
                                return e.scalar_tensor_tensor(out=acc[s], in0=rq[s][:, j, :], scalar=qmask[:, j:j + 1], in1=acc[s], op0=ALU.mult, op1=ALU.add)
                            S.op("dve", f, reads=[t_rq[s], t_qmask, t_acc[s]] if j else [t_rq[s], t_qmask], writes=[t_acc[s]])
                        S.op("dve", lambda e, s=s, chn=chn, tb=tb: e.scalar_tensor_tensor(out=SZ[:, chn, tb * 512:(tb + 1) * 512], in0=acc[s], scalar=gngs[:, 0:1], in1=SZ[:, chn, tb * 512:(tb + 1) * 512], op0=ALU.mult, op1=ALU.mult),
                             reads=[t_acc[s], t_gngs, t_SZ[chn][tb]], writes=[t_SZ[chn][tb]])
            S.barrier()
            A.release(mY)
            cut4(3)
            S.barrier()
            mM = A.mark()
            MG = A.alloc([8, OWN], BF16); t_MG = [[Tk() for _ in range(4)] for _ in range(8)]
            sga = [A.alloc([512], BF16) for _ in range(2)]; t_sga = [Tk(), Tk()]
            sgb = [A.alloc([512], BF16) for _ in range(2)]; t_sgb = [Tk(), Tk()]
            m1 = [A.alloc([512]) for _ in range(2)]; t_m1 = [Tk(), Tk()]
            m2 = [A.alloc([512]) for _ in range(2)]; t_m2 = [Tk(), Tk()]
            sub = [(wstg[i][:, :, j * 128:(j + 1) * 128], wbf[i][:, :, j * 128:(j + 1) * 128], Tk(), Tk()) for i in range(2) for j in range(4)]
            subc = [0]

            def stream_small(src_cols):
                stg, bfw, tst, tbf = sub[subc[0] % 8]
                subc[0] += 1
                v = src_cols.rearrange("(k p) n -> p k n", p=128)
                S.op("sp", lambda e, sig: sig(e.dma_start(out=stg, in_=v)), writes=[tst], dma=1, key=f"sub{(subc[0] - 1) % 8}")
                S.op("pool", lambda e: e.tensor_copy(out=bfw, in_=stg), reads=[tst], writes=[tbf])
                return bfw, tbf
            cntr = 0
            for dc in range(8):
                ws = [stream_small(w_a[:, dc * 128:(dc + 1) * 128]), stream_small(w_b[:, dc * 128:(dc + 1) * 128]),
                      stream_small(w_rest[:, 3072 + dc * 128:3072 + (dc + 1) * 128]), stream_small(w_rest[:, 4096 + dc * 128:4096 + (dc + 1) * 128])]
                for tb in range(4):
                    s = cntr % 2; cntr += 1
                    outs = []
                    for wi, (src, tsrc) in enumerate(((SZ, t_SZ), (GU, t_GU), (hxo, None), (hxo, None))):
                        wv_, tw_ = ws[wi]
                        pp, tp = p512.get()

                        def f(e, pp=pp, wv_=wv_, src=src, tb=tb):
                            for k in range(8):
                                i = e.matmul(pp, lhsT=wv_[:, k, :], rhs=src[:, k, tb * 512:(tb + 1) * 512], start=(k == 0), stop=(k == 7))
                            return i
                        rds = [tw_] + ([tsrc[k][tb] for k in range(8)] if tsrc is not None else t_hxo[tb * 4:(tb + 1) * 4])
                        S.op("pe", f, reads=rds, writes=[tp])
                        outs.append((pp, tp))
                    S.op("act", lambda e, s=s, pp=outs[2][0]: e.activation(out=sga[s], in_=pp, func=AF.Sigmoid), reads=[outs[2][1]], writes=[t_sga[s]])
                    S.op("act", lambda e, s=s, pp=outs[3][0]: e.activation(out=sgb[s], in_=pp, func=AF.Sigmoid), reads=[outs[3][1]], writes=[t_sgb[s]])
                    S.op("dve", lambda e, s=s, pp=outs[0][0]: e.tensor_tensor(out=m1[s], in0=pp, in1=sga[s], op=ALU.mult), reads=[outs[0][1], t_sga[s]], writes=[t_m1[s]])
                    S.op("dve", lambda e, s=s, pp=outs[1][0]: e.tensor_tensor(out=m2[s], in0=pp, in1=sgb[s], op=ALU.mult), reads=[outs[1][1], t_sgb[s]], writes=[t_m2[s]])
                    S.op("pool", lambda e, s=s, dc=dc, tb=tb: e.tensor_tensor(out=MG[:, dc, tb * 512:(tb + 1) * 512], in0=m1[s], in1=m2[s], op=ALU.add),
                         reads=[t_m1[s], t_m2[s]], writes=[t_MG[dc][tb]])
            S.barrier()
            if stage == 4:
                for i_, (buf_, tk_) in enumerate(((SZ, t_SZ), (GU, t_GU), (MG, t_MG))):
                    for h_ in range(2):
                        dumpb(buf_[:, h_ * 4:(h_ + 1) * 4, :].rearrange("p a b -> p (a b)"), i_ * 256 + h_ * 128, 128, 0, SEQ)
                raise _Stop()
            hx2 = hxo; t_hx2 = [Tk() for _ in range(NOT)]
            Amain = A
            A = Arena(arena_t, 16384, base=offS)
            wo_b = A.alloc([8, D], BF16); t_wo_b = Tk()
            for hh in range(2):
                sl = hh
                S.op("sp", lambda e, sig, hh=hh, sl=sl: (sig(e.dma_start(out=wstg[sl][:, 0:4, :], in_=w_out.ap().rearrange("(k p) n -> p k n", p=128)[:, 0:4, hh * 512:(hh + 1) * 512])),
                                                        sig(e.dma_start(out=wstg[sl][:, 4:8, :], in_=w_out.ap().rearrange("(k p) n -> p k n", p=128)[:, 4:8, hh * 512:(hh + 1) * 512]))),
                     writes=[t_wstg[sl]], dma=2, key=f"wstg{sl}")
                for k in range(8):
                    S.op("pool", lambda e, k=k, hh=hh, sl=sl: e.tensor_tensor(out=wo_b[:, k, hh * 512:(hh + 1) * 512], in0=wstg[sl][:, k, :], in1=gmix[:, hh * 512:(hh + 1) * 512], op=ALU.mult),
                         reads=[t_wstg[sl], t_gmix], writes=[t_wo_b])
            rwf = A.alloc([8, 36]); t_rwf = Tk(); load(rwf, rw.ap().rearrange("(k p) n -> p k n", p=128), t_rwf, "c15")
            rbb = A.alloc([36]); t_rbb = Tk(); load(rbb, rb[:, :], t_rbb, "c16")
            x1 = [A.alloc([D]) for _ in range(2)]; t_x1 = [Tk(), Tk()]
            xt_D = [A.alloc([D]) for _ in range(2)]; t_xt_D = [Tk(), Tk()]
            junk_D = A.alloc([D]); t_junk_D = Tk()
            ssq_D = [A.alloc([1]) for _ in range(2)]; t_ssq_D = [Tk(), Tk()]
            xsf = [A.alloc([8, 128]) for _ in range(2)]; t_xsf = [Tk(), Tk()]
            hxf = [A.alloc([8, 128]) for _ in range(2)]; t_hxf = [Tk(), Tk()]
            rs_ = [A.alloc([64]) for _ in range(2)]; t_rs = [Tk(), Tk()]
            x1_ops = []
            for t in range(NOT):
                s2 = t % 2
                S.op("sp", lambda e, sig, t=t, s2=s2: sig(e.dma_start(out=xt_D[s2], in_=xo[t * 128:(t + 1) * 128, :])), writes=[t_xt_D[s2]], dma=1, key=f"dxt{s2}")
                for hh in range(2):
                    pp, tp = p512.get()

                    def f(e, pp=pp, hh=hh, t=t):
                        for k in range(8):
                            i = e.matmul(pp, lhsT=MG[:, k, t * 128:(t + 1) * 128], rhs=wo_b[:, k, hh * 512:(hh + 1) * 512], start=(k == 0), stop=(k == 7))
                        return i
                    S.op("pe", f, reads=[t_wo_b] + [t_MG[k][t // 4] for k in range(8)], writes=[tp])
                    S.op("dve", lambda e, pp=pp, hh=hh, s2=s2: e.tensor_tensor(out=x1[s2][:, hh * 512:(hh + 1) * 512], in0=pp, in1=xt_D[s2][:, hh * 512:(hh + 1) * 512], op=ALU.add),
                         reads=[tp, t_xt_D[s2]], writes=[t_x1[s2]])
                o_ = S.op("pool", lambda e, sig, t=t, s2=s2: sig(e.dma_start(out=x1d[t * 128:(t + 1) * 128, :], in_=x1[s2])), reads=[t_x1[s2]], dma=1, key=f"x1d{s2}")
                x1_ops.append(o_)
                S.op("act", lambda e, s2=s2: e.activation(out=junk_D, in_=x1[s2], func=AF.Square, accum_out=ssq_D[s2]), reads=[t_x1[s2]], writes=[t_ssq_D[s2]])
                S.op("act", lambda e, s2=s2: e.activation(out=ssq_D[s2], in_=ssq_D[s2], func=AF.Sqrt, scale=1.0 / D, bias=EPS), reads=[t_ssq_D[s2]], writes=[t_ssq_D[s2]])
                S.op("dve", lambda e, s2=s2: e.reciprocal(out=ssq_D[s2], in_=ssq_D[s2]), reads=[t_ssq_D[s2]], writes=[t_ssq_D[s2]])
                S.op("pool", lambda e, s2=s2: e.tensor_scalar(out=xsf[s2].rearrange("p a b -> p (a b)"), in0=x1[s2], scalar1=ssq_D[s2], scalar2=1.0, op0=ALU.mult, op1=ALU.mult),
                     reads=[t_x1[s2], t_ssq_D[s2]], writes=[t_xsf[s2]])
                for q4 in range(2):
                    pp, tp = p512.get()

                    def tr(e, pp=pp, q4=q4, s2=s2):
                        for k in range(4):
                            i = e.transpose(out=pp[:, k * 128:(k + 1) * 128], in_=xsf[s2][:, q4 * 4 + k, :], identity=identf)
                        return i
                    S.op("pe", tr, reads=[t_xsf[s2], t_identf], writes=[tp])

                    def ev(e, pp=pp, q4=q4, s2=s2, t=t):
                        for k in range(4):
                            kk = q4 * 4 + k
                            i = e.tensor_scalar(out=hxf[s2][:, kk, :], in0=pp[:, k * 128:(k + 1) * 128], scalar1=modc[:, 5, kk:kk + 1], scalar2=modc[:, 4, kk:kk + 1], op0=ALU.mult, op1=ALU.add)
                        return i
                    S.op("dve", ev, reads=[tp, t_modc], writes=[t_hxf[s2]])
                S.op("pool", lambda e, s2=s2, t=t: e.tensor_copy(out=hx2[:, :, t * 128:(t + 1) * 128], in_=hxf[s2]), reads=[t_hxf[s2]], writes=[t_hx2[t]])
                pr, tpr = p128.get()

                def rt(e, pr=pr, s2=s2):
                    for k in range(8):
                        i = e.matmul(pr[:, 0:36], lhsT=hxf[s2][:, k, :], rhs=rwf[:, k, :], start=(k == 0), stop=(k == 7))
                    return i
                S.op("pe", rt, reads=[t_hxf[s2], t_rwf], writes=[tpr])
                r = rs_[s2]; tr_ = t_rs[s2]
                S.op("dve", lambda e, pr=pr, r=r: e.tensor_tensor(out=r[:, 0:36], in0=pr[:, 0:36], in1=rbb, op=ALU.add), reads=[tpr, t_rbb], writes=[tr_])
                S.op("dve", lambda e, r=r: e.tensor_reduce(out=r[:, 40:41], in_=r[:, 0:4], axis=mybir.AxisListType.X, op=ALU.max), reads=[tr_], writes=[tr_])
                S.op("dve", lambda e, r=r: e.tensor_scalar(out=r[:, 36:40], in0=r[:, 0:4], scalar1=r[:, 40:41], scalar2=None, op0=ALU.is_ge), reads=[tr_], writes=[tr_])
                S.op("dve", lambda e, r=r: e.tensor_scalar(out=r[:, 60:64], in0=r[:, 0:4], scalar1=r[:, 40:41], scalar2=None, op0=ALU.subtract), reads=[tr_], writes=[tr_])
                S.op("act", lambda e, r=r: e.activation(out=r[:, 60:64], in_=r[:, 60:64], func=AF.Exp, accum_out=r[:, 41:42]), reads=[tr_], writes=[tr_])
                S.op("dve", lambda e, r=r: e.reciprocal(out=r[:, 41:42], in_=r[:, 41:42]), reads=[tr_], writes=[tr_])
                S.op("dve", lambda e, r=r: e.tensor_scalar(out=r[:, 42:50], in0=r[:, 4:12], scalar1=r[:, 36:37], scalar2=None, op0=ALU.mult), reads=[tr_], writes=[tr_])
                for g in range(1, 4):
                    S.op("dve", lambda e, r=r, g=g: e.scalar_tensor_tensor(out=r[:, 42:50], in0=r[:, 4 + 8 * g:12 + 8 * g], scalar=r[:, 36 + g:37 + g], in1=r[:, 42:50], op0=ALU.mult, op1=ALU.add),
                         reads=[tr_], writes=[tr_])
                S.op("dve", lambda e, r=r: e.tensor_reduce(out=r[:, 50:51], in_=r[:, 42:50], axis=mybir.AxisListType.X, op=ALU.max), reads=[tr_], writes=[tr_])
                S.op("dve", lambda e, r=r: e.tensor_scalar(out=r[:, 42:50], in0=r[:, 42:50], scalar1=r[:, 50:51], scalar2=None, op0=ALU.subtract), reads=[tr_], writes=[tr_])
                S.op("act", lambda e, r=r: e.activation(out=r[:, 42:50], in_=r[:, 42:50], func=AF.Exp), reads=[tr_], writes=[tr_])
                S.op("dve", lambda e, r=r: e.tensor_scalar(out=r[:, 52:60], in0=r[:, 42:50], scalar1=1.0, scalar2=None, op0=ALU.is_ge), reads=[tr_], writes=[tr_])
                S.op("dve", lambda e, r=r: e.scalar_tensor_tensor(out=r[:, 4:12], in0=r[:, 52:60], scalar=-2.0, in1=r[:, 42:50], op0=ALU.mult, op1=ALU.add), reads=[tr_], writes=[tr_])
                S.op("dve", lambda e, r=r: e.tensor_reduce(out=r[:, 51:52], in_=r[:, 4:12], axis=mybir.AxisListType.X, op=ALU.max), reads=[tr_], writes=[tr_])
                S.op("dve", lambda e, r=r: e.tensor_scalar(out=r[:, 12:20], in0=r[:, 4:12], scalar1=r[:, 51:52], scalar2=None, op0=ALU.is_ge), reads=[tr_], writes=[tr_])
                S.op("dve", lambda e, r=r: e.scalar_tensor_tensor(out=r[:, 20:28], in0=r[:, 12:20], scalar=r[:, 51:52], in1=r[:, 52:60], op0=ALU.mult, op1=ALU.add), reads=[tr_], writes=[tr_])
                S.op("dve", lambda e, r=r: e.tensor_scalar(out=r[:, 50:51], in0=r[:, 51:52], scalar1=1.0, scalar2=None, op0=ALU.add), reads=[tr_], writes=[tr_])
                S.op("dve", lambda e, r=r: e.reciprocal(out=r[:, 50:51], in_=r[:, 50:51]), reads=[tr_], writes=[tr_])
                S.op("dve", lambda e, r=r: e.tensor_tensor(out=r[:, 50:51], in0=r[:, 50:51], in1=r[:, 41:42], op=ALU.mult), reads=[tr_], writes=[tr_])
                S.op("dve", lambda e, r=r: e.tensor_scalar(out=r[:, 20:28], in0=r[:, 20:28], scalar1=r[:, 50:51], scalar2=None, op0=ALU.mult), reads=[tr_], writes=[tr_])
                for g in range(4):
                    S.op("dve", lambda e, r=r, g=g, t=t: e.tensor_scalar(out=GT[:, t, g * 8:(g + 1) * 8], in0=r[:, 20:28], scalar1=r[:, 36 + g:37 + g], scalar2=None, op0=ALU.mult),
                         reads=[tr_], writes=[t_GT[t]])
            S.barrier()
            A = Amain
            A.release(markH)
            if stage == 5:
                dump(GT.rearrange("p a b -> p (a b)"), 0, 512)
                for i_ in range(4):
                    o_ = S.op("sp", lambda e, sig, i_=i_: sig(e.dma_start(out=y[i_ * 512:(i_ + 1) * 512, :], in_=x1d[i_ * 512:(i_ + 1) * 512, :])), extra=x1_ops, dma=1, key="dbg")
                    dump_ops.append(o_)
                dumpb(hx2[:, 0:4, :].rearrange("p a b -> p (a b)"), 0, 128, 0, SEQ)
                raise _Stop()
            p512 = Pool([(psb[b_][:, :], Tk(f"p512x_{b_}", excl=True)) for b_ in range(8)])
            ACC = A.alloc([NOT, D]); t_ACC = [[Tk(), Tk()] for _ in range(NOT)]
            for t in range(NOT):
                S.op("pool", lambda e, t=t: e.memset(ACC[:, t, :], 0.0), writes=t_ACC[t])
            e1f = A.alloc([8, 512]); t_e1f = Tk()
            e2f = A.alloc([2, D]); t_e2f = Tk()
            e1b = [A.alloc([8, 512], BF16) for _ in range(2)]; t_e1b = [Tk(), Tk()]
            e2b = [A.alloc([2, D], BF16) for _ in range(2)]; t_e2b = [Tk(), Tk()]
            sil = [A.alloc([512], BF16) for _ in range(2)]; t_sil = [Tk(), Tk()]
            hid = [A.alloc([512], BF16) for _ in range(4)]; t_hid = [Tk() for _ in range(4)]
            hc = 0
            for ex in range(NE):
                s = ex % 2
                v1 = ew1[ex].rearrange("(k p) n -> p k n", p=128); v3 = ew3[ex].rearrange("(k p) n -> p k n", p=128)
                v2 = ew2[ex].rearrange("(k p) n -> p k n", p=128)
                S.op("sp", lambda e, sig, v1=v1, v3=v3: (sig(e.dma_start(out=e1f[:, :, 0:256], in_=v1)), sig(e.dma_start(out=e1f[:, :, 256:512], in_=v3))), writes=[t_e1f], dma=2, key="e1f")
                S.op("sp", lambda e, sig, v2=v2: sig(e.dma_start(out=e2f, in_=v2)), writes=[t_e2f], dma=1, key="e2f")
                S.op("pool", lambda e, s=s: e.tensor_copy(out=e1b[s], in_=e1f), reads=[t_e1f], writes=[t_e1b[s]])
                S.op("pool", lambda e, s=s: e.tensor_copy(out=e2b[s], in_=e2f), reads=[t_e2f], writes=[t_e2b[s]])
                for tb in range(4):
                    hs = []
                    for fc in range(2):
                        p1, tp1 = p512.get(); p3, tp3 = p512.get()

                        def f1(e, p1=p1, s=s, fc=fc, tb=tb):
                            for k in range(8):
                                i = e.matmul(p1, lhsT=e1b[s][:, k, fc * 128:(fc + 1) * 128], rhs=hx2[:, k, tb * 512:(tb + 1) * 512], start=(k == 0), stop=(k == 7))
                            return i

                        def f3(e, p3=p3, s=s, fc=fc, tb=tb):
                            for k in range(8):
                                i = e.matmul(p3, lhsT=e1b[s][:, k, 256 + fc * 128:256 + (fc + 1) * 128], rhs=hx2[:, k, tb * 512:(tb + 1) * 512], start=(k == 0), stop=(k == 7))
                            return i
                        S.op("pe", f1, reads=[t_e1b[s]] + t_hx2[tb * 4:(tb + 1) * 4], writes=[tp1])
                        S.op("pe", f3, reads=[t_e1b[s]] + t_hx2[tb * 4:(tb + 1) * 4], writes=[tp3])
                        ss = hc % 2; h4 = hc % 4; hc += 1
                        S.op("act", lambda e, p1=p1, ss=ss: e.activation(out=sil[ss], in_=p1, func=AF.Silu), reads=[tp1], writes=[t_sil[ss]])
                        S.op("dve", lambda e, p3=p3, ss=ss, h4=h4: e.tensor_tensor(out=hid[h4], in0=p3, in1=sil[ss], op=ALU.mult), reads=[tp3, t_sil[ss]], writes=[t_hid[h4]])
                        hs.append(h4)
                    for tt in range(4):
                        t = tb * 4 + tt
                        for hh in range(2):
                            po, tpo = p512.get()

                            def f2(e, po=po, s=s, tt=tt, hh=hh, hs=tuple(hs)):
                                for fc in range(2):
                                    i = e.matmul(po, lhsT=hid[hs[fc]][:, tt * 128:(tt + 1) * 128], rhs=e2b[s][:, fc, hh * 512:(hh + 1) * 512], start=(fc == 0), stop=(fc == 1))
                                return i
                            S.op("pe", f2, reads=[t_hid[hs[0]], t_hid[hs[1]], t_e2b[s]], writes=[tpo])
                            S.op("dve", lambda e, po=po, t=t, hh=hh, ex=ex: e.scalar_tensor_tensor(out=ACC[:, t, hh * 512:(hh + 1) * 512], in0=po, scalar=GT[:, t, ex:ex + 1], in1=ACC[:, t, hh * 512:(hh + 1) * 512], op0=ALU.mult, op1=ALU.add),
                                 reads=[tpo, t_ACC[t][hh]], writes=[t_ACC[t][hh]])
            fngb = A.alloc([D]); t_fngb = Tk(); load(fngb, fng_bc[:, :], t_fngb, "c17")
            x1r = [A.alloc([D]) for _ in range(2)]; t_x1r = [Tk(), Tk()]
            x2 = [A.alloc([D]) for _ in range(2)]; t_x2 = [Tk(), Tk()]
            junk_E = A.alloc([D]); t_junk_E = Tk()
            ssq_E = [A.alloc([1]) for _ in range(2)]; t_ssq_E = [Tk(), Tk()]
            outs_ = []
            for t in range(NOT):
                s2 = t % 2
                S.op("sp", lambda e, sig, t=t, s2=s2: sig(e.dma_start(out=x1r[s2], in_=x1d[t * 128:(t + 1) * 128, :])), extra=[x1_ops[t]], writes=[t_x1r[s2]], dma=1, key=f"x1r{s2}")
                S.op("dve", lambda e, t=t, s2=s2: e.tensor_tensor(out=x2[s2], in0=ACC[:, t, :], in1=gffn, op=ALU.mult), reads=t_ACC[t] + [t_gffn], writes=[t_x2[s2]])
                S.op("dve", lambda e, s2=s2: e.tensor_tensor(out=x2[s2], in0=x2[s2], in1=x1r[s2], op=ALU.add), reads=[t_x2[s2], t_x1r[s2]], writes=[t_x2[s2]])
                S.op("act", lambda e, s2=s2: e.activation(out=junk_E, in_=x2[s2], func=AF.Square, accum_out=ssq_E[s2]), reads=[t_x2[s2]], writes=[t_ssq_E[s2]])
                S.op("act", lambda e, s2=s2: e.activation(out=ssq_E[s2], in_=ssq_E[s2], func=AF.Sqrt, scale=1.0 / D, bias=EPS), reads=[t_ssq_E[s2]], writes=[t_ssq_E[s2]])
                S.op("dve", lambda e, s2=s2: e.reciprocal(out=ssq_E[s2], in_=ssq_E[s2]), reads=[t_ssq_E[s2]], writes=[t_ssq_E[s2]])
                S.op("dve", lambda e, s2=s2: e.scalar_tensor_tensor(out=x2[s2], in0=x2[s2], scalar=ssq_E[s2], in1=fngb, op0=ALU.mult, op1=ALU.mult), reads=[t_x2[s2], t_ssq_E[s2], t_fngb], writes=[t_x2[s2]])
                o_ = S.op("sp", lambda e, sig, t=t, s2=s2: sig(e.dma_start(out=y[t * 128:(t + 1) * 128, :], in_=x2[s2])), reads=[t_x2[s2]], dma=1, key=f"yo{s2}")
                outs_.append(o_)
            S.op("sp", None, extra=outs_)
        try:
            author()
        except _Stop:
            pass
        if dump_ops:
            S.op("sp", None, extra=dump_ops)
        S.emit(block, sems)
    return nc


def _prep(inputs):
    f = lambda a: np.ascontiguousarray(np.asarray(a, dtype=np.float32))
    x = f(inputs["x"]); c = f(inputs["c"]); ctx = f(inputs["ctx"]); c_ctx = f(inputs["c_ctx"])
    w_in = f(inputs["w_in"])[0]
    conv_w = f(inputs["conv_w"])[0]
    a_log = f(inputs["a_log"])[0]; dt_bias = f(inputs["dt_bias"])[0]
    idx = np.arange(128)
    tri = np.stack([(idx[:, None] <= idx[None, :]), (idx[:, None] > idx[None, :]), (idx[:, None] >= idx[None, :]), (idx[:, None] < idx[None, :])]).astype(np.float32)
    negm = np.stack([(idx[:, None] <= idx[None, :]), (idx[None, :] < idx[:, None]), (idx[:, None] >= idx[None, :]), (idx[None, :] > idx[:, None])]).astype(np.float32) * -100.0
    blk = lambda n: (idx[:, None] // n == idx[None, :] // n)
    dcm = [blk(8)]
    for n in (16, 32, 64, 128):
        low = blk(n) & ((idx[:, None] % n) >= n // 2) & ((idx[None, :] % n) < n // 2)
        dcm += [low, low.T]
    import ml_dtypes
    dcm = np.stack(dcm).astype(np.float32).astype(ml_dtypes.bfloat16)
    common = {
        "dcm": dcm,
        "ada_w": f(inputs["ada_w"])[0], "ada_b": f(inputs["ada_b"]).reshape(1, -1),
        "gcols": np.ascontiguousarray(np.stack([f(inputs["norm_mix_g"])[0].reshape(8, 128).T, f(inputs["norm_ffn_g"])[0].reshape(8, 128).T], axis=-1)),
        "gng": f(inputs["gdn_norm_g"])[0].reshape(128, 1),
        "ident": np.eye(128, dtype=np.float32), "tri": tri, "negm": negm,
        "w_rest": np.ascontiguousarray(w_in[:, COL_Z:]),
        "sgu_wT": np.ascontiguousarray(f(inputs["sgu_w"])[0].transpose(0, 2, 1)),
        "sgu_bb": np.ascontiguousarray(np.broadcast_to(f(inputs["sgu_b"])[0][None], (128, 8, 128))),
        "lng_c": np.ascontiguousarray(f(inputs["sgu_ln_g"])[0].reshape(8, 128).T),
        "lnb_bc": np.ascontiguousarray(np.broadcast_to(f(inputs["sgu_ln_b"])[0][None], (128, D))),
        "w_a": f(inputs["w_branch_a"])[0], "w_b": f(inputs["w_branch_b"])[0], "w_out": f(inputs["w_out"])[0],
        "rw": np.ascontiguousarray(np.concatenate([f(inputs["router_group_w"])[0], f(inputs["router_expert_w"])[0]], axis=1)),
        "rb": np.ascontiguousarray(np.broadcast_to(np.concatenate([f(inputs["router_group_b"])[0], f(inputs["router_expert_b"])[0]])[None], (128, 36))),
        "ew1": f(inputs["expert_w1"])[0], "ew3": f(inputs["expert_w3"])[0], "ew2": f(inputs["expert_w2"])[0],
        "fng_bc": np.ascontiguousarray(np.broadcast_to(f(inputs["final_norm_g"])[None], (128, D))),
    }
    maps = []
    for core in range(8):
        b, r = core // 4, core % 4
        heads = (2 * r, 2 * r + 1)
        qcols = np.concatenate([np.arange(base + h * 128, base + (h + 1) * 128) for base in (0, 1024, 2048) for h in heads])
        bacols = np.array([COL_BETA + d * 8 + h for d in range(2) for h in heads] + [COL_A + d * 8 + h for d in range(2) for h in heads])
        conv_c = np.ascontiguousarray(conv_w[:, qcols].reshape(5, 6, 128).transpose(2, 1, 0))
        gsc = np.concatenate([np.array([a_log[d, h] for d in range(2) for h in heads]), np.array([dt_bias[d, h] for d in range(2) for h in heads])]).astype(np.float32)
        qm = np.zeros((128, 4), np.float32); qm[:, r] = 1.0
        m = dict(common)
        m.update({
            "xb": x[b], "ctxb": ctx[b], "xo": np.ascontiguousarray(x[b, r * OWN:(r + 1) * OWN]),
            "cvT": np.ascontiguousarray(np.stack([c[b].reshape(8, 128).T, c_ctx.reshape(8, 128).T], axis=-1)),
            "w_qkv": np.ascontiguousarray(w_in[:, qcols]), "w_ba": np.ascontiguousarray(w_in[:, bacols]),
            "gsc66": np.ascontiguousarray(np.broadcast_to(gsc.reshape(1, 2, 1, 4), (128, 2, NT, 4))),
            "conv_c": conv_c, "gsc": np.ascontiguousarray(np.broadcast_to(gsc[None], (128, 8))), "qmask": qm,
        })
        maps.append(m)
    return maps


_NC = None


def kernel(**inputs):
    global _NC
    if _NC is None:
        _NC = build_program()
    maps = _prep(inputs)
    res = run_bass_kernel_spmd(_NC, maps, core_ids=list(range(8)))
    out = np.zeros((2, SEQ, D), np.float32)
    for core in range(8):
        b, r = core // 4, core % 4
        out[b, r * OWN:(r + 1) * OWN] = np.asarray(res.results[core]["y"], dtype=np.float32)
    return out
```

```python
import contextlib
import os
import numpy as np
import concourse.bass as bass
import concourse.mybir as mybir
from concourse.bass_utils import run_bass_kernel_spmd

F32 = mybir.dt.float32
BF16 = mybir.dt.bfloat16
ALU = mybir.AluOpType
AF = mybir.ActivationFunctionType

D = 1024
SEQ = 8192
CTX = 256
NT = 66
OWN = 2048
NOT = 16
NE = 32
DE = 256
COL_BETA = 3072
COL_A = COL_BETA + 16
COL_Z = COL_A + 16
EPS = 1e-6
ARENA = 53000


SAME_ENGINE_WAITS = os.environ.get('SAMEENG', '1') == '1'


class Tk:
    __slots__ = ("name", "w", "rd", "excl")

    def __init__(self, name="", excl=False):
        self.name = name
        self.w = None
        self.rd = []
        self.excl = excl


class Op:
    __slots__ = ("eng", "fn", "deps", "used", "sem", "val", "dma", "key", "idx", "inc")


class Sched:
    ENGS = ("pe", "act", "dve", "pool", "sp")

    def __init__(self, nc):
        self.nc = nc
        self.ops = {e: [] for e in self.ENGS}
        self.all = []
        self.dma_since_barrier = []

    def op(self, eng, fn, reads=(), writes=(), dma=0, key=None, extra=(), inc=16):
        o = Op()
        o.eng = eng; o.fn = fn; o.used = False; o.sem = None; o.val = None
        o.dma = dma; o.key = key; o.idx = len(self.all); o.inc = inc
        deps = set(extra)
        reads = list(reads); writes = list(writes)
        for r in list(reads):
            if r.excl and r not in writes:
                writes.append(r)
        for r in reads:
            if r.w is not None:
                deps.add(r.w)
        for w in writes:
            if w.w is not None:
                deps.add(w.w)
            for x in w.rd:
                deps.add(x)
        for r in reads:
            r.rd.append(o)
        for w in writes:
            w.w = o
            w.rd = []
        o.deps = [d for d in deps if d is not o and not (d.eng == "pe" and eng == "pe" and not d.dma and not dma)
                  and not (SAME_ENGINE_WAITS is False and d.eng == eng and eng in ("act", "dve", "pool") and not d.dma and not dma)]
        for d in o.deps:
            d.used = True
        if dma:
            assert key is not None
            self.dma_since_barrier.append(o)
        self.ops[eng].append(o)
        self.all.append(o)
        return o

    def barrier(self, skip_cc=False):
        last = []
        for e in self.ENGS:
            for x in reversed(self.ops[e]):
                if x.fn is None:
                    break
                if skip_cc and x.dma and x.inc == 1:
                    continue
                last.append(x)
                break
        dmas = [x for x in self.dma_since_barrier if not (skip_cc and x.inc == 1)]
        self.dma_since_barrier = [x for x in self.dma_since_barrier if (skip_cc and x.inc == 1)]
        for e in self.ENGS:
            self.op(e, None, extra=[x for x in last if x.eng != e] + dmas)

    def emit(self, block, sems):
        nc = self.nc
        sems = list(sems)
        eng_sem = {e: sems.pop() for e in ("pe", "act", "dve", "pool")}
        keysem = {}
        cnt = {e: 0 for e in eng_sem}
        kcnt = {}
        for o in self.all:
            if o.dma:
                if o.key not in keysem:
                    keysem[o.key] = sems.pop()
                    kcnt[o.key] = 0
                kcnt[o.key] += o.inc * o.dma
                o.sem = keysem[o.key]; o.val = kcnt[o.key]
            elif o.used:
                assert o.fn is not None
                cnt[o.eng] += 1
                o.sem = eng_sem[o.eng]; o.val = cnt[o.eng]
        engobj = {"pe": nc.tensor, "act": nc.scalar, "dve": nc.vector, "pool": nc.gpsimd, "sp": nc.sync}
        deco = {"pe": block.tensor, "act": block.scalar, "dve": block.vector, "pool": block.gpsimd, "sp": block.sync}

        def run(ename):
            def body(e):
                known = {}
                for o in self.ops[ename]:
                    for d in sorted(o.deps, key=lambda x: x.idx):
                        sid = id(d.sem)
                        if known.get(sid, 0) >= d.val:
                            continue
                        e.wait_ge(d.sem, d.val)
                        known[sid] = d.val
                    if o.fn is None:
                        continue
                    if o.dma:
                        n = [0]

                        def sig(inst, o=o, n=n):
                            if o.inc == 16:
                                inst.then_inc(o.sem, 16)
                            else:
                                inst.then_inc(o.sem)
                            n[0] += 1
                            return inst
                        o.fn(e, sig)
                        assert n[0] == o.dma, (n[0], o.dma)
                    else:
                        inst = o.fn(e)
                        if o.used:
                            inst.then_inc(o.sem, 1)
            return body
        for ename in self.ENGS:
            deco[ename](run(ename))


class Arena:
    def __init__(self, ap_f32, nf32, base=0):
        self.ap = ap_f32
        self.n = base + nf32
        self.off = base
        self.hi = 0

    def mark(self):
        return self.off

    def release(self, m):
        self.off = m

    def alloc(self, free_shape, dtype=F32):
        n = int(np.prod(free_shape))
        nf = n if dtype == F32 else (n + 1) // 2
        nf = (nf + 1) // 2 * 2
        assert self.off + nf <= self.n, ("arena overflow", self.off, nf, self.n)
        v = self.ap[:, self.off:self.off + nf]
        self.off += nf
        self.hi = max(self.hi, self.off)
        if dtype != F32:
            v = v.bitcast(dtype)[:, 0:n]
        else:
            v = v[:, 0:n]
        if len(free_shape) == 2:
            v = v.rearrange("p (a b) -> p a b", a=free_shape[0])
        elif len(free_shape) == 3:
            v = v.rearrange("p (a b c) -> p a b c", a=free_shape[0], b=free_shape[1])
        return v


class _Stop(Exception):
    pass


def build_program(debug=False, stage=99):
    KCUT = int(os.environ.get('KCUT', '0')); NTL = int(os.environ.get('NTL', str(NT)))
    nc = bass.Bass("TRN2", target_bir_lowering=False)

    def din(name, shape, dt=F32):
        return nc.dram_tensor(name, list(shape), dt, kind="ExternalInput")

    xb = din("xb", [SEQ, D]); ctxb = din("ctxb", [CTX, D]); xo = din("xo", [OWN, D])
    cvT = din("cvT", [128, 8, 2]); ada_w = din("ada_w", [D, 6 * D]); ada_b = din("ada_b", [1, 6 * D])
    gcols = din("gcols", [128, 8, 2])
    w_qkv = din("w_qkv", [D, 768]); w_ba = din("w_ba", [D, 8])
    gsc66 = din("gsc66", [128, 2, NT, 4])
    conv_c = din("conv_c", [128, 6, 5]); gsc = din("gsc", [128, 8]); gng = din("gng", [128, 1])
    dcm_d = din("dcm", [9, 128, 128], BF16)
    ident_d = din("ident", [128, 128]); tri_d = din("tri", [4, 128, 128]); neg_d = din("negm", [4, 128, 128])
    w_rest = din("w_rest", [D, 5120]); sgu_wT = din("sgu_wT", [8, 128, 128]); sgu_bb = din("sgu_bb", [128, 8, 128])
    lng_c = din("lng_c", [128, 8]); lnb_bc = din("lnb_bc", [128, D])
    w_a = din("w_a", [D, D]); w_b = din("w_b", [D, D]); w_out = din("w_out", [D, D])
    rw = din("rw", [D, 36]); rb = din("rb", [128, 36])
    ew1 = din("ew1", [NE, D, DE]); ew3 = din("ew3", [NE, D, DE]); ew2 = din("ew2", [NE, DE, D])
    fng_bc = din("fng_bc", [128, D]); qmask_d = din("qmask", [128, 4])
    y = nc.dram_tensor("y", [OWN, D], F32, kind="ExternalOutput")
    snd = [nc.dram_tensor(f"snd{j}", [2 * 128, 1024], F32) for j in range(4)]
    rcv = [nc.dram_tensor(f"rcv{j}", [4 * 2 * 128, 1024], F32) for j in range(4)]
    x1d = nc.dram_tensor("x1d", [OWN, D], F32)
    oacc_d = nc.dram_tensor("oacc_d", [128, 128, 128], BF16)
    dbg = None
    if debug:
        dbg = nc.dram_tensor("dbg", [128, 4096], F32, kind="ExternalOutput")
        dbgb = nc.dram_tensor("dbgb", [768, SEQ], BF16, kind="ExternalOutput")

    es = contextlib.ExitStack()
    with es:
        arena_t = es.enter_context(nc.sbuf_tensor("arena", [128, ARENA], F32))
        psb = [es.enter_context(nc.psum_tensor(f"psb{i}", [128, 512], F32)) for i in range(8)]
        sems = [es.enter_context(nc.semaphore(f"s{i}")) for i in range(100)]
        block = es.enter_context(nc.Block())
        A = Arena(arena_t, ARENA)
        S = Sched(nc)

        dump_ops = []

        def dump(ap, col0, ncols, reads=()):
            o_ = S.op("sp", lambda e, sig: sig(e.dma_start(out=dbg[:, col0:col0 + ncols], in_=ap)), reads=list(reads), dma=1, key="dbg")
            dump_ops.append(o_)

        def cut(k, dumps):
            if stage == k:
                S.barrier()
                for d_ in dumps():
                    dump(*d_)
                raise _Stop()

        def dumpb(ap, row0, nrows, col0, ncols, reads=(), extra=()):
            o_ = S.op("sp", lambda e, sig: sig(e.dma_start(out=dbgb[row0:row0 + nrows, col0:col0 + ncols], in_=ap)), reads=list(reads), extra=list(extra), dma=1, key="dbg")
            dump_ops.append(o_)

        def author():
            nonlocal A
            class Pool:
                def __init__(self, items):
                    self.items = items
                    self.i = 0

                def get(self):
                    it = self.items[self.i % len(self.items)]
                    self.i += 1
                    return it
            p128 = Pool([(psb[b][:, 0:128], Tk(f"p128_{b}", excl=True)) for b in range(4)])
            p512 = Pool([(psb[b][:, :], Tk(f"p512_{b}", excl=True)) for b in range(4, 8)])

            def bfv(ap):
                n = ap.shape[-1]
                return ap.bitcast(BF16)[:, 0:n]

            def load(dst, src, tk, key):
                return S.op("sp", lambda e, sig: sig(e.dma_start(out=dst, in_=src)), writes=[tk], dma=1, key=key)

            identf = A.alloc([128]); t_identf = Tk(); load(identf, ident_d[:, :], t_identf, "c0")
            identb = A.alloc([128], BF16); t_identb = Tk()
            S.op("pool", lambda e: e.tensor_copy(out=identb, in_=identf), reads=[t_identf], writes=[t_identb])
            trif = A.alloc([4, 128]); t_trif = Tk(); load(trif, tri_d.ap().rearrange("a p q -> p a q"), t_trif, "c1")
            negf = A.alloc([4, 128]); t_negf = Tk(); load(negf, neg_d.ap().rearrange("a p q -> p a q"), t_negf, "c2")
            negb = A.alloc([4, 128], BF16); t_negb = Tk()
            S.op("pool", lambda e: e.tensor_copy(out=negb, in_=negf), reads=[t_negf], writes=[t_negb])
            dcm = A.alloc([9, 128], BF16); t_dcm = Tk(); load(dcm, dcm_d.ap().rearrange("a p q -> p a q"), t_dcm, "c2b")
            onesf = A.alloc([128]); t_onesf = Tk()
            S.op("pool", lambda e: e.memset(onesf, 1.0), writes=[t_onesf])
            Uf, Vf, Ub, Vb = (trif[:, i, :] for i in range(4))
            gscs = A.alloc([8]); t_gscs = Tk(); load(gscs, gsc[:, :], t_gscs, "c3")
            negA = A.alloc([4]); t_negA = Tk()
            S.op("act", lambda e: e.activation(out=negA, in_=gscs[:, 0:4], func=AF.Exp), reads=[t_gscs], writes=[t_negA])
            S.op("dve", lambda e: e.tensor_scalar(out=negA, in0=negA, scalar1=-1.0, scalar2=None, op0=ALU.mult), reads=[t_negA], writes=[t_negA])
            dtb = gscs[:, 4:8]
            convc = A.alloc([6, 5]); t_convc = Tk(); load(convc, conv_c[:, :, :], t_convc, "c4")
            gngs = A.alloc([1]); t_gngs = Tk(); load(gngs, gng[:, :], t_gngs, "c5")
            qmask = A.alloc([4]); t_qmask = Tk(); load(qmask, qmask_d[:, :], t_qmask, "c6")
            gcol = A.alloc([8, 2]); t_gcol = Tk(); load(gcol, gcols[:, :, :], t_gcol, "c7")
            modc = A.alloc([6, 8]); t_modc = Tk("modc")
            gmix = A.alloc([D]); t_gmix = Tk("gmix")
            gffn = A.alloc([D]); t_gffn = Tk("gffn")
            GT = A.alloc([NOT, 32]); t_GT = [Tk() for _ in range(NOT)]

            m0 = A.mark()
            scT = A.alloc([8, 2]); t_scT = Tk(); load(scT, cvT[:, :, :], t_scT, "c8")
            S.op("act", lambda e: e.activation(out=scT, in_=scT, func=AF.Silu), reads=[t_scT], writes=[t_scT])
            modrow = A.alloc([6 * D]); t_modrow = Tk("modrow")
            modrow_c = A.alloc([6 * D]); t_modrow_c = Tk("modrow_c")
            adb = A.alloc([6 * D]); t_adb = Tk()
            S.op("sp", lambda e, sig: (sig(e.dma_start(out=adb[0:1, :], in_=ada_b[:, :])), sig(e.dma_start(out=adb[1:2, :], in_=ada_b[:, :]))),
                 writes=[t_adb], dma=2, key="c9")
            adw = [A.alloc([8, 512]) for _ in range(2)]; t_adw = [Tk(), Tk()]
            ada_v = ada_w.ap().rearrange("(k p) n -> p k n", p=128)
            for nb in range(12):
                sl = nb % 2
                S.op("sp", lambda e, sig, nb=nb, sl=sl: (sig(e.dma_start(out=adw[sl][:, 0:4, :], in_=ada_v[:, 0:4, nb * 512:(nb + 1) * 512])),
                                                        sig(e.dma_start(out=adw[sl][:, 4:8, :], in_=ada_v[:, 4:8, nb * 512:(nb + 1) * 512]))),
                     writes=[t_adw[sl]], dma=2, key=f"adw{sl}")
                pp, tp = p512.get()

                def f(e, sl=sl, pp=pp):
                    for k in range(8):
                        i = e.matmul(pp[0:2, :], lhsT=scT[:, k, :], rhs=adw[sl][:, k, :], start=(k == 0), stop=(k == 7))
                    return i
                S.op("pe", f, reads=[t_scT, t_adw[sl]], writes=[tp])
                S.op("dve", lambda e, nb=nb, pp=pp: e.tensor_tensor(out=modrow[0:2, nb * 512:(nb + 1) * 512], in0=pp[0:2, :], in1=adb[0:2, nb * 512:(nb + 1) * 512], op=ALU.add),
                     reads=[tp, t_adb], writes=[t_modrow])
            S.op("sp", lambda e, sig: sig(e.dma_start(out=modrow_c[0:1, :], in_=modrow[1:2, :])), reads=[t_modrow], writes=[t_modrow_c], dma=1, key="c10")
            pp, tp = p128.get()
            vecs = [(modrow, 0), (modrow, 1), (modrow_c, 0), (modrow_c, 1), (modrow, 3), (modrow, 4)]

            def f(e, pp=pp):
                for vi, (row, m) in enumerate(vecs):
                    for k in range(8):
                        i = e.matmul(pp[:, vi * 8 + k:vi * 8 + k + 1], lhsT=row[0:1, m * D + k * 128:m * D + (k + 1) * 128], rhs=onesf[0:1, 0:1], start=True, stop=True)
                return i
            S.op("pe", f, reads=[t_modrow, t_modrow_c, t_onesf], writes=[tp])
            S.op("dve", lambda e, pp=pp: e.tensor_copy(out=modc.rearrange("p a b -> p (a b)"), in_=pp[:, 0:48]), reads=[tp], writes=[t_modc])
            for vi, gi in ((1, 0), (3, 0), (5, 1)):
                S.op("dve", lambda e, vi=vi, gi=gi: e.scalar_tensor_tensor(out=modc[:, vi, :], in0=modc[:, vi, :], scalar=1.0, in1=gcol[:, :, gi], op0=ALU.add, op1=ALU.mult),
                     reads=[t_modc, t_gcol], writes=[t_modc])
            for dst, tdst, m in ((gmix, t_gmix, 2), (gffn, t_gffn, 5)):
                for h in range(2):
                    pp, tp = p512.get()
                    S.op("pe", lambda e, pp=pp, m=m, h=h: e.matmul(pp, lhsT=onesf[0:1, :], rhs=modrow[0:1, m * D + h * 512:m * D + (h + 1) * 512], start=True, stop=True),
                         reads=[t_modrow, t_onesf], writes=[tp])
                    S.op("act", lambda e, pp=pp, dst=dst, h=h: e.copy(out=dst[:, h * 512:(h + 1) * 512], in_=pp), reads=[tp], writes=[tdst])
            S.barrier()
            A.release(m0)
            if stage == 0:
                dump(modc.rearrange("p a b -> p (a b)"), 0, 48); dump(gmix[:, 0:256], 64, 256); dump(gffn[:, 0:256], 320, 256)
                raise _Stop()

            mA = A.mark()
            SC = A.alloc([NT, 28]); t_SC = [Tk("SC")] * NT
            QN = A.alloc([NT, 2, 128], BF16); KN = A.alloc([NT, 2, 128], BF16); VV = A.alloc([NT, 2, 128], BF16)
            t_QKV = [[Tk() for _ in range(6)] for t in range(NT)]
            mA1 = A.mark()
            wst = A.alloc([8, 776]); t_wst = Tk()
            wq = A.alloc([8, 896], BF16); t_wq = Tk()
            S.op("sp", lambda e, sig: (sig(e.dma_start(out=wst[:, :, 0:768], in_=w_qkv.ap().rearrange("(k p) n -> p k n", p=128))),
                                       sig(e.dma_start(out=wst[:, :, 768:776], in_=w_ba.ap().rearrange("(k p) n -> p k n", p=128)))),
                 writes=[t_wst], dma=2, key="wst")
            S.op("pool", lambda e: e.tensor_copy(out=wq[:, :, 0:776], in_=wst), reads=[t_wst], writes=[t_wq])
            xt_A = [A.alloc([D]) for _ in range(3)]; t_xt_A = [Tk() for _ in range(3)]
            junk_A = A.alloc([D]); t_junk_A = Tk()
            ssq_A = [A.alloc([1]) for _ in range(3)]; t_ssq_A = [Tk() for _ in range(3)]
            xs_A = [A.alloc([8, 128], BF16) for _ in range(2)]; t_xs_A = [Tk(), Tk()]
            hxT = [A.alloc([8, 128], BF16) for _ in range(2)]; t_hxT = [Tk(), Tk()]; t_hxTb = [Tk(), Tk()]
            PRE = [A.alloc([6, 132]) for _ in range(4)]; t_PRE = [Tk() for _ in range(4)]
            CV = A.alloc([6, 128]); t_CVc = [Tk() for _ in range(6)]
            SQ = A.alloc([6, 128], BF16); t_SQ = Tk()
            sm = [A.alloc([40]) for _ in range(2)]; t_sm = [Tk(), Tk()]
            nr = [A.alloc([8]) for _ in range(2)]; t_nr = [Tk(), Tk()]

            wst_flat = wst.rearrange("p a b -> p (a b)")
            BA = wst_flat[:, 0:NT * 8].rearrange("p (a b) -> p a b", b=8); t_BA = Tk("BA")
            g66 = A.alloc([2, NT, 4]); t_g66 = Tk(); load(g66, gsc66[:, :, :, :], t_g66, "c3b")
            Mx = [wst_flat[:, 1024 + i_ * 512:1024 + i_ * 512 + NT * 4].rearrange("p (a b) -> p a b", b=4) for i_ in range(4)]; t_Mx = [Tk() for _ in range(4)]

            def rows_of(t):
                if t < 2:
                    return ctxb[t * 128:(t + 1) * 128, :]
                return xb[(t - 2) * 128:(t - 1) * 128, :]

            def front(t, part):
                s3 = t % 3; s2 = t % 2
                if part == "b":
                    return front_b(t, s3, s2)
                isctx = t < 2
                shc = modc[:, 2 if isctx else 0, :]; scc = modc[:, 3 if isctx else 1, :]
                S.op("sp", lambda e, sig: sig(e.dma_start(out=xt_A[s3], in_=rows_of(t))), writes=[t_xt_A[s3]], dma=1, key=f"xt_A{s3}")
                S.op("act", lambda e: e.activation(out=junk_A, in_=xt_A[s3], func=AF.Square, accum_out=ssq_A[s3]), reads=[t_xt_A[s3]], writes=[t_ssq_A[s3]])
                S.op("act", lambda e: e.activation(out=ssq_A[s3], in_=ssq_A[s3], func=AF.Sqrt, scale=1.0 / D, bias=EPS), reads=[t_ssq_A[s3]], writes=[t_ssq_A[s3]])
                S.op("dve", lambda e: e.reciprocal(out=ssq_A[s3], in_=ssq_A[s3]), reads=[t_ssq_A[s3]], writes=[t_ssq_A[s3]])
                S.op("pool", lambda e: e.tensor_scalar(out=xs_A[s2].rearrange("p a b -> p (a b)"), in0=xt_A[s3], scalar1=ssq_A[s3], scalar2=1.0, op0=ALU.mult, op1=ALU.mult),
                     reads=[t_xt_A[s3], t_ssq_A[s3]], writes=[t_xs_A[s2]])
                pp, tp = p512.get()
                ppb = pp.bitcast(BF16)

                def tr(e):
                    for k in range(8):
                        i = e.transpose(out=ppb[:, k * 128:(k + 1) * 128], in_=xs_A[s2][:, k, :], identity=identb)
                    return i
                S.op("pe", tr, reads=[t_xs_A[s2], t_identb], writes=[tp])

                def ev_d(e):
                    for k in range(8):
                        i = e.tensor_scalar(out=hxT[s2][:, k, :], in0=ppb[:, k * 128:(k + 1) * 128], scalar1=scc[:, k:k + 1], scalar2=shc[:, k:k + 1], op0=ALU.mult, op1=ALU.add)
                    return i
                t_h2 = t_hxTb[s2]
                S.op("dve", ev_d, reads=[tp, t_modc], writes=[t_hxT[s2], t_h2])
                return

            def front_b(t, s3, s2):
                s3 = t % 4
                pA, tA = p512.get()
                pB, tB = p512.get()

                def pjA(e):
                    for ch in range(4):
                        for k in range(8):
                            i = e.matmul(pA[:, ch * 128:(ch + 1) * 128], lhsT=wq[:, k, ch * 128:(ch + 1) * 128], rhs=hxT[s2][:, k, :], start=(k == 0), stop=(k == 7))
                    return i

                def pjB(e):
                    for ch in range(4, 6):
                        for k in range(8):
                            i = e.matmul(pB[:, (ch - 4) * 128:(ch - 3) * 128], lhsT=wq[:, k, ch * 128:(ch + 1) * 128], rhs=hxT[s2][:, k, :], start=(k == 0), stop=(k == 7))
                    for k in range(8):
                        i = e.matmul(pB[:, 256:264], lhsT=hxT[s2][:, k, :], rhs=wq[:, k, 768:776], start=(k == 0), stop=(k == 7))
                    return i
                S.op("pe", pjA, reads=[t_wq, t_hxT[s2]], writes=[tA])
                S.op("pe", pjB, reads=[t_wq, t_hxT[s2]], writes=[tB])
                pba = pB[:, 256:264]; tba = tB
                S.op("dve", lambda e: e.tensor_copy(out=PRE[s3][:, 0:4, 2:130], in_=pA.rearrange("p (a b) -> p a b", a=4)), reads=[tA], writes=[t_PRE[s3]])
                S.op("dve", lambda e: e.tensor_copy(out=PRE[s3][:, 4:6, 2:130], in_=pB[:, 0:256].rearrange("p (a b) -> p a b", a=2)), reads=[tB], writes=[t_PRE[s3]])
                if KCUT == 2:
                    return
                first = t in (0, 2); last = t in (1, NT - 1)
                if first:
                    S.op("pool", lambda e: e.memset(PRE[s3][:, :, 0:2], 0.0), writes=[t_PRE[s3]])
                else:
                    sp_ = (t - 1) % 4
                    S.op("pool", lambda e: e.tensor_copy(out=PRE[sp_][:, :, 130:132], in_=PRE[s3][:, :, 2:4]), reads=[t_PRE[s3]], writes=[t_PRE[sp_]])
                if last:
                    S.op("pool", lambda e: e.memset(PRE[s3][:, :, 130:132], 0.0), writes=[t_PRE[s3]])
                else:
                    sn = (t + 1) % 4
                    S.op("pool", lambda e: e.tensor_copy(out=PRE[sn][:, :, 0:2], in_=PRE[s3][:, :, 128:130]), reads=[t_PRE[s3]], writes=[t_PRE[sn]])
                S.op("dve", lambda e: e.tensor_copy(out=BA[:, t, :], in_=pba), reads=[tba], writes=[t_BA])

            def lag(t):
                s3 = t % 4; s2 = t % 2
                for j in range(5):
                    for ch in range(6):
                        tcv = t_CVc[ch]

                        def cvj(e, ch=ch, j=j):
                            if j == 0:
                                return e.tensor_scalar(out=CV[:, ch, :], in0=PRE[s3][:, ch, 0:128], scalar1=convc[:, ch, 0:1], scalar2=None, op0=ALU.mult)
                            return e.scalar_tensor_tensor(out=CV[:, ch, :], in0=PRE[s3][:, ch, j:j + 128], scalar=convc[:, ch, j:j + 1], in1=CV[:, ch, :], op0=ALU.mult, op1=ALU.add)
                        S.op("dve", cvj, reads=[t_PRE[s3], t_convc] + ([tcv] if j else []), writes=[tcv])
                S.op("act", lambda e: e.activation(out=SQ, in_=CV, func=AF.Silu), reads=t_CVc, writes=[t_SQ])
                pT, tT = p512.get()
                pTb = pT.bitcast(BF16)

                def trs(e):
                    for ch in range(6):
                        i = e.transpose(out=pTb[:, ch * 128:(ch + 1) * 128], in_=SQ[:, ch, :], identity=identb)
                    return i
                S.op("pe", trs, reads=[t_SQ, t_identb], writes=[tT])
                pts = [(pTb[:, ch * 128:(ch + 1) * 128], tT) for ch in range(6)]
                n = nr[s2]; tn = t_nr[s2]
                for ch in range(4):
                    S.op("act", lambda e, ch=ch: e.activation(out=junk_A[:, 0:128], in_=pts[ch][0], func=AF.Square, accum_out=n[:, ch:ch + 1]),
                         reads=[pts[ch][1]], writes=[tn])
                S.op("act", lambda e: e.activation(out=n[:, 0:4], in_=n[:, 0:4], func=AF.Sqrt, bias=EPS), reads=[tn], writes=[tn])
                S.op("dve", lambda e: e.reciprocal(out=n[:, 0:4], in_=n[:, 0:4]), reads=[tn], writes=[tn])
                S.op("dve", lambda e: e.tensor_scalar(out=n[:, 0:2], in0=n[:, 0:2], scalar1=128.0 ** -0.5, scalar2=None, op0=ALU.mult), reads=[tn], writes=[tn])
                dsts = [QN[:, t, 0, :], QN[:, t, 1, :], KN[:, t, 0, :], KN[:, t, 1, :], VV[:, t, 0, :], VV[:, t, 1, :]]
                for ch in range(6):
                    if ch < 4:
                        S.op("dve", lambda e, ch=ch: e.tensor_scalar(out=dsts[ch], in0=pts[ch][0], scalar1=n[:, ch:ch + 1], scalar2=None, op0=ALU.mult),
                             reads=[pts[ch][1], tn], writes=[t_QKV[t][ch]])
                    else:
                        S.op("act", lambda e, ch=ch: e.copy(out=dsts[ch], in_=pts[ch][0]), reads=[pts[ch][1]], writes=[t_QKV[t][ch]])

            front(0, "a")
            for i in range(NTL + 2):
                if i + 1 < NTL:
                    front(i + 1, "a")
                if i < NTL:
                    front(i, "b")
                if i >= 2 and KCUT in (0, 5):
                    lag(i - 2)
            tS = t_SC[0]
            SCk = lambda k: SC[:, :, k * 4:(k + 1) * 4]
            M0, M1, M2, M3 = Mx; tM0, tM1, tM2, tM3 = t_Mx
            S.op("act", lambda e: e.activation(out=g66[:, 0, :, :], in_=g66[:, 0, :, :], func=AF.Exp), reads=[t_g66], writes=[t_g66])
            S.op("act", lambda e: e.activation(out=SCk(3), in_=BA[:, :, 0:4], func=AF.Sigmoid), reads=[t_BA], writes=[tS])
            S.op("dve", lambda e: e.tensor_tensor(out=M0, in0=BA[:, :, 4:8], in1=g66[:, 1, :, :], op=ALU.add), reads=[t_BA, t_g66], writes=[tM0])
            S.op("dve", lambda e: e.tensor_scalar(out=M1, in0=M0, scalar1=-1.0, scalar2=None, op0=ALU.mult), reads=[tM0], writes=[tM1])
            S.op("dve", lambda e: e.tensor_tensor(out=M1, in0=M1, in1=M0, op=ALU.min), reads=[tM0, tM1], writes=[tM1])
            S.op("act", lambda e: e.activation(out=M1, in_=M1, func=AF.Exp), reads=[tM1], writes=[tM1])
            S.op("act", lambda e: e.activation(out=M1, in_=M1, func=AF.Ln, bias=1.0), reads=[tM1], writes=[tM1])
            S.op("dve", lambda e: e.tensor_scalar(out=M2, in0=M0, scalar1=0.0, scalar2=None, op0=ALU.max), reads=[tM0], writes=[tM2])
            S.op("dve", lambda e: e.tensor_tensor(out=M2, in0=M2, in1=M1, op=ALU.add), reads=[tM1, tM2], writes=[tM2])
            S.op("dve", lambda e: e.scalar_tensor_tensor(out=SCk(6), in0=M2, scalar=-1.0, in1=g66[:, 0, :, :], op0=ALU.mult, op1=ALU.mult), reads=[tM2, t_g66], writes=[tS])
            pcA, tcA = p512.get(); pcB, tcB = p512.get()

            def cums(e):
                e.matmul(pcA[:, 0:2 * NT], lhsT=Uf, rhs=SC[:, :, 24:26], start=True, stop=True)
                return e.matmul(pcA[:, 2 * NT:4 * NT], lhsT=Ub, rhs=SC[:, :, 26:28], start=True, stop=True)
            S.op("pe", cums, reads=[tS, t_trif], writes=[tcA])
            S.op("pe", lambda e: e.matmul(pcB[:, 0:4 * NT], lhsT=onesf, rhs=SC[:, :, 24:28], start=True, stop=True), reads=[tS, t_onesf], writes=[tcB])
            Gf_ps = pcA[:, 0:2 * NT].rearrange("p (a b) -> p a b", b=2); Gb_ps = pcA[:, 2 * NT:4 * NT].rearrange("p (a b) -> p a b", b=2)
            Gt_ps = pcB[:, 0:4 * NT].rearrange("p (a b) -> p a b", b=4)
            S.op("act", lambda e: e.activation(out=SC[:, :, 16:18], in_=Gf_ps, func=AF.Exp), reads=[tcA], writes=[tS])
            S.op("act", lambda e: e.activation(out=SC[:, :, 18:20], in_=Gb_ps, func=AF.Exp), reads=[tcA], writes=[tS])
            S.op("act", lambda e: e.activation(out=SCk(5), in_=Gt_ps, func=AF.Exp), reads=[tcB], writes=[tS])
            S.op("dve", lambda e: e.tensor_copy(out=M3[:, :, 0:2], in_=Gf_ps), reads=[tcA], writes=[tM3])
            S.op("dve", lambda e: e.tensor_copy(out=M3[:, :, 2:4], in_=Gb_ps), reads=[tcA], writes=[tM3])
            S.op("dve", lambda e: e.tensor_tensor(out=M3, in0=Gt_ps, in1=M3, op=ALU.subtract), reads=[tcB, tM3], writes=[tM3])
            S.op("act", lambda e: e.activation(out=SCk(2), in_=M3, func=AF.Exp), reads=[tM3], writes=[tS])
            S.op("dve", lambda e: e.tensor_scalar(out=SCk(0), in0=SCk(3), scalar1=-1.0, scalar2=None, op0=ALU.mult), reads=[tS], writes=[tS])
            S.op("dve", lambda e: e.tensor_tensor(out=SCk(1), in0=SCk(3), in1=SCk(4), op=ALU.mult), reads=[tS], writes=[tS])
            S.barrier()
            A.release(mA1)
            if stage == 1:
                for i_, t_ in enumerate((0, 1, 2, 3, 33, 65)):
                    dump(SC[:, t_, :], i_ * 32, 28)
                    dumpb(QN[:, t_, :, :].rearrange("p a b -> p (a b)"), 0, 128, i_ * 768, 256)
                    dumpb(KN[:, t_, :, :].rearrange("p a b -> p (a b)"), 0, 128, i_ * 768 + 256, 256)
                    dumpb(VV[:, t_, :, :].rearrange("p a b -> p (a b)"), 0, 128, i_ * 768 + 512, 256)
                raise _Stop()

            p128_small = p128
            p128 = Pool([(psb[b_][:, 0:128], Tk(f"p128x_{b_}", excl=True)) for b_ in range(8)])
            chains = [(hl, d) for hl in range(2) for d in range(2)]
            cb = {}
            for c in chains:
                Sf = A.alloc([128]); tSf = Tk("S"); Sbb = A.alloc([128], BF16); tSbb = Tk("Sb")
                S.op("pool", lambda e, Sf=Sf: e.memset(Sf, 0.0), writes=[tSf])
                S.op("pool", lambda e, Sbb=Sbb: e.memset(Sbb, 0.0), writes=[tSbb])
                for st_ in range(2):
                    b = {}
                    for nm in ("knT", "qnT", "qdT", "X0", "XT0", "Xb", "XbT", "X0s", "XT0s", "X1s", "XT1s", "P0", "P1", "PT0", "PT1", "No", "NoT", "Wb", "Vb", "kbg", "kd", "vb", "nwT", "vnew", "QKm", "qd", "E", "ET", "ob"):
                        b[nm] = A.alloc([128], BF16); b["t_" + nm] = Tk(nm)
                    for nm in ("gV", "gU"):
                        b[nm] = A.alloc([128]); b["t_" + nm] = Tk(nm)
                    b["S"] = Sf; b["t_S"] = tSf; b["Sb"] = Sbb; b["t_Sb"] = tSbb
                    cb[(c, st_)] = b
            t_OACC = [[Tk() for _ in range(2)] for _ in range(64)]
            ost = [A.alloc([128], BF16) for _ in range(4)]; t_ost = [Tk() for _ in range(4)]
            ost_cnt = [0]
            ofin = [A.alloc([128]) for _ in range(2)]; t_ofin = [Tk(), Tk()]
            onb = [A.alloc([128], BF16) for _ in range(2)]; t_onb = [Tk(), Tk()]
            onT = [A.alloc([128], BF16) for _ in range(4)]; t_onT = [Tk() for _ in range(4)]
            fsm = [A.alloc([4]) for _ in range(2)]; t_fsm = [Tk(), Tk()]
            junk_B = A.alloc([128])
            visited = set()
            snd_ops = []
            qops = [[] for _ in range(4)]
            ccs = []
            fin_cnt = [0]
            evq = [0]

            def evac_copy(dst, src, rd, wr, scale=None):
                evq[0] += 1
                if evq[0] % 2 == 0:
                    if scale is None:
                        return S.op("act", lambda e: e.copy(out=dst, in_=src), reads=rd, writes=wr)
                    return S.op("act", lambda e: e.activation(out=dst, in_=src, func=AF.Copy, scale=scale), reads=rd, writes=wr)
                if scale is None:
                    return S.op("dve", lambda e: e.tensor_copy(out=dst, in_=src), reads=rd, writes=wr)
                return S.op("dve", lambda e: e.tensor_scalar(out=dst, in0=src, scalar1=scale, scalar2=None, op0=ALU.mult), reads=rd, writes=wr)

            def chain_step(c, t, st_):
                hl, d = c
                b = cb[(c, st_)]
                col = d * 2 + hl
                latent = t >= 2
                kn = KN[:, t, hl, :]; qn = QN[:, t, hl, :]; vv = VV[:, t, hl, :]
                tqq = t_QKV[t][hl]; tqk = t_QKV[t][2 + hl]; tqv = t_QKV[t][4 + hl]; tsc = t_SC[t]

                def scol(kind):
                    return SC[:, t, kind * 4 + col:kind * 4 + col + 1]
                U_, V_ = (Uf, Vf) if d == 0 else (Ub, Vb)
                negs = negb[:, 0 if d == 0 else 2, :]; negi = negb[:, 1 if d == 0 else 3, :]
                pk, tpk = p128.get()
                S.op("pe", lambda e: e.transpose(out=bfv(pk), in_=kn, identity=identb), reads=[tqk, t_identb], writes=[tpk])
                evac_copy(b["knT"], bfv(pk), [tpk], [b["t_knT"]])
                S.op("act", lambda e: e.activation(out=b["gV"], in_=V_, func=AF.Copy, scale=scol(6)), reads=[t_trif, tsc], writes=[b["t_gV"]])
                S.op("act", lambda e: e.activation(out=b["gU"], in_=U_, func=AF.Copy, scale=scol(6)), reads=[t_trif, tsc], writes=[b["t_gU"]])
                S.op("dve", lambda e: e.tensor_scalar(out=b["kbg"], in0=kn, scalar1=scol(1), scalar2=None, op0=ALU.mult), reads=[tqk, tsc], writes=[b["t_kbg"]])
                S.op("pool", lambda e: e.tensor_scalar(out=b["kd"], in0=kn, scalar1=scol(2), scalar2=1.0, op0=ALU.mult, op1=ALU.mult), reads=[tqk, tsc], writes=[b["t_kd"]])
                S.op("pool", lambda e: e.tensor_scalar(out=b["vb"], in0=vv, scalar1=scol(3), scalar2=1.0, op0=ALU.mult, op1=ALU.mult), reads=[tqv, tsc], writes=[b["t_vb"]])
                yield
                pd, tpd = p128.get()

                def dm(e):
                    e.matmul(pd, lhsT=identb, rhs=negs, start=True, stop=False)
                    return e.matmul(pd, lhsT=U_, rhs=b["gV"], start=False, stop=True)
                S.op("pe", dm, reads=[t_identb, t_negb, t_trif, b["t_gV"]], writes=[tpd])
                S.op("act", lambda e: e.activation(out=b["E"], in_=pd, func=AF.Exp), reads=[tpd], writes=[b["t_E"]])
                pkk, tpkk = p128.get()
                S.op("pe", lambda e: e.matmul(pkk, lhsT=b["knT"], rhs=b["knT"], start=True, stop=True), reads=[b["t_knT"]], writes=[tpkk])
                S.op("dve", lambda e: e.scalar_tensor_tensor(out=b["X0"], in0=pkk, scalar=scol(0), in1=b["E"], op0=ALU.mult, op1=ALU.mult),
                     reads=[tpkk, tsc, b["t_E"]], writes=[b["t_X0"]])
                if latent:
                    pdt, tpdt = p128.get()

                    def dmt(e):
                        e.matmul(pdt, lhsT=identb, rhs=negi, start=True, stop=False)
                        return e.matmul(pdt, lhsT=V_, rhs=b["gU"], start=False, stop=True)
                    S.op("pe", dmt, reads=[t_identb, t_negb, t_trif, b["t_gU"]], writes=[tpdt])
                    S.op("act", lambda e: e.activation(out=b["ET"], in_=pdt, func=AF.Exp), reads=[tpdt], writes=[b["t_ET"]])
                    pq_, tpq = p128.get()
                    S.op("pe", lambda e: e.transpose(out=bfv(pq_), in_=qn, identity=identb), reads=[tqq, t_identb], writes=[tpq])
                    evac_copy(b["qnT"], bfv(pq_), [tpq], [b["t_qnT"]])
                    S.op("pool", lambda e: e.tensor_scalar(out=b["qd"], in0=qn, scalar1=scol(4), scalar2=1.0, op0=ALU.mult, op1=ALU.mult), reads=[tqq, tsc], writes=[b["t_qd"]])
                yield
                px, tpx = p128.get()
                S.op("pe", lambda e: e.transpose(out=bfv(px), in_=b["X0"], identity=identb), reads=[b["t_X0"], t_identb], writes=[tpx])
                evac_copy(b["XT0"], bfv(px), [tpx], [b["t_XT0"]])
                if latent:
                    pqd, tpqd = p128.get()
                    S.op("pe", lambda e: e.transpose(out=bfv(pqd), in_=b["qd"], identity=identb), reads=[b["t_qd"], t_identb], writes=[tpqd])
                    evac_copy(b["qdT"], bfv(pqd), [tpqd], [b["t_qdT"]])
                    pqk, tpqk = p128.get()
                    S.op("pe", lambda e: e.matmul(pqk, lhsT=b["knT"], rhs=b["qnT"], start=True, stop=True), reads=[b["t_knT"], b["t_qnT"]], writes=[tpqk])
                    S.op("dve", lambda e: e.tensor_tensor(out=b["QKm"], in0=pqk, in1=b["ET"], op=ALU.mult), reads=[tpqk, b["t_ET"]], writes=[b["t_QKm"]])
                yield
                mk = lambda nm: (b[nm], b["t_" + nm])
                Xb, tXb = mk("Xb"); XbT, tXbT = mk("XbT")
                S.op("pool", lambda e: e.tensor_tensor(out=Xb, in0=b["X0"], in1=dcm[:, 0, :], op=ALU.mult), reads=[b["t_X0"], t_dcm], writes=[tXb])
                S.op("pool", lambda e: e.tensor_tensor(out=XbT, in0=b["XT0"], in1=dcm[:, 0, :], op=ALU.mult), reads=[b["t_XT0"], t_dcm], writes=[tXbT])
                S.op("pool", lambda e: e.tensor_tensor(out=b["P0"], in0=Xb, in1=identb, op=ALU.add), reads=[tXb, t_identb], writes=[b["t_P0"]])
                S.op("pool", lambda e: e.tensor_tensor(out=b["PT0"], in0=XbT, in1=identb, op=ALU.add), reads=[tXbT, t_identb], writes=[b["t_PT0"]])
                yield
                cur = 0
                cX, tcX, cXT, tcXT = Xb, tXb, XbT, tXbT
                for lev in range(2):
                    nX, tnX = mk(f"X{lev}s"); nXT, tnXT = mk(f"XT{lev}s")
                    p1, tp1 = p128.get()
                    S.op("pe", lambda e, p1=p1, cX=cX, cXT=cXT: e.matmul(p1, lhsT=cXT, rhs=cX, start=True, stop=True), reads=[tcX, tcXT], writes=[tp1])
                    evac_copy(nX, p1, [tp1], [tnX])
                    p2, tp2 = p128.get()
                    S.op("pe", lambda e, p2=p2, cX=cX, cXT=cXT: e.matmul(p2, lhsT=cX, rhs=cXT, start=True, stop=True), reads=[tcX, tcXT], writes=[tp2])
                    evac_copy(nXT, p2, [tp2], [tnXT])
                    yield
                    P = b[f"P{cur}"]; tP = b[f"t_P{cur}"]; nP = b[f"P{1 - cur}"]; tnP = b[f"t_P{1 - cur}"]
                    PT = b[f"PT{cur}"]; tPT = b[f"t_PT{cur}"]; nPT = b[f"PT{1 - cur}"]; tnPT = b[f"t_PT{1 - cur}"]
                    p3, tp3 = p128.get()
                    S.op("pe", lambda e, p3=p3, nXT=nXT, P=P: e.matmul(p3, lhsT=nXT, rhs=P, start=True, stop=True), reads=[tnXT, tP], writes=[tp3])
                    S.op("dve", lambda e, p3=p3, P=P, nP=nP: e.tensor_tensor(out=nP, in0=p3, in1=P, op=ALU.add), reads=[tp3, tP], writes=[tnP])
                    p4, tp4 = p128.get()
                    S.op("pe", lambda e, p4=p4, nX=nX, PT=PT: e.matmul(p4, lhsT=nX, rhs=PT, start=True, stop=True), reads=[tnX, tPT], writes=[tp4])
                    S.op("dve", lambda e, p4=p4, PT=PT, nPT=nPT: e.tensor_tensor(out=nPT, in0=p4, in1=PT, op=ALU.add), reads=[tp4, tPT], writes=[tnPT])
                    cur = 1 - cur
                    cX, tcX, cXT, tcXT = nX, tnX, nXT, tnXT
                    yield
                for li in range(4):
                    mi = 1 + 2 * li + (0 if d == 0 else 1)
                    miT = 1 + 2 * li + (1 if d == 0 else 0)
                    No, tNo = mk("No"); NoT, tNoT = mk("NoT")
                    P = b[f"P{cur}"]; tP = b[f"t_P{cur}"]; nP = b[f"P{1 - cur}"]; tnP = b[f"t_P{1 - cur}"]
                    PT = b[f"PT{cur}"]; tPT = b[f"t_PT{cur}"]; nPT = b[f"PT{1 - cur}"]; tnPT = b[f"t_PT{1 - cur}"]
                    S.op("pool", lambda e, mi=mi, No=No: e.tensor_tensor(out=No, in0=b["X0"], in1=dcm[:, mi, :], op=ALU.mult), reads=[b["t_X0"], t_dcm], writes=[tNo])
                    pw_, tpw_ = p128.get()
                    S.op("pe", lambda e, pw_=pw_, No=No, PT=PT: e.matmul(pw_, lhsT=No, rhs=PT, start=True, stop=True), reads=[tNo, tPT], writes=[tpw_])
                    Wb, tWb = mk("Wb")
                    evac_copy(Wb, pw_, [tpw_], [tWb])
                    if li < 3:
                        S.op("pool", lambda e, miT=miT, NoT=NoT: e.tensor_tensor(out=NoT, in0=b["XT0"], in1=dcm[:, miT, :], op=ALU.mult), reads=[b["t_XT0"], t_dcm], writes=[tNoT])
                        pv_, tpv_ = p128.get()
                        S.op("pe", lambda e, pv_=pv_, NoT=NoT, P=P: e.matmul(pv_, lhsT=NoT, rhs=P, start=True, stop=True), reads=[tNoT, tP], writes=[tpv_])
                        Vb_, tVb_ = mk("Vb")
                        evac_copy(Vb_, pv_, [tpv_], [tVb_])
                    yield
                    p5, tp5 = p128.get()
                    S.op("pe", lambda e, p5=p5, P=P, Wb=Wb: e.matmul(p5, lhsT=P, rhs=Wb, start=True, stop=True), reads=[tP, tWb], writes=[tp5])
                    S.op("dve", lambda e, p5=p5, PT=PT, nPT=nPT: e.tensor_tensor(out=nPT, in0=p5, in1=PT, op=ALU.add), reads=[tp5, tPT], writes=[tnPT])
                    if li < 3:
                        p6, tp6 = p128.get()
                        S.op("pe", lambda e, p6=p6, PT=PT, Vb_=Vb_: e.matmul(p6, lhsT=PT, rhs=Vb_, start=True, stop=True), reads=[tPT, tVb_], writes=[tp6])
                        S.op("dve", lambda e, p6=p6, P=P, nP=nP: e.tensor_tensor(out=nP, in0=p6, in1=P, op=ALU.add), reads=[tp6, tP], writes=[tnP])
                    cur = 1 - cur
                    yield
                TT = b[f"PT{cur}"]; tTT = b[f"t_PT{cur}"]
                pw, tpw = p128.get()
                S.op("pe", lambda e: e.matmul(pw, lhsT=b["kbg"], rhs=TT, start=True, stop=True), reads=[b["t_kbg"], tTT], writes=[tpw])
                evac_copy(b["nwT"], pw, [tpw], [b["t_nwT"]], scale=-1.0)
                yield
                pv, tpv = p128.get()

                def vn(e):
                    e.matmul(pv, lhsT=TT, rhs=b["vb"], start=True, stop=False)
                    return e.matmul(pv, lhsT=b["nwT"], rhs=b["Sb"], start=False, stop=True)
                S.op("pe", vn, reads=[tTT, b["t_vb"], b["t_nwT"], b["t_Sb"]], writes=[tpv])
                evac_copy(b["vnew"], pv, [tpv], [b["t_vnew"]])
                yield
                if latent:
                    lt = t - 2
                    po, tpo = p128.get()

                    def om(e):
                        e.matmul(po, lhsT=b["qdT"], rhs=b["Sb"], start=True, stop=False)
                        return e.matmul(po, lhsT=b["QKm"], rhs=b["vnew"], start=False, stop=True)
                    S.op("pe", om, reads=[b["t_qdT"], b["t_Sb"], b["t_QKm"], b["t_vnew"]], writes=[tpo])
                    if (lt, hl) not in visited:
                        visited.add((lt, hl))
                        k4o = ost_cnt[0] % 4; ost_cnt[0] += 1
                        evac_copy(ost[k4o], po, [tpo], [t_ost[k4o]])
                        S.op("pool", lambda e, sig: sig(e.dma_start(out=oacc_d[lt * 2 + hl], in_=ost[k4o])), reads=[t_ost[k4o]], writes=[t_OACC[lt][hl]], dma=1, key=f"oaw{k4o}")
                    else:
                        k2 = fin_cnt[0] % 2; k4 = fin_cnt[0] % 4
                        fin_cnt[0] += 1
                        of = ofin[k2]; tof = t_ofin[k2]; fs = fsm[k2]; tfs = t_fsm[k2]
                        S.op("sp", lambda e, sig: sig(e.dma_start(out=b["ob"], in_=oacc_d[lt * 2 + hl])), reads=[t_OACC[lt][hl]], writes=[b["t_ob"]], dma=1, key=f"oar{hl}{d}{st_}")
                        S.op("dve", lambda e: e.tensor_tensor(out=of, in0=po, in1=b["ob"], op=ALU.add), reads=[tpo, b["t_ob"]], writes=[tof])
                        S.op("act", lambda e: e.activation(out=junk_B, in_=of, func=AF.Square, accum_out=fs[:, 0:1]), reads=[tof], writes=[tfs])
                        S.op("act", lambda e: e.activation(out=fs[:, 0:1], in_=fs[:, 0:1], func=AF.Sqrt, scale=1.0 / 128, bias=EPS), reads=[tfs], writes=[tfs])
                        S.op("dve", lambda e: e.reciprocal(out=fs[:, 0:1], in_=fs[:, 0:1]), reads=[tfs], writes=[tfs])
                        S.op("dve", lambda e: e.tensor_scalar(out=onb[k2], in0=of, scalar1=fs[:, 0:1], scalar2=None, op0=ALU.mult), reads=[tof, tfs], writes=[t_onb[k2]])
                        pt_, tpt = p128.get()
                        S.op("pe", lambda e: e.transpose(out=bfv(pt_), in_=onb[k2], identity=identb), reads=[t_onb[k2], t_identb], writes=[tpt])
                        evac_copy(onT[k4], bfv(pt_), [tpt], [t_onT[k4]])
                        o_ = S.op("pool", lambda e, sig: sig(e.dma_start(out=snd[lt // 16][hl * 128:(hl + 1) * 128, (lt % 16) * 64:(lt % 16 + 1) * 64], in_=onT[k4].bitcast(F32))),
                                  reads=[t_onT[k4]], dma=1, key=f"snd{k4}")
                        snd_ops.append(o_)
                        qops[lt // 16].append(o_)
                        if len(qops[lt // 16]) == 32:
                            jq = lt // 16
                            ccs.append(S.op("pool", lambda e, sig: sig(e.collective_compute("AllGather", ALU.bypass, replica_groups=[[0, 1, 2, 3], [4, 5, 6, 7]],
                                                                                       ins=[snd[jq].ap().opt()], outs=[rcv[jq].ap().opt()])),
                                            extra=qops[jq], dma=1, key=f"cc{jq}", inc=1))
                ps_, tps = p128.get()
                S.op("pe", lambda e: e.matmul(ps_, lhsT=b["kd"], rhs=b["vnew"], start=True, stop=True), reads=[b["t_kd"], b["t_vnew"]], writes=[tps])
                S.op("dve", lambda e: e.scalar_tensor_tensor(out=b["S"], in0=b["S"], scalar=scol(5), in1=ps_, op0=ALU.mult, op1=ALU.add),
                     reads=[b["t_S"], tsc, tps], writes=[b["t_S"]])
                S.op("act", lambda e: e.copy(out=b["Sb"], in_=b["S"]), reads=[b["t_S"]], writes=[b["t_Sb"]])
                yield

            def bwd_tile(i):
                return 1 - i if i < 2 else NT + 1 - i

            HALF = 11
            alive = []
            nstart = 0
            tick = 0
            while nstart < NT or alive:
                if nstart < NT and tick % HALF == 0:
                    i = nstart; nstart += 1
                    for c in chains:
                        t = i if c[1] == 0 else bwd_tile(i)
                        alive.append([chain_step(c, t, i % 2), 0])
                nxt = []
                for g in alive:
                    try:
                        next(g[0]); g[1] += 1
                        assert g[1] < 2 * HALF, "chain step too long for the 2-deep pipeline"
                        nxt.append(g)
                    except StopIteration:
                        pass
                alive = nxt
                tick += 1
            if stage == 2:
                for i_ in range(4):
                    o_ = S.op("sp", lambda e, sig, i_=i_: sig(e.dma_start(out=dbgb[0:256, i_ * 2048:(i_ + 1) * 2048].bitcast(F32), in_=snd[i_][:, :])), extra=snd_ops, dma=1, key="dbg")
                    dump_ops.append(o_)
                for i_, c_ in enumerate(chains):
                    dump(cb[c_]["S"], i_ * 128, 128, reads=[cb[c_]["t_S"]])
                raise _Stop()
            p128 = p128_small
            assert len(ccs) == 4
            S.barrier(skip_cc=True)
            A.release(mA)
            if stage == 3:
                raise _Stop()

            hxo = A.alloc([8, OWN], BF16); t_hxo = [Tk() for _ in range(NOT)]
            markH = A.mark()
            offS = A.mark()
            SZ = A.alloc([8, OWN], BF16); t_SZ = [[Tk() for _ in range(4)] for _ in range(8)]
            GU = A.alloc([8, OWN], BF16); t_GU = [[Tk() for _ in range(4)] for _ in range(8)]
            wstg = [A.alloc([8, 512]) for _ in range(2)]; t_wstg = [Tk(), Tk()]
            wbf = [A.alloc([8, 512], BF16) for _ in range(2)]; t_wbf = [Tk(), Tk()]
            mC1 = A.mark()
            xt_C = [A.alloc([D]) for _ in range(2)]; t_xt_C = [Tk(), Tk()]
            junk_C = A.alloc([D]); t_junk_C = Tk()
            ssq_C = [A.alloc([1]) for _ in range(2)]; t_ssq_C = [Tk(), Tk()]
            xs_C = [A.alloc([8, 128], BF16) for _ in range(2)]; t_xs_C = [Tk(), Tk()]
            for t in range(NOT):
                s2 = t % 2
                S.op("sp", lambda e, sig, t=t, s2=s2: sig(e.dma_start(out=xt_C[s2], in_=xo[t * 128:(t + 1) * 128, :])), writes=[t_xt_C[s2]], dma=1, key=f"cxt{s2}")
                S.op("act", lambda e, s2=s2: e.activation(out=junk_C, in_=xt_C[s2], func=AF.Square, accum_out=ssq_C[s2]), reads=[t_xt_C[s2]], writes=[t_ssq_C[s2]])
                S.op("act", lambda e, s2=s2: e.activation(out=ssq_C[s2], in_=ssq_C[s2], func=AF.Sqrt, scale=1.0 / D, bias=EPS), reads=[t_ssq_C[s2]], writes=[t_ssq_C[s2]])
                S.op("dve", lambda e, s2=s2: e.reciprocal(out=ssq_C[s2], in_=ssq_C[s2]), reads=[t_ssq_C[s2]], writes=[t_ssq_C[s2]])
                S.op("pool", lambda e, s2=s2: e.tensor_scalar(out=xs_C[s2].rearrange("p a b -> p (a b)"), in0=xt_C[s2], scalar1=ssq_C[s2], scalar2=1.0, op0=ALU.mult, op1=ALU.mult),
                     reads=[t_xt_C[s2], t_ssq_C[s2]], writes=[t_xs_C[s2]])
                pp, tp = p512.get()
                ppb = pp.bitcast(BF16)

                def tr(e, s2=s2, ppb=ppb):
                    for k in range(8):
                        i = e.transpose(out=ppb[:, k * 128:(k + 1) * 128], in_=xs_C[s2][:, k, :], identity=identb)
                    return i
                S.op("pe", tr, reads=[t_xs_C[s2], t_identb], writes=[tp])

                def ev_d(e, t=t, ppb=ppb):
                    for k in range(8):
                        i = e.tensor_scalar(out=hxo[:, k, t * 128:(t + 1) * 128], in0=ppb[:, k * 128:(k + 1) * 128], scalar1=modc[:, 1, k:k + 1], scalar2=modc[:, 0, k:k + 1], op0=ALU.mult, op1=ALU.add)
                    return i
                S.op("dve", ev_d, reads=[tp, t_modc], writes=[t_hxo[t]])
            S.barrier()
            A.release(mC1)
            wcnt = [0]

            def stream_w(src_ap_cols, ncols):
                s = wcnt[0] % 2
                wcnt[0] += 1
                v = src_ap_cols.rearrange("(k p) n -> p k n", p=128)
                S.op("sp", lambda e, sig: (sig(e.dma_start(out=wstg[s][:, 0:4, 0:ncols], in_=v[:, 0:4, :])), sig(e.dma_start(out=wstg[s][:, 4:8, 0:ncols], in_=v[:, 4:8, :]))),
                     writes=[t_wstg[s]], dma=2, key=f"wstg{s}")
                S.op("pool", lambda e: e.tensor_copy(out=wbf[s][:, :, 0:ncols], in_=wstg[s][:, :, 0:ncols]), reads=[t_wstg[s]], writes=[t_wbf[s]])
                return wbf[s], t_wbf[s]
            for cbk in range(4):
                wv, twv = stream_w(w_rest[:, cbk * 512:(cbk + 1) * 512], 512)
                dst, tdst, fn = (SZ, t_SZ, AF.Silu) if cbk < 2 else (GU, t_GU, AF.Gelu)
                for cc_ in range(4):
                    chn = (cbk % 2) * 4 + cc_
                    for tb in range(4):
                        pp, tp = p512.get()

                        def f(e, pp=pp, wv=wv, cc_=cc_, tb=tb):
                            for k in range(8):
                                i = e.matmul(pp, lhsT=wv[:, k, cc_ * 128:(cc_ + 1) * 128], rhs=hxo[:, k, tb * 512:(tb + 1) * 512], start=(k == 0), stop=(k == 7))
                            return i
                        S.op("pe", f, reads=[twv] + t_hxo[tb * 4:(tb + 1) * 4], writes=[tp])
                        S.op("act", lambda e, pp=pp, dst=dst, chn=chn, tb=tb, fn=fn: e.activation(out=dst[:, chn, tb * 512:(tb + 1) * 512], in_=pp, func=fn),
                             reads=[tp], writes=[tdst[chn][tb]])
            def cut4(k):
                if stage == 4 and int(os.environ.get("CUT4", "0")) == k:
                    S.barrier()
                    for i_, buf_ in enumerate((SZ, GU, hxo)):
                        for h_ in range(2):
                            dumpb(buf_[:, h_ * 4:(h_ + 1) * 4, :].rearrange("p a b -> p (a b)"), i_ * 256 + h_ * 128, 128, 0, SEQ)
                    raise _Stop()
            cut4(1)
            mV = A.mark()
            junk_V = A.alloc([D])
            wcnt[0] = 0
            wvh = [stream_w(w_rest[:, 2048 + hh * 512:2048 + (hh + 1) * 512], 512) for hh in range(2)]
            swf = A.alloc([8, 128]); t_swf = Tk(); load(swf, sgu_wT.ap().rearrange("g q p -> q g p"), t_swf, "c11")
            swb = A.alloc([8, 128], BF16); t_swb = Tk()
            S.op("pool", lambda e: e.tensor_copy(out=swb, in_=swf), reads=[t_swf], writes=[t_swb])
            lnbb = A.alloc([D]); t_lnbb = Tk(); load(lnbb, lnb_bc[:, :], t_lnbb, "c12")
            sbb = A.alloc([8, 128]); t_sbb = Tk(); load(sbb, sgu_bb[:, :, :], t_sbb, "c13")
            lngc = A.alloc([8]); t_lngc = Tk(); load(lngc, lng_c[:, :], t_lngc, "c14")
            BIAS = A.alloc([8, 128]); t_BIAS = Tk()
            for g in range(8):
                pp, tp = p128.get()
                S.op("pe", lambda e, pp=pp, g=g: e.matmul(pp, lhsT=lnbb[:, g * 128:(g + 1) * 128], rhs=swf[:, g, :], start=True, stop=True), reads=[t_lnbb, t_swf], writes=[tp])
                S.op("dve", lambda e, pp=pp, g=g: e.tensor_tensor(out=BIAS[:, g, :], in0=pp, in1=sbb[:, g, :], op=ALU.add), reads=[tp, t_sbb], writes=[t_BIAS])
            gv = [A.alloc([D])] * 2; t_gv = [Tk()] * 2
            vnb = [A.alloc([D], BF16) for _ in range(2)]; t_vnb = [Tk(), Tk()]
            lst = [A.alloc([8]) for _ in range(2)]; t_lst = [Tk(), Tk()]
            mtmp = [A.alloc([128]) for _ in range(2)]; t_mtmp = [Tk(), Tk()]
            for t in range(NOT):
                s2 = t % 2
                for hh in range(2):
                    pp, tp = p512.get()

                    def f(e, pp=pp, hh=hh, t=t):
                        for k in range(8):
                            i = e.matmul(pp, lhsT=hxo[:, k, t * 128:(t + 1) * 128], rhs=wvh[hh][0][:, k, :], start=(k == 0), stop=(k == 7))
                        return i
                    S.op("pe", f, reads=[wvh[hh][1], t_hxo[t]], writes=[tp])
                    S.op("act", lambda e, pp=pp, hh=hh, s2=s2: e.activation(out=gv[s2][:, hh * 512:(hh + 1) * 512], in_=pp, func=AF.Gelu), reads=[tp], writes=[t_gv[s2]])
                ls = lst[s2]; tls = t_lst[s2]
                S.op("dve", lambda e, s2=s2, ls=ls: e.tensor_reduce(out=ls[:, 0:1], in_=gv[s2], axis=mybir.AxisListType.X, op=ALU.add), reads=[t_gv[s2]], writes=[tls])
                S.op("act", lambda e, s2=s2, ls=ls: e.activation(out=junk_V, in_=gv[s2], func=AF.Square, accum_out=ls[:, 1:2]), reads=[t_gv[s2]], writes=[tls])
                S.op("dve", lambda e, ls=ls: e.tensor_scalar(out=ls[:, 0:2], in0=ls[:, 0:2], scalar1=1.0 / D, scalar2=None, op0=ALU.mult), reads=[tls], writes=[tls])
                S.op("dve", lambda e, ls=ls: e.tensor_tensor(out=ls[:, 2:3], in0=ls[:, 0:1], in1=ls[:, 0:1], op=ALU.mult), reads=[tls], writes=[tls])
                S.op("dve", lambda e, ls=ls: e.tensor_tensor(out=ls[:, 2:3], in0=ls[:, 1:2], in1=ls[:, 2:3], op=ALU.subtract), reads=[tls], writes=[tls])
                S.op("act", lambda e, ls=ls: e.activation(out=ls[:, 2:3], in_=ls[:, 2:3], func=AF.Sqrt, bias=EPS), reads=[tls], writes=[tls])
                S.op("dve", lambda e, ls=ls: e.reciprocal(out=ls[:, 2:3], in_=ls[:, 2:3]), reads=[tls], writes=[tls])
                S.op("dve", lambda e, s2=s2, ls=ls: e.tensor_scalar(out=vnb[s2], in0=gv[s2], scalar1=ls[:, 0:1], scalar2=ls[:, 2:3], op0=ALU.subtract, op1=ALU.mult),
                     reads=[t_gv[s2], tls], writes=[t_vnb[s2]])
                for g in range(8):
                    pp, tp = p128.get()
                    S.op("pe", lambda e, pp=pp, g=g, s2=s2: e.matmul(pp, lhsT=vnb[s2][:, g * 128:(g + 1) * 128], rhs=swb[:, g, :], start=True, stop=True),
                         reads=[t_vnb[s2], t_swb], writes=[tp])
                    mt = mtmp[g % 2]; tmt = t_mtmp[g % 2]
                    S.op("dve", lambda e, pp=pp, g=g, mt=mt: e.scalar_tensor_tensor(out=mt, in0=pp, scalar=lngc[:, g:g + 1], in1=BIAS[:, g, :], op0=ALU.mult, op1=ALU.add),
                         reads=[tp, t_lngc, t_BIAS], writes=[tmt])
                    S.op("pool", lambda e, g=g, t=t, mt=mt: e.tensor_tensor(out=GU[:, g, t * 128:(t + 1) * 128], in0=GU[:, g, t * 128:(t + 1) * 128], in1=mt, op=ALU.mult),
                         reads=[tmt, t_GU[g][t // 4]], writes=[t_GU[g][t // 4]])
            S.barrier()
            A.release(mV)
            cut4(2)
            mY = A.mark()
            rq = [A.alloc([4, 512], BF16) for _ in range(2)]; t_rq = [Tk(), Tk()]
            acc = [A.alloc([512]) for _ in range(2)]; t_acc = [Tk(), Tk()]
            cntr = 0
            for hp in range(4):
                for hl in range(2):
                    chn = hp * 2 + hl
                    for tb in range(4):
                        s = cntr % 2; cntr += 1
                        S.op("sp", lambda e, sig, s=s, chn=chn, tb=tb: tuple(sig(e.dma_start(out=rq[s][:, j, :].bitcast(F32), in_=rcv[j][chn * 128:(chn + 1) * 128, tb * 256:(tb + 1) * 256])) for j in range(4)),
                             extra=ccs, writes=[t_rq[s]], dma=4, key=f"rq{s}")
                        for j in range(4):
                            def f(e, s=s, j=j):
                                if j == 0:
                                    return e.tensor_scalar(out=acc[s], in0=rq[s][:, 0, :], scalar1=qmask[:, 0:1], scalar2=None, op0=ALU.mult)
                                return e.scalar_tensor_tensor(out=acc[s], in0=rq[s][:, j, :], scalar=qmask[:, j:j + 1], in1=acc[s], op0=ALU.mult, op1=ALU.add)
                            S.op("dve", f, reads=[t_rq[s], t_qmask, t_acc[s]] if j else [t_rq[s], t_qmask], writes=[t_acc[s]])
                        S.op("dve", lambda e, s=s, chn=chn, tb=tb: e.scalar_tensor_tensor(out=SZ[:, chn, tb * 512:(tb + 1) * 512], in0=acc[s], scalar=gngs[:, 0:1], in1=SZ[:, chn, tb * 512:(tb + 1) * 512], op0=ALU.mult, op1=ALU.mult),
                             reads=[t_acc[s], t_gngs, t_SZ[chn][tb]], writes=[t_SZ[chn][tb]])
            S.barrier()
            A.release(mY)
            cut4(3)
            S.barrier()
            mM = A.mark()
            MG = A.alloc([8, OWN], BF16); t_MG = [[Tk() for _ in range(4)] for _ in range(8)]
            sga = [A.alloc([512], BF16) for _ in range(2)]; t_sga = [Tk(), Tk()]
            sgb = [A.alloc([512], BF16) for _ in range(2)]; t_sgb = [Tk(), Tk()]
            m1 = [A.alloc([512]) for _ in range(2)]; t_m1 = [Tk(), Tk()]
            m2 = [A.alloc([512]) for _ in range(2)]; t_m2 = [Tk(), Tk()]
            sub = [(wstg[i][:, :, j * 128:(j + 1) * 128], wbf[i][:, :, j * 128:(j + 1) * 128], Tk(), Tk()) for i in range(2) for j in range(4)]
            subc = [0]

            def stream_small(src_cols):
                stg, bfw, tst, tbf = sub[subc[0] % 8]
                subc[0] += 1
                v = src_cols.rearrange("(k p) n -> p k n", p=128)
                S.op("sp", lambda e, sig: sig(e.dma_start(out=stg, in_=v)), writes=[tst], dma=1, key=f"sub{(subc[0] - 1) % 8}")
                S.op("pool", lambda e: e.tensor_copy(out=bfw, in_=stg), reads=[tst], writes=[tbf])
                return bfw, tbf
            cntr = 0
            for dc in range(8):
                ws = [stream_small(w_a[:, dc * 128:(dc + 1) * 128]), stream_small(w_b[:, dc * 128:(dc + 1) * 128]),
                      stream_small(w_rest[:, 3072 + dc * 128:3072 + (dc + 1) * 128]), stream_small(w_rest[:, 4096 + dc * 128:4096 + (dc + 1) * 128])]
                for tb in range(4):
                    s = cntr % 2; cntr += 1
                    outs = []
                    for wi, (src, tsrc) in enumerate(((SZ, t_SZ), (GU, t_GU), (hxo, None), (hxo, None))):
                        wv_, tw_ = ws[wi]
                        pp, tp = p512.get()

                        def f(e, pp=pp, wv_=wv_, src=src, tb=tb):
                            for k in range(8):
                                i = e.matmul(pp, lhsT=wv_[:, k, :], rhs=src[:, k, tb * 512:(tb + 1) * 512], start=(k == 0), stop=(k == 7))
                            return i
                        rds = [tw_] + ([tsrc[k][tb] for k in range(8)] if tsrc is not None else t_hxo[tb * 4:(tb + 1) * 4])
                        S.op("pe", f, reads=rds, writes=[tp])
                        outs.append((pp, tp))
                    S.op("act", lambda e, s=s, pp=outs[2][0]: e.activation(out=sga[s], in_=pp, func=AF.Sigmoid), reads=[outs[2][1]], writes=[t_sga[s]])
                    S.op("act", lambda e, s=s, pp=outs[3][0]: e.activation(out=sgb[s], in_=pp, func=AF.Sigmoid), reads=[outs[3][1]], writes=[t_sgb[s]])
                    S.op("dve", lambda e, s=s, pp=outs[0][0]: e.tensor_tensor(out=m1[s], in0=pp, in1=sga[s], op=ALU.mult), reads=[outs[0][1], t_sga[s]], writes=[t_m1[s]])
                    S.op("dve", lambda e, s=s, pp=outs[1][0]: e.tensor_tensor(out=m2[s], in0=pp, in1=sgb[s], op=ALU.mult), reads=[outs[1][1], t_sgb[s]], writes=[t_m2[s]])
                    S.op("pool", lambda e, s=s, dc=dc, tb=tb: e.tensor_tensor(out=MG[:, dc, tb * 512:(tb + 1) * 512], in0=m1[s], in1=m2[s], op=ALU.add),
                         reads=[t_m1[s], t_m2[s]], writes=[t_MG[dc][tb]])
            S.barrier()
            if stage == 4:
                for i_, (buf_, tk_) in enumerate(((SZ, t_SZ), (GU, t_GU), (MG, t_MG))):
                    for h_ in range(2):
                        dumpb(buf_[:, h_ * 4:(h_ + 1) * 4, :].rearrange("p a b -> p (a b)"), i_ * 256 + h_ * 128, 128, 0, SEQ)
                raise _Stop()
            hx2 = hxo; t_hx2 = [Tk() for _ in range(NOT)]
            Amain = A
            A = Arena(arena_t, 16384, base=offS)
            wo_b = A.alloc([8, D], BF16); t_wo_b = Tk()
            for hh in range(2):
                sl = hh
                S.op("sp", lambda e, sig, hh=hh, sl=sl: (sig(e.dma_start(out=wstg[sl][:, 0:4, :], in_=w_out.ap().rearrange("(k p) n -> p k n", p=128)[:, 0:4, hh * 512:(hh + 1) * 512])),
                                                        sig(e.dma_start(out=wstg[sl][:, 4:8, :], in_=w_out.ap().rearrange("(k p) n -> p k n", p=128)[:, 4:8, hh * 512:(hh + 1) * 512]))),
                     writes=[t_wstg[sl]], dma=2, key=f"wstg{sl}")
                for k in range(8):
                    S.op("pool", lambda e, k=k, hh=hh, sl=sl: e.tensor_tensor(out=wo_b[:, k, hh * 512:(hh + 1) * 512], in0=wstg[sl][:, k, :], in1=gmix[:, hh * 512:(hh + 1) * 512], op=ALU.mult),
                         reads=[t_wstg[sl], t_gmix], writes=[t_wo_b])
            rwf = A.alloc([8, 36]); t_rwf = Tk(); load(rwf, rw.ap().rearrange("(k p) n -> p k n", p=128), t_rwf, "c15")
            rbb = A.alloc([36]); t_rbb = Tk(); load(rbb, rb[:, :], t_rbb, "c16")
            x1 = [A.alloc([D]) for _ in range(2)]; t_x1 = [Tk(), Tk()]
            xt_D = [A.alloc([D]) for _ in range(2)]; t_xt_D = [Tk(), Tk()]
            junk_D = A.alloc([D]); t_junk_D = Tk()
            ssq_D = [A.alloc([1]) for _ in range(2)]; t_ssq_D = [Tk(), Tk()]
            xsf = [A.alloc([8, 128]) for _ in range(2)]; t_xsf = [Tk(), Tk()]
            hxf = [A.alloc([8, 128]) for _ in range(2)]; t_hxf = [Tk(), Tk()]
            rs_ = [A.alloc([64]) for _ in range(2)]; t_rs = [Tk(), Tk()]
            x1_ops = []
            for t in range(NOT):
                s2 = t % 2
                S.op("sp", lambda e, sig, t=t, s2=s2: sig(e.dma_start(out=xt_D[s2], in_=xo[t * 128:(t + 1) * 128, :])), writes=[t_xt_D[s2]], dma=1, key=f"dxt{s2}")
                for hh in range(2):
                    pp, tp = p512.get()

                    def f(e, pp=pp, hh=hh, t=t):
                        for k in range(8):
                            i = e.matmul(pp, lhsT=MG[:, k, t * 128:(t + 1) * 128], rhs=wo_b[:, k, hh * 512:(hh + 1) * 512], start=(k == 0), stop=(k == 7))
                        return i
                    S.op("pe", f, reads=[t_wo_b] + [t_MG[k][t // 4] for k in range(8)], writes=[tp])
                    S.op("dve", lambda e, pp=pp, hh=hh, s2=s2: e.tensor_tensor(out=x1[s2][:, hh * 512:(hh + 1) * 512], in0=pp, in1=xt_D[s2][:, hh * 512:(hh + 1) * 512], op=ALU.add),
                         reads=[tp, t_xt_D[s2]], writes=[t_x1[s2]])
                o_ = S.op("pool", lambda e, sig, t=t, s2=s2: sig(e.dma_start(out=x1d[t * 128:(t + 1) * 128, :], in_=x1[s2])), reads=[t_x1[s2]], dma=1, key=f"x1d{s2}")
                x1_ops.append(o_)
                S.op("act", lambda e, s2=s2: e.activation(out=junk_D, in_=x1[s2], func=AF.Square, accum_out=ssq_D[s2]), reads=[t_x1[s2]], writes=[t_ssq_D[s2]])
                S.op("act", lambda e, s2=s2: e.activation(out=ssq_D[s2], in_=ssq_D[s2], func=AF.Sqrt, scale=1.0 / D, bias=EPS), reads=[t_ssq_D[s2]], writes=[t_ssq_D[s2]])
                S.op("dve", lambda e, s2=s2: e.reciprocal(out=ssq_D[s2], in_=ssq_D[s2]), reads=[t_ssq_D[s2]], writes=[t_ssq_D[s2]])
                S.op("pool", lambda e, s2=s2: e.tensor_scalar(out=xsf[s2].rearrange("p a b -> p (a b)"), in0=x1[s2], scalar1=ssq_D[s2], scalar2=1.0, op0=ALU.mult, op1=ALU.mult),
                     reads=[t_x1[s2], t_ssq_D[s2]], writes=[t_xsf[s2]])
                for q4 in range(2):
                    pp, tp = p512.get()

                    def tr(e, pp=pp, q4=q4, s2=s2):
                        for k in range(4):
                            i = e.transpose(out=pp[:, k * 128:(k + 1) * 128], in_=xsf[s2][:, q4 * 4 + k, :], identity=identf)
                        return i
                    S.op("pe", tr, reads=[t_xsf[s2], t_identf], writes=[tp])

                    def ev(e, pp=pp, q4=q4, s2=s2, t=t):
                        for k in range(4):
                            kk = q4 * 4 + k
                            i = e.tensor_scalar(out=hxf[s2][:, kk, :], in0=pp[:, k * 128:(k + 1) * 128], scalar1=modc[:, 5, kk:kk + 1], scalar2=modc[:, 4, kk:kk + 1], op0=ALU.mult, op1=ALU.add)
                        return i
                    S.op("dve", ev, reads=[tp, t_modc], writes=[t_hxf[s2]])
                S.op("pool", lambda e, s2=s2, t=t: e.tensor_copy(out=hx2[:, :, t * 128:(t + 1) * 128], in_=hxf[s2]), reads=[t_hxf[s2]], writes=[t_hx2[t]])
                pr, tpr = p128.get()

                def rt(e, pr=pr, s2=s2):
                    for k in range(8):
                        i = e.matmul(pr[:, 0:36], lhsT=hxf[s2][:, k, :], rhs=rwf[:, k, :], start=(k == 0), stop=(k == 7))
                    return i
                S.op("pe", rt, reads=[t_hxf[s2], t_rwf], writes=[tpr])
                r = rs_[s2]; tr_ = t_rs[s2]
                S.op("dve", lambda e, pr=pr, r=r: e.tensor_tensor(out=r[:, 0:36], in0=pr[:, 0:36], in1=rbb, op=ALU.add), reads=[tpr, t_rbb], writes=[tr_])
                S.op("dve", lambda e, r=r: e.tensor_reduce(out=r[:, 40:41], in_=r[:, 0:4], axis=mybir.AxisListType.X, op=ALU.max), reads=[tr_], writes=[tr_])
                S.op("dve", lambda e, r=r: e.tensor_scalar(out=r[:, 36:40], in0=r[:, 0:4], scalar1=r[:, 40:41], scalar2=None, op0=ALU.is_ge), reads=[tr_], writes=[tr_])
                S.op("dve", lambda e, r=r: e.tensor_scalar(out=r[:, 60:64], in0=r[:, 0:4], scalar1=r[:, 40:41], scalar2=None, op0=ALU.subtract), reads=[tr_], writes=[tr_])
                S.op("act", lambda e, r=r: e.activation(out=r[:, 60:64], in_=r[:, 60:64], func=AF.Exp, accum_out=r[:, 41:42]), reads=[tr_], writes=[tr_])
                S.op("dve", lambda e, r=r: e.reciprocal(out=r[:, 41:42], in_=r[:, 41:42]), reads=[tr_], writes=[tr_])
                S.op("dve", lambda e, r=r: e.tensor_scalar(out=r[:, 42:50], in0=r[:, 4:12], scalar1=r[:, 36:37], scalar2=None, op0=ALU.mult), reads=[tr_], writes=[tr_])
                for g in range(1, 4):
                    S.op("dve", lambda e, r=r, g=g: e.scalar_tensor_tensor(out=r[:, 42:50], in0=r[:, 4 + 8 * g:12 + 8 * g], scalar=r[:, 36 + g:37 + g], in1=r[:, 42:50], op0=ALU.mult, op1=ALU.add),
                         reads=[tr_], writes=[tr_])
                S.op("dve", lambda e, r=r: e.tensor_reduce(out=r[:, 50:51], in_=r[:, 42:50], axis=mybir.AxisListType.X, op=ALU.max), reads=[tr_], writes=[tr_])
                S.op("dve", lambda e, r=r: e.tensor_scalar(out=r[:, 42:50], in0=r[:, 42:50], scalar1=r[:, 50:51], scalar2=None, op0=ALU.subtract), reads=[tr_], writes=[tr_])
                S.op("act", lambda e, r=r: e.activation(out=r[:, 42:50], in_=r[:, 42:50], func=AF.Exp), reads=[tr_], writes=[tr_])
                S.op("dve", lambda e, r=r: e.tensor_scalar(out=r[:, 52:60], in0=r[:, 42:50], scalar1=1.0, scalar2=None, op0=ALU.is_ge), reads=[tr_], writes=[tr_])
                S.op("dve", lambda e, r=r: e.scalar_tensor_tensor(out=r[:, 4:12], in0=r[:, 52:60], scalar=-2.0, in1=r[:, 42:50], op0=ALU.mult, op1=ALU.add), reads=[tr_], writes=[tr_])
                S.op("dve", lambda e, r=r: e.tensor_reduce(out=r[:, 51:52], in_=r[:, 4:12], axis=mybir.AxisListType.X, op=ALU.max), reads=[tr_], writes=[tr_])
                S.op("dve", lambda e, r=r: e.tensor_scalar(out=r[:, 12:20], in0=r[:, 4:12], scalar1=r[:, 51:52], scalar2=None, op0=ALU.is_ge), reads=[tr_], writes=[tr_])
                S.op("dve", lambda e, r=r: e.scalar_tensor_tensor(out=r[:, 20:28], in0=r[:, 12:20], scalar=r[:, 51:52], in1=r[:, 52:60], op0=ALU.mult, op1=ALU.add), reads=[tr_], writes=[tr_])
                S.op("dve", lambda e, r=r: e.tensor_scalar(out=r[:, 50:51], in0=r[:, 51:52], scalar1=1.0, scalar2=None, op0=ALU.add), reads=[tr_], writes=[tr_])
                S.op("dve", lambda e, r=r: e.reciprocal(out=r[:, 50:51], in_=r[:, 50:51]), reads=[tr_], writes=[tr_])
                S.op("dve", lambda e, r=r: e.tensor_tensor(out=r[:, 50:51], in0=r[:, 50:51], in1=r[:, 41:42], op=ALU.mult), reads=[tr_], writes=[tr_])
                S.op("dve", lambda e, r=r: e.tensor_scalar(out=r[:, 20:28], in0=r[:, 20:28], scalar1=r[:, 50:51], scalar2=None, op0=ALU.mult), reads=[tr_], writes=[tr_])
                for g in range(4):
                    S.op("dve", lambda e, r=r, g=g, t=t: e.tensor_scalar(out=GT[:, t, g * 8:(g + 1) * 8], in0=r[:, 20:28], scalar1=r[:, 36 + g:37 + g], scalar2=None, op0=ALU.mult),
                         reads=[tr_], writes=[t_GT[t]])
            S.barrier()
            A = Amain
            A.release(markH)
            if stage == 5:
                dump(GT.rearrange("p a b -> p (a b)"), 0, 512)
                for i_ in range(4):
                    o_ = S.op("sp", lambda e, sig, i_=i_: sig(e.dma_start(out=y[i_ * 512:(i_ + 1) * 512, :], in_=x1d[i_ * 512:(i_ + 1) * 512, :])), extra=x1_ops, dma=1, key="dbg")
                    dump_ops.append(o_)
                dumpb(hx2[:, 0:4, :].rearrange("p a b -> p (a b)"), 0, 128, 0, SEQ)
                raise _Stop()
            p512 = Pool([(psb[b_][:, :], Tk(f"p512x_{b_}", excl=True)) for b_ in range(8)])
            ACC = A.alloc([NOT, D]); t_ACC = [[Tk(), Tk()] for _ in range(NOT)]
            for t in range(NOT):
                S.op("pool", lambda e, t=t: e.memset(ACC[:, t, :], 0.0), writes=t_ACC[t])
            e1f = A.alloc([8, 512]); t_e1f = Tk()
            e2f = A.alloc([2, D]); t_e2f = Tk()
            e1b = [A.alloc([8, 512], BF16) for _ in range(2)]; t_e1b = [Tk(), Tk()]
            e2b = [A.alloc([2, D], BF16) for _ in range(2)]; t_e2b = [Tk(), Tk()]
            sil = [A.alloc([512], BF16) for _ in range(2)]; t_sil = [Tk(), Tk()]
            hid = [A.alloc([512], BF16) for _ in range(4)]; t_hid = [Tk() for _ in range(4)]
            hc = 0
            for ex in range(NE):
                s = ex % 2
                v1 = ew1[ex].rearrange("(k p) n -> p k n", p=128); v3 = ew3[ex].rearrange("(k p) n -> p k n", p=128)
                v2 = ew2[ex].rearrange("(k p) n -> p k n", p=128)
                S.op("sp", lambda e, sig, v1=v1, v3=v3: (sig(e.dma_start(out=e1f[:, :, 0:256], in_=v1)), sig(e.dma_start(out=e1f[:, :, 256:512], in_=v3))), writes=[t_e1f], dma=2, key="e1f")
                S.op("sp", lambda e, sig, v2=v2: sig(e.dma_start(out=e2f, in_=v2)), writes=[t_e2f], dma=1, key="e2f")
                S.op("pool", lambda e, s=s: e.tensor_copy(out=e1b[s], in_=e1f), reads=[t_e1f], writes=[t_e1b[s]])
                S.op("pool", lambda e, s=s: e.tensor_copy(out=e2b[s], in_=e2f), reads=[t_e2f], writes=[t_e2b[s]])
                for tb in range(4):
                    hs = []
                    for fc in range(2):
                        p1, tp1 = p512.get(); p3, tp3 = p512.get()

                        def f1(e, p1=p1, s=s, fc=fc, tb=tb):
                            for k in range(8):
                                i = e.matmul(p1, lhsT=e1b[s][:, k, fc * 128:(fc + 1) * 128], rhs=hx2[:, k, tb * 512:(tb + 1) * 512], start=(k == 0), stop=(k == 7))
                            return i

                        def f3(e, p3=p3, s=s, fc=fc, tb=tb):
                            for k in range(8):
                                i = e.matmul(p3, lhsT=e1b[s][:, k, 256 + fc * 128:256 + (fc + 1) * 128], rhs=hx2[:, k, tb * 512:(tb + 1) * 512], start=(k == 0), stop=(k == 7))
                            return i
                        S.op("pe", f1, reads=[t_e1b[s]] + t_hx2[tb * 4:(tb + 1) * 4], writes=[tp1])
                        S.op("pe", f3, reads=[t_e1b[s]] + t_hx2[tb * 4:(tb + 1) * 4], writes=[tp3])
                        ss = hc % 2; h4 = hc % 4; hc += 1
                        S.op("act", lambda e, p1=p1, ss=ss: e.activation(out=sil[ss], in_=p1, func=AF.Silu), reads=[tp1], writes=[t_sil[ss]])
                        S.op("dve", lambda e, p3=p3, ss=ss, h4=h4: e.tensor_tensor(out=hid[h4], in0=p3, in1=sil[ss], op=ALU.mult), reads=[tp3, t_sil[ss]], writes=[t_hid[h4]])
                        hs.append(h4)
                    for tt in range(4):
                        t = tb * 4 + tt
                        for hh in range(2):
                            po, tpo = p512.get()

                            def f2(e, po=po, s=s, tt=tt, hh=hh, hs=tuple(hs)):
                                for fc in range(2):
                                    i = e.matmul(po, lhsT=hid[hs[fc]][:, tt * 128:(tt + 1) * 128], rhs=e2b[s][:, fc, hh * 512:(hh + 1) * 512], start=(fc == 0), stop=(fc == 1))
                                return i
                            S.op("pe", f2, reads=[t_hid[hs[0]], t_hid[hs[1]], t_e2b[s]], writes=[tpo])
                            S.op("dve", lambda e, po=po, t=t, hh=hh, ex=ex: e.scalar_tensor_tensor(out=ACC[:, t, hh * 512:(hh + 1) * 512], in0=po, scalar=GT[:, t, ex:ex + 1], in1=ACC[:, t, hh * 512:(hh + 1) * 512], op0=ALU.mult, op1=ALU.add),
                                 reads=[tpo, t_ACC[t][hh]], writes=[t_ACC[t][hh]])
            fngb = A.alloc([D]); t_fngb = Tk(); load(fngb, fng_bc[:, :], t_fngb, "c17")
            x1r = [A.alloc([D]) for _ in range(2)]; t_x1r = [Tk(), Tk()]
            x2 = [A.alloc([D]) for _ in range(2)]; t_x2 = [Tk(), Tk()]
            junk_E = A.alloc([D]); t_junk_E = Tk()
            ssq_E = [A.alloc([1]) for _ in range(2)]; t_ssq_E = [Tk(), Tk()]
            outs_ = []
            for t in range(NOT):
                s2 = t % 2
                S.op("sp", lambda e, sig, t=t, s2=s2: sig(e.dma_start(out=x1r[s2], in_=x1d[t * 128:(t + 1) * 128, :])), extra=[x1_ops[t]], writes=[t_x1r[s2]], dma=1, key=f"x1r{s2}")
                S.op("dve", lambda e, t=t, s2=s2: e.tensor_tensor(out=x2[s2], in0=ACC[:, t, :], in1=gffn, op=ALU.mult), reads=t_ACC[t] + [t_gffn], writes=[t_x2[s2]])
                S.op("dve", lambda e, s2=s2: e.tensor_tensor(out=x2[s2], in0=x2[s2], in1=x1r[s2], op=ALU.add), reads=[t_x2[s2], t_x1r[s2]], writes=[t_x2[s2]])
                S.op("act", lambda e, s2=s2: e.activation(out=junk_E, in_=x2[s2], func=AF.Square, accum_out=ssq_E[s2]), reads=[t_x2[s2]], writes=[t_ssq_E[s2]])
                S.op("act", lambda e, s2=s2: e.activation(out=ssq_E[s2], in_=ssq_E[s2], func=AF.Sqrt, scale=1.0 / D, bias=EPS), reads=[t_ssq_E[s2]], writes=[t_ssq_E[s2]])
                S.op("dve", lambda e, s2=s2: e.reciprocal(out=ssq_E[s2], in_=ssq_E[s2]), reads=[t_ssq_E[s2]], writes=[t_ssq_E[s2]])
                S.op("dve", lambda e, s2=s2: e.scalar_tensor_tensor(out=x2[s2], in0=x2[s2], scalar=ssq_E[s2], in1=fngb, op0=ALU.mult, op1=ALU.mult), reads=[t_x2[s2], t_ssq_E[s2], t_fngb], writes=[t_x2[s2]])
                o_ = S.op("sp", lambda e, sig, t=t, s2=s2: sig(e.dma_start(out=y[t * 128:(t + 1) * 128, :], in_=x2[s2])), reads=[t_x2[s2]], dma=1, key=f"yo{s2}")
                outs_.append(o_)
            S.op("sp", None, extra=outs_)
        try:
            author()
        except _Stop:
            pass
        if dump_ops:
            S.op("sp", None, extra=dump_ops)
        S.emit(block, sems)
    return nc


def _prep(inputs):
    f = lambda a: np.ascontiguousarray(np.asarray(a, dtype=np.float32))
    x = f(inputs["x"]); c = f(inputs["c"]); ctx = f(inputs["ctx"]); c_ctx = f(inputs["c_ctx"])
    w_in = f(inputs["w_in"])[0]
    conv_w = f(inputs["conv_w"])[0]
    a_log = f(inputs["a_log"])[0]; dt_bias = f(inputs["dt_bias"])[0]
    idx = np.arange(128)
    tri = np.stack([(idx[:, None] <= idx[None, :]), (idx[:, None] > idx[None, :]), (idx[:, None] >= idx[None, :]), (idx[:, None] < idx[None, :])]).astype(np.float32)
    negm = np.stack([(idx[:, None] <= idx[None, :]), (idx[None, :] < idx[:, None]), (idx[:, None] >= idx[None, :]), (idx[None, :] > idx[:, None])]).astype(np.float32) * -100.0
    blk = lambda n: (idx[:, None] // n == idx[None, :] // n)
    dcm = [blk(8)]
    for n in (16, 32, 64, 128):
        low = blk(n) & ((idx[:, None] % n) >= n // 2) & ((idx[None, :] % n) < n // 2)
        dcm += [low, low.T]
    import ml_dtypes
    dcm = np.stack(dcm).astype(np.float32).astype(ml_dtypes.bfloat16)
    common = {
        "dcm": dcm,
        "ada_w": f(inputs["ada_w"])[0], "ada_b": f(inputs["ada_b"]).reshape(1, -1),
        "gcols": np.ascontiguousarray(np.stack([f(inputs["norm_mix_g"])[0].reshape(8, 128).T, f(inputs["norm_ffn_g"])[0].reshape(8, 128).T], axis=-1)),
        "gng": f(inputs["gdn_norm_g"])[0].reshape(128, 1),
        "ident": np.eye(128, dtype=np.float32), "tri": tri, "negm": negm,
        "w_rest": np.ascontiguousarray(w_in[:, COL_Z:]),
        "sgu_wT": np.ascontiguousarray(f(inputs["sgu_w"])[0].transpose(0, 2, 1)),
        "sgu_bb": np.ascontiguousarray(np.broadcast_to(f(inputs["sgu_b"])[0][None], (128, 8, 128))),
        "lng_c": np.ascontiguousarray(f(inputs["sgu_ln_g"])[0].reshape(8, 128).T),
        "lnb_bc": np.ascontiguousarray(np.broadcast_to(f(inputs["sgu_ln_b"])[0][None], (128, D))),
        "w_a": f(inputs["w_branch_a"])[0], "w_b": f(inputs["w_branch_b"])[0], "w_out": f(inputs["w_out"])[0],
        "rw": np.ascontiguousarray(np.concatenate([f(inputs["router_group_w"])[0], f(inputs["router_expert_w"])[0]], axis=1)),
        "rb": np.ascontiguousarray(np.broadcast_to(np.concatenate([f(inputs["router_group_b"])[0], f(inputs["router_expert_b"])[0]])[None], (128, 36))),
        "ew1": f(inputs["expert_w1"])[0], "ew3": f(inputs["expert_w3"])[0], "ew2": f(inputs["expert_w2"])[0],
        "fng_bc": np.ascontiguousarray(np.broadcast_to(f(inputs["final_norm_g"])[None], (128, D))),
    }
    maps = []
    for core in range(8):
        b, r = core // 4, core % 4
        heads = (2 * r, 2 * r + 1)
        qcols = np.concatenate([np.arange(base + h * 128, base + (h + 1) * 128) for base in (0, 1024, 2048) for h in heads])
        bacols = np.array([COL_BETA + d * 8 + h for d in range(2) for h in heads] + [COL_A + d * 8 + h for d in range(2) for h in heads])
        conv_c = np.ascontiguousarray(conv_w[:, qcols].reshape(5, 6, 128).transpose(2, 1, 0))
        gsc = np.concatenate([np.array([a_log[d, h] for d in range(2) for h in heads]), np.array([dt_bias[d, h] for d in range(2) for h in heads])]).astype(np.float32)
        qm = np.zeros((128, 4), np.float32); qm[:, r] = 1.0
        m = dict(common)
        m.update({
            "xb": x[b], "ctxb": ctx[b], "xo": np.ascontiguousarray(x[b, r * OWN:(r + 1) * OWN]),
            "cvT": np.ascontiguousarray(np.stack([c[b].reshape(8, 128).T, c_ctx.reshape(8, 128).T], axis=-1)),
            "w_qkv": np.ascontiguousarray(w_in[:, qcols]), "w_ba": np.ascontiguousarray(w_in[:, bacols]),
            "gsc66": np.ascontiguousarray(np.broadcast_to(gsc.reshape(1, 2, 1, 4), (128, 2, NT, 4))),
            "conv_c": conv_c, "gsc": np.ascontiguousarray(np.broadcast_to(gsc[None], (128, 8))), "qmask": qm,
        })
        maps.append(m)
    return maps


_NC = None


def kernel(**inputs):
    global _NC
    if _NC is None:
        _NC = build_program()
    maps = _prep(inputs)
    res = run_bass_kernel_spmd(_NC, maps, core_ids=list(range(8)))
    out = np.zeros((2, SEQ, D), np.float32)
    for core in range(8):
        b, r = core // 4, core % 4
        out[b, r * OWN:(r + 1) * OWN] = np.asarray(res.results[core]["y"], dtype=np.float32)
    return out
```

```python
import contextlib
import os
import sys
import numpy as np
import concourse.bass as bass
import concourse.mybir as mybir
from concourse.bass_utils import run_bass_kernel_spmd

F32 = mybir.dt.float32
BF16 = mybir.dt.bfloat16
ALU = mybir.AluOpType
AF = mybir.ActivationFunctionType

D = 1024
SEQ = 8192
CTX = 256
NT = 66
OWN = 2048
NOT = 16
NE = 32
DE = 256
COL_BETA = 3072
COL_A = COL_BETA + 16
COL_Z = COL_A + 16
EPS = 1e-6
ARENA = 53000


SAME_ENGINE_WAITS = os.environ.get('SAMEENG', '1') == '1'


class Tk:
    __slots__ = ("name", "w", "rd", "excl")

    def __init__(self, name="", excl=False):
        self.name = name
        self.w = None
        self.rd = []
        self.excl = excl


class Op:
    __slots__ = ("eng", "fn", "deps", "used", "sem", "val", "dma", "key", "idx", "inc", "src")


class Sched:
    ENGS = ("pe", "act", "dve", "pool", "sp")

    def __init__(self, nc):
        self.nc = nc
        self.ops = {e: [] for e in self.ENGS}
        self.all = []
        self.dma_since_barrier = []

    def op(self, eng, fn, reads=(), writes=(), dma=0, key=None, extra=(), inc=16):
        o = Op()
        o.eng = eng; o.fn = fn; o.used = False; o.sem = None; o.val = None
        o.dma = dma; o.key = key; o.idx = len(self.all); o.inc = inc
        o.src = sys._getframe(1).f_lineno
        deps = set(extra)
        reads = list(reads); writes = list(writes)
        for r in list(reads):
            if r.excl and r not in writes:
                writes.append(r)
        for r in reads:
            if r.w is not None:
                deps.add(r.w)
        for w in writes:
            if w.w is not None:
                deps.add(w.w)
            for x in w.rd:
                deps.add(x)
        for r in reads:
            r.rd.append(o)
        for w in writes:
            w.w = o
            w.rd = []
        o.deps = [d for d in deps if d is not o and not (d.eng == "pe" and eng == "pe" and not d.dma and not dma)
                  and not (SAME_ENGINE_WAITS is False and d.eng == eng and eng in ("act", "dve", "pool") and not d.dma and not dma)]
        for d in o.deps:
            d.used = True
        if dma:
            assert key is not None
            self.dma_since_barrier.append(o)
        self.ops[eng].append(o)
        self.all.append(o)
        return o

    def barrier(self, skip_cc=False):
        last = []
        for e in self.ENGS:
            for x in reversed(self.ops[e]):
                if x.fn is None:
                    break
                if skip_cc and x.dma and x.inc == 1:
                    continue
                last.append(x)
                break
        dmas = [x for x in self.dma_since_barrier if not (skip_cc and x.inc == 1)]
        self.dma_since_barrier = [x for x in self.dma_since_barrier if (skip_cc and x.inc == 1)]
        for e in self.ENGS:
            self.op(e, None, extra=[x for x in last if x.eng != e] + dmas)

    def emit(self, block, sems):
        nc = self.nc
        sems = list(sems)
        eng_sem = {e: sems.pop() for e in ("pe", "act", "dve", "pool")}
        keysem = {}
        cnt = {e: 0 for e in eng_sem}
        kcnt = {}
        for o in self.all:
            if o.dma:
                if o.key not in keysem:
                    keysem[o.key] = sems.pop()
                    kcnt[o.key] = 0
                kcnt[o.key] += o.inc * o.dma
                o.sem = keysem[o.key]; o.val = kcnt[o.key]
            elif o.used:
                assert o.fn is not None
                cnt[o.eng] += 1
                o.sem = eng_sem[o.eng]; o.val = cnt[o.eng]
        engobj = {"pe": nc.tensor, "act": nc.scalar, "dve": nc.vector, "pool": nc.gpsimd, "sp": nc.sync}
        deco = {"pe": block.tensor, "act": block.scalar, "dve": block.vector, "pool": block.gpsimd, "sp": block.sync}

        def run(ename):
            def body(e):
                known = {}
                for o in self.ops[ename]:
                    for d in sorted(o.deps, key=lambda x: x.idx):
                        sid = id(d.sem)
                        if known.get(sid, 0) >= d.val:
                            continue
                        e.wait_ge(d.sem, d.val)
                        known[sid] = d.val
                    if o.fn is None:
                        continue
                    if o.dma:
                        n = [0]

                        def sig(inst, o=o, n=n):
                            if o.inc == 16:
                                inst.then_inc(o.sem, 16)
                            else:
                                inst.then_inc(o.sem)
                            n[0] += 1
                            return inst
                        o.fn(e, sig)
                        assert n[0] == o.dma, (n[0], o.dma)
                    else:
                        inst = o.fn(e)
                        if o.used:
                            inst.then_inc(o.sem, 1)
            return body
        for ename in self.ENGS:
            deco[ename](run(ename))


class Arena:
    def __init__(self, ap_f32, nf32, base=0):
        self.ap = ap_f32
        self.n = base + nf32
        self.off = base
        self.hi = 0

    def mark(self):
        return self.off

    def release(self, m):
        self.off = m

    def alloc(self, free_shape, dtype=F32):
        n = int(np.prod(free_shape))
        nf = n if dtype == F32 else (n + 1) // 2
        nf = (nf + 1) // 2 * 2
        assert self.off + nf <= self.n, ("arena overflow", self.off, nf, self.n)
        v = self.ap[:, self.off:self.off + nf]
        self.off += nf
        self.hi = max(self.hi, self.off)
        if dtype != F32:
            v = v.bitcast(dtype)[:, 0:n]
        else:
            v = v[:, 0:n]
        if len(free_shape) == 2:
            v = v.rearrange("p (a b) -> p a b", a=free_shape[0])
        elif len(free_shape) == 3:
            v = v.rearrange("p (a b c) -> p a b c", a=free_shape[0], b=free_shape[1])
        return v


class _Stop(Exception):
    pass


def build_program(debug=False, stage=99):
    KCUT = int(os.environ.get('KCUT', '0')); NTL = int(os.environ.get('NTL', str(NT)))
    nc = bass.Bass("TRN2", target_bir_lowering=False)

    def din(name, shape, dt=F32):
        return nc.dram_tensor(name, list(shape), dt, kind="ExternalInput")

    xb = din("xb", [SEQ, D]); ctxb = din("ctxb", [CTX, D]); xo = din("xo", [OWN, D])
    cvT = din("cvT", [128, 8, 2]); ada_w = din("ada_w", [D, 6 * D]); ada_b = din("ada_b", [1, 6 * D])
    gcols = din("gcols", [128, 8, 2])
    w_qkv = din("w_qkv", [D, 768]); w_ba = din("w_ba", [D, 8])
    gsc66 = din("gsc66", [128, 2, NT, 4])
    conv_c = din("conv_c", [128, 6, 5]); gsc = din("gsc", [128, 8]); gng = din("gng", [128, 1])
    dcm_d = din("dcm", [9, 128, 128], BF16)
    ident_d = din("ident", [128, 128]); tri_d = din("tri", [4, 128, 128]); neg_d = din("negm", [4, 128, 128])
    w_rest = din("w_rest", [D, 5120]); sgu_wT = din("sgu_wT", [8, 128, 128]); sgu_bb = din("sgu_bb", [128, 8, 128])
    lng_c = din("lng_c", [128, 8]); lnb_bc = din("lnb_bc", [128, D])
    w_a = din("w_a", [D, D]); w_b = din("w_b", [D, D]); w_out = din("w_out", [D, D])
    rw = din("rw", [D, 36]); rb = din("rb", [128, 36])
    ew1 = din("ew1", [NE, D, DE]); ew3 = din("ew3", [NE, D, DE]); ew2 = din("ew2", [NE, DE, D])
    fng_bc = din("fng_bc", [128, D]); qmask_d = din("qmask", [128, 4])
    y = nc.dram_tensor("y", [OWN, D], F32, kind="ExternalOutput")
    snd = [nc.dram_tensor(f"snd{j}", [2 * 128, 1024], F32) for j in range(4)]
    rcv = [nc.dram_tensor(f"rcv{j}", [4 * 2 * 128, 1024], F32) for j in range(4)]
    x1d = nc.dram_tensor("x1d", [OWN, D], F32)
    oacc_d = nc.dram_tensor("oacc_d", [128, 128, 128], BF16)
    dbg = None
    if debug:
        dbg = nc.dram_tensor("dbg", [128, 4096], F32, kind="ExternalOutput")
        dbgb = nc.dram_tensor("dbgb", [768, SEQ], BF16, kind="ExternalOutput")

    es = contextlib.ExitStack()
    with es:
        arena_t = es.enter_context(nc.sbuf_tensor("arena", [128, ARENA], F32))
        psb = [es.enter_context(nc.psum_tensor(f"psb{i}", [128, 512], F32)) for i in range(8)]
        sems = [es.enter_context(nc.semaphore(f"s{i}")) for i in range(100)]
        block = es.enter_context(nc.Block())
        A = Arena(arena_t, ARENA)
        S = Sched(nc)

        dump_ops = []

        def dump(ap, col0, ncols, reads=()):
            o_ = S.op("sp", lambda e, sig: sig(e.dma_start(out=dbg[:, col0:col0 + ncols], in_=ap)), reads=list(reads), dma=1, key="dbg")
            dump_ops.append(o_)

        def cut(k, dumps):
            if stage == k:
                S.barrier()
                for d_ in dumps():
                    dump(*d_)
                raise _Stop()

        def dumpb(ap, row0, nrows, col0, ncols, reads=(), extra=()):
            o_ = S.op("sp", lambda e, sig: sig(e.dma_start(out=dbgb[row0:row0 + nrows, col0:col0 + ncols], in_=ap)), reads=list(reads), extra=list(extra), dma=1, key="dbg")
            dump_ops.append(o_)

        def author():
            nonlocal A
            class Pool:
                def __init__(self, items):
                    self.items = items
                    self.i = 0

                def get(self):
                    it = self.items[self.i % len(self.items)]
                    self.i += 1
                    return it
            p128 = Pool([(psb[b][:, 0:128], Tk(f"p128_{b}", excl=True)) for b in range(4)])
            p512 = Pool([(psb[b][:, :], Tk(f"p512_{b}", excl=True)) for b in range(4, 8)])

            def bfv(ap):
                n = ap.shape[-1]
                return ap.bitcast(BF16)[:, 0:n]

            def load(dst, src, tk, key):
                return S.op("sp", lambda e, sig: sig(e.dma_start(out=dst, in_=src)), writes=[tk], dma=1, key=key)

            identf = A.alloc([128]); t_identf = Tk(); load(identf, ident_d[:, :], t_identf, "c0")
            identb = A.alloc([128], BF16); t_identb = Tk()
            S.op("pool", lambda e: e.tensor_copy(out=identb, in_=identf), reads=[t_identf], writes=[t_identb])
            trif = A.alloc([4, 128]); t_trif = Tk(); load(trif, tri_d.ap().rearrange("a p q -> p a q"), t_trif, "c1")
            negf = A.alloc([4, 128]); t_negf = Tk(); load(negf, neg_d.ap().rearrange("a p q -> p a q"), t_negf, "c2")
            negb = A.alloc([4, 128], BF16); t_negb = Tk()
            S.op("pool", lambda e: e.tensor_copy(out=negb, in_=negf), reads=[t_negf], writes=[t_negb])
            dcm = A.alloc([9, 128], BF16); t_dcm = Tk(); load(dcm, dcm_d.ap().rearrange("a p q -> p a q"), t_dcm, "c2b")
            onesf = A.alloc([128]); t_onesf = Tk()
            S.op("pool", lambda e: e.memset(onesf, 1.0), writes=[t_onesf])
            Uf, Vf, Ub, Vb = (trif[:, i, :] for i in range(4))
            gscs = A.alloc([8]); t_gscs = Tk(); load(gscs, gsc[:, :], t_gscs, "c3")
            negA = A.alloc([4]); t_negA = Tk()
            S.op("act", lambda e: e.activation(out=negA, in_=gscs[:, 0:4], func=AF.Exp), reads=[t_gscs], writes=[t_negA])
            S.op("dve", lambda e: e.tensor_scalar(out=negA, in0=negA, scalar1=-1.0, scalar2=None, op0=ALU.mult), reads=[t_negA], writes=[t_negA])
            dtb = gscs[:, 4:8]
            convc = A.alloc([6, 5]); t_convc = Tk(); load(convc, conv_c[:, :, :], t_convc, "c4")
            gngs = A.alloc([1]); t_gngs = Tk(); load(gngs, gng[:, :], t_gngs, "c5")
            qmask = A.alloc([4]); t_qmask = Tk(); load(qmask, qmask_d[:, :], t_qmask, "c6")
            gcol = A.alloc([8, 2]); t_gcol = Tk(); load(gcol, gcols[:, :, :], t_gcol, "c7")
            modc = A.alloc([6, 8]); t_modc = Tk("modc")
            gmix = A.alloc([D]); t_gmix = Tk("gmix")
            gffn = A.alloc([D]); t_gffn = Tk("gffn")
            GT = A.alloc([NOT, 32]); t_GT = [Tk() for _ in range(NOT)]

            m0 = A.mark()
            scT = A.alloc([8, 2]); t_scT = Tk(); load(scT, cvT[:, :, :], t_scT, "c8")
            S.op("act", lambda e: e.activation(out=scT, in_=scT, func=AF.Silu), reads=[t_scT], writes=[t_scT])
            modrow = A.alloc([6 * D]); t_modrow = Tk("modrow")
            modrow_c = A.alloc([6 * D]); t_modrow_c = Tk("modrow_c")
            adb = A.alloc([6 * D]); t_adb = Tk()
            S.op("sp", lambda e, sig: (sig(e.dma_start(out=adb[0:1, :], in_=ada_b[:, :])), sig(e.dma_start(out=adb[1:2, :], in_=ada_b[:, :]))),
                 writes=[t_adb], dma=2, key="c9")
            adw = [A.alloc([8, 512]) for _ in range(2)]; t_adw = [Tk(), Tk()]
            ada_v = ada_w.ap().rearrange("(k p) n -> p k n", p=128)
            for nb in range(12):
                sl = nb % 2
                S.op("sp", lambda e, sig, nb=nb, sl=sl: (sig(e.dma_start(out=adw[sl][:, 0:4, :], in_=ada_v[:, 0:4, nb * 512:(nb + 1) * 512])),
                                                        sig(e.dma_start(out=adw[sl][:, 4:8, :], in_=ada_v[:, 4:8, nb * 512:(nb + 1) * 512]))),
                     writes=[t_adw[sl]], dma=2, key=f"adw{sl}")
                pp, tp = p512.get()

                def f(e, sl=sl, pp=pp):
                    for k in range(8):
                        i = e.matmul(pp[0:2, :], lhsT=scT[:, k, :], rhs=adw[sl][:, k, :], start=(k == 0), stop=(k == 7))
                    return i
                S.op("pe", f, reads=[t_scT, t_adw[sl]], writes=[tp])
                S.op("dve", lambda e, nb=nb, pp=pp: e.tensor_tensor(out=modrow[0:2, nb * 512:(nb + 1) * 512], in0=pp[0:2, :], in1=adb[0:2, nb * 512:(nb + 1) * 512], op=ALU.add),
                     reads=[tp, t_adb], writes=[t_modrow])
            S.op("sp", lambda e, sig: sig(e.dma_start(out=modrow_c[0:1, :], in_=modrow[1:2, :])), reads=[t_modrow], writes=[t_modrow_c], dma=1, key="c10")
            pp, tp = p128.get()
            vecs = [(modrow, 0), (modrow, 1), (modrow_c, 0), (modrow_c, 1), (modrow, 3), (modrow, 4)]

            def f(e, pp=pp):
                for vi, (row, m) in enumerate(vecs):
                    for k in range(8):
                        i = e.matmul(pp[:, vi * 8 + k:vi * 8 + k + 1], lhsT=row[0:1, m * D + k * 128:m * D + (k + 1) * 128], rhs=onesf[0:1, 0:1], start=True, stop=True)
                return i
            S.op("pe", f, reads=[t_modrow, t_modrow_c, t_onesf], writes=[tp])
            S.op("dve", lambda e, pp=pp: e.tensor_copy(out=modc.rearrange("p a b -> p (a b)"), in_=pp[:, 0:48]), reads=[tp], writes=[t_modc])
            for vi, gi in ((1, 0), (3, 0), (5, 1)):
                S.op("dve", lambda e, vi=vi, gi=gi: e.scalar_tensor_tensor(out=modc[:, vi, :], in0=modc[:, vi, :], scalar=1.0, in1=gcol[:, :, gi], op0=ALU.add, op1=ALU.mult),
                     reads=[t_modc, t_gcol], writes=[t_modc])
            for dst, tdst, m in ((gmix, t_gmix, 2), (gffn, t_gffn, 5)):
                for h in range(2):
                    pp, tp = p512.get()
                    S.op("pe", lambda e, pp=pp, m=m, h=h: e.matmul(pp, lhsT=onesf[0:1, :], rhs=modrow[0:1, m * D + h * 512:m * D + (h + 1) * 512], start=True, stop=True),
                         reads=[t_modrow, t_onesf], writes=[tp])
                    S.op("act", lambda e, pp=pp, dst=dst, h=h: e.copy(out=dst[:, h * 512:(h + 1) * 512], in_=pp), reads=[tp], writes=[tdst])
            S.barrier()
            A.release(m0)
            if stage == 0:
                dump(modc.rearrange("p a b -> p (a b)"), 0, 48); dump(gmix[:, 0:256], 64, 256); dump(gffn[:, 0:256], 320, 256)
                raise _Stop()

            mA = A.mark()
            SC = A.alloc([NT, 28]); t_SC = [Tk("SC")] * NT
            QN = A.alloc([NT, 2, 128], BF16); KN = A.alloc([NT, 2, 128], BF16); VV = A.alloc([NT, 2, 128], BF16)
            t_QKV = [[Tk() for _ in range(6)] for t in range(NT)]
            mA1 = A.mark()
            wst = A.alloc([8, 776]); t_wst = Tk()
            wq = A.alloc([8, 896], BF16); t_wq = Tk()
            S.op("sp", lambda e, sig: (sig(e.dma_start(out=wst[:, :, 0:768], in_=w_qkv.ap().rearrange("(k p) n -> p k n", p=128))),
                                       sig(e.dma_start(out=wst[:, :, 768:776], in_=w_ba.ap().rearrange("(k p) n -> p k n", p=128)))),
                 writes=[t_wst], dma=2, key="wst")
            S.op("pool", lambda e: e.tensor_copy(out=wq[:, :, 0:776], in_=wst), reads=[t_wst], writes=[t_wq])
            xt_A = [A.alloc([D]) for _ in range(3)]; t_xt_A = [Tk() for _ in range(3)]
            junk_A = A.alloc([D], BF16); t_junk_A = Tk()
            ssq_A = [A.alloc([1]) for _ in range(3)]; t_ssq_A = [Tk() for _ in range(3)]
            xs_A = [A.alloc([8, 128], BF16) for _ in range(2)]; t_xs_A = [Tk(), Tk()]
            hxT = [A.alloc([8, 128], BF16) for _ in range(2)]; t_hxT = [Tk(), Tk()]; t_hxTb = [Tk(), Tk()]
            PRE = [A.alloc([6, 132]) for _ in range(4)]; t_PRE = [Tk() for _ in range(4)]
            CV = A.alloc([6, 128]); t_CVc = [Tk() for _ in range(6)]
            SQs = [A.alloc([6, 128], BF16) for _ in range(2)]; t_SQs = [Tk(), Tk()]
            sm = [A.alloc([40]) for _ in range(2)]; t_sm = [Tk(), Tk()]
            nr = [A.alloc([8]) for _ in range(2)]; t_nr = [Tk(), Tk()]

            wst_flat = wst.rearrange("p a b -> p (a b)")
            BA = wst_flat[:, 0:NT * 8].rearrange("p (a b) -> p a b", b=8); t_BA = Tk("BA")
            g66 = A.alloc([2, NT, 4]); t_g66 = Tk(); load(g66, gsc66[:, :, :, :], t_g66, "c3b")
            Mx = [wst_flat[:, 1024 + i_ * 512:1024 + i_ * 512 + NT * 4].rearrange("p (a b) -> p a b", b=4) for i_ in range(4)]; t_Mx = [Tk() for _ in range(4)]

            def rows_of(t):
                if t < 2:
                    return ctxb[t * 128:(t + 1) * 128, :]
                return xb[(t - 2) * 128:(t - 1) * 128, :]

            def front(t, part):
                s3 = t % 3; s2 = t % 2
                if part == "b":
                    return front_b(t, s3, s2)
                isctx = t < 2
                shc = modc[:, 2 if isctx else 0, :]; scc = modc[:, 3 if isctx else 1, :]
                S.op("sp", lambda e, sig: sig(e.dma_start(out=xt_A[s3], in_=rows_of(t))), writes=[t_xt_A[s3]], dma=1, key=f"xt_A{s3}")
                S.op("act", lambda e: e.activation(out=junk_A, in_=xt_A[s3], func=AF.Square, accum_out=ssq_A[s3]), reads=[t_xt_A[s3]], writes=[t_ssq_A[s3]])
                S.op("act", lambda e: e.activation(out=ssq_A[s3], in_=ssq_A[s3], func=AF.Sqrt, scale=1.0 / D, bias=EPS), reads=[t_ssq_A[s3]], writes=[t_ssq_A[s3]])
                S.op("dve", lambda e: e.reciprocal(out=ssq_A[s3], in_=ssq_A[s3]), reads=[t_ssq_A[s3]], writes=[t_ssq_A[s3]])
                S.op("pool", lambda e: e.tensor_scalar(out=xs_A[s2].rearrange("p a b -> p (a b)"), in0=xt_A[s3], scalar1=ssq_A[s3], scalar2=1.0, op0=ALU.mult, op1=ALU.mult),
                     reads=[t_xt_A[s3], t_ssq_A[s3]], writes=[t_xs_A[s2]])
                pp, tp = p512.get()
                ppb = pp.bitcast(BF16)

                def tr(e):
                    for k in range(8):
                        i = e.transpose(out=ppb[:, k * 128:(k + 1) * 128], in_=xs_A[s2][:, k, :], identity=identb)
                    return i
                S.op("pe", tr, reads=[t_xs_A[s2], t_identb], writes=[tp])

                def ev_d(e):
                    for k in range(8):
                        i = e.tensor_scalar(out=hxT[s2][:, k, :], in0=ppb[:, k * 128:(k + 1) * 128], scalar1=scc[:, k:k + 1], scalar2=shc[:, k:k + 1], op0=ALU.mult, op1=ALU.add)
                    return i
                t_h2 = t_hxTb[s2]
                S.op("dve", ev_d, reads=[tp, t_modc], writes=[t_hxT[s2], t_h2])
                return

            def front_b(t, s3, s2):
                s3 = t % 4
                pA, tA = p512.get()
                pB, tB = p512.get()

                def pjA(e):
                    for ch in range(4):
                        for k in range(8):
                            i = e.matmul(pA[:, ch * 128:(ch + 1) * 128], lhsT=wq[:, k, ch * 128:(ch + 1) * 128], rhs=hxT[s2][:, k, :], start=(k == 0), stop=(k == 7))
                    return i

                def pjB(e):
                    for ch in range(4, 6):
                        for k in range(8):
                            i = e.matmul(pB[:, (ch - 4) * 128:(ch - 3) * 128], lhsT=wq[:, k, ch * 128:(ch + 1) * 128], rhs=hxT[s2][:, k, :], start=(k == 0), stop=(k == 7))
                    for k in range(8):
                        i = e.matmul(pB[:, 256:264], lhsT=hxT[s2][:, k, :], rhs=wq[:, k, 768:776], start=(k == 0), stop=(k == 7))
                    return i
                S.op("pe", pjA, reads=[t_wq, t_hxT[s2]], writes=[tA])
                S.op("pe", pjB, reads=[t_wq, t_hxT[s2]], writes=[tB])
                pba = pB[:, 256:264]; tba = tB
                S.op("dve", lambda e: e.tensor_copy(out=PRE[s3][:, 0:4, 2:130], in_=pA.rearrange("p (a b) -> p a b", a=4)), reads=[tA], writes=[t_PRE[s3]])
                S.op("dve", lambda e: e.tensor_copy(out=PRE[s3][:, 4:6, 2:130], in_=pB[:, 0:256].rearrange("p (a b) -> p a b", a=2)), reads=[tB], writes=[t_PRE[s3]])
                if KCUT == 2:
                    return
                first = t in (0, 2); last = t in (1, NT - 1)
                if first:
                    S.op("pool", lambda e: e.memset(PRE[s3][:, :, 0:2], 0.0), writes=[t_PRE[s3]])
                else:
                    sp_ = (t - 1) % 4
                    S.op("pool", lambda e: e.tensor_copy(out=PRE[sp_][:, :, 130:132], in_=PRE[s3][:, :, 2:4]), reads=[t_PRE[s3]], writes=[t_PRE[sp_]])
                if last:
                    S.op("pool", lambda e: e.memset(PRE[s3][:, :, 130:132], 0.0), writes=[t_PRE[s3]])
                else:
                    sn = (t + 1) % 4
                    S.op("pool", lambda e: e.tensor_copy(out=PRE[sn][:, :, 0:2], in_=PRE[s3][:, :, 128:130]), reads=[t_PRE[s3]], writes=[t_PRE[sn]])
                S.op("dve", lambda e: e.tensor_copy(out=BA[:, t, :], in_=pba), reads=[tba], writes=[t_BA])

            def lag(t, part):
                s3 = t % 4; s2 = t % 2
                SQ = SQs[s2]; t_SQ = t_SQs[s2]
                if part == "rest":
                    return lag_rest(t, s2, SQ, t_SQ)
                for j in range(5):
                    for ch in range(6):
                        tcv = t_CVc[ch]

                        def cvj(e, ch=ch, j=j):
                            if j == 0:
                                return e.tensor_scalar(out=CV[:, ch, :], in0=PRE[s3][:, ch, 0:128], scalar1=convc[:, ch, 0:1], scalar2=None, op0=ALU.mult)
                            return e.scalar_tensor_tensor(out=CV[:, ch, :], in0=PRE[s3][:, ch, j:j + 128], scalar=convc[:, ch, j:j + 1], in1=CV[:, ch, :], op0=ALU.mult, op1=ALU.add)
                        S.op("dve", cvj, reads=[t_PRE[s3], t_convc] + ([tcv] if j else []), writes=[tcv])
                S.op("act", lambda e: e.activation(out=SQ, in_=CV, func=AF.Silu), reads=t_CVc, writes=[t_SQ])
                return

            def lag_rest(t, s2, SQ, t_SQ):
                pT, tT = p512.get()
                pTb = pT.bitcast(BF16)

                def trs(e):
                    for ch in range(6):
                        i = e.transpose(out=pTb[:, ch * 128:(ch + 1) * 128], in_=SQ[:, ch, :], identity=identb)
                    return i
                S.op("pe", trs, reads=[t_SQ, t_identb], writes=[tT])
                pts = [(pTb[:, ch * 128:(ch + 1) * 128], tT) for ch in range(6)]
                n = nr[s2]; tn = t_nr[s2]
                for ch in range(4):
                    S.op("act", lambda e, ch=ch: e.activation(out=junk_A[:, 0:128], in_=pts[ch][0], func=AF.Square, accum_out=n[:, ch:ch + 1]),
                         reads=[pts[ch][1]], writes=[tn])
                S.op("act", lambda e: e.activation(out=n[:, 0:4], in_=n[:, 0:4], func=AF.Sqrt, bias=EPS), reads=[tn], writes=[tn])
                S.op("dve", lambda e: e.reciprocal(out=n[:, 0:4], in_=n[:, 0:4]), reads=[tn], writes=[tn])
                S.op("dve", lambda e: e.tensor_scalar(out=n[:, 0:2], in0=n[:, 0:2], scalar1=128.0 ** -0.5, scalar2=None, op0=ALU.mult), reads=[tn], writes=[tn])
                dsts = [QN[:, t, 0, :], QN[:, t, 1, :], KN[:, t, 0, :], KN[:, t, 1, :], VV[:, t, 0, :], VV[:, t, 1, :]]
                for ch in range(6):
                    if ch < 4:
                        S.op("dve", lambda e, ch=ch: e.tensor_scalar(out=dsts[ch], in0=pts[ch][0], scalar1=n[:, ch:ch + 1], scalar2=None, op0=ALU.mult),
                             reads=[pts[ch][1], tn], writes=[t_QKV[t][ch]])
                    else:
                        S.op("act", lambda e, ch=ch: e.copy(out=dsts[ch], in_=pts[ch][0]), reads=[pts[ch][1]], writes=[t_QKV[t][ch]])

            front(0, "a")
            for i in range(NTL + 3):
                if i + 1 < NTL:
                    front(i + 1, "a")
                if 2 <= i < NTL + 2:
                    lag(i - 2, "conv")
                if i < NTL:
                    front(i, "b")
                if i >= 3:
                    lag(i - 3, "rest")
            tS = t_SC[0]
            SCk = lambda k: SC[:, :, k * 4:(k + 1) * 4]
            M0, M1, M2, M3 = Mx; tM0, tM1, tM2, tM3 = t_Mx
            S.op("act", lambda e: e.activation(out=g66[:, 0, :, :], in_=g66[:, 0, :, :], func=AF.Exp), reads=[t_g66], writes=[t_g66])
            S.op("act", lambda e: e.activation(out=SCk(3), in_=BA[:, :, 0:4], func=AF.Sigmoid), reads=[t_BA], writes=[tS])
            S.op("dve", lambda e: e.tensor_tensor(out=M0, in0=BA[:, :, 4:8], in1=g66[:, 1, :, :], op=ALU.add), reads=[t_BA, t_g66], writes=[tM0])
            S.op("dve", lambda e: e.tensor_scalar(out=M1, in0=M0, scalar1=-1.0, scalar2=None, op0=ALU.mult), reads=[tM0], writes=[tM1])
            S.op("dve", lambda e: e.tensor_tensor(out=M1, in0=M1, in1=M0, op=ALU.min), reads=[tM0, tM1], writes=[tM1])
            S.op("act", lambda e: e.activation(out=M1, in_=M1, func=AF.Exp), reads=[tM1], writes=[tM1])
            S.op("act", lambda e: e.activation(out=M1, in_=M1, func=AF.Ln, bias=1.0), reads=[tM1], writes=[tM1])
            S.op("dve", lambda e: e.tensor_scalar(out=M2, in0=M0, scalar1=0.0, scalar2=None, op0=ALU.max), reads=[tM0], writes=[tM2])
            S.op("dve", lambda e: e.tensor_tensor(out=M2, in0=M2, in1=M1, op=ALU.add), reads=[tM1, tM2], writes=[tM2])
            S.op("dve", lambda e: e.scalar_tensor_tensor(out=SCk(6), in0=M2, scalar=-1.0, in1=g66[:, 0, :, :], op0=ALU.mult, op1=ALU.mult), reads=[tM2, t_g66], writes=[tS])
            pcA, tcA = p512.get(); pcB, tcB = p512.get()

            def cums(e):
                e.matmul(pcA[:, 0:2 * NT], lhsT=Uf, rhs=SC[:, :, 24:26], start=True, stop=True)
                return e.matmul(pcA[:, 2 * NT:4 * NT], lhsT=Ub, rhs=SC[:, :, 26:28], start=True, stop=True)
            S.op("pe", cums, reads=[tS, t_trif], writes=[tcA])
            S.op("pe", lambda e: e.matmul(pcB[:, 0:4 * NT], lhsT=onesf, rhs=SC[:, :, 24:28], start=True, stop=True), reads=[tS, t_onesf], writes=[tcB])
            Gf_ps = pcA[:, 0:2 * NT].rearrange("p (a b) -> p a b", b=2); Gb_ps = pcA[:, 2 * NT:4 * NT].rearrange("p (a b) -> p a b", b=2)
            Gt_ps = pcB[:, 0:4 * NT].rearrange("p (a b) -> p a b", b=4)
            S.op("act", lambda e: e.activation(out=SC[:, :, 16:18], in_=Gf_ps, func=AF.Exp), reads=[tcA], writes=[tS])
            S.op("act", lambda e: e.activation(out=SC[:, :, 18:20], in_=Gb_ps, func=AF.Exp), reads=[tcA], writes=[tS])
            S.op("act", lambda e: e.activation(out=SCk(5), in_=Gt_ps, func=AF.Exp), reads=[tcB], writes=[tS])
            S.op("dve", lambda e: e.tensor_copy(out=M3[:, :, 0:2], in_=Gf_ps), reads=[tcA], writes=[tM3])
            S.op("dve", lambda e: e.tensor_copy(out=M3[:, :, 2:4], in_=Gb_ps), reads=[tcA], writes=[tM3])
            S.op("dve", lambda e: e.tensor_tensor(out=M3, in0=Gt_ps, in1=M3, op=ALU.subtract), reads=[tcB, tM3], writes=[tM3])
            S.op("act", lambda e: e.activation(out=SCk(2), in_=M3, func=AF.Exp), reads=[tM3], writes=[tS])
            S.op("dve", lambda e: e.tensor_scalar(out=SCk(0), in0=SCk(3), scalar1=-1.0, scalar2=None, op0=ALU.mult), reads=[tS], writes=[tS])
            S.op("dve", lambda e: e.tensor_tensor(out=SCk(1), in0=SCk(3), in1=SCk(4), op=ALU.mult), reads=[tS], writes=[tS])
            S.barrier()
            A.release(mA1)
            if stage == 1:
                for i_, t_ in enumerate((0, 1, 2, 3, 33, 65)):
                    dump(SC[:, t_, :], i_ * 32, 28)
                    dumpb(QN[:, t_, :, :].rearrange("p a b -> p (a b)"), 0, 128, i_ * 768, 256)
                    dumpb(KN[:, t_, :, :].rearrange("p a b -> p (a b)"), 0, 128, i_ * 768 + 256, 256)
                    dumpb(VV[:, t_, :, :].rearrange("p a b -> p (a b)"), 0, 128, i_ * 768 + 512, 256)
                raise _Stop()

            p128_small = p128
            p128 = Pool([(psb[b_][:, 0:128], Tk(f"p128x_{b_}", excl=True)) for b_ in range(8)])
            chains = [(hl, d) for hl in range(2) for d in range(2)]
            cb = {}
            for c in chains:
                Sf = A.alloc([128]); tSf = Tk("S"); Sbb = A.alloc([128], BF16); tSbb = Tk("Sb")
                S.op("pool", lambda e, Sf=Sf: e.memset(Sf, 0.0), writes=[tSf])
                S.op("pool", lambda e, Sbb=Sbb: e.memset(Sbb, 0.0), writes=[tSbb])
                for st_ in range(2):
                    b = {}
                    for nm in ("knT", "qnT", "qdT", "X0", "XT0", "Xb", "XbT", "X0s", "XT0s", "X1s", "XT1s", "P0", "P1", "PT0", "PT1", "No", "NoT", "Wb", "Vb", "kbg", "kd", "vb", "nwT", "vnew", "QKm", "qd", "E", "ET", "ob"):
                        b[nm] = A.alloc([128], BF16); b["t_" + nm] = Tk(nm)
                    for nm in ("gV", "gU"):
                        b[nm] = A.alloc([128]); b["t_" + nm] = Tk(nm)
                    b["S"] = Sf; b["t_S"] = tSf; b["Sb"] = Sbb; b["t_Sb"] = tSbb
                    cb[(c, st_)] = b
            t_OACC = [[Tk() for _ in range(2)] for _ in range(64)]
            ost = [A.alloc([128], BF16) for _ in range(4)]; t_ost = [Tk() for _ in range(4)]
            ost_cnt = [0]
            ofin = [A.alloc([128]) for _ in range(2)]; t_ofin = [Tk(), Tk()]
            onb = [A.alloc([128], BF16) for _ in range(2)]; t_onb = [Tk(), Tk()]
            onT = [A.alloc([128], BF16) for _ in range(4)]; t_onT = [Tk() for _ in range(4)]
            fsm = [A.alloc([4]) for _ in range(2)]; t_fsm = [Tk(), Tk()]
            junk_B = A.alloc([128])
            visited = set()
            snd_ops = []
            qops = [[] for _ in range(4)]
            ccs = []
            fin_cnt = [0]
            evq = [0]

            def evac_copy(dst, src, rd, wr, scale=None):
                evq[0] += 1
                if evq[0] % 2 == 0:
                    if scale is None:
                        return S.op("act", lambda e: e.copy(out=dst, in_=src), reads=rd, writes=wr)
                    return S.op("act", lambda e: e.activation(out=dst, in_=src, func=AF.Copy, scale=scale), reads=rd, writes=wr)
                if scale is None:
                    return S.op("dve", lambda e: e.tensor_copy(out=dst, in_=src), reads=rd, writes=wr)
                return S.op("dve", lambda e: e.tensor_scalar(out=dst, in0=src, scalar1=scale, scalar2=None, op0=ALU.mult), reads=rd, writes=wr)

            def chain_step(c, t, st_):
                hl, d = c
                b = cb[(c, st_)]
                col = d * 2 + hl
                latent = t >= 2
                kn = KN[:, t, hl, :]; qn = QN[:, t, hl, :]; vv = VV[:, t, hl, :]
                tqq = t_QKV[t][hl]; tqk = t_QKV[t][2 + hl]; tqv = t_QKV[t][4 + hl]; tsc = t_SC[t]

                def scol(kind):
                    return SC[:, t, kind * 4 + col:kind * 4 + col + 1]
                U_, V_ = (Uf, Vf) if d == 0 else (Ub, Vb)
                negs = negb[:, 0 if d == 0 else 2, :]; negi = negb[:, 1 if d == 0 else 3, :]
                pk, tpk = p128.get()
                S.op("pe", lambda e: e.transpose(out=bfv(pk), in_=kn, identity=identb), reads=[tqk, t_identb], writes=[tpk])
                evac_copy(b["knT"], bfv(pk), [tpk], [b["t_knT"]])
                S.op("act", lambda e: e.activation(out=b["gV"], in_=V_, func=AF.Copy, scale=scol(6)), reads=[t_trif, tsc], writes=[b["t_gV"]])
                S.op("act", lambda e: e.activation(out=b["gU"], in_=U_, func=AF.Copy, scale=scol(6)), reads=[t_trif, tsc], writes=[b["t_gU"]])
                S.op("dve", lambda e: e.tensor_scalar(out=b["kbg"], in0=kn, scalar1=scol(1), scalar2=None, op0=ALU.mult), reads=[tqk, tsc], writes=[b["t_kbg"]])
                S.op("pool", lambda e: e.tensor_scalar(out=b["kd"], in0=kn, scalar1=scol(2), scalar2=1.0, op0=ALU.mult, op1=ALU.mult), reads=[tqk, tsc], writes=[b["t_kd"]])
                S.op("pool", lambda e: e.tensor_scalar(out=b["vb"], in0=vv, scalar1=scol(3), scalar2=1.0, op0=ALU.mult, op1=ALU.mult), reads=[tqv, tsc], writes=[b["t_vb"]])
                yield
                pd, tpd = p128.get()

                def dm(e):
                    e.matmul(pd, lhsT=identb, rhs=negs, start=True, stop=False)
                    return e.matmul(pd, lhsT=U_, rhs=b["gV"], start=False, stop=True)
                S.op("pe", dm, reads=[t_identb, t_negb, t_trif, b["t_gV"]], writes=[tpd])
                S.op("act", lambda e: e.activation(out=b["E"], in_=pd, func=AF.Exp), reads=[tpd], writes=[b["t_E"]])
                pkk, tpkk = p128.get()
                S.op("pe", lambda e: e.matmul(pkk, lhsT=b["knT"], rhs=b["knT"], start=True, stop=True), reads=[b["t_knT"]], writes=[tpkk])
                S.op("dve", lambda e: e.scalar_tensor_tensor(out=b["X0"], in0=pkk, scalar=scol(0), in1=b["E"], op0=ALU.mult, op1=ALU.mult),
                     reads=[tpkk, tsc, b["t_E"]], writes=[b["t_X0"]])
                if latent:
                    pdt, tpdt = p128.get()

                    def dmt(e):
                        e.matmul(pdt, lhsT=identb, rhs=negi, start=True, stop=False)
                        return e.matmul(pdt, lhsT=V_, rhs=b["gU"], start=False, stop=True)
                    S.op("pe", dmt, reads=[t_identb, t_negb, t_trif, b["t_gU"]], writes=[tpdt])
                    S.op("act", lambda e: e.activation(out=b["ET"], in_=pdt, func=AF.Exp), reads=[tpdt], writes=[b["t_ET"]])
                    pq_, tpq = p128.get()
                    S.op("pe", lambda e: e.transpose(out=bfv(pq_), in_=qn, identity=identb), reads=[tqq, t_identb], writes=[tpq])
                    evac_copy(b["qnT"], bfv(pq_), [tpq], [b["t_qnT"]])
                    S.op("pool", lambda e: e.tensor_scalar(out=b["qd"], in0=qn, scalar1=scol(4), scalar2=1.0, op0=ALU.mult, op1=ALU.mult), reads=[tqq, tsc], writes=[b["t_qd"]])
                yield
                px, tpx = p128.get()
                S.op("pe", lambda e: e.transpose(out=bfv(px), in_=b["X0"], identity=identb), reads=[b["t_X0"], t_identb], writes=[tpx])
                evac_copy(b["XT0"], bfv(px), [tpx], [b["t_XT0"]])
                if latent:
                    pqd, tpqd = p128.get()
                    S.op("pe", lambda e: e.transpose(out=bfv(pqd), in_=b["qd"], identity=identb), reads=[b["t_qd"], t_identb], writes=[tpqd])
                    evac_copy(b["qdT"], bfv(pqd), [tpqd], [b["t_qdT"]])
                    pqk, tpqk = p128.get()
                    S.op("pe", lambda e: e.matmul(pqk, lhsT=b["knT"], rhs=b["qnT"], start=True, stop=True), reads=[b["t_knT"], b["t_qnT"]], writes=[tpqk])
                    S.op("dve", lambda e: e.tensor_tensor(out=b["QKm"], in0=pqk, in1=b["ET"], op=ALU.mult), reads=[tpqk, b["t_ET"]], writes=[b["t_QKm"]])
                yield
                mk = lambda nm: (b[nm], b["t_" + nm])
                Xb, tXb = mk("Xb"); XbT, tXbT = mk("XbT")
                S.op("pool", lambda e: e.tensor_tensor(out=Xb, in0=b["X0"], in1=dcm[:, 0, :], op=ALU.mult), reads=[b["t_X0"], t_dcm], writes=[tXb])
                S.op("pool", lambda e: e.tensor_tensor(out=XbT, in0=b["XT0"], in1=dcm[:, 0, :], op=ALU.mult), reads=[b["t_XT0"], t_dcm], writes=[tXbT])
                S.op("pool", lambda e: e.tensor_tensor(out=b["P0"], in0=Xb, in1=identb, op=ALU.add), reads=[tXb, t_identb], writes=[b["t_P0"]])
                S.op("pool", lambda e: e.tensor_tensor(out=b["PT0"], in0=XbT, in1=identb, op=ALU.add), reads=[tXbT, t_identb], writes=[b["t_PT0"]])
                yield
                cur = 0
                cX, tcX, cXT, tcXT = Xb, tXb, XbT, tXbT
                for lev in range(2):
                    nX, tnX = mk(f"X{lev}s"); nXT, tnXT = mk(f"XT{lev}s")
                    p1, tp1 = p128.get()
                    S.op("pe", lambda e, p1=p1, cX=cX, cXT=cXT: e.matmul(p1, lhsT=cXT, rhs=cX, start=True, stop=True), reads=[tcX, tcXT], writes=[tp1])
                    evac_copy(nX, p1, [tp1], [tnX])
                    p2, tp2 = p128.get()
                    S.op("pe", lambda e, p2=p2, cX=cX, cXT=cXT: e.matmul(p2, lhsT=cX, rhs=cXT, start=True, stop=True), reads=[tcX, tcXT], writes=[tp2])
                    evac_copy(nXT, p2, [tp2], [tnXT])
                    yield
                    P = b[f"P{cur}"]; tP = b[f"t_P{cur}"]; nP = b[f"P{1 - cur}"]; tnP = b[f"t_P{1 - cur}"]
                    PT = b[f"PT{cur}"]; tPT = b[f"t_PT{cur}"]; nPT = b[f"PT{1 - cur}"]; tnPT = b[f"t_PT{1 - cur}"]
                    p3, tp3 = p128.get()
                    S.op("pe", lambda e, p3=p3, nXT=nXT, P=P: e.matmul(p3, lhsT=nXT, rhs=P, start=True, stop=True), reads=[tnXT, tP], writes=[tp3])
                    S.op("dve", lambda e, p3=p3, P=P, nP=nP: e.tensor_tensor(out=nP, in0=p3, in1=P, op=ALU.add), reads=[tp3, tP], writes=[tnP])
                    p4, tp4 = p128.get()
                    S.op("pe", lambda e, p4=p4, nX=nX, PT=PT: e.matmul(p4, lhsT=nX, rhs=PT, start=True, stop=True), reads=[tnX, tPT], writes=[tp4])
                    S.op("dve", lambda e, p4=p4, PT=PT, nPT=nPT: e.tensor_tensor(out=nPT, in0=p4, in1=PT, op=ALU.add), reads=[tp4, tPT], writes=[tnPT])
                    cur = 1 - cur
                    cX, tcX, cXT, tcXT = nX, tnX, nXT, tnXT
                    yield
                for li in range(4):
                    mi = 1 + 2 * li + (0 if d == 0 else 1)
                    miT = 1 + 2 * li + (1 if d == 0 else 0)
                    No, tNo = mk("No"); NoT, tNoT = mk("NoT")
                    P = b[f"P{cur}"]; tP = b[f"t_P{cur}"]; nP = b[f"P{1 - cur}"]; tnP = b[f"t_P{1 - cur}"]
                    PT = b[f"PT{cur}"]; tPT = b[f"t_PT{cur}"]; nPT = b[f"PT{1 - cur}"]; tnPT = b[f"t_PT{1 - cur}"]
                    S.op("pool", lambda e, mi=mi, No=No: e.tensor_tensor(out=No, in0=b["X0"], in1=dcm[:, mi, :], op=ALU.mult), reads=[b["t_X0"], t_dcm], writes=[tNo])
                    pw_, tpw_ = p128.get()
                    S.op("pe", lambda e, pw_=pw_, No=No, PT=PT: e.matmul(pw_, lhsT=No, rhs=PT, start=True, stop=True), reads=[tNo, tPT], writes=[tpw_])
                    Wb, tWb = mk("Wb")
                    evac_copy(Wb, pw_, [tpw_], [tWb])
                    if li < 3:
                        S.op("pool", lambda e, miT=miT, NoT=NoT: e.tensor_tensor(out=NoT, in0=b["XT0"], in1=dcm[:, miT, :], op=ALU.mult), reads=[b["t_XT0"], t_dcm], writes=[tNoT])
                        pv_, tpv_ = p128.get()
                        S.op("pe", lambda e, pv_=pv_, NoT=NoT, P=P: e.matmul(pv_, lhsT=NoT, rhs=P, start=True, stop=True), reads=[tNoT, tP], writes=[tpv_])
                        Vb_, tVb_ = mk("Vb")
                        evac_copy(Vb_, pv_, [tpv_], [tVb_])
                    yield
                    p5, tp5 = p128.get()
                    S.op("pe", lambda e, p5=p5, P=P, Wb=Wb: e.matmul(p5, lhsT=P, rhs=Wb, start=True, stop=True), reads=[tP, tWb], writes=[tp5])
                    S.op("dve", lambda e, p5=p5, PT=PT, nPT=nPT: e.tensor_tensor(out=nPT, in0=p5, in1=PT, op=ALU.add), reads=[tp5, tPT], writes=[tnPT])
                    if li < 3:
                        p6, tp6 = p128.get()
                        S.op("pe", lambda e, p6=p6, PT=PT, Vb_=Vb_: e.matmul(p6, lhsT=PT, rhs=Vb_, start=True, stop=True), reads=[tPT, tVb_], writes=[tp6])
                        S.op("dve", lambda e, p6=p6, P=P, nP=nP: e.tensor_tensor(out=nP, in0=p6, in1=P, op=ALU.add), reads=[tp6, tP], writes=[tnP])
                    cur = 1 - cur
                    yield
                TT = b[f"PT{cur}"]; tTT = b[f"t_PT{cur}"]
                pw, tpw = p128.get()
                S.op("pe", lambda e: e.matmul(pw, lhsT=b["kbg"], rhs=TT, start=True, stop=True), reads=[b["t_kbg"], tTT], writes=[tpw])
                evac_copy(b["nwT"], pw, [tpw], [b["t_nwT"]], scale=-1.0)
                yield
                pv, tpv = p128.get()

                def vn(e):
                    e.matmul(pv, lhsT=TT, rhs=b["vb"], start=True, stop=False)
                    return e.matmul(pv, lhsT=b["nwT"], rhs=b["Sb"], start=False, stop=True)
                S.op("pe", vn, reads=[tTT, b["t_vb"], b["t_nwT"], b["t_Sb"]], writes=[tpv])
                evac_copy(b["vnew"], pv, [tpv], [b["t_vnew"]])
                yield
                if latent:
                    lt = t - 2
                    po, tpo = p128.get()

                    def om(e):
                        e.matmul(po, lhsT=b["qdT"], rhs=b["Sb"], start=True, stop=False)
                        return e.matmul(po, lhsT=b["QKm"], rhs=b["vnew"], start=False, stop=True)
                    S.op("pe", om, reads=[b["t_qdT"], b["t_Sb"], b["t_QKm"], b["t_vnew"]], writes=[tpo])
                    if (lt, hl) not in visited:
                        visited.add((lt, hl))
                        k4o = ost_cnt[0] % 4; ost_cnt[0] += 1
                        evac_copy(ost[k4o], po, [tpo], [t_ost[k4o]])
                        S.op("pool", lambda e, sig: sig(e.dma_start(out=oacc_d[lt * 2 + hl], in_=ost[k4o])), reads=[t_ost[k4o]], writes=[t_OACC[lt][hl]], dma=1, key=f"oaw{k4o}")
                    else:
                        k2 = fin_cnt[0] % 2; k4 = fin_cnt[0] % 4
                        fin_cnt[0] += 1
                        of = ofin[k2]; tof = t_ofin[k2]; fs = fsm[k2]; tfs = t_fsm[k2]
                        S.op("sp", lambda e, sig: sig(e.dma_start(out=b["ob"], in_=oacc_d[lt * 2 + hl])), reads=[t_OACC[lt][hl]], writes=[b["t_ob"]], dma=1, key=f"oar{hl}{d}{st_}")
                        S.op("dve", lambda e: e.tensor_tensor(out=of, in0=po, in1=b["ob"], op=ALU.add), reads=[tpo, b["t_ob"]], writes=[tof])
                        S.op("act", lambda e: e.activation(out=junk_B, in_=of, func=AF.Square, accum_out=fs[:, 0:1]), reads=[tof], writes=[tfs])
                        S.op("act", lambda e: e.activation(out=fs[:, 0:1], in_=fs[:, 0:1], func=AF.Sqrt, scale=1.0 / 128, bias=EPS), reads=[tfs], writes=[tfs])
                        S.op("dve", lambda e: e.reciprocal(out=fs[:, 0:1], in_=fs[:, 0:1]), reads=[tfs], writes=[tfs])
                        S.op("dve", lambda e: e.tensor_scalar(out=onb[k2], in0=of, scalar1=fs[:, 0:1], scalar2=None, op0=ALU.mult), reads=[tof, tfs], writes=[t_onb[k2]])
                        pt_, tpt = p128.get()
                        S.op("pe", lambda e: e.transpose(out=bfv(pt_), in_=onb[k2], identity=identb), reads=[t_onb[k2], t_identb], writes=[tpt])
                        evac_copy(onT[k4], bfv(pt_), [tpt], [t_onT[k4]])
                        o_ = S.op("pool", lambda e, sig: sig(e.dma_start(out=snd[lt // 16][hl * 128:(hl + 1) * 128, (lt % 16) * 64:(lt % 16 + 1) * 64], in_=onT[k4].bitcast(F32))),
                                  reads=[t_onT[k4]], dma=1, key=f"snd{k4}")
                        snd_ops.append(o_)
                        qops[lt // 16].append(o_)
                        if len(qops[lt // 16]) == 32:
                            jq = lt // 16
                            ccs.append(S.op("pool", lambda e, sig: sig(e.collective_compute("AllGather", ALU.bypass, replica_groups=[[0, 1, 2, 3], [4, 5, 6, 7]],
                                                                                       ins=[snd[jq].ap().opt()], outs=[rcv[jq].ap().opt()])),
                                            extra=qops[jq], dma=1, key=f"cc{jq}", inc=1))
                ps_, tps = p128.get()
                S.op("pe", lambda e: e.matmul(ps_, lhsT=b["kd"], rhs=b["vnew"], start=True, stop=True), reads=[b["t_kd"], b["t_vnew"]], writes=[tps])
                S.op("dve", lambda e: e.scalar_tensor_tensor(out=b["S"], in0=b["S"], scalar=scol(5), in1=ps_, op0=ALU.mult, op1=ALU.add),
                     reads=[b["t_S"], tsc, tps], writes=[b["t_S"]])
                S.op("act", lambda e: e.copy(out=b["Sb"], in_=b["S"]), reads=[b["t_S"]], writes=[b["t_Sb"]])
                yield

            def bwd_tile(i):
                return 1 - i if i < 2 else NT + 1 - i

            HALF = 11
            alive = []
            nstart = 0
            tick = 0
            while nstart < NT or alive:
                if nstart < NT and tick % HALF == 0:
                    i = nstart; nstart += 1
                    for c in chains:
                        t = i if c[1] == 0 else bwd_tile(i)
                        alive.append([chain_step(c, t, i % 2), 0])
                nxt = []
                for g in alive:
                    try:
                        next(g[0]); g[1] += 1
                        assert g[1] < 2 * HALF, "chain step too long for the 2-deep pipeline"
                        nxt.append(g)
                    except StopIteration:
                        pass
                alive = nxt
                tick += 1
            if stage == 2:
                for i_ in range(4 if debug else 0):
                    o_ = S.op("sp", lambda e, sig, i_=i_: sig(e.dma_start(out=dbgb[0:256, i_ * 2048:(i_ + 1) * 2048].bitcast(F32), in_=snd[i_][:, :])), extra=snd_ops, dma=1, key="dbg")
                    dump_ops.append(o_)
                for i_, c_ in enumerate(chains if debug else []):
                    dump(cb[(c_, 0)]["S"], i_ * 128, 128, reads=[cb[(c_, 0)]["t_S"]])
                raise _Stop()
            p128 = p128_small
            assert len(ccs) == 4
            S.barrier(skip_cc=True)
            A.release(mA)
            if stage == 3:
                raise _Stop()

            hxo = A.alloc([8, OWN], BF16); t_hxo = [Tk() for _ in range(NOT)]
            markH = A.mark()
            offS = A.mark()
            SZ = A.alloc([8, OWN], BF16); t_SZ = [[Tk() for _ in range(4)] for _ in range(8)]
            GU = A.alloc([8, OWN], BF16); t_GU = [[Tk() for _ in range(4)] for _ in range(8)]
            wstg = [A.alloc([8, 512]) for _ in range(2)]; t_wstg = [Tk(), Tk()]
            wbf = [A.alloc([8, 512], BF16) for _ in range(2)]; t_wbf = [Tk(), Tk()]
            mC1 = A.mark()
            xt_C = [A.alloc([D]) for _ in range(2)]; t_xt_C = [Tk(), Tk()]
            junk_C = A.alloc([D]); t_junk_C = Tk()
            ssq_C = [A.alloc([1]) for _ in range(2)]; t_ssq_C = [Tk(), Tk()]
            xs_C = [A.alloc([8, 128], BF16) for _ in range(2)]; t_xs_C = [Tk(), Tk()]
            for t in range(NOT):
                s2 = t % 2
                S.op("sp", lambda e, sig, t=t, s2=s2: sig(e.dma_start(out=xt_C[s2], in_=xo[t * 128:(t + 1) * 128, :])), writes=[t_xt_C[s2]], dma=1, key=f"cxt{s2}")
                S.op("act", lambda e, s2=s2: e.activation(out=junk_C, in_=xt_C[s2], func=AF.Square, accum_out=ssq_C[s2]), reads=[t_xt_C[s2]], writes=[t_ssq_C[s2]])
                S.op("act", lambda e, s2=s2: e.activation(out=ssq_C[s2], in_=ssq_C[s2], func=AF.Sqrt, scale=1.0 / D, bias=EPS), reads=[t_ssq_C[s2]], writes=[t_ssq_C[s2]])
                S.op("dve", lambda e, s2=s2: e.reciprocal(out=ssq_C[s2], in_=ssq_C[s2]), reads=[t_ssq_C[s2]], writes=[t_ssq_C[s2]])
                S.op("pool", lambda e, s2=s2: e.tensor_scalar(out=xs_C[s2].rearrange("p a b -> p (a b)"), in0=xt_C[s2], scalar1=ssq_C[s2], scalar2=1.0, op0=ALU.mult, op1=ALU.mult),
                     reads=[t_xt_C[s2], t_ssq_C[s2]], writes=[t_xs_C[s2]])
                pp, tp = p512.get()
                ppb = pp.bitcast(BF16)

                def tr(e, s2=s2, ppb=ppb):
                    for k in range(8):
                        i = e.transpose(out=ppb[:, k * 128:(k + 1) * 128], in_=xs_C[s2][:, k, :], identity=identb)
                    return i
                S.op("pe", tr, reads=[t_xs_C[s2], t_identb], writes=[tp])

                def ev_d(e, t=t, ppb=ppb):
                    for k in range(8):
                        i = e.tensor_scalar(out=hxo[:, k, t * 128:(t + 1) * 128], in0=ppb[:, k * 128:(k + 1) * 128], scalar1=modc[:, 1, k:k + 1], scalar2=modc[:, 0, k:k + 1], op0=ALU.mult, op1=ALU.add)
                    return i
                S.op("dve", ev_d, reads=[tp, t_modc], writes=[t_hxo[t]])
            S.barrier()
            A.release(mC1)
            wcnt = [0]

            def stream_w(src_ap_cols, ncols):
                s = wcnt[0] % 2
                wcnt[0] += 1
                v = src_ap_cols.rearrange("(k p) n -> p k n", p=128)
                S.op("sp", lambda e, sig: (sig(e.dma_start(out=wstg[s][:, 0:4, 0:ncols], in_=v[:, 0:4, :])), sig(e.dma_start(out=wstg[s][:, 4:8, 0:ncols], in_=v[:, 4:8, :]))),
                     writes=[t_wstg[s]], dma=2, key=f"wstg{s}")
                S.op("pool", lambda e: e.tensor_copy(out=wbf[s][:, :, 0:ncols], in_=wstg[s][:, :, 0:ncols]), reads=[t_wstg[s]], writes=[t_wbf[s]])
                return wbf[s], t_wbf[s]
            for cbk in range(4):
                wv, twv = stream_w(w_rest[:, cbk * 512:(cbk + 1) * 512], 512)
                dst, tdst, fn = (SZ, t_SZ, AF.Silu) if cbk < 2 else (GU, t_GU, AF.Gelu_apprx_tanh)
                for cc_ in range(4):
                    chn = (cbk % 2) * 4 + cc_
                    for tb in range(4):
                        pp, tp = p512.get()

                        def f(e, pp=pp, wv=wv, cc_=cc_, tb=tb):
                            for k in range(8):
                                i = e.matmul(pp, lhsT=wv[:, k, cc_ * 128:(cc_ + 1) * 128], rhs=hxo[:, k, tb * 512:(tb + 1) * 512], start=(k == 0), stop=(k == 7))
                            return i
                        S.op("pe", f, reads=[twv] + t_hxo[tb * 4:(tb + 1) * 4], writes=[tp])
                        S.op("act", lambda e, pp=pp, dst=dst, chn=chn, tb=tb, fn=fn: e.activation(out=dst[:, chn, tb * 512:(tb + 1) * 512], in_=pp, func=fn),
                             reads=[tp], writes=[tdst[chn][tb]])
            def cut4(k):
                if stage == 4 and int(os.environ.get("CUT4", "0")) == k:
                    S.barrier()
                    for i_, buf_ in enumerate((SZ, GU, hxo)):
                        for h_ in range(2):
                            dumpb(buf_[:, h_ * 4:(h_ + 1) * 4, :].rearrange("p a b -> p (a b)"), i_ * 256 + h_ * 128, 128, 0, SEQ)
                    raise _Stop()
            cut4(1)
            mV = A.mark()
            junk_V = A.alloc([D])
            wcnt[0] = 0
            wvh = [stream_w(w_rest[:, 2048 + hh * 512:2048 + (hh + 1) * 512], 512) for hh in range(2)]
            swf = A.alloc([8, 128]); t_swf = Tk(); load(swf, sgu_wT.ap().rearrange("g q p -> q g p"), t_swf, "c11")
            swb = A.alloc([8, 128], BF16); t_swb = Tk()
            S.op("pool", lambda e: e.tensor_copy(out=swb, in_=swf), reads=[t_swf], writes=[t_swb])
            lnbb = A.alloc([D]); t_lnbb = Tk(); load(lnbb, lnb_bc[:, :], t_lnbb, "c12")
            sbb = A.alloc([8, 128]); t_sbb = Tk(); load(sbb, sgu_bb[:, :, :], t_sbb, "c13")
            lngc = A.alloc([8]); t_lngc = Tk(); load(lngc, lng_c[:, :], t_lngc, "c14")
            BIAS = A.alloc([8, 128]); t_BIAS = Tk()
            for g in range(8):
                pp, tp = p128.get()
                S.op("pe", lambda e, pp=pp, g=g: e.matmul(pp, lhsT=lnbb[:, g * 128:(g + 1) * 128], rhs=swf[:, g, :], start=True, stop=True), reads=[t_lnbb, t_swf], writes=[tp])
                S.op("dve", lambda e, pp=pp, g=g: e.tensor_tensor(out=BIAS[:, g, :], in0=pp, in1=sbb[:, g, :], op=ALU.add), reads=[tp, t_sbb], writes=[t_BIAS])
            gv = [A.alloc([D])] * 2; t_gv = [Tk()] * 2
            vnb = [A.alloc([D], BF16) for _ in range(2)]; t_vnb = [Tk(), Tk()]
            lst = [A.alloc([8]) for _ in range(2)]; t_lst = [Tk(), Tk()]
            mtmp = [A.alloc([128]) for _ in range(2)]; t_mtmp = [Tk(), Tk()]
            for t in range(NOT):
                s2 = t % 2
                for hh in range(2):
                    pp, tp = p512.get()

                    def f(e, pp=pp, hh=hh, t=t):
                        for k in range(8):
                            i = e.matmul(pp, lhsT=hxo[:, k, t * 128:(t + 1) * 128], rhs=wvh[hh][0][:, k, :], start=(k == 0), stop=(k == 7))
                        return i
                    S.op("pe", f, reads=[wvh[hh][1], t_hxo[t]], writes=[tp])
                    S.op("act", lambda e, pp=pp, hh=hh, s2=s2: e.activation(out=gv[s2][:, hh * 512:(hh + 1) * 512], in_=pp, func=AF.Gelu_apprx_tanh), reads=[tp], writes=[t_gv[s2]])
                ls = lst[s2]; tls = t_lst[s2]
                S.op("dve", lambda e, s2=s2, ls=ls: e.tensor_reduce(out=ls[:, 0:1], in_=gv[s2], axis=mybir.AxisListType.X, op=ALU.add), reads=[t_gv[s2]], writes=[tls])
                S.op("act", lambda e, s2=s2, ls=ls: e.activation(out=junk_V, in_=gv[s2], func=AF.Square, accum_out=ls[:, 1:2]), reads=[t_gv[s2]], writes=[tls])
                S.op("dve", lambda e, ls=ls: e.tensor_scalar(out=ls[:, 0:2], in0=ls[:, 0:2], scalar1=1.0 / D, scalar2=None, op0=ALU.mult), reads=[tls], writes=[tls])
                S.op("dve", lambda e, ls=ls: e.tensor_tensor(out=ls[:, 2:3], in0=ls[:, 0:1], in1=ls[:, 0:1], op=ALU.mult), reads=[tls], writes=[tls])
                S.op("dve", lambda e, ls=ls: e.tensor_tensor(out=ls[:, 2:3], in0=ls[:, 1:2], in1=ls[:, 2:3], op=ALU.subtract), reads=[tls], writes=[tls])
                S.op("act", lambda e, ls=ls: e.activation(out=ls[:, 2:3], in_=ls[:, 2:3], func=AF.Sqrt, bias=EPS), reads=[tls], writes=[tls])
                S.op("dve", lambda e, ls=ls: e.reciprocal(out=ls[:, 2:3], in_=ls[:, 2:3]), reads=[tls], writes=[tls])
                S.op("dve", lambda e, s2=s2, ls=ls: e.tensor_scalar(out=vnb[s2], in0=gv[s2], scalar1=ls[:, 0:1], scalar2=ls[:, 2:3], op0=ALU.subtract, op1=ALU.mult),
                     reads=[t_gv[s2], tls], writes=[t_vnb[s2]])
                for g in range(8):
                    pp, tp = p128.get()
                    S.op("pe", lambda e, pp=pp, g=g, s2=s2: e.matmul(pp, lhsT=vnb[s2][:, g * 128:(g + 1) * 128], rhs=swb[:, g, :], start=True, stop=True),
                         reads=[t_vnb[s2], t_swb], writes=[tp])
                    mt = mtmp[g % 2]; tmt = t_mtmp[g % 2]
                    S.op("dve", lambda e, pp=pp, g=g, mt=mt: e.scalar_tensor_tensor(out=mt, in0=pp, scalar=lngc[:, g:g + 1], in1=BIAS[:, g, :], op0=ALU.mult, op1=ALU.add),
                         reads=[tp, t_lngc, t_BIAS], writes=[tmt])
                    S.op("pool", lambda e, g=g, t=t, mt=mt: e.tensor_tensor(out=GU[:, g, t * 128:(t + 1) * 128], in0=GU[:, g, t * 128:(t + 1) * 128], in1=mt, op=ALU.mult),
                         reads=[tmt, t_GU[g][t // 4]], writes=[t_GU[g][t // 4]])
            S.barrier()
            A.release(mV)
            cut4(2)
            mY = A.mark()
            rq = [A.alloc([4, 512], BF16) for _ in range(2)]; t_rq = [Tk(), Tk()]
            acc = [A.alloc([512]) for _ in range(2)]; t_acc = [Tk(), Tk()]
            cntr = 0
            for hp in range(4):
                for hl in range(2):
                    chn = hp * 2 + hl
                    for tb in range(4):
                        s = cntr % 2; cntr += 1
                        S.op("sp", lambda e, sig, s=s, chn=chn, tb=tb: tuple(sig(e.dma_start(out=rq[s][:, j, :].bitcast(F32), in_=rcv[j][chn * 128:(chn + 1) * 128, tb * 256:(tb + 1) * 256])) for j in range(4)),
                             extra=ccs, writes=[t_rq[s]], dma=4, key=f"rq{s}")
                        for j in range(4):
                            def f(e, s=s, j=j):
                                if j == 0:
                                    return e.tensor_scalar(out=acc[s], in0=rq[s][:, 0, :], scalar1=qmask[:, 0:1], scalar2=None, op0=ALU.mult)
                                return e.scalar_tensor_tensor(out=acc[s], in0=rq[s][:, j, :], scalar=qmask[:, j:j + 1], in1=acc[s], op0=ALU.mult, op1=ALU.add)
                            S.op("dve", f, reads=[t_rq[s], t_qmask, t_acc[s]] if j else [t_rq[s], t_qmask], writes=[t_acc[s]])
                        S.op("dve", lambda e, s=s, chn=chn, tb=tb: e.scalar_tensor_tensor(out=SZ[:, chn, tb * 512:(tb + 1) * 512], in0=acc[s], scalar=gngs[:, 0:1], in1=SZ[:, chn, tb * 512:(tb + 1) * 512], op0=ALU.mult, op1=ALU.mult),
                             reads=[t_acc[s], t_gngs, t_SZ[chn][tb]], writes=[t_SZ[chn][tb]])
            S.barrier()
            A.release(mY)
            cut4(3)
            S.barrier()
            mM = A.mark()
            MG = A.alloc([8, OWN], BF16); t_MG = [[Tk() for _ in range(4)] for _ in range(8)]
            sga = [A.alloc([512], BF16) for _ in range(2)]; t_sga = [Tk(), Tk()]
            sgb = [A.alloc([512], BF16) for _ in range(2)]; t_sgb = [Tk(), Tk()]
            m1 = [A.alloc([512]) for _ in range(2)]; t_m1 = [Tk(), Tk()]
            m2 = [A.alloc([512]) for _ in range(2)]; t_m2 = [Tk(), Tk()]
            sub = [(wstg[i][:, :, j * 128:(j + 1) * 128], wbf[i][:, :, j * 128:(j + 1) * 128], Tk(), Tk()) for i in range(2) for j in range(4)]
            subc = [0]

            def stream_small(src_cols):
                stg, bfw, tst, tbf = sub[subc[0] % 8]
                subc[0] += 1
                v = src_cols.rearrange("(k p) n -> p k n", p=128)
                S.op("sp", lambda e, sig: sig(e.dma_start(out=stg, in_=v)), writes=[tst], dma=1, key=f"sub{(subc[0] - 1) % 8}")
                S.op("pool", lambda e: e.tensor_copy(out=bfw, in_=stg), reads=[tst], writes=[tbf])
                return bfw, tbf
            cntr = 0
            for dc in range(8):
                ws = [stream_small(w_a[:, dc * 128:(dc + 1) * 128]), stream_small(w_b[:, dc * 128:(dc + 1) * 128]),
                      stream_small(w_rest[:, 3072 + dc * 128:3072 + (dc + 1) * 128]), stream_small(w_rest[:, 4096 + dc * 128:4096 + (dc + 1) * 128])]
                for tb in range(4):
                    s = cntr % 2; cntr += 1
                    outs = []
                    for wi, (src, tsrc) in enumerate(((SZ, t_SZ), (GU, t_GU), (hxo, None), (hxo, None))):
                        wv_, tw_ = ws[wi]
                        pp, tp = p512.get()

                        def f(e, pp=pp, wv_=wv_, src=src, tb=tb):
                            for k in range(8):
                                i = e.matmul(pp, lhsT=wv_[:, k, :], rhs=src[:, k, tb * 512:(tb + 1) * 512], start=(k == 0), stop=(k == 7))
                            return i
                        rds = [tw_] + ([tsrc[k][tb] for k in range(8)] if tsrc is not None else t_hxo[tb * 4:(tb + 1) * 4])
                        S.op("pe", f, reads=rds, writes=[tp])
                        outs.append((pp, tp))
                    S.op("act", lambda e, s=s, pp=outs[2][0]: e.activation(out=sga[s], in_=pp, func=AF.Sigmoid), reads=[outs[2][1]], writes=[t_sga[s]])
                    S.op("act", lambda e, s=s, pp=outs[3][0]: e.activation(out=sgb[s], in_=pp, func=AF.Sigmoid), reads=[outs[3][1]], writes=[t_sgb[s]])
                    S.op("dve", lambda e, s=s, pp=outs[0][0]: e.tensor_tensor(out=m1[s], in0=pp, in1=sga[s], op=ALU.mult), reads=[outs[0][1], t_sga[s]], writes=[t_m1[s]])
                    S.op("dve", lambda e, s=s, pp=outs[1][0]: e.tensor_tensor(out=m2[s], in0=pp, in1=sgb[s], op=ALU.mult), reads=[outs[1][1], t_sgb[s]], writes=[t_m2[s]])
                    S.op("pool", lambda e, s=s, dc=dc, tb=tb: e.tensor_tensor(out=MG[:, dc, tb * 512:(tb + 1) * 512], in0=m1[s], in1=m2[s], op=ALU.add),
                         reads=[t_m1[s], t_m2[s]], writes=[t_MG[dc][tb]])
            S.barrier()
            if stage == 4:
                for i_, (buf_, tk_) in enumerate(((SZ, t_SZ), (GU, t_GU), (MG, t_MG))):
                    for h_ in range(2):
                        dumpb(buf_[:, h_ * 4:(h_ + 1) * 4, :].rearrange("p a b -> p (a b)"), i_ * 256 + h_ * 128, 128, 0, SEQ)
                raise _Stop()
            hx2 = hxo; t_hx2 = [Tk() for _ in range(NOT)]
            Amain = A
            A = Arena(arena_t, 16384, base=offS)
            wo_b = A.alloc([8, D], BF16); t_wo_b = Tk()
            for hh in range(2):
                sl = hh
                S.op("sp", lambda e, sig, hh=hh, sl=sl: (sig(e.dma_start(out=wstg[sl][:, 0:4, :], in_=w_out.ap().rearrange("(k p) n -> p k n", p=128)[:, 0:4, hh * 512:(hh + 1) * 512])),
                                                        sig(e.dma_start(out=wstg[sl][:, 4:8, :], in_=w_out.ap().rearrange("(k p) n -> p k n", p=128)[:, 4:8, hh * 512:(hh + 1) * 512]))),
                     writes=[t_wstg[sl]], dma=2, key=f"wstg{sl}")
                for k in range(8):
                    S.op("pool", lambda e, k=k, hh=hh, sl=sl: e.tensor_tensor(out=wo_b[:, k, hh * 512:(hh + 1) * 512], in0=wstg[sl][:, k, :], in1=gmix[:, hh * 512:(hh + 1) * 512], op=ALU.mult),
                         reads=[t_wstg[sl], t_gmix], writes=[t_wo_b])
            rwf = A.alloc([8, 36]); t_rwf = Tk(); load(rwf, rw.ap().rearrange("(k p) n -> p k n", p=128), t_rwf, "c15")
            rbb = A.alloc([36]); t_rbb = Tk(); load(rbb, rb[:, :], t_rbb, "c16")
            x1 = [A.alloc([D]) for _ in range(2)]; t_x1 = [Tk(), Tk()]
            xt_D = [A.alloc([D]) for _ in range(2)]; t_xt_D = [Tk(), Tk()]
            junk_D = A.alloc([D]); t_junk_D = Tk()
            ssq_D = [A.alloc([1]) for _ in range(2)]; t_ssq_D = [Tk(), Tk()]
            xsf = [A.alloc([8, 128]) for _ in range(2)]; t_xsf = [Tk(), Tk()]
            hxf = [A.alloc([8, 128]) for _ in range(2)]; t_hxf = [Tk(), Tk()]
            rs_ = [A.alloc([64]) for _ in range(2)]; t_rs = [Tk(), Tk()]
            x1_ops = []
            for t in range(NOT):
                s2 = t % 2
                S.op("sp", lambda e, sig, t=t, s2=s2: sig(e.dma_start(out=xt_D[s2], in_=xo[t * 128:(t + 1) * 128, :])), writes=[t_xt_D[s2]], dma=1, key=f"dxt{s2}")
                for hh in range(2):
                    pp, tp = p512.get()

                    def f(e, pp=pp, hh=hh, t=t):
                        for k in range(8):
                            i = e.matmul(pp, lhsT=MG[:, k, t * 128:(t + 1) * 128], rhs=wo_b[:, k, hh * 512:(hh + 1) * 512], start=(k == 0), stop=(k == 7))
                        return i
                    S.op("pe", f, reads=[t_wo_b] + [t_MG[k][t // 4] for k in range(8)], writes=[tp])
                    S.op("dve", lambda e, pp=pp, hh=hh, s2=s2: e.tensor_tensor(out=x1[s2][:, hh * 512:(hh + 1) * 512], in0=pp, in1=xt_D[s2][:, hh * 512:(hh + 1) * 512], op=ALU.add),
                         reads=[tp, t_xt_D[s2]], writes=[t_x1[s2]])
                o_ = S.op("pool", lambda e, sig, t=t, s2=s2: sig(e.dma_start(out=x1d[t * 128:(t + 1) * 128, :], in_=x1[s2])), reads=[t_x1[s2]], dma=1, key=f"x1d{s2}")
                x1_ops.append(o_)
                S.op("act", lambda e, s2=s2: e.activation(out=junk_D, in_=x1[s2], func=AF.Square, accum_out=ssq_D[s2]), reads=[t_x1[s2]], writes=[t_ssq_D[s2]])
                S.op("act", lambda e, s2=s2: e.activation(out=ssq_D[s2], in_=ssq_D[s2], func=AF.Sqrt, scale=1.0 / D, bias=EPS), reads=[t_ssq_D[s2]], writes=[t_ssq_D[s2]])
                S.op("dve", lambda e, s2=s2: e.reciprocal(out=ssq_D[s2], in_=ssq_D[s2]), reads=[t_ssq_D[s2]], writes=[t_ssq_D[s2]])
                S.op("pool", lambda e, s2=s2: e.tensor_scalar(out=xsf[s2].rearrange("p a b -> p (a b)"), in0=x1[s2], scalar1=ssq_D[s2], scalar2=1.0, op0=ALU.mult, op1=ALU.mult),
                     reads=[t_x1[s2], t_ssq_D[s2]], writes=[t_xsf[s2]])
                for q4 in range(2):
                    pp, tp = p512.get()

                    def tr(e, pp=pp, q4=q4, s2=s2):
                        for k in range(4):
                            i = e.transpose(out=pp[:, k * 128:(k + 1) * 128], in_=xsf[s2][:, q4 * 4 + k, :], identity=identf)
                        return i
                    S.op("pe", tr, reads=[t_xsf[s2], t_identf], writes=[tp])

                    def ev(e, pp=pp, q4=q4, s2=s2, t=t):
                        for k in range(4):
                            kk = q4 * 4 + k
                            i = e.tensor_scalar(out=hxf[s2][:, kk, :], in0=pp[:, k * 128:(k + 1) * 128], scalar1=modc[:, 5, kk:kk + 1], scalar2=modc[:, 4, kk:kk + 1], op0=ALU.mult, op1=ALU.add)
                        return i
                    S.op("dve", ev, reads=[tp, t_modc], writes=[t_hxf[s2]])
                S.op("pool", lambda e, s2=s2, t=t: e.tensor_copy(out=hx2[:, :, t * 128:(t + 1) * 128], in_=hxf[s2]), reads=[t_hxf[s2]], writes=[t_hx2[t]])
                pr, tpr = p128.get()

                def rt(e, pr=pr, s2=s2):
                    for k in range(8):
                        i = e.matmul(pr[:, 0:36], lhsT=hxf[s2][:, k, :], rhs=rwf[:, k, :], start=(k == 0), stop=(k == 7))
                    return i
                S.op("pe", rt, reads=[t_hxf[s2], t_rwf], writes=[tpr])
                r = rs_[s2]; tr_ = t_rs[s2]
                S.op("dve", lambda e, pr=pr, r=r: e.tensor_tensor(out=r[:, 0:36], in0=pr[:, 0:36], in1=rbb, op=ALU.add), reads=[tpr, t_rbb], writes=[tr_])
                S.op("dve", lambda e, r=r: e.tensor_reduce(out=r[:, 40:41], in_=r[:, 0:4], axis=mybir.AxisListType.X, op=ALU.max), reads=[tr_], writes=[tr_])
                S.op("dve", lambda e, r=r: e.tensor_scalar(out=r[:, 36:40], in0=r[:, 0:4], scalar1=r[:, 40:41], scalar2=None, op0=ALU.is_ge), reads=[tr_], writes=[tr_])
                S.op("dve", lambda e, r=r: e.tensor_scalar(out=r[:, 60:64], in0=r[:, 0:4], scalar1=r[:, 40:41], scalar2=None, op0=ALU.subtract), reads=[tr_], writes=[tr_])
                S.op("act", lambda e, r=r: e.activation(out=r[:, 60:64], in_=r[:, 60:64], func=AF.Exp, accum_out=r[:, 41:42]), reads=[tr_], writes=[tr_])
                S.op("dve", lambda e, r=r: e.reciprocal(out=r[:, 41:42], in_=r[:, 41:42]), reads=[tr_], writes=[tr_])
                S.op("dve", lambda e, r=r: e.tensor_scalar(out=r[:, 42:50], in0=r[:, 4:12], scalar1=r[:, 36:37], scalar2=None, op0=ALU.mult), reads=[tr_], writes=[tr_])
                for g in range(1, 4):
                    S.op("dve", lambda e, r=r, g=g: e.scalar_tensor_tensor(out=r[:, 42:50], in0=r[:, 4 + 8 * g:12 + 8 * g], scalar=r[:, 36 + g:37 + g], in1=r[:, 42:50], op0=ALU.mult, op1=ALU.add),
                         reads=[tr_], writes=[tr_])
                S.op("dve", lambda e, r=r: e.tensor_reduce(out=r[:, 50:51], in_=r[:, 42:50], axis=mybir.AxisListType.X, op=ALU.max), reads=[tr_], writes=[tr_])
                S.op("dve", lambda e, r=r: e.tensor_scalar(out=r[:, 42:50], in0=r[:, 42:50], scalar1=r[:, 50:51], scalar2=None, op0=ALU.subtract), reads=[tr_], writes=[tr_])
                S.op("act", lambda e, r=r: e.activation(out=r[:, 42:50], in_=r[:, 42:50], func=AF.Exp), reads=[tr_], writes=[tr_])
                S.op("dve", lambda e, r=r: e.tensor_scalar(out=r[:, 52:60], in0=r[:, 42:50], scalar1=1.0, scalar2=None, op0=ALU.is_ge), reads=[tr_], writes=[tr_])
                S.op("dve", lambda e, r=r: e.scalar_tensor_tensor(out=r[:, 4:12], in0=r[:, 52:60], scalar=-2.0, in1=r[:, 42:50], op0=ALU.mult, op1=ALU.add), reads=[tr_], writes=[tr_])
                S.op("dve", lambda e, r=r: e.tensor_reduce(out=r[:, 51:52], in_=r[:, 4:12], axis=mybir.AxisListType.X, op=ALU.max), reads=[tr_], writes=[tr_])
                S.op("dve", lambda e, r=r: e.tensor_scalar(out=r[:, 12:20], in0=r[:, 4:12], scalar1=r[:, 51:52], scalar2=None, op0=ALU.is_ge), reads=[tr_], writes=[tr_])
                S.op("dve", lambda e, r=r: e.scalar_tensor_tensor(out=r[:, 20:28], in0=r[:, 12:20], scalar=r[:, 51:52], in1=r[:, 52:60], op0=ALU.mult, op1=ALU.add), reads=[tr_], writes=[tr_])
                S.op("dve", lambda e, r=r: e.tensor_scalar(out=r[:, 50:51], in0=r[:, 51:52], scalar1=1.0, scalar2=None, op0=ALU.add), reads=[tr_], writes=[tr_])
                S.op("dve", lambda e, r=r: e.reciprocal(out=r[:, 50:51], in_=r[:, 50:51]), reads=[tr_], writes=[tr_])
                S.op("dve", lambda e, r=r: e.tensor_tensor(out=r[:, 50:51], in0=r[:, 50:51], in1=r[:, 41:42], op=ALU.mult), reads=[tr_], writes=[tr_])
                S.op("dve", lambda e, r=r: e.tensor_scalar(out=r[:, 20:28], in0=r[:, 20:28], scalar1=r[:, 50:51], scalar2=None, op0=ALU.mult), reads=[tr_], writes=[tr_])
                for g in range(4):
                    S.op("dve", lambda e, r=r, g=g, t=t: e.tensor_scalar(out=GT[:, t, g * 8:(g + 1) * 8], in0=r[:, 20:28], scalar1=r[:, 36 + g:37 + g], scalar2=None, op0=ALU.mult),
                         reads=[tr_], writes=[t_GT[t]])
            S.barrier()
            A = Amain
            A.release(markH)
            if stage == 5:
                dump(GT.rearrange("p a b -> p (a b)"), 0, 512)
                for i_ in range(4):
                    o_ = S.op("sp", lambda e, sig, i_=i_: sig(e.dma_start(out=y[i_ * 512:(i_ + 1) * 512, :], in_=x1d[i_ * 512:(i_ + 1) * 512, :])), extra=x1_ops, dma=1, key="dbg")
                    dump_ops.append(o_)
                dumpb(hx2[:, 0:4, :].rearrange("p a b -> p (a b)"), 0, 128, 0, SEQ)
                raise _Stop()
            p512 = Pool([(psb[b_][:, :], Tk(f"p512x_{b_}", excl=True)) for b_ in range(8)])
            ACC = A.alloc([NOT, D]); t_ACC = [[Tk(), Tk()] for _ in range(NOT)]
            for t in range(NOT):
                S.op("pool", lambda e, t=t: e.memset(ACC[:, t, :], 0.0), writes=t_ACC[t])
            e1f = A.alloc([8, 512]); t_e1f = Tk()
            e2f = A.alloc([2, D]); t_e2f = Tk()
            e1b = [A.alloc([8, 512], BF16) for _ in range(2)]; t_e1b = [Tk(), Tk()]
            e2b = [A.alloc([2, D], BF16) for _ in range(2)]; t_e2b = [Tk(), Tk()]
            sil = [A.alloc([512], BF16) for _ in range(2)]; t_sil = [Tk(), Tk()]
            hid = [A.alloc([512], BF16) for _ in range(4)]; t_hid = [Tk() for _ in range(4)]
            hc = 0
            for ex in range(NE):
                s = ex % 2
                v1 = ew1[ex].rearrange("(k p) n -> p k n", p=128); v3 = ew3[ex].rearrange("(k p) n -> p k n", p=128)
                v2 = ew2[ex].rearrange("(k p) n -> p k n", p=128)
                S.op("sp", lambda e, sig, v1=v1, v3=v3: (sig(e.dma_start(out=e1f[:, :, 0:256], in_=v1)), sig(e.dma_start(out=e1f[:, :, 256:512], in_=v3))), writes=[t_e1f], dma=2, key="e1f")
                S.op("sp", lambda e, sig, v2=v2: sig(e.dma_start(out=e2f, in_=v2)), writes=[t_e2f], dma=1, key="e2f")
                S.op("pool", lambda e, s=s: e.tensor_copy(out=e1b[s], in_=e1f), reads=[t_e1f], writes=[t_e1b[s]])
                S.op("pool", lambda e, s=s: e.tensor_copy(out=e2b[s], in_=e2f), reads=[t_e2f], writes=[t_e2b[s]])
                for tb in range(4):
                    hs = []
                    for fc in range(2):
                        p1, tp1 = p512.get(); p3, tp3 = p512.get()

                        def f1(e, p1=p1, s=s, fc=fc, tb=tb):
                            for k in range(8):
                                i = e.matmul(p1, lhsT=e1b[s][:, k, fc * 128:(fc + 1) * 128], rhs=hx2[:, k, tb * 512:(tb + 1) * 512], start=(k == 0), stop=(k == 7))
                            return i

                        def f3(e, p3=p3, s=s, fc=fc, tb=tb):
                            for k in range(8):
                                i = e.matmul(p3, lhsT=e1b[s][:, k, 256 + fc * 128:256 + (fc + 1) * 128], rhs=hx2[:, k, tb * 512:(tb + 1) * 512], start=(k == 0), stop=(k == 7))
                            return i
                        S.op("pe", f1, reads=[t_e1b[s]] + t_hx2[tb * 4:(tb + 1) * 4], writes=[tp1])
                        S.op("pe", f3, reads=[t_e1b[s]] + t_hx2[tb * 4:(tb + 1) * 4], writes=[tp3])
                        ss = hc % 2; h4 = hc % 4; hc += 1
                        S.op("act", lambda e, p1=p1, ss=ss: e.activation(out=sil[ss], in_=p1, func=AF.Silu), reads=[tp1], writes=[t_sil[ss]])
                        S.op("dve", lambda e, p3=p3, ss=ss, h4=h4: e.tensor_tensor(out=hid[h4], in0=p3, in1=sil[ss], op=ALU.mult), reads=[tp3, t_sil[ss]], writes=[t_hid[h4]])
                        hs.append(h4)
                    for tt in range(4):
                        t = tb * 4 + tt
                        for hh in range(2):
                            po, tpo = p512.get()

                            def f2(e, po=po, s=s, tt=tt, hh=hh, hs=tuple(hs)):
                                for fc in range(2):
                                    i = e.matmul(po, lhsT=hid[hs[fc]][:, tt * 128:(tt + 1) * 128], rhs=e2b[s][:, fc, hh * 512:(hh + 1) * 512], start=(fc == 0), stop=(fc == 1))
                                return i
                            S.op("pe", f2, reads=[t_hid[hs[0]], t_hid[hs[1]], t_e2b[s]], writes=[tpo])
                            S.op("dve", lambda e, po=po, t=t, hh=hh, ex=ex: e.scalar_tensor_tensor(out=ACC[:, t, hh * 512:(hh + 1) * 512], in0=po, scalar=GT[:, t, ex:ex + 1], in1=ACC[:, t, hh * 512:(hh + 1) * 512], op0=ALU.mult, op1=ALU.add),
                                 reads=[tpo, t_ACC[t][hh]], writes=[t_ACC[t][hh]])
            fngb = A.alloc([D]); t_fngb = Tk(); load(fngb, fng_bc[:, :], t_fngb, "c17")
            x1r = [A.alloc([D]) for _ in range(2)]; t_x1r = [Tk(), Tk()]
            x2 = [A.alloc([D]) for _ in range(2)]; t_x2 = [Tk(), Tk()]
            junk_E = A.alloc([D]); t_junk_E = Tk()
            ssq_E = [A.alloc([1]) for _ in range(2)]; t_ssq_E = [Tk(), Tk()]
            outs_ = []
            for t in range(NOT):
                s2 = t % 2
                S.op("sp", lambda e, sig, t=t, s2=s2: sig(e.dma_start(out=x1r[s2], in_=x1d[t * 128:(t + 1) * 128, :])), extra=[x1_ops[t]], writes=[t_x1r[s2]], dma=1, key=f"x1r{s2}")
                S.op("dve", lambda e, t=t, s2=s2: e.tensor_tensor(out=x2[s2], in0=ACC[:, t, :], in1=gffn, op=ALU.mult), reads=t_ACC[t] + [t_gffn], writes=[t_x2[s2]])
                S.op("dve", lambda e, s2=s2: e.tensor_tensor(out=x2[s2], in0=x2[s2], in1=x1r[s2], op=ALU.add), reads=[t_x2[s2], t_x1r[s2]], writes=[t_x2[s2]])
                S.op("act", lambda e, s2=s2: e.activation(out=junk_E, in_=x2[s2], func=AF.Square, accum_out=ssq_E[s2]), reads=[t_x2[s2]], writes=[t_ssq_E[s2]])
                S.op("act", lambda e, s2=s2: e.activation(out=ssq_E[s2], in_=ssq_E[s2], func=AF.Sqrt, scale=1.0 / D, bias=EPS), reads=[t_ssq_E[s2]], writes=[t_ssq_E[s2]])
                S.op("dve", lambda e, s2=s2: e.reciprocal(out=ssq_E[s2], in_=ssq_E[s2]), reads=[t_ssq_E[s2]], writes=[t_ssq_E[s2]])
                S.op("dve", lambda e, s2=s2: e.scalar_tensor_tensor(out=x2[s2], in0=x2[s2], scalar=ssq_E[s2], in1=fngb, op0=ALU.mult, op1=ALU.mult), reads=[t_x2[s2], t_ssq_E[s2], t_fngb], writes=[t_x2[s2]])
                o_ = S.op("sp", lambda e, sig, t=t, s2=s2: sig(e.dma_start(out=y[t * 128:(t + 1) * 128, :], in_=x2[s2])), reads=[t_x2[s2]], dma=1, key=f"yo{s2}")
                outs_.append(o_)
            S.op("sp", None, extra=outs_)
        try:
            author()
        except _Stop:
            pass
        if dump_ops:
            S.op("sp", None, extra=dump_ops)
        S.emit(block, sems)
    nc._sched = S
    return nc


def _prep(inputs):
    f = lambda a: np.ascontiguousarray(np.asarray(a, dtype=np.float32))
    x = f(inputs["x"]); c = f(inputs["c"]); ctx = f(inputs["ctx"]); c_ctx = f(inputs["c_ctx"])
    w_in = f(inputs["w_in"])[0]
    conv_w = f(inputs["conv_w"])[0]
    a_log = f(inputs["a_log"])[0]; dt_bias = f(inputs["dt_bias"])[0]
    idx = np.arange(128)
    tri = np.stack([(idx[:, None] <= idx[None, :]), (idx[:, None] > idx[None, :]), (idx[:, None] >= idx[None, :]), (idx[:, None] < idx[None, :])]).astype(np.float32)
    negm = np.stack([(idx[:, None] <= idx[None, :]), (idx[None, :] < idx[:, None]), (idx[:, None] >= idx[None, :]), (idx[None, :] > idx[:, None])]).astype(np.float32) * -100.0
    blk = lambda n: (idx[:, None] // n == idx[None, :] // n)
    dcm = [blk(8)]
    for n in (16, 32, 64, 128):
        low = blk(n) & ((idx[:, None] % n) >= n // 2) & ((idx[None, :] % n) < n // 2)
        dcm += [low, low.T]
    import ml_dtypes
    dcm = np.stack(dcm).astype(np.float32).astype(ml_dtypes.bfloat16)
    common = {
        "dcm": dcm,
        "ada_w": f(inputs["ada_w"])[0], "ada_b": f(inputs["ada_b"]).reshape(1, -1),
        "gcols": np.ascontiguousarray(np.stack([f(inputs["norm_mix_g"])[0].reshape(8, 128).T, f(inputs["norm_ffn_g"])[0].reshape(8, 128).T], axis=-1)),
        "gng": f(inputs["gdn_norm_g"])[0].reshape(128, 1),
        "ident": np.eye(128, dtype=np.float32), "tri": tri, "negm": negm,
        "w_rest": np.ascontiguousarray(w_in[:, COL_Z:]),
        "sgu_wT": np.ascontiguousarray(f(inputs["sgu_w"])[0].transpose(0, 2, 1)),
        "sgu_bb": np.ascontiguousarray(np.broadcast_to(f(inputs["sgu_b"])[0][None], (128, 8, 128))),
        "lng_c": np.ascontiguousarray(f(inputs["sgu_ln_g"])[0].reshape(8, 128).T),
        "lnb_bc": np.ascontiguousarray(np.broadcast_to(f(inputs["sgu_ln_b"])[0][None], (128, D))),
        "w_a": f(inputs["w_branch_a"])[0], "w_b": f(inputs["w_branch_b"])[0], "w_out": f(inputs["w_out"])[0],
        "rw": np.ascontiguousarray(np.concatenate([f(inputs["router_group_w"])[0], f(inputs["router_expert_w"])[0]], axis=1)),
        "rb": np.ascontiguousarray(np.broadcast_to(np.concatenate([f(inputs["router_group_b"])[0], f(inputs["router_expert_b"])[0]])[None], (128, 36))),
        "ew1": f(inputs["expert_w1"])[0], "ew3": f(inputs["expert_w3"])[0], "ew2": f(inputs["expert_w2"])[0],
        "fng_bc": np.ascontiguousarray(np.broadcast_to(f(inputs["final_norm_g"])[None], (128, D))),
    }
    maps = []
    for core in range(8):
        b, r = core // 4, core % 4
        heads = (2 * r, 2 * r + 1)
        qcols = np.concatenate([np.arange(base + h * 128, base + (h + 1) * 128) for base in (0, 1024, 2048) for h in heads])
        bacols = np.array([COL_BETA + d * 8 + h for d in range(2) for h in heads] + [COL_A + d * 8 + h for d in range(2) for h in heads])
        conv_c = np.ascontiguousarray(conv_w[:, qcols].reshape(5, 6, 128).transpose(2, 1, 0))
        gsc = np.concatenate([np.array([a_log[d, h] for d in range(2) for h in heads]), np.array([dt_bias[d, h] for d in range(2) for h in heads])]).astype(np.float32)
        qm = np.zeros((128, 4), np.float32); qm[:, r] = 1.0
        m = dict(common)
        m.update({
            "xb": x[b], "ctxb": ctx[b], "xo": np.ascontiguousarray(x[b, r * OWN:(r + 1) * OWN]),
            "cvT": np.ascontiguousarray(np.stack([c[b].reshape(8, 128).T, c_ctx.reshape(8, 128).T], axis=-1)),
            "w_qkv": np.ascontiguousarray(w_in[:, qcols]), "w_ba": np.ascontiguousarray(w_in[:, bacols]),
            "gsc66": np.ascontiguousarray(np.broadcast_to(gsc.reshape(1, 2, 1, 4), (128, 2, NT, 4))),
            "conv_c": conv_c, "gsc": np.ascontiguousarray(np.broadcast_to(gsc[None], (128, 8))), "qmask": qm,
        })
        maps.append(m)
    return maps


_NC = None


def kernel(**inputs):
    global _NC
    if _NC is None:
        _NC = build_program()
    maps = _prep(inputs)
    res = run_bass_kernel_spmd(_NC, maps, core_ids=list(range(8)))
    out = np.zeros((2, SEQ, D), np.float32)
    for core in range(8):
        b, r = core // 4, core % 4
        out[b, r * OWN:(r + 1) * OWN] = np.asarray(res.results[core]["y"], dtype=np.float32)
    return out
```

```python
import contextlib
import os
import sys
import numpy as np
import concourse.bass as bass
import concourse.mybir as mybir
from concourse.bass_utils import run_bass_kernel_spmd

F32 = mybir.dt.float32
BF16 = mybir.dt.bfloat16
ALU = mybir.AluOpType
AF = mybir.ActivationFunctionType

D = 1024
SEQ = 8192
CTX = 256
NT = 66
OWN = 2048
NOT = 16
NE = 32
DE = 256
COL_BETA = 3072
COL_A = COL_BETA + 16
COL_Z = COL_A + 16
EPS = 1e-6
ARENA = 53000


SAME_ENGINE_WAITS = os.environ.get('SAMEENG', '1') == '1'


class Tk:
    __slots__ = ("name", "w", "rd", "excl")

    def __init__(self, name="", excl=False):
        self.name = name
        self.w = None
        self.rd = []
        self.excl = excl


class Op:
    __slots__ = ("eng", "fn", "deps", "used", "sem", "val", "dma", "key", "idx", "inc", "src")


class Sched:
    ENGS = ("pe", "act", "dve", "pool", "sp")

    def __init__(self, nc):
        self.nc = nc
        self.ops = {e: [] for e in self.ENGS}
        self.all = []
        self.dma_since_barrier = []

    def op(self, eng, fn, reads=(), writes=(), dma=0, key=None, extra=(), inc=16):
        o = Op()
        o.eng = eng; o.fn = fn; o.used = False; o.sem = None; o.val = None
        o.dma = dma; o.key = key; o.idx = len(self.all); o.inc = inc
        o.src = sys._getframe(1).f_lineno
        deps = set(extra)
        reads = list(reads); writes = list(writes)
        for r in list(reads):
            if r.excl and r not in writes:
                writes.append(r)
        for r in reads:
            if r.w is not None:
                deps.add(r.w)
        for w in writes:
            if w.w is not None:
                deps.add(w.w)
            for x in w.rd:
                deps.add(x)
        for r in reads:
            r.rd.append(o)
        for w in writes:
            w.w = o
            w.rd = []
        o.deps = [d for d in deps if d is not o and not (d.eng == "pe" and eng == "pe" and not d.dma and not dma)
                  and not (SAME_ENGINE_WAITS is False and d.eng == eng and eng in ("act", "dve", "pool") and not d.dma and not dma)]
        for d in o.deps:
            d.used = True
        if dma:
            assert key is not None
            self.dma_since_barrier.append(o)
        self.ops[eng].append(o)
        self.all.append(o)
        return o

    def barrier(self, skip_cc=False):
        last = []
        for e in self.ENGS:
            for x in reversed(self.ops[e]):
                if x.fn is None:
                    break
                if skip_cc and x.dma and x.inc == 1:
                    continue
                last.append(x)
                break
        dmas = [x for x in self.dma_since_barrier if not (skip_cc and x.inc == 1)]
        self.dma_since_barrier = [x for x in self.dma_since_barrier if (skip_cc and x.inc == 1)]
        for e in self.ENGS:
            self.op(e, None, extra=[x for x in last if x.eng != e] + dmas)

    def emit(self, block, sems):
        nc = self.nc
        sems = list(sems)
        eng_sem = {e: sems.pop() for e in ("pe", "act", "dve", "pool")}
        keysem = {}
        cnt = {e: 0 for e in eng_sem}
        kcnt = {}
        for o in self.all:
            if o.dma:
                if o.key not in keysem:
                    keysem[o.key] = sems.pop()
                    kcnt[o.key] = 0
                kcnt[o.key] += o.inc * o.dma
                o.sem = keysem[o.key]; o.val = kcnt[o.key]
            elif o.used:
                assert o.fn is not None
                cnt[o.eng] += 1
                o.sem = eng_sem[o.eng]; o.val = cnt[o.eng]
        engobj = {"pe": nc.tensor, "act": nc.scalar, "dve": nc.vector, "pool": nc.gpsimd, "sp": nc.sync}
        deco = {"pe": block.tensor, "act": block.scalar, "dve": block.vector, "pool": block.gpsimd, "sp": block.sync}

        def run(ename):
            def body(e):
                known = {}
                for o in self.ops[ename]:
                    for d in sorted(o.deps, key=lambda x: x.idx):
                        sid = id(d.sem)
                        if known.get(sid, 0) >= d.val:
                            continue
                        e.wait_ge(d.sem, d.val)
                        known[sid] = d.val
                    if o.fn is None:
                        continue
                    if o.dma:
                        n = [0]

                        def sig(inst, o=o, n=n):
                            if o.inc == 16:
                                inst.then_inc(o.sem, 16)
                            else:
                                inst.then_inc(o.sem)
                            n[0] += 1
                            return inst
                        o.fn(e, sig)
                        assert n[0] == o.dma, (n[0], o.dma)
                    else:
                        inst = o.fn(e)
                        if o.used:
                            inst.then_inc(o.sem, 1)
            return body
        for ename in self.ENGS:
            deco[ename](run(ename))


class Arena:
    def __init__(self, ap_f32, nf32, base=0):
        self.ap = ap_f32
        self.n = base + nf32
        self.off = base
        self.hi = 0

    def mark(self):
        return self.off

    def release(self, m):
        self.off = m

    def alloc(self, free_shape, dtype=F32):
        n = int(np.prod(free_shape))
        nf = n if dtype == F32 else (n + 1) // 2
        nf = (nf + 1) // 2 * 2
        assert self.off + nf <= self.n, ("arena overflow", self.off, nf, self.n)
        v = self.ap[:, self.off:self.off + nf]
        self.off += nf
        self.hi = max(self.hi, self.off)
        if dtype != F32:
            v = v.bitcast(dtype)[:, 0:n]
        else:
            v = v[:, 0:n]
        if len(free_shape) == 2:
            v = v.rearrange("p (a b) -> p a b", a=free_shape[0])
        elif len(free_shape) == 3:
            v = v.rearrange("p (a b c) -> p a b c", a=free_shape[0], b=free_shape[1])
        return v


class _Stop(Exception):
    pass


def build_program(debug=False, stage=99):
    KCUT = int(os.environ.get('KCUT', '0')); NTL = int(os.environ.get('NTL', str(NT)))
    nc = bass.Bass("TRN2", target_bir_lowering=False)

    def din(name, shape, dt=F32):
        return nc.dram_tensor(name, list(shape), dt, kind="ExternalInput")

    xb = din("xb", [SEQ, D]); ctxb = din("ctxb", [CTX, D]); xo = din("xo", [OWN, D])
    cvT = din("cvT", [128, 8, 2]); ada_w = din("ada_w", [D, 6 * D]); ada_b = din("ada_b", [1, 6 * D])
    gcols = din("gcols", [128, 8, 2])
    w_qkv = din("w_qkv", [D, 768]); w_ba = din("w_ba", [D, 8])
    gsc66 = din("gsc66", [128, 2, NT, 4])
    conv_c = din("conv_c", [128, 6, 5]); gsc = din("gsc", [128, 8]); gng = din("gng", [128, 1])
    dcm_d = din("dcm", [9, 128, 128], BF16)
    ident_d = din("ident", [128, 128]); tri_d = din("tri", [4, 128, 128]); neg_d = din("negm", [4, 128, 128], BF16)
    w_rest = din("w_rest", [D, 5120]); sgu_wT = din("sgu_wT", [8, 128, 128]); sgu_bb = din("sgu_bb", [128, 8, 128])
    lng_c = din("lng_c", [128, 8]); lnb_bc = din("lnb_bc", [128, D])
    w_a = din("w_a", [D, D]); w_b = din("w_b", [D, D]); w_out = din("w_out", [D, D])
    rw = din("rw", [D, 36]); rb = din("rb", [128, 36])
    ew1 = din("ew1", [NE, D, DE]); ew3 = din("ew3", [NE, D, DE]); ew2 = din("ew2", [NE, DE, D])
    fng_bc = din("fng_bc", [128, D]); qmask_d = din("qmask", [128, 4])
    y = nc.dram_tensor("y", [OWN, D], F32, kind="ExternalOutput")
    snd = [nc.dram_tensor(f"snd{j}", [2 * 128, 1024], F32) for j in range(4)]
    rcv = [nc.dram_tensor(f"rcv{j}", [4 * 2 * 128, 1024], F32) for j in range(4)]
    x1d = nc.dram_tensor("x1d", [OWN, D], F32)
    oacc_d = nc.dram_tensor("oacc_d", [128, 128, 128], BF16)
    dbg = None
    if debug:
        dbg = nc.dram_tensor("dbg", [128, 4096], F32, kind="ExternalOutput")
        dbgb = nc.dram_tensor("dbgb", [768, SEQ], BF16, kind="ExternalOutput")

    es = contextlib.ExitStack()
    with es:
        arena_t = es.enter_context(nc.sbuf_tensor("arena", [128, ARENA], F32))
        psb = [es.enter_context(nc.psum_tensor(f"psb{i}", [128, 512], F32)) for i in range(8)]
        sems = [es.enter_context(nc.semaphore(f"s{i}")) for i in range(100)]
        block = es.enter_context(nc.Block())
        A = Arena(arena_t, ARENA)
        S = Sched(nc)

        dump_ops = []

        def dump(ap, col0, ncols, reads=()):
            o_ = S.op("sp", lambda e, sig: sig(e.dma_start(out=dbg[:, col0:col0 + ncols], in_=ap)), reads=list(reads), dma=1, key="dbg")
            dump_ops.append(o_)

        def cut(k, dumps):
            if stage == k:
                S.barrier()
                for d_ in dumps():
                    dump(*d_)
                raise _Stop()

        def dumpb(ap, row0, nrows, col0, ncols, reads=(), extra=()):
            o_ = S.op("sp", lambda e, sig: sig(e.dma_start(out=dbgb[row0:row0 + nrows, col0:col0 + ncols], in_=ap)), reads=list(reads), extra=list(extra), dma=1, key="dbg")
            dump_ops.append(o_)

        def author():
            nonlocal A
            class Pool:
                def __init__(self, items):
                    self.items = items
                    self.i = 0

                def get(self):
                    it = self.items[self.i % len(self.items)]
                    self.i += 1
                    return it
            p128 = Pool([(psb[b][:, 0:128], Tk(f"p128_{b}", excl=True)) for b in range(4)])
            p512 = Pool([(psb[b][:, :], Tk(f"p512_{b}", excl=True)) for b in range(4, 8)])

            def bfv(ap):
                n = ap.shape[-1]
                return ap.bitcast(BF16)[:, 0:n]

            def load(dst, src, tk, key):
                return S.op("sp", lambda e, sig: sig(e.dma_start(out=dst, in_=src)), writes=[tk], dma=1, key=key)

            identf = A.alloc([128]); t_identf = Tk(); load(identf, ident_d[:, :], t_identf, "c0")
            identb = A.alloc([128], BF16); t_identb = Tk()
            S.op("pool", lambda e: e.tensor_copy(out=identb, in_=identf), reads=[t_identf], writes=[t_identb])
            trif = A.alloc([4, 128]); t_trif = Tk(); load(trif, tri_d.ap().rearrange("a p q -> p a q"), t_trif, "c1")
            negb = A.alloc([4, 128], BF16); t_negb = Tk(); load(negb, neg_d.ap().rearrange("a p q -> p a q"), t_negb, "c2")
            dcm = A.alloc([9, 128], BF16); t_dcm = Tk(); load(dcm, dcm_d.ap().rearrange("a p q -> p a q"), t_dcm, "c2b")
            onesf = A.alloc([128]); t_onesf = Tk()
            S.op("pool", lambda e: e.memset(onesf, 1.0), writes=[t_onesf])
            Uf, Vf, Ub, Vb = (trif[:, i, :] for i in range(4))
            gscs = A.alloc([8]); t_gscs = Tk(); load(gscs, gsc[:, :], t_gscs, "c3")
            negA = A.alloc([4]); t_negA = Tk()
            S.op("act", lambda e: e.activation(out=negA, in_=gscs[:, 0:4], func=AF.Exp), reads=[t_gscs], writes=[t_negA])
            S.op("dve", lambda e: e.tensor_scalar(out=negA, in0=negA, scalar1=-1.0, scalar2=None, op0=ALU.mult), reads=[t_negA], writes=[t_negA])
            dtb = gscs[:, 4:8]
            convc = A.alloc([6, 5]); t_convc = Tk(); load(convc, conv_c[:, :, :], t_convc, "c4")
            gngs = A.alloc([1]); t_gngs = Tk(); load(gngs, gng[:, :], t_gngs, "c5")
            qmask = A.alloc([4]); t_qmask = Tk(); load(qmask, qmask_d[:, :], t_qmask, "c6")
            gcol = A.alloc([8, 2]); t_gcol = Tk(); load(gcol, gcols[:, :, :], t_gcol, "c7")
            modc = A.alloc([6, 8]); t_modc = Tk("modc")
            gmix = A.alloc([D]); t_gmix = Tk("gmix")
            gffn = A.alloc([D]); t_gffn = Tk("gffn")
            GT = A.alloc([NOT, 32]); t_GT = [Tk() for _ in range(NOT)]

            m0 = A.mark()
            scT = A.alloc([8, 2]); t_scT = Tk(); load(scT, cvT[:, :, :], t_scT, "c8")
            S.op("act", lambda e: e.activation(out=scT, in_=scT, func=AF.Silu), reads=[t_scT], writes=[t_scT])
            modrow = A.alloc([6 * D]); t_modrow = Tk("modrow")
            modrow_c = A.alloc([6 * D]); t_modrow_c = Tk("modrow_c")
            adb = A.alloc([6 * D]); t_adb = Tk()
            S.op("sp", lambda e, sig: (sig(e.dma_start(out=adb[0:1, :], in_=ada_b[:, :])), sig(e.dma_start(out=adb[1:2, :], in_=ada_b[:, :]))),
                 writes=[t_adb], dma=2, key="c9")
            adw = [A.alloc([8, 512]) for _ in range(2)]; t_adw = [Tk(), Tk()]
            ada_v = ada_w.ap().rearrange("(k p) n -> p k n", p=128)
            for nb in range(12):
                sl = nb % 2
                S.op("sp", lambda e, sig, nb=nb, sl=sl: (sig(e.dma_start(out=adw[sl][:, 0:4, :], in_=ada_v[:, 0:4, nb * 512:(nb + 1) * 512])),
                                                        sig(e.dma_start(out=adw[sl][:, 4:8, :], in_=ada_v[:, 4:8, nb * 512:(nb + 1) * 512]))),
                     writes=[t_adw[sl]], dma=2, key=f"adw{sl}")
                pp, tp = p512.get()

                def f(e, sl=sl, pp=pp):
                    for k in range(8):
                        i = e.matmul(pp[0:2, :], lhsT=scT[:, k, :], rhs=adw[sl][:, k, :], start=(k == 0), stop=(k == 7))
                    return i
                S.op("pe", f, reads=[t_scT, t_adw[sl]], writes=[tp])
                S.op("dve", lambda e, nb=nb, pp=pp: e.tensor_tensor(out=modrow[0:2, nb * 512:(nb + 1) * 512], in0=pp[0:2, :], in1=adb[0:2, nb * 512:(nb + 1) * 512], op=ALU.add),
                     reads=[tp, t_adb], writes=[t_modrow])
            S.op("sp", lambda e, sig: sig(e.dma_start(out=modrow_c[0:1, :], in_=modrow[1:2, :])), reads=[t_modrow], writes=[t_modrow_c], dma=1, key="c10")
            pp, tp = p128.get()
            vecs = [(modrow, 0), (modrow, 1), (modrow_c, 0), (modrow_c, 1), (modrow, 3), (modrow, 4)]

            def f(e, pp=pp):
                for vi, (row, m) in enumerate(vecs):
                    for k in range(8):
                        i = e.matmul(pp[:, vi * 8 + k:vi * 8 + k + 1], lhsT=row[0:1, m * D + k * 128:m * D + (k + 1) * 128], rhs=onesf[0:1, 0:1], start=True, stop=True)
                return i
            S.op("pe", f, reads=[t_modrow, t_modrow_c, t_onesf], writes=[tp])
            S.op("dve", lambda e, pp=pp: e.tensor_copy(out=modc.rearrange("p a b -> p (a b)"), in_=pp[:, 0:48]), reads=[tp], writes=[t_modc])
            for vi, gi in ((1, 0), (3, 0), (5, 1)):
                S.op("dve", lambda e, vi=vi, gi=gi: e.scalar_tensor_tensor(out=modc[:, vi, :], in0=modc[:, vi, :], scalar=1.0, in1=gcol[:, :, gi], op0=ALU.add, op1=ALU.mult),
                     reads=[t_modc, t_gcol], writes=[t_modc])
            for dst, tdst, m in ((gmix, t_gmix, 2), (gffn, t_gffn, 5)):
                for h in range(2):
                    pp, tp = p512.get()
                    S.op("pe", lambda e, pp=pp, m=m, h=h: e.matmul(pp, lhsT=onesf[0:1, :], rhs=modrow[0:1, m * D + h * 512:m * D + (h + 1) * 512], start=True, stop=True),
                         reads=[t_modrow, t_onesf], writes=[tp])
                    S.op("act", lambda e, pp=pp, dst=dst, h=h: e.copy(out=dst[:, h * 512:(h + 1) * 512], in_=pp), reads=[tp], writes=[tdst])
            S.barrier()
            A.release(m0)
            if stage == 0:
                dump(modc.rearrange("p a b -> p (a b)"), 0, 48); dump(gmix[:, 0:256], 64, 256); dump(gffn[:, 0:256], 320, 256)
                raise _Stop()

            mA = A.mark()
            SC = A.alloc([NT, 28]); t_SC = [Tk("SC")] * NT
            QN = A.alloc([NT, 2, 128], BF16); KN = A.alloc([NT, 2, 128], BF16); VV = A.alloc([NT, 2, 128], BF16)
            t_QKV = [[Tk() for _ in range(6)] for t in range(NT)]
            mA1 = A.mark()
            wst = A.alloc([8, 776]); t_wst = Tk()
            wq = A.alloc([8, 896], BF16); t_wq = Tk()
            S.op("sp", lambda e, sig: (sig(e.dma_start(out=wst[:, :, 0:768], in_=w_qkv.ap().rearrange("(k p) n -> p k n", p=128))),
                                       sig(e.dma_start(out=wst[:, :, 768:776], in_=w_ba.ap().rearrange("(k p) n -> p k n", p=128)))),
                 writes=[t_wst], dma=2, key="wst")
            S.op("pool", lambda e: e.tensor_copy(out=wq[:, :, 0:776], in_=wst), reads=[t_wst], writes=[t_wq])
            xt_A = [A.alloc([D]) for _ in range(3)]; t_xt_A = [Tk() for _ in range(3)]
            junk_A = A.alloc([D], BF16); t_junk_A = Tk()
            ssq_A = [A.alloc([1]) for _ in range(3)]; t_ssq_A = [Tk() for _ in range(3)]
            xs_A = [A.alloc([8, 128], BF16) for _ in range(3)]; t_xs_A = [Tk(), Tk(), Tk()]
            hxT = [A.alloc([8, 128], BF16) for _ in range(2)]; t_hxT = [Tk(), Tk()]; t_hxTb = [Tk(), Tk()]
            PRE = [A.alloc([6, 132]) for _ in range(4)]; t_PRE = [Tk() for _ in range(4)]
            CV = A.alloc([6, 128]); t_CVc = [Tk() for _ in range(6)]
            SQs = [A.alloc([6, 128], BF16) for _ in range(2)]; t_SQs = [Tk(), Tk()]
            sm = [A.alloc([40]) for _ in range(2)]; t_sm = [Tk(), Tk()]
            nr = [A.alloc([8]) for _ in range(2)]; t_nr = [Tk(), Tk()]

            wst_flat = wst.rearrange("p a b -> p (a b)")
            BA = wst_flat[:, 0:NT * 8].rearrange("p (a b) -> p a b", b=8); t_BA = Tk("BA")
            g66 = A.alloc([2, NT, 4]); t_g66 = Tk(); load(g66, gsc66[:, :, :, :], t_g66, "c3b")
            Mx = [wst_flat[:, 1024 + i_ * 512:1024 + i_ * 512 + NT * 4].rearrange("p (a b) -> p a b", b=4) for i_ in range(4)]; t_Mx = [Tk() for _ in range(4)]

            def rows_of(t):
                if t < 2:
                    return ctxb[t * 128:(t + 1) * 128, :]
                return xb[(t - 2) * 128:(t - 1) * 128, :]

            def front(t, part):
                s3 = t % 3; s2 = t % 2
                if part == "b":
                    return front_b(t, s3, s2)
                isctx = t < 2
                if part == "tr":
                    return front_tr(t, s3, s2, isctx)
                shc = modc[:, 2 if isctx else 0, :]; scc = modc[:, 3 if isctx else 1, :]
                S.op("sp", lambda e, sig: sig(e.dma_start(out=xt_A[s3], in_=rows_of(t))), writes=[t_xt_A[s3]], dma=1, key=f"xt_A{s3}")
                S.op("act", lambda e: e.activation(out=junk_A, in_=xt_A[s3], func=AF.Square, accum_out=ssq_A[s3]), reads=[t_xt_A[s3]], writes=[t_ssq_A[s3]])
                S.op("act", lambda e: e.activation(out=ssq_A[s3], in_=ssq_A[s3], func=AF.Sqrt, scale=1.0 / D, bias=EPS), reads=[t_ssq_A[s3]], writes=[t_ssq_A[s3]])
                S.op("dve", lambda e: e.reciprocal(out=ssq_A[s3], in_=ssq_A[s3]), reads=[t_ssq_A[s3]], writes=[t_ssq_A[s3]])
                S.op("pool", lambda e: e.tensor_scalar(out=xs_A[s3].rearrange("p a b -> p (a b)"), in0=xt_A[s3], scalar1=ssq_A[s3], scalar2=1.0, op0=ALU.mult, op1=ALU.mult),
                     reads=[t_xt_A[s3], t_ssq_A[s3]], writes=[t_xs_A[s3]])
                return

            def front_tr(t, s3, s2, isctx):
                shc = modc[:, 2 if isctx else 0, :]; scc = modc[:, 3 if isctx else 1, :]
                pp, tp = p512.get()
                ppb = pp.bitcast(BF16)

                def tr(e):
                    for k in range(8):
                        i = e.transpose(out=ppb[:, k * 128:(k + 1) * 128], in_=xs_A[s3][:, k, :], identity=identb)
                    return i
                S.op("pe", tr, reads=[t_xs_A[s3], t_identb], writes=[tp])

                def ev_d(e):
                    for k in range(8):
                        i = e.tensor_scalar(out=hxT[s2][:, k, :], in0=ppb[:, k * 128:(k + 1) * 128], scalar1=scc[:, k:k + 1], scalar2=shc[:, k:k + 1], op0=ALU.mult, op1=ALU.add)
                    return i
                t_h2 = t_hxTb[s2]
                S.op("dve", ev_d, reads=[tp, t_modc], writes=[t_hxT[s2], t_h2])
                return

            def front_b(t, s3, s2):
                s3 = t % 4
                pA, tA = p512.get()
                pB, tB = p512.get()

                def pjA(e):
                    for ch in range(4):
                        for k in range(8):
                            i = e.matmul(pA[:, ch * 128:(ch + 1) * 128], lhsT=wq[:, k, ch * 128:(ch + 1) * 128], rhs=hxT[s2][:, k, :], start=(k == 0), stop=(k == 7))
                    return i

                def pjB(e):
                    for ch in range(4, 6):
                        for k in range(8):
                            i = e.matmul(pB[:, (ch - 4) * 128:(ch - 3) * 128], lhsT=wq[:, k, ch * 128:(ch + 1) * 128], rhs=hxT[s2][:, k, :], start=(k == 0), stop=(k == 7))
                    for k in range(8):
                        i = e.matmul(pB[:, 256:264], lhsT=hxT[s2][:, k, :], rhs=wq[:, k, 768:776], start=(k == 0), stop=(k == 7))
                    return i
                S.op("pe", pjA, reads=[t_wq, t_hxT[s2]], writes=[tA])
                S.op("pe", pjB, reads=[t_wq, t_hxT[s2]], writes=[tB])
                pba = pB[:, 256:264]; tba = tB
                S.op("dve", lambda e: e.tensor_copy(out=PRE[s3][:, 0:4, 2:130], in_=pA.rearrange("p (a b) -> p a b", a=4)), reads=[tA], writes=[t_PRE[s3]])
                S.op("dve", lambda e: e.tensor_copy(out=PRE[s3][:, 4:6, 2:130], in_=pB[:, 0:256].rearrange("p (a b) -> p a b", a=2)), reads=[tB], writes=[t_PRE[s3]])
                if KCUT == 2:
                    return
                first = t in (0, 2); last = t in (1, NT - 1)
                if first:
                    S.op("pool", lambda e: e.memset(PRE[s3][:, :, 0:2], 0.0), writes=[t_PRE[s3]])
                else:
                    sp_ = (t - 1) % 4
                    S.op("pool", lambda e: e.tensor_copy(out=PRE[sp_][:, :, 130:132], in_=PRE[s3][:, :, 2:4]), reads=[t_PRE[s3]], writes=[t_PRE[sp_]])
                if last:
                    S.op("pool", lambda e: e.memset(PRE[s3][:, :, 130:132], 0.0), writes=[t_PRE[s3]])
                else:
                    sn = (t + 1) % 4
                    S.op("pool", lambda e: e.tensor_copy(out=PRE[sn][:, :, 0:2], in_=PRE[s3][:, :, 128:130]), reads=[t_PRE[s3]], writes=[t_PRE[sn]])
                S.op("dve", lambda e: e.tensor_copy(out=BA[:, t, :], in_=pba), reads=[tba], writes=[t_BA])

            def lag(t, part):
                s3 = t % 4; s2 = t % 2
                SQ = SQs[s2]; t_SQ = t_SQs[s2]
                if part == "rest":
                    return lag_rest(t, s2, SQ, t_SQ)
                for j in range(5):
                    for ch in range(6):
                        tcv = t_CVc[ch]

                        def cvj(e, ch=ch, j=j):
                            if j == 0:
                                return e.tensor_scalar(out=CV[:, ch, :], in0=PRE[s3][:, ch, 0:128], scalar1=convc[:, ch, 0:1], scalar2=None, op0=ALU.mult)
                            return e.scalar_tensor_tensor(out=CV[:, ch, :], in0=PRE[s3][:, ch, j:j + 128], scalar=convc[:, ch, j:j + 1], in1=CV[:, ch, :], op0=ALU.mult, op1=ALU.add)
                        S.op("dve", cvj, reads=[t_PRE[s3], t_convc] + ([tcv] if j else []), writes=[tcv])
                S.op("act", lambda e: e.activation(out=SQ, in_=CV, func=AF.Silu), reads=t_CVc, writes=[t_SQ])
                return

            def lag_rest(t, s2, SQ, t_SQ):
                pT, tT = p512.get()
                pTb = pT.bitcast(BF16)

                def trs(e):
                    for ch in range(6):
                        i = e.transpose(out=pTb[:, ch * 128:(ch + 1) * 128], in_=SQ[:, ch, :], identity=identb)
                    return i
                S.op("pe", trs, reads=[t_SQ, t_identb], writes=[tT])
                pts = [(pTb[:, ch * 128:(ch + 1) * 128], tT) for ch in range(6)]
                n = nr[s2]; tn = t_nr[s2]
                for ch in range(4):
                    S.op("act", lambda e, ch=ch: e.activation(out=junk_A[:, 0:128], in_=pts[ch][0], func=AF.Square, accum_out=n[:, ch:ch + 1]),
                         reads=[pts[ch][1]], writes=[tn])
                S.op("act", lambda e: e.activation(out=n[:, 0:4], in_=n[:, 0:4], func=AF.Sqrt, bias=EPS), reads=[tn], writes=[tn])
                S.op("dve", lambda e: e.reciprocal(out=n[:, 0:4], in_=n[:, 0:4]), reads=[tn], writes=[tn])
                S.op("dve", lambda e: e.tensor_scalar(out=n[:, 0:2], in0=n[:, 0:2], scalar1=128.0 ** -0.5, scalar2=None, op0=ALU.mult), reads=[tn], writes=[tn])
                dsts = [QN[:, t, 0, :], QN[:, t, 1, :], KN[:, t, 0, :], KN[:, t, 1, :], VV[:, t, 0, :], VV[:, t, 1, :]]
                for ch in range(6):
                    if ch < 4:
                        S.op("dve", lambda e, ch=ch: e.tensor_scalar(out=dsts[ch], in0=pts[ch][0], scalar1=n[:, ch:ch + 1], scalar2=None, op0=ALU.mult),
                             reads=[pts[ch][1], tn], writes=[t_QKV[t][ch]])
                    else:
                        S.op("act", lambda e, ch=ch: e.copy(out=dsts[ch], in_=pts[ch][0]), reads=[pts[ch][1]], writes=[t_QKV[t][ch]])

            front(0, "a"); front(1, "a"); front(0, "tr")
            for i in range(NTL + 3):
                if i + 2 < NTL:
                    front(i + 2, "a")
                if i + 1 < NTL:
                    front(i + 1, "tr")
                if 2 <= i < NTL + 2:
                    lag(i - 2, "conv")
                if i < NTL:
                    front(i, "b")
                if i >= 3:
                    lag(i - 3, "rest")
            tS = t_SC[0]
            SCk = lambda k: SC[:, :, k * 4:(k + 1) * 4]
            M0, M1, M2, M3 = Mx; tM0, tM1, tM2, tM3 = t_Mx
            S.op("act", lambda e: e.activation(out=g66[:, 0, :, :], in_=g66[:, 0, :, :], func=AF.Exp), reads=[t_g66], writes=[t_g66])
            S.op("act", lambda e: e.activation(out=SCk(3), in_=BA[:, :, 0:4], func=AF.Sigmoid), reads=[t_BA], writes=[tS])
            S.op("dve", lambda e: e.tensor_tensor(out=M0, in0=BA[:, :, 4:8], in1=g66[:, 1, :, :], op=ALU.add), reads=[t_BA, t_g66], writes=[tM0])
            S.op("dve", lambda e: e.tensor_scalar(out=M1, in0=M0, scalar1=-1.0, scalar2=None, op0=ALU.mult), reads=[tM0], writes=[tM1])
            S.op("dve", lambda e: e.tensor_tensor(out=M1, in0=M1, in1=M0, op=ALU.min), reads=[tM0, tM1], writes=[tM1])
            S.op("act", lambda e: e.activation(out=M1, in_=M1, func=AF.Exp), reads=[tM1], writes=[tM1])
            S.op("act", lambda e: e.activation(out=M1, in_=M1, func=AF.Ln, bias=1.0), reads=[tM1], writes=[tM1])
            S.op("dve", lambda e: e.tensor_scalar(out=M2, in0=M0, scalar1=0.0, scalar2=None, op0=ALU.max), reads=[tM0], writes=[tM2])
            S.op("dve", lambda e: e.tensor_tensor(out=M2, in0=M2, in1=M1, op=ALU.add), reads=[tM1, tM2], writes=[tM2])
            S.op("dve", lambda e: e.scalar_tensor_tensor(out=SCk(6), in0=M2, scalar=-1.0, in1=g66[:, 0, :, :], op0=ALU.mult, op1=ALU.mult), reads=[tM2, t_g66], writes=[tS])
            pcA, tcA = p512.get(); pcB, tcB = p512.get()

            def cums(e):
                e.matmul(pcA[:, 0:2 * NT], lhsT=Uf, rhs=SC[:, :, 24:26], start=True, stop=True)
                return e.matmul(pcA[:, 2 * NT:4 * NT], lhsT=Ub, rhs=SC[:, :, 26:28], start=True, stop=True)
            S.op("pe", cums, reads=[tS, t_trif], writes=[tcA])
            S.op("pe", lambda e: e.matmul(pcB[:, 0:4 * NT], lhsT=onesf, rhs=SC[:, :, 24:28], start=True, stop=True), reads=[tS, t_onesf], writes=[tcB])
            Gf_ps = pcA[:, 0:2 * NT].rearrange("p (a b) -> p a b", b=2); Gb_ps = pcA[:, 2 * NT:4 * NT].rearrange("p (a b) -> p a b", b=2)
            Gt_ps = pcB[:, 0:4 * NT].rearrange("p (a b) -> p a b", b=4)
            S.op("act", lambda e: e.activation(out=SC[:, :, 16:18], in_=Gf_ps, func=AF.Exp), reads=[tcA], writes=[tS])
            S.op("act", lambda e: e.activation(out=SC[:, :, 18:20], in_=Gb_ps, func=AF.Exp), reads=[tcA], writes=[tS])
            S.op("act", lambda e: e.activation(out=SCk(5), in_=Gt_ps, func=AF.Exp), reads=[tcB], writes=[tS])
            S.op("dve", lambda e: e.tensor_copy(out=M3[:, :, 0:2], in_=Gf_ps), reads=[tcA], writes=[tM3])
            S.op("dve", lambda e: e.tensor_copy(out=M3[:, :, 2:4], in_=Gb_ps), reads=[tcA], writes=[tM3])
            S.op("dve", lambda e: e.tensor_tensor(out=M3, in0=Gt_ps, in1=M3, op=ALU.subtract), reads=[tcB, tM3], writes=[tM3])
            S.op("act", lambda e: e.activation(out=SCk(2), in_=M3, func=AF.Exp), reads=[tM3], writes=[tS])
            S.op("dve", lambda e: e.tensor_scalar(out=SCk(0), in0=SCk(3), scalar1=-1.0, scalar2=None, op0=ALU.mult), reads=[tS], writes=[tS])
            S.op("dve", lambda e: e.tensor_tensor(out=SCk(1), in0=SCk(3), in1=SCk(4), op=ALU.mult), reads=[tS], writes=[tS])
            S.barrier()
            A.release(mA1)
            if stage == 1:
                for i_, t_ in enumerate((0, 1, 2, 3, 33, 65)):
                    dump(SC[:, t_, :], i_ * 32, 28)
                    dumpb(QN[:, t_, :, :].rearrange("p a b -> p (a b)"), 0, 128, i_ * 768, 256)
                    dumpb(KN[:, t_, :, :].rearrange("p a b -> p (a b)"), 0, 128, i_ * 768 + 256, 256)
                    dumpb(VV[:, t_, :, :].rearrange("p a b -> p (a b)"), 0, 128, i_ * 768 + 512, 256)
                raise _Stop()

            p128_small = p128
            p128 = Pool([(psb[b_][:, 0:128], Tk(f"p128x_{b_}", excl=True)) for b_ in range(8)])
            chains = [(hl, d) for hl in range(2) for d in range(2)]
            cb = {}
            for c in chains:
                Sf = A.alloc([128]); tSf = Tk("S"); Sbb = A.alloc([128], BF16); tSbb = Tk("Sb")
                S.op("pool", lambda e, Sf=Sf: e.memset(Sf, 0.0), writes=[tSf])
                S.op("pool", lambda e, Sbb=Sbb: e.memset(Sbb, 0.0), writes=[tSbb])
                for st_ in range(2):
                    b = {}
                    for nm in ("knT", "qnT", "qdT", "X0", "XT0", "Xb", "XbT", "X0s", "XT0s", "X1s", "XT1s", "P0", "P1", "PT0", "PT1", "No", "NoT", "Wb", "Vb", "kbg", "kd", "vb", "nwT", "vnew", "QKm", "qd", "E", "ET", "ob"):
                        b[nm] = A.alloc([128], BF16); b["t_" + nm] = Tk(nm)
                    for nm in ("gV", "gU"):
                        b[nm] = A.alloc([128]); b["t_" + nm] = Tk(nm)
                    b["S"] = Sf; b["t_S"] = tSf; b["Sb"] = Sbb; b["t_Sb"] = tSbb
                    cb[(c, st_)] = b
            t_OACC = [[Tk() for _ in range(2)] for _ in range(64)]
            ost = [A.alloc([128], BF16) for _ in range(4)]; t_ost = [Tk() for _ in range(4)]
            ost_cnt = [0]
            ofin = [A.alloc([128]) for _ in range(2)]; t_ofin = [Tk(), Tk()]
            onb = [A.alloc([128], BF16) for _ in range(2)]; t_onb = [Tk(), Tk()]
            onT = [A.alloc([128], BF16) for _ in range(4)]; t_onT = [Tk() for _ in range(4)]
            fsm = [A.alloc([4]) for _ in range(2)]; t_fsm = [Tk(), Tk()]
            junk_B = A.alloc([128])
            visited = set()
            snd_ops = []
            qops = [[] for _ in range(4)]
            ccs = []
            fin_cnt = [0]
            evq = [0]

            def evac_copy(dst, src, rd, wr, scale=None):
                evq[0] += 1
                if evq[0] % 2 == 0:
                    if scale is None:
                        return S.op("act", lambda e: e.copy(out=dst, in_=src), reads=rd, writes=wr)
                    return S.op("act", lambda e: e.activation(out=dst, in_=src, func=AF.Copy, scale=scale), reads=rd, writes=wr)
                if scale is None:
                    return S.op("dve", lambda e: e.tensor_copy(out=dst, in_=src), reads=rd, writes=wr)
                return S.op("dve", lambda e: e.tensor_scalar(out=dst, in0=src, scalar1=scale, scalar2=None, op0=ALU.mult), reads=rd, writes=wr)

            def chain_step(c, t, st_):
                hl, d = c
                b = cb[(c, st_)]
                col = d * 2 + hl
                latent = t >= 2
                kn = KN[:, t, hl, :]; qn = QN[:, t, hl, :]; vv = VV[:, t, hl, :]
                tqq = t_QKV[t][hl]; tqk = t_QKV[t][2 + hl]; tqv = t_QKV[t][4 + hl]; tsc = t_SC[t]

                def scol(kind):
                    return SC[:, t, kind * 4 + col:kind * 4 + col + 1]
                U_, V_ = (Uf, Vf) if d == 0 else (Ub, Vb)
                negs = negb[:, 0 if d == 0 else 2, :]; negi = negb[:, 1 if d == 0 else 3, :]
                pk, tpk = p128.get()
                S.op("pe", lambda e: e.transpose(out=bfv(pk), in_=kn, identity=identb), reads=[tqk, t_identb], writes=[tpk])
                evac_copy(b["knT"], bfv(pk), [tpk], [b["t_knT"]])
                S.op("act", lambda e: e.activation(out=b["gV"], in_=V_, func=AF.Copy, scale=scol(6)), reads=[t_trif, tsc], writes=[b["t_gV"]])
                S.op("act", lambda e: e.activation(out=b["gU"], in_=U_, func=AF.Copy, scale=scol(6)), reads=[t_trif, tsc], writes=[b["t_gU"]])
                S.op("dve", lambda e: e.tensor_scalar(out=b["kbg"], in0=kn, scalar1=scol(1), scalar2=None, op0=ALU.mult), reads=[tqk, tsc], writes=[b["t_kbg"]])
                S.op("pool", lambda e: e.tensor_scalar(out=b["kd"], in0=kn, scalar1=scol(2), scalar2=1.0, op0=ALU.mult, op1=ALU.mult), reads=[tqk, tsc], writes=[b["t_kd"]])
                S.op("pool", lambda e: e.tensor_scalar(out=b["vb"], in0=vv, scalar1=scol(3), scalar2=1.0, op0=ALU.mult, op1=ALU.mult), reads=[tqv, tsc], writes=[b["t_vb"]])
                yield
                pd, tpd = p128.get()

                def dm(e):
                    e.matmul(pd, lhsT=identb, rhs=negs, start=True, stop=False)
                    return e.matmul(pd, lhsT=U_, rhs=b["gV"], start=False, stop=True)
                S.op("pe", dm, reads=[t_identb, t_negb, t_trif, b["t_gV"]], writes=[tpd])
                S.op("act", lambda e: e.activation(out=b["E"], in_=pd, func=AF.Exp), reads=[tpd], writes=[b["t_E"]])
                pkk, tpkk = p128.get()
                S.op("pe", lambda e: e.matmul(pkk, lhsT=b["knT"], rhs=b["knT"], start=True, stop=True), reads=[b["t_knT"]], writes=[tpkk])
                S.op("dve", lambda e: e.scalar_tensor_tensor(out=b["X0"], in0=pkk, scalar=scol(0), in1=b["E"], op0=ALU.mult, op1=ALU.mult),
                     reads=[tpkk, tsc, b["t_E"]], writes=[b["t_X0"]])
                if latent:
                    pdt, tpdt = p128.get()

                    def dmt(e):
                        e.matmul(pdt, lhsT=identb, rhs=negi, start=True, stop=False)
                        return e.matmul(pdt, lhsT=V_, rhs=b["gU"], start=False, stop=True)
                    S.op("pe", dmt, reads=[t_identb, t_negb, t_trif, b["t_gU"]], writes=[tpdt])
                    S.op("act", lambda e: e.activation(out=b["ET"], in_=pdt, func=AF.Exp), reads=[tpdt], writes=[b["t_ET"]])
                    pq_, tpq = p128.get()
                    S.op("pe", lambda e: e.transpose(out=bfv(pq_), in_=qn, identity=identb), reads=[tqq, t_identb], writes=[tpq])
                    evac_copy(b["qnT"], bfv(pq_), [tpq], [b["t_qnT"]])
                    S.op("pool", lambda e: e.tensor_scalar(out=b["qd"], in0=qn, scalar1=scol(4), scalar2=1.0, op0=ALU.mult, op1=ALU.mult), reads=[tqq, tsc], writes=[b["t_qd"]])
                yield
                px, tpx = p128.get()
                S.op("pe", lambda e: e.transpose(out=bfv(px), in_=b["X0"], identity=identb), reads=[b["t_X0"], t_identb], writes=[tpx])
                evac_copy(b["XT0"], bfv(px), [tpx], [b["t_XT0"]])
                if latent:
                    pqd, tpqd = p128.get()
                    S.op("pe", lambda e: e.transpose(out=bfv(pqd), in_=b["qd"], identity=identb), reads=[b["t_qd"], t_identb], writes=[tpqd])
                    evac_copy(b["qdT"], bfv(pqd), [tpqd], [b["t_qdT"]])
                    pqk, tpqk = p128.get()
                    S.op("pe", lambda e: e.matmul(pqk, lhsT=b["knT"], rhs=b["qnT"], start=True, stop=True), reads=[b["t_knT"], b["t_qnT"]], writes=[tpqk])
                    S.op("dve", lambda e: e.tensor_tensor(out=b["QKm"], in0=pqk, in1=b["ET"], op=ALU.mult), reads=[tpqk, b["t_ET"]], writes=[b["t_QKm"]])
                yield
                mk = lambda nm: (b[nm], b["t_" + nm])
                Xb, tXb = mk("Xb"); XbT, tXbT = mk("XbT")
                S.op("pool", lambda e: e.tensor_tensor(out=Xb, in0=b["X0"], in1=dcm[:, 0, :], op=ALU.mult), reads=[b["t_X0"], t_dcm], writes=[tXb])
                S.op("pool", lambda e: e.tensor_tensor(out=XbT, in0=b["XT0"], in1=dcm[:, 0, :], op=ALU.mult), reads=[b["t_XT0"], t_dcm], writes=[tXbT])
                S.op("pool", lambda e: e.tensor_tensor(out=b["P0"], in0=Xb, in1=identb, op=ALU.add), reads=[tXb, t_identb], writes=[b["t_P0"]])
                S.op("pool", lambda e: e.tensor_tensor(out=b["PT0"], in0=XbT, in1=identb, op=ALU.add), reads=[tXbT, t_identb], writes=[b["t_PT0"]])
                yield
                cur = 0
                cX, tcX, cXT, tcXT = Xb, tXb, XbT, tXbT
                for lev in range(2):
                    nX, tnX = mk(f"X{lev}s"); nXT, tnXT = mk(f"XT{lev}s")
                    p1, tp1 = p128.get()
                    S.op("pe", lambda e, p1=p1, cX=cX, cXT=cXT: e.matmul(p1, lhsT=cXT, rhs=cX, start=True, stop=True), reads=[tcX, tcXT], writes=[tp1])
                    evac_copy(nX, p1, [tp1], [tnX])
                    p2, tp2 = p128.get()
                    S.op("pe", lambda e, p2=p2, cX=cX, cXT=cXT: e.matmul(p2, lhsT=cX, rhs=cXT, start=True, stop=True), reads=[tcX, tcXT], writes=[tp2])
                    evac_copy(nXT, p2, [tp2], [tnXT])
                    yield
                    P = b[f"P{cur}"]; tP = b[f"t_P{cur}"]; nP = b[f"P{1 - cur}"]; tnP = b[f"t_P{1 - cur}"]
                    PT = b[f"PT{cur}"]; tPT = b[f"t_PT{cur}"]; nPT = b[f"PT{1 - cur}"]; tnPT = b[f"t_PT{1 - cur}"]
                    p3, tp3 = p128.get()
                    S.op("pe", lambda e, p3=p3, nXT=nXT, P=P: e.matmul(p3, lhsT=nXT, rhs=P, start=True, stop=True), reads=[tnXT, tP], writes=[tp3])
                    S.op("dve", lambda e, p3=p3, P=P, nP=nP: e.tensor_tensor(out=nP, in0=p3, in1=P, op=ALU.add), reads=[tp3, tP], writes=[tnP])
                    p4, tp4 = p128.get()
                    S.op("pe", lambda e, p4=p4, nX=nX, PT=PT: e.matmul(p4, lhsT=nX, rhs=PT, start=True, stop=True), reads=[tnX, tPT], writes=[tp4])
                    S.op("dve", lambda e, p4=p4, PT=PT, nPT=nPT: e.tensor_tensor(out=nPT, in0=p4, in1=PT, op=ALU.add), reads=[tp4, tPT], writes=[tnPT])
                    cur = 1 - cur
                    cX, tcX, cXT, tcXT = nX, tnX, nXT, tnXT
                    yield
                for li in range(4):
                    mi = 1 + 2 * li + (0 if d == 0 else 1)
                    miT = 1 + 2 * li + (1 if d == 0 else 0)
                    No, tNo = mk("No"); NoT, tNoT = mk("NoT")
                    P = b[f"P{cur}"]; tP = b[f"t_P{cur}"]; nP = b[f"P{1 - cur}"]; tnP = b[f"t_P{1 - cur}"]
                    PT = b[f"PT{cur}"]; tPT = b[f"t_PT{cur}"]; nPT = b[f"PT{1 - cur}"]; tnPT = b[f"t_PT{1 - cur}"]
                    S.op("pool", lambda e, mi=mi, No=No: e.tensor_tensor(out=No, in0=b["X0"], in1=dcm[:, mi, :], op=ALU.mult), reads=[b["t_X0"], t_dcm], writes=[tNo])
                    pw_, tpw_ = p128.get()
                    S.op("pe", lambda e, pw_=pw_, No=No, PT=PT: e.matmul(pw_, lhsT=No, rhs=PT, start=True, stop=True), reads=[tNo, tPT], writes=[tpw_])
                    Wb, tWb = mk("Wb")
                    evac_copy(Wb, pw_, [tpw_], [tWb])
                    if li < 3:
                        S.op("pool", lambda e, miT=miT, NoT=NoT: e.tensor_tensor(out=NoT, in0=b["XT0"], in1=dcm[:, miT, :], op=ALU.mult), reads=[b["t_XT0"], t_dcm], writes=[tNoT])
                        pv_, tpv_ = p128.get()
                        S.op("pe", lambda e, pv_=pv_, NoT=NoT, P=P: e.matmul(pv_, lhsT=NoT, rhs=P, start=True, stop=True), reads=[tNoT, tP], writes=[tpv_])
                        Vb_, tVb_ = mk("Vb")
                        evac_copy(Vb_, pv_, [tpv_], [tVb_])
                    yield
                    p5, tp5 = p128.get()
                    S.op("pe", lambda e, p5=p5, P=P, Wb=Wb: e.matmul(p5, lhsT=P, rhs=Wb, start=True, stop=True), reads=[tP, tWb], writes=[tp5])
                    S.op("dve", lambda e, p5=p5, PT=PT, nPT=nPT: e.tensor_tensor(out=nPT, in0=p5, in1=PT, op=ALU.add), reads=[tp5, tPT], writes=[tnPT])
                    if li < 3:
                        p6, tp6 = p128.get()
                        S.op("pe", lambda e, p6=p6, PT=PT, Vb_=Vb_: e.matmul(p6, lhsT=PT, rhs=Vb_, start=True, stop=True), reads=[tPT, tVb_], writes=[tp6])
                        S.op("dve", lambda e, p6=p6, P=P, nP=nP: e.tensor_tensor(out=nP, in0=p6, in1=P, op=ALU.add), reads=[tp6, tP], writes=[tnP])
                    cur = 1 - cur
                    yield
                TT = b[f"PT{cur}"]; tTT = b[f"t_PT{cur}"]
                pw, tpw = p128.get()
                S.op("pe", lambda e: e.matmul(pw, lhsT=b["kbg"], rhs=TT, start=True, stop=True), reads=[b["t_kbg"], tTT], writes=[tpw])
                evac_copy(b["nwT"], pw, [tpw], [b["t_nwT"]], scale=-1.0)
                yield
                pv, tpv = p128.get()

                def vn(e):
                    e.matmul(pv, lhsT=TT, rhs=b["vb"], start=True, stop=False)
                    return e.matmul(pv, lhsT=b["nwT"], rhs=b["Sb"], start=False, stop=True)
                S.op("pe", vn, reads=[tTT, b["t_vb"], b["t_nwT"], b["t_Sb"]], writes=[tpv])
                evac_copy(b["vnew"], pv, [tpv], [b["t_vnew"]])
                yield
                if latent:
                    lt = t - 2
                    po, tpo = p128.get()

                    def om(e):
                        e.matmul(po, lhsT=b["qdT"], rhs=b["Sb"], start=True, stop=False)
                        return e.matmul(po, lhsT=b["QKm"], rhs=b["vnew"], start=False, stop=True)
                    S.op("pe", om, reads=[b["t_qdT"], b["t_Sb"], b["t_QKm"], b["t_vnew"]], writes=[tpo])
                    if (lt, hl) not in visited:
                        visited.add((lt, hl))
                        k4o = ost_cnt[0] % 4; ost_cnt[0] += 1
                        evac_copy(ost[k4o], po, [tpo], [t_ost[k4o]])
                        S.op("pool", lambda e, sig: sig(e.dma_start(out=oacc_d[lt * 2 + hl], in_=ost[k4o])), reads=[t_ost[k4o]], writes=[t_OACC[lt][hl]], dma=1, key=f"oaw{k4o}")
                    else:
                        k2 = fin_cnt[0] % 2; k4 = fin_cnt[0] % 4
                        fin_cnt[0] += 1
                        of = ofin[k2]; tof = t_ofin[k2]; fs = fsm[k2]; tfs = t_fsm[k2]
                        S.op("sp", lambda e, sig: sig(e.dma_start(out=b["ob"], in_=oacc_d[lt * 2 + hl])), reads=[t_OACC[lt][hl]], writes=[b["t_ob"]], dma=1, key=f"oar{hl}{d}{st_}")
                        S.op("dve", lambda e: e.tensor_tensor(out=of, in0=po, in1=b["ob"], op=ALU.add), reads=[tpo, b["t_ob"]], writes=[tof])
                        S.op("act", lambda e: e.activation(out=junk_B, in_=of, func=AF.Square, accum_out=fs[:, 0:1]), reads=[tof], writes=[tfs])
                        S.op("act", lambda e: e.activation(out=fs[:, 0:1], in_=fs[:, 0:1], func=AF.Sqrt, scale=1.0 / 128, bias=EPS), reads=[tfs], writes=[tfs])
                        S.op("dve", lambda e: e.reciprocal(out=fs[:, 0:1], in_=fs[:, 0:1]), reads=[tfs], writes=[tfs])
                        S.op("dve", lambda e: e.tensor_scalar(out=onb[k2], in0=of, scalar1=fs[:, 0:1], scalar2=None, op0=ALU.mult), reads=[tof, tfs], writes=[t_onb[k2]])
                        pt_, tpt = p128.get()
                        S.op("pe", lambda e: e.transpose(out=bfv(pt_), in_=onb[k2], identity=identb), reads=[t_onb[k2], t_identb], writes=[tpt])
                        evac_copy(onT[k4], bfv(pt_), [tpt], [t_onT[k4]])
                        o_ = S.op("pool", lambda e, sig: sig(e.dma_start(out=snd[lt // 16][hl * 128:(hl + 1) * 128, (lt % 16) * 64:(lt % 16 + 1) * 64], in_=onT[k4].bitcast(F32))),
                                  reads=[t_onT[k4]], dma=1, key=f"snd{k4}")
                        snd_ops.append(o_)
                        qops[lt // 16].append(o_)
                        if len(qops[lt // 16]) == 32:
                            jq = lt // 16
                            ccs.append(S.op("pool", lambda e, sig: sig(e.collective_compute("AllGather", ALU.bypass, replica_groups=[[0, 1, 2, 3], [4, 5, 6, 7]],
                                                                                       ins=[snd[jq].ap().opt()], outs=[rcv[jq].ap().opt()])),
                                            extra=qops[jq], dma=1, key=f"cc{jq}", inc=1))
                ps_, tps = p128.get()
                S.op("pe", lambda e: e.matmul(ps_, lhsT=b["kd"], rhs=b["vnew"], start=True, stop=True), reads=[b["t_kd"], b["t_vnew"]], writes=[tps])
                S.op("dve", lambda e: e.scalar_tensor_tensor(out=b["S"], in0=b["S"], scalar=scol(5), in1=ps_, op0=ALU.mult, op1=ALU.add),
                     reads=[b["t_S"], tsc, tps], writes=[b["t_S"]])
                S.op("act", lambda e: e.copy(out=b["Sb"], in_=b["S"]), reads=[b["t_S"]], writes=[b["t_Sb"]])
                yield

            def bwd_tile(i):
                return 1 - i if i < 2 else NT + 1 - i

            HALF = 11
            alive = []
            nstart = 0
            tick = 0
            while nstart < NT or alive:
                if nstart < NT and tick % HALF == 0:
                    i = nstart; nstart += 1
                    for c in chains:
                        t = i if c[1] == 0 else bwd_tile(i)
                        alive.append([chain_step(c, t, i % 2), 0])
                nxt = []
                for g in alive:
                    try:
                        next(g[0]); g[1] += 1
                        assert g[1] < 2 * HALF, "chain step too long for the 2-deep pipeline"
                        nxt.append(g)
                    except StopIteration:
                        pass
                alive = nxt
                tick += 1
            if stage == 2:
                for i_ in range(4 if debug else 0):
                    o_ = S.op("sp", lambda e, sig, i_=i_: sig(e.dma_start(out=dbgb[0:256, i_ * 2048:(i_ + 1) * 2048].bitcast(F32), in_=snd[i_][:, :])), extra=snd_ops, dma=1, key="dbg")
                    dump_ops.append(o_)
                for i_, c_ in enumerate(chains if debug else []):
                    dump(cb[(c_, 0)]["S"], i_ * 128, 128, reads=[cb[(c_, 0)]["t_S"]])
                raise _Stop()
            p128 = p128_small
            assert len(ccs) == 4
            S.barrier(skip_cc=True)
            A.release(mA)
            if stage == 3:
                raise _Stop()

            hxo = A.alloc([8, OWN], BF16); t_hxo = [Tk() for _ in range(NOT)]
            markH = A.mark()
            offS = A.mark()
            SZ = A.alloc([8, OWN], BF16); t_SZ = [[Tk() for _ in range(4)] for _ in range(8)]
            GU = A.alloc([8, OWN], BF16); t_GU = [[Tk() for _ in range(4)] for _ in range(8)]
            wstg = [A.alloc([8, 512]) for _ in range(2)]; t_wstg = [Tk(), Tk()]
            wbf = [A.alloc([8, 512], BF16) for _ in range(2)]; t_wbf = [Tk(), Tk()]
            mC1 = A.mark()
            xt_C = [A.alloc([D]) for _ in range(2)]; t_xt_C = [Tk(), Tk()]
            junk_C = A.alloc([D]); t_junk_C = Tk()
            ssq_C = [A.alloc([1]) for _ in range(2)]; t_ssq_C = [Tk(), Tk()]
            xs_C = [A.alloc([8, 128], BF16) for _ in range(2)]; t_xs_C = [Tk(), Tk()]
            for t in range(NOT):
                s2 = t % 2
                S.op("sp", lambda e, sig, t=t, s2=s2: sig(e.dma_start(out=xt_C[s2], in_=xo[t * 128:(t + 1) * 128, :])), writes=[t_xt_C[s2]], dma=1, key=f"cxt{s2}")
                S.op("act", lambda e, s2=s2: e.activation(out=junk_C, in_=xt_C[s2], func=AF.Square, accum_out=ssq_C[s2]), reads=[t_xt_C[s2]], writes=[t_ssq_C[s2]])
                S.op("act", lambda e, s2=s2: e.activation(out=ssq_C[s2], in_=ssq_C[s2], func=AF.Sqrt, scale=1.0 / D, bias=EPS), reads=[t_ssq_C[s2]], writes=[t_ssq_C[s2]])
                S.op("dve", lambda e, s2=s2: e.reciprocal(out=ssq_C[s2], in_=ssq_C[s2]), reads=[t_ssq_C[s2]], writes=[t_ssq_C[s2]])
                S.op("pool", lambda e, s2=s2: e.tensor_scalar(out=xs_C[s2].rearrange("p a b -> p (a b)"), in0=xt_C[s2], scalar1=ssq_C[s2], scalar2=1.0, op0=ALU.mult, op1=ALU.mult),
                     reads=[t_xt_C[s2], t_ssq_C[s2]], writes=[t_xs_C[s2]])
                pp, tp = p512.get()
                ppb = pp.bitcast(BF16)

                def tr(e, s2=s2, ppb=ppb):
                    for k in range(8):
                        i = e.transpose(out=ppb[:, k * 128:(k + 1) * 128], in_=xs_C[s2][:, k, :], identity=identb)
                    return i
                S.op("pe", tr, reads=[t_xs_C[s2], t_identb], writes=[tp])

                def ev_d(e, t=t, ppb=ppb):
                    for k in range(8):
                        i = e.tensor_scalar(out=hxo[:, k, t * 128:(t + 1) * 128], in0=ppb[:, k * 128:(k + 1) * 128], scalar1=modc[:, 1, k:k + 1], scalar2=modc[:, 0, k:k + 1], op0=ALU.mult, op1=ALU.add)
                    return i
                S.op("dve", ev_d, reads=[tp, t_modc], writes=[t_hxo[t]])
            S.barrier()
            A.release(mC1)
            wcnt = [0]

            def stream_w(src_ap_cols, ncols):
                s = wcnt[0] % 2
                wcnt[0] += 1
                v = src_ap_cols.rearrange("(k p) n -> p k n", p=128)
                S.op("sp", lambda e, sig: (sig(e.dma_start(out=wstg[s][:, 0:4, 0:ncols], in_=v[:, 0:4, :])), sig(e.dma_start(out=wstg[s][:, 4:8, 0:ncols], in_=v[:, 4:8, :]))),
                     writes=[t_wstg[s]], dma=2, key=f"wstg{s}")
                S.op("pool", lambda e: e.tensor_copy(out=wbf[s][:, :, 0:ncols], in_=wstg[s][:, :, 0:ncols]), reads=[t_wstg[s]], writes=[t_wbf[s]])
                return wbf[s], t_wbf[s]
            for cbk in range(4):
                wv, twv = stream_w(w_rest[:, cbk * 512:(cbk + 1) * 512], 512)
                dst, tdst, fn = (SZ, t_SZ, AF.Silu) if cbk < 2 else (GU, t_GU, AF.Gelu_apprx_tanh)
                for cc_ in range(4):
                    chn = (cbk % 2) * 4 + cc_
                    for tb in range(4):
                        pp, tp = p512.get()

                        def f(e, pp=pp, wv=wv, cc_=cc_, tb=tb):
                            for k in range(8):
                                i = e.matmul(pp, lhsT=wv[:, k, cc_ * 128:(cc_ + 1) * 128], rhs=hxo[:, k, tb * 512:(tb + 1) * 512], start=(k == 0), stop=(k == 7))
                            return i
                        S.op("pe", f, reads=[twv] + t_hxo[tb * 4:(tb + 1) * 4], writes=[tp])
                        S.op("act", lambda e, pp=pp, dst=dst, chn=chn, tb=tb, fn=fn: e.activation(out=dst[:, chn, tb * 512:(tb + 1) * 512], in_=pp, func=fn),
                             reads=[tp], writes=[tdst[chn][tb]])
            def cut4(k):
                if stage == 4 and int(os.environ.get("CUT4", "0")) == k:
                    S.barrier()
                    for i_, buf_ in enumerate((SZ, GU, hxo)):
                        for h_ in range(2):
                            dumpb(buf_[:, h_ * 4:(h_ + 1) * 4, :].rearrange("p a b -> p (a b)"), i_ * 256 + h_ * 128, 128, 0, SEQ)
                    raise _Stop()
            cut4(1)
            mV = A.mark()
            junk_V = A.alloc([D])
            wcnt[0] = 0
            wvh = [stream_w(w_rest[:, 2048 + hh * 512:2048 + (hh + 1) * 512], 512) for hh in range(2)]
            swf = A.alloc([8, 128]); t_swf = Tk(); load(swf, sgu_wT.ap().rearrange("g q p -> q g p"), t_swf, "c11")
            swb = A.alloc([8, 128], BF16); t_swb = Tk()
            S.op("pool", lambda e: e.tensor_copy(out=swb, in_=swf), reads=[t_swf], writes=[t_swb])
            lnbb = A.alloc([D]); t_lnbb = Tk(); load(lnbb, lnb_bc[:, :], t_lnbb, "c12")
            sbb = A.alloc([8, 128]); t_sbb = Tk(); load(sbb, sgu_bb[:, :, :], t_sbb, "c13")
            lngc = A.alloc([8]); t_lngc = Tk(); load(lngc, lng_c[:, :], t_lngc, "c14")
            BIAS = A.alloc([8, 128]); t_BIAS = Tk()
            for g in range(8):
                pp, tp = p128.get()
                S.op("pe", lambda e, pp=pp, g=g: e.matmul(pp, lhsT=lnbb[:, g * 128:(g + 1) * 128], rhs=swf[:, g, :], start=True, stop=True), reads=[t_lnbb, t_swf], writes=[tp])
                S.op("dve", lambda e, pp=pp, g=g: e.tensor_tensor(out=BIAS[:, g, :], in0=pp, in1=sbb[:, g, :], op=ALU.add), reads=[tp, t_sbb], writes=[t_BIAS])
            gv = [A.alloc([D])] * 2; t_gv = [Tk()] * 2
            vnb = [A.alloc([D], BF16) for _ in range(2)]; t_vnb = [Tk(), Tk()]
            lst = [A.alloc([8]) for _ in range(2)]; t_lst = [Tk(), Tk()]
            mtmp = [A.alloc([128]) for _ in range(2)]; t_mtmp = [Tk(), Tk()]
            for t in range(NOT):
                s2 = t % 2
                for hh in range(2):
                    pp, tp = p512.get()

                    def f(e, pp=pp, hh=hh, t=t):
                        for k in range(8):
                            i = e.matmul(pp, lhsT=hxo[:, k, t * 128:(t + 1) * 128], rhs=wvh[hh][0][:, k, :], start=(k == 0), stop=(k == 7))
                        return i
                    S.op("pe", f, reads=[wvh[hh][1], t_hxo[t]], writes=[tp])
                    S.op("act", lambda e, pp=pp, hh=hh, s2=s2: e.activation(out=gv[s2][:, hh * 512:(hh + 1) * 512], in_=pp, func=AF.Gelu_apprx_tanh), reads=[tp], writes=[t_gv[s2]])
                ls = lst[s2]; tls = t_lst[s2]
                S.op("dve", lambda e, s2=s2, ls=ls: e.tensor_reduce(out=ls[:, 0:1], in_=gv[s2], axis=mybir.AxisListType.X, op=ALU.add), reads=[t_gv[s2]], writes=[tls])
                S.op("act", lambda e, s2=s2, ls=ls: e.activation(out=junk_V, in_=gv[s2], func=AF.Square, accum_out=ls[:, 1:2]), reads=[t_gv[s2]], writes=[tls])
                S.op("dve", lambda e, ls=ls: e.tensor_scalar(out=ls[:, 0:2], in0=ls[:, 0:2], scalar1=1.0 / D, scalar2=None, op0=ALU.mult), reads=[tls], writes=[tls])
                S.op("dve", lambda e, ls=ls: e.tensor_tensor(out=ls[:, 2:3], in0=ls[:, 0:1], in1=ls[:, 0:1], op=ALU.mult), reads=[tls], writes=[tls])
                S.op("dve", lambda e, ls=ls: e.tensor_tensor(out=ls[:, 2:3], in0=ls[:, 1:2], in1=ls[:, 2:3], op=ALU.subtract), reads=[tls], writes=[tls])
                S.op("act", lambda e, ls=ls: e.activation(out=ls[:, 2:3], in_=ls[:, 2:3], func=AF.Sqrt, bias=EPS), reads=[tls], writes=[tls])
                S.op("dve", lambda e, ls=ls: e.reciprocal(out=ls[:, 2:3], in_=ls[:, 2:3]), reads=[tls], writes=[tls])
                S.op("dve", lambda e, s2=s2, ls=ls: e.tensor_scalar(out=vnb[s2], in0=gv[s2], scalar1=ls[:, 0:1], scalar2=ls[:, 2:3], op0=ALU.subtract, op1=ALU.mult),
                     reads=[t_gv[s2], tls], writes=[t_vnb[s2]])
                for g in range(8):
                    pp, tp = p128.get()
                    S.op("pe", lambda e, pp=pp, g=g, s2=s2: e.matmul(pp, lhsT=vnb[s2][:, g * 128:(g + 1) * 128], rhs=swb[:, g, :], start=True, stop=True),
                         reads=[t_vnb[s2], t_swb], writes=[tp])
                    mt = mtmp[g % 2]; tmt = t_mtmp[g % 2]
                    S.op("dve", lambda e, pp=pp, g=g, mt=mt: e.scalar_tensor_tensor(out=mt, in0=pp, scalar=lngc[:, g:g + 1], in1=BIAS[:, g, :], op0=ALU.mult, op1=ALU.add),
                         reads=[tp, t_lngc, t_BIAS], writes=[tmt])
                    S.op("pool", lambda e, g=g, t=t, mt=mt: e.tensor_tensor(out=GU[:, g, t * 128:(t + 1) * 128], in0=GU[:, g, t * 128:(t + 1) * 128], in1=mt, op=ALU.mult),
                         reads=[tmt, t_GU[g][t // 4]], writes=[t_GU[g][t // 4]])
            S.barrier()
            A.release(mV)
            cut4(2)
            mY = A.mark()
            rq = [A.alloc([4, 512], BF16) for _ in range(2)]; t_rq = [Tk(), Tk()]
            acc = [A.alloc([512]) for _ in range(2)]; t_acc = [Tk(), Tk()]
            cntr = 0
            for hp in range(4):
                for hl in range(2):
                    chn = hp * 2 + hl
                    for tb in range(4):
                        s = cntr % 2; cntr += 1
                        S.op("sp", lambda e, sig, s=s, chn=chn, tb=tb: tuple(sig(e.dma_start(out=rq[s][:, j, :].bitcast(F32), in_=rcv[j][chn * 128:(chn + 1) * 128, tb * 256:(tb + 1) * 256])) for j in range(4)),
                             extra=ccs, writes=[t_rq[s]], dma=4, key=f"rq{s}")
                        for j in range(4):
                            def f(e, s=s, j=j):
                                if j == 0:
                                    return e.tensor_scalar(out=acc[s], in0=rq[s][:, 0, :], scalar1=qmask[:, 0:1], scalar2=None, op0=ALU.mult)
                                return e.scalar_tensor_tensor(out=acc[s], in0=rq[s][:, j, :], scalar=qmask[:, j:j + 1], in1=acc[s], op0=ALU.mult, op1=ALU.add)
                            S.op("dve", f, reads=[t_rq[s], t_qmask, t_acc[s]] if j else [t_rq[s], t_qmask], writes=[t_acc[s]])
                        S.op("dve", lambda e, s=s, chn=chn, tb=tb: e.scalar_tensor_tensor(out=SZ[:, chn, tb * 512:(tb + 1) * 512], in0=acc[s], scalar=gngs[:, 0:1], in1=SZ[:, chn, tb * 512:(tb + 1) * 512], op0=ALU.mult, op1=ALU.mult),
                             reads=[t_acc[s], t_gngs, t_SZ[chn][tb]], writes=[t_SZ[chn][tb]])
            S.barrier()
            A.release(mY)
            cut4(3)
            S.barrier()
            mM = A.mark()
            MG = A.alloc([8, OWN], BF16); t_MG = [[Tk() for _ in range(4)] for _ in range(8)]
            sga = [A.alloc([512], BF16) for _ in range(2)]; t_sga = [Tk(), Tk()]
            sgb = [A.alloc([512], BF16) for _ in range(2)]; t_sgb = [Tk(), Tk()]
            m1 = [A.alloc([512]) for _ in range(2)]; t_m1 = [Tk(), Tk()]
            m2 = [A.alloc([512]) for _ in range(2)]; t_m2 = [Tk(), Tk()]
            sub = [(wstg[i][:, :, j * 128:(j + 1) * 128], wbf[i][:, :, j * 128:(j + 1) * 128], Tk(), Tk()) for i in range(2) for j in range(4)]
            subc = [0]

            def stream_small(src_cols):
                stg, bfw, tst, tbf = sub[subc[0] % 8]
                subc[0] += 1
                v = src_cols.rearrange("(k p) n -> p k n", p=128)
                S.op("sp", lambda e, sig: sig(e.dma_start(out=stg, in_=v)), writes=[tst], dma=1, key=f"sub{(subc[0] - 1) % 8}")
                S.op("pool", lambda e: e.tensor_copy(out=bfw, in_=stg), reads=[tst], writes=[tbf])
                return bfw, tbf
            cntr = 0
            for dc in range(8):
                ws = [stream_small(w_a[:, dc * 128:(dc + 1) * 128]), stream_small(w_b[:, dc * 128:(dc + 1) * 128]),
                      stream_small(w_rest[:, 3072 + dc * 128:3072 + (dc + 1) * 128]), stream_small(w_rest[:, 4096 + dc * 128:4096 + (dc + 1) * 128])]
                for tb in range(4):
                    s = cntr % 2; cntr += 1
                    outs = []
                    for wi, (src, tsrc) in enumerate(((SZ, t_SZ), (GU, t_GU), (hxo, None), (hxo, None))):
                        wv_, tw_ = ws[wi]
                        pp, tp = p512.get()

                        def f(e, pp=pp, wv_=wv_, src=src, tb=tb):
                            for k in range(8):
                                i = e.matmul(pp, lhsT=wv_[:, k, :], rhs=src[:, k, tb * 512:(tb + 1) * 512], start=(k == 0), stop=(k == 7))
                            return i
                        rds = [tw_] + ([tsrc[k][tb] for k in range(8)] if tsrc is not None else t_hxo[tb * 4:(tb + 1) * 4])
                        S.op("pe", f, reads=rds, writes=[tp])
                        outs.append((pp, tp))
                    S.op("act", lambda e, s=s, pp=outs[2][0]: e.activation(out=sga[s], in_=pp, func=AF.Sigmoid), reads=[outs[2][1]], writes=[t_sga[s]])
                    S.op("act", lambda e, s=s, pp=outs[3][0]: e.activation(out=sgb[s], in_=pp, func=AF.Sigmoid), reads=[outs[3][1]], writes=[t_sgb[s]])
                    S.op("dve", lambda e, s=s, pp=outs[0][0]: e.tensor_tensor(out=m1[s], in0=pp, in1=sga[s], op=ALU.mult), reads=[outs[0][1], t_sga[s]], writes=[t_m1[s]])
                    S.op("dve", lambda e, s=s, pp=outs[1][0]: e.tensor_tensor(out=m2[s], in0=pp, in1=sgb[s], op=ALU.mult), reads=[outs[1][1], t_sgb[s]], writes=[t_m2[s]])
                    S.op("pool", lambda e, s=s, dc=dc, tb=tb: e.tensor_tensor(out=MG[:, dc, tb * 512:(tb + 1) * 512], in0=m1[s], in1=m2[s], op=ALU.add),
                         reads=[t_m1[s], t_m2[s]], writes=[t_MG[dc][tb]])
            S.barrier()
            if stage == 4:
                for i_, (buf_, tk_) in enumerate(((SZ, t_SZ), (GU, t_GU), (MG, t_MG))):
                    for h_ in range(2):
                        dumpb(buf_[:, h_ * 4:(h_ + 1) * 4, :].rearrange("p a b -> p (a b)"), i_ * 256 + h_ * 128, 128, 0, SEQ)
                raise _Stop()
            hx2 = hxo; t_hx2 = [Tk() for _ in range(NOT)]
            Amain = A
            A = Arena(arena_t, 16384, base=offS)
            wo_b = A.alloc([8, D], BF16); t_wo_b = Tk()
            for hh in range(2):
                sl = hh
                S.op("sp", lambda e, sig, hh=hh, sl=sl: (sig(e.dma_start(out=wstg[sl][:, 0:4, :], in_=w_out.ap().rearrange("(k p) n -> p k n", p=128)[:, 0:4, hh * 512:(hh + 1) * 512])),
                                                        sig(e.dma_start(out=wstg[sl][:, 4:8, :], in_=w_out.ap().rearrange("(k p) n -> p k n", p=128)[:, 4:8, hh * 512:(hh + 1) * 512]))),
                     writes=[t_wstg[sl]], dma=2, key=f"wstg{sl}")
                for k in range(8):
                    S.op("pool", lambda e, k=k, hh=hh, sl=sl: e.tensor_tensor(out=wo_b[:, k, hh * 512:(hh + 1) * 512], in0=wstg[sl][:, k, :], in1=gmix[:, hh * 512:(hh + 1) * 512], op=ALU.mult),
                         reads=[t_wstg[sl], t_gmix], writes=[t_wo_b])
            rwf = A.alloc([8, 36]); t_rwf = Tk(); load(rwf, rw.ap().rearrange("(k p) n -> p k n", p=128), t_rwf, "c15")
            rbb = A.alloc([36]); t_rbb = Tk(); load(rbb, rb[:, :], t_rbb, "c16")
            x1 = [A.alloc([D]) for _ in range(2)]; t_x1 = [Tk(), Tk()]
            xt_D = [A.alloc([D]) for _ in range(2)]; t_xt_D = [Tk(), Tk()]
            junk_D = A.alloc([D]); t_junk_D = Tk()
            ssq_D = [A.alloc([1]) for _ in range(2)]; t_ssq_D = [Tk(), Tk()]
            xsf = [A.alloc([8, 128]) for _ in range(2)]; t_xsf = [Tk(), Tk()]
            hxf = [A.alloc([8, 128]) for _ in range(2)]; t_hxf = [Tk(), Tk()]
            rs_ = [A.alloc([64]) for _ in range(2)]; t_rs = [Tk(), Tk()]
            x1_ops = []
            for t in range(NOT):
                s2 = t % 2
                S.op("sp", lambda e, sig, t=t, s2=s2: sig(e.dma_start(out=xt_D[s2], in_=xo[t * 128:(t + 1) * 128, :])), writes=[t_xt_D[s2]], dma=1, key=f"dxt{s2}")
                for hh in range(2):
                    pp, tp = p512.get()

                    def f(e, pp=pp, hh=hh, t=t):
                        for k in range(8):
                            i = e.matmul(pp, lhsT=MG[:, k, t * 128:(t + 1) * 128], rhs=wo_b[:, k, hh * 512:(hh + 1) * 512], start=(k == 0), stop=(k == 7))
                        return i
                    S.op("pe", f, reads=[t_wo_b] + [t_MG[k][t // 4] for k in range(8)], writes=[tp])
                    S.op("dve", lambda e, pp=pp, hh=hh, s2=s2: e.tensor_tensor(out=x1[s2][:, hh * 512:(hh + 1) * 512], in0=pp, in1=xt_D[s2][:, hh * 512:(hh + 1) * 512], op=ALU.add),
                         reads=[tp, t_xt_D[s2]], writes=[t_x1[s2]])
                o_ = S.op("pool", lambda e, sig, t=t, s2=s2: sig(e.dma_start(out=x1d[t * 128:(t + 1) * 128, :], in_=x1[s2])), reads=[t_x1[s2]], dma=1, key=f"x1d{s2}")
                x1_ops.append(o_)
                S.op("act", lambda e, s2=s2: e.activation(out=junk_D, in_=x1[s2], func=AF.Square, accum_out=ssq_D[s2]), reads=[t_x1[s2]], writes=[t_ssq_D[s2]])
                S.op("act", lambda e, s2=s2: e.activation(out=ssq_D[s2], in_=ssq_D[s2], func=AF.Sqrt, scale=1.0 / D, bias=EPS), reads=[t_ssq_D[s2]], writes=[t_ssq_D[s2]])
                S.op("dve", lambda e, s2=s2: e.reciprocal(out=ssq_D[s2], in_=ssq_D[s2]), reads=[t_ssq_D[s2]], writes=[t_ssq_D[s2]])
                S.op("pool", lambda e, s2=s2: e.tensor_scalar(out=xsf[s2].rearrange("p a b -> p (a b)"), in0=x1[s2], scalar1=ssq_D[s2], scalar2=1.0, op0=ALU.mult, op1=ALU.mult),
                     reads=[t_x1[s2], t_ssq_D[s2]], writes=[t_xsf[s2]])
                for q4 in range(2):
                    pp, tp = p512.get()

                    def tr(e, pp=pp, q4=q4, s2=s2):
                        for k in range(4):
                            i = e.transpose(out=pp[:, k * 128:(k + 1) * 128], in_=xsf[s2][:, q4 * 4 + k, :], identity=identf)
                        return i
                    S.op("pe", tr, reads=[t_xsf[s2], t_identf], writes=[tp])

                    def ev(e, pp=pp, q4=q4, s2=s2, t=t):
                        for k in range(4):
                            kk = q4 * 4 + k
                            i = e.tensor_scalar(out=hxf[s2][:, kk, :], in0=pp[:, k * 128:(k + 1) * 128], scalar1=modc[:, 5, kk:kk + 1], scalar2=modc[:, 4, kk:kk + 1], op0=ALU.mult, op1=ALU.add)
                        return i
                    S.op("dve", ev, reads=[tp, t_modc], writes=[t_hxf[s2]])
                S.op("pool", lambda e, s2=s2, t=t: e.tensor_copy(out=hx2[:, :, t * 128:(t + 1) * 128], in_=hxf[s2]), reads=[t_hxf[s2]], writes=[t_hx2[t]])
                pr, tpr = p128.get()

                def rt(e, pr=pr, s2=s2):
                    for k in range(8):
                        i = e.matmul(pr[:, 0:36], lhsT=hxf[s2][:, k, :], rhs=rwf[:, k, :], start=(k == 0), stop=(k == 7))
                    return i
                S.op("pe", rt, reads=[t_hxf[s2], t_rwf], writes=[tpr])
                r = rs_[s2]; tr_ = t_rs[s2]
                S.op("dve", lambda e, pr=pr, r=r: e.tensor_tensor(out=r[:, 0:36], in0=pr[:, 0:36], in1=rbb, op=ALU.add), reads=[tpr, t_rbb], writes=[tr_])
                S.op("dve", lambda e, r=r: e.tensor_reduce(out=r[:, 40:41], in_=r[:, 0:4], axis=mybir.AxisListType.X, op=ALU.max), reads=[tr_], writes=[tr_])
                S.op("dve", lambda e, r=r: e.tensor_scalar(out=r[:, 36:40], in0=r[:, 0:4], scalar1=r[:, 40:41], scalar2=None, op0=ALU.is_ge), reads=[tr_], writes=[tr_])
                S.op("dve", lambda e, r=r: e.tensor_scalar(out=r[:, 60:64], in0=r[:, 0:4], scalar1=r[:, 40:41], scalar2=None, op0=ALU.subtract), reads=[tr_], writes=[tr_])
                S.op("act", lambda e, r=r: e.activation(out=r[:, 60:64], in_=r[:, 60:64], func=AF.Exp, accum_out=r[:, 41:42]), reads=[tr_], writes=[tr_])
                S.op("dve", lambda e, r=r: e.reciprocal(out=r[:, 41:42], in_=r[:, 41:42]), reads=[tr_], writes=[tr_])
                S.op("dve", lambda e, r=r: e.tensor_scalar(out=r[:, 42:50], in0=r[:, 4:12], scalar1=r[:, 36:37], scalar2=None, op0=ALU.mult), reads=[tr_], writes=[tr_])
                for g in range(1, 4):
                    S.op("dve", lambda e, r=r, g=g: e.scalar_tensor_tensor(out=r[:, 42:50], in0=r[:, 4 + 8 * g:12 + 8 * g], scalar=r[:, 36 + g:37 + g], in1=r[:, 42:50], op0=ALU.mult, op1=ALU.add),
                         reads=[tr_], writes=[tr_])
                S.op("dve", lambda e, r=r: e.tensor_reduce(out=r[:, 50:51], in_=r[:, 42:50], axis=mybir.AxisListType.X, op=ALU.max), reads=[tr_], writes=[tr_])
                S.op("dve", lambda e, r=r: e.tensor_scalar(out=r[:, 42:50], in0=r[:, 42:50], scalar1=r[:, 50:51], scalar2=None, op0=ALU.subtract), reads=[tr_], writes=[tr_])
                S.op("act", lambda e, r=r: e.activation(out=r[:, 42:50], in_=r[:, 42:50], func=AF.Exp), reads=[tr_], writes=[tr_])
                S.op("dve", lambda e, r=r: e.tensor_scalar(out=r[:, 52:60], in0=r[:, 42:50], scalar1=1.0, scalar2=None, op0=ALU.is_ge), reads=[tr_], writes=[tr_])
                S.op("dve", lambda e, r=r: e.scalar_tensor_tensor(out=r[:, 4:12], in0=r[:, 52:60], scalar=-2.0, in1=r[:, 42:50], op0=ALU.mult, op1=ALU.add), reads=[tr_], writes=[tr_])
                S.op("dve", lambda e, r=r: e.tensor_reduce(out=r[:, 51:52], in_=r[:, 4:12], axis=mybir.AxisListType.X, op=ALU.max), reads=[tr_], writes=[tr_])
                S.op("dve", lambda e, r=r: e.tensor_scalar(out=r[:, 12:20], in0=r[:, 4:12], scalar1=r[:, 51:52], scalar2=None, op0=ALU.is_ge), reads=[tr_], writes=[tr_])
                S.op("dve", lambda e, r=r: e.scalar_tensor_tensor(out=r[:, 20:28], in0=r[:, 12:20], scalar=r[:, 51:52], in1=r[:, 52:60], op0=ALU.mult, op1=ALU.add), reads=[tr_], writes=[tr_])
                S.op("dve", lambda e, r=r: e.tensor_scalar(out=r[:, 50:51], in0=r[:, 51:52], scalar1=1.0, scalar2=None, op0=ALU.add), reads=[tr_], writes=[tr_])
                S.op("dve", lambda e, r=r: e.reciprocal(out=r[:, 50:51], in_=r[:, 50:51]), reads=[tr_], writes=[tr_])
                S.op("dve", lambda e, r=r: e.tensor_tensor(out=r[:, 50:51], in0=r[:, 50:51], in1=r[:, 41:42], op=ALU.mult), reads=[tr_], writes=[tr_])
                S.op("dve", lambda e, r=r: e.tensor_scalar(out=r[:, 20:28], in0=r[:, 20:28], scalar1=r[:, 50:51], scalar2=None, op0=ALU.mult), reads=[tr_], writes=[tr_])
                for g in range(4):
                    S.op("dve", lambda e, r=r, g=g, t=t: e.tensor_scalar(out=GT[:, t, g * 8:(g + 1) * 8], in0=r[:, 20:28], scalar1=r[:, 36 + g:37 + g], scalar2=None, op0=ALU.mult),
                         reads=[tr_], writes=[t_GT[t]])
            S.barrier()
            A = Amain
            A.release(markH)
            if stage == 5:
                dump(GT.rearrange("p a b -> p (a b)"), 0, 512)
                for i_ in range(4):
                    o_ = S.op("sp", lambda e, sig, i_=i_: sig(e.dma_start(out=y[i_ * 512:(i_ + 1) * 512, :], in_=x1d[i_ * 512:(i_ + 1) * 512, :])), extra=x1_ops, dma=1, key="dbg")
                    dump_ops.append(o_)
                dumpb(hx2[:, 0:4, :].rearrange("p a b -> p (a b)"), 0, 128, 0, SEQ)
                raise _Stop()
            p512 = Pool([(psb[b_][:, :], Tk(f"p512x_{b_}", excl=True)) for b_ in range(8)])
            ACC = A.alloc([NOT, D]); t_ACC = [[Tk(), Tk()] for _ in range(NOT)]
            for t in range(NOT):
                S.op("pool", lambda e, t=t: e.memset(ACC[:, t, :], 0.0), writes=t_ACC[t])
            e1f = A.alloc([8, 512]); t_e1f = Tk()
            e2f = A.alloc([2, D]); t_e2f = Tk()
            e1b = [A.alloc([8, 512], BF16) for _ in range(2)]; t_e1b = [Tk(), Tk()]
            e2b = [A.alloc([2, D], BF16) for _ in range(2)]; t_e2b = [Tk(), Tk()]
            sil = [A.alloc([512], BF16) for _ in range(2)]; t_sil = [Tk(), Tk()]
            hid = [A.alloc([512], BF16) for _ in range(4)]; t_hid = [Tk() for _ in range(4)]
            hc = 0
            for ex in range(NE):
                s = ex % 2
                v1 = ew1[ex].rearrange("(k p) n -> p k n", p=128); v3 = ew3[ex].rearrange("(k p) n -> p k n", p=128)
                v2 = ew2[ex].rearrange("(k p) n -> p k n", p=128)
                S.op("sp", lambda e, sig, v1=v1, v3=v3: (sig(e.dma_start(out=e1f[:, :, 0:256], in_=v1)), sig(e.dma_start(out=e1f[:, :, 256:512], in_=v3))), writes=[t_e1f], dma=2, key="e1f")
                S.op("sp", lambda e, sig, v2=v2: sig(e.dma_start(out=e2f, in_=v2)), writes=[t_e2f], dma=1, key="e2f")
                S.op("pool", lambda e, s=s: e.tensor_copy(out=e1b[s], in_=e1f), reads=[t_e1f], writes=[t_e1b[s]])
                S.op("pool", lambda e, s=s: e.tensor_copy(out=e2b[s], in_=e2f), reads=[t_e2f], writes=[t_e2b[s]])
                for tb in range(4):
                    hs = []
                    for fc in range(2):
                        p1, tp1 = p512.get(); p3, tp3 = p512.get()

                        def f1(e, p1=p1, s=s, fc=fc, tb=tb):
                            for k in range(8):
                                i = e.matmul(p1, lhsT=e1b[s][:, k, fc * 128:(fc + 1) * 128], rhs=hx2[:, k, tb * 512:(tb + 1) * 512], start=(k == 0), stop=(k == 7))
                            return i

                        def f3(e, p3=p3, s=s, fc=fc, tb=tb):
                            for k in range(8):
                                i = e.matmul(p3, lhsT=e1b[s][:, k, 256 + fc * 128:256 + (fc + 1) * 128], rhs=hx2[:, k, tb * 512:(tb + 1) * 512], start=(k == 0), stop=(k == 7))
                            return i
                        S.op("pe", f1, reads=[t_e1b[s]] + t_hx2[tb * 4:(tb + 1) * 4], writes=[tp1])
                        S.op("pe", f3, reads=[t_e1b[s]] + t_hx2[tb * 4:(tb + 1) * 4], writes=[tp3])
                        ss = hc % 2; h4 = hc % 4; hc += 1
                        S.op("act", lambda e, p1=p1, ss=ss: e.activation(out=sil[ss], in_=p1, func=AF.Silu), reads=[tp1], writes=[t_sil[ss]])
                        S.op("dve", lambda e, p3=p3, ss=ss, h4=h4: e.tensor_tensor(out=hid[h4], in0=p3, in1=sil[ss], op=ALU.mult), reads=[tp3, t_sil[ss]], writes=[t_hid[h4]])
                        hs.append(h4)
                    for tt in range(4):
                        t = tb * 4 + tt
                        for hh in range(2):
                            po, tpo = p512.get()

                            def f2(e, po=po, s=s, tt=tt, hh=hh, hs=tuple(hs)):
                                for fc in range(2):
                                    i = e.matmul(po, lhsT=hid[hs[fc]][:, tt * 128:(tt + 1) * 128], rhs=e2b[s][:, fc, hh * 512:(hh + 1) * 512], start=(fc == 0), stop=(fc == 1))
                                return i
                            S.op("pe", f2, reads=[t_hid[hs[0]], t_hid[hs[1]], t_e2b[s]], writes=[tpo])
                            S.op("dve", lambda e, po=po, t=t, hh=hh, ex=ex: e.scalar_tensor_tensor(out=ACC[:, t, hh * 512:(hh + 1) * 512], in0=po, scalar=GT[:, t, ex:ex + 1], in1=ACC[:, t, hh * 512:(hh + 1) * 512], op0=ALU.mult, op1=ALU.add),
                                 reads=[tpo, t_ACC[t][hh]], writes=[t_ACC[t][hh]])
            fngb = A.alloc([D]); t_fngb = Tk(); load(fngb, fng_bc[:, :], t_fngb, "c17")
            x1r = [A.alloc([D]) for _ in range(2)]; t_x1r = [Tk(), Tk()]
            x2 = [A.alloc([D]) for _ in range(2)]; t_x2 = [Tk(), Tk()]
            junk_E = A.alloc([D]); t_junk_E = Tk()
            ssq_E = [A.alloc([1]) for _ in range(2)]; t_ssq_E = [Tk(), Tk()]
            outs_ = []
            for t in range(NOT):
                s2 = t % 2
                S.op("sp", lambda e, sig, t=t, s2=s2: sig(e.dma_start(out=x1r[s2], in_=x1d[t * 128:(t + 1) * 128, :])), extra=[x1_ops[t]], writes=[t_x1r[s2]], dma=1, key=f"x1r{s2}")
                S.op("dve", lambda e, t=t, s2=s2: e.tensor_tensor(out=x2[s2], in0=ACC[:, t, :], in1=gffn, op=ALU.mult), reads=t_ACC[t] + [t_gffn], writes=[t_x2[s2]])
                S.op("dve", lambda e, s2=s2: e.tensor_tensor(out=x2[s2], in0=x2[s2], in1=x1r[s2], op=ALU.add), reads=[t_x2[s2], t_x1r[s2]], writes=[t_x2[s2]])
                S.op("act", lambda e, s2=s2: e.activation(out=junk_E, in_=x2[s2], func=AF.Square, accum_out=ssq_E[s2]), reads=[t_x2[s2]], writes=[t_ssq_E[s2]])
                S.op("act", lambda e, s2=s2: e.activation(out=ssq_E[s2], in_=ssq_E[s2], func=AF.Sqrt, scale=1.0 / D, bias=EPS), reads=[t_ssq_E[s2]], writes=[t_ssq_E[s2]])
                S.op("dve", lambda e, s2=s2: e.reciprocal(out=ssq_E[s2], in_=ssq_E[s2]), reads=[t_ssq_E[s2]], writes=[t_ssq_E[s2]])
                S.op("dve", lambda e, s2=s2: e.scalar_tensor_tensor(out=x2[s2], in0=x2[s2], scalar=ssq_E[s2], in1=fngb, op0=ALU.mult, op1=ALU.mult), reads=[t_x2[s2], t_ssq_E[s2], t_fngb], writes=[t_x2[s2]])
                o_ = S.op("sp", lambda e, sig, t=t, s2=s2: sig(e.dma_start(out=y[t * 128:(t + 1) * 128, :], in_=x2[s2])), reads=[t_x2[s2]], dma=1, key=f"yo{s2}")
                outs_.append(o_)
            S.op("sp", None, extra=outs_)
        try:
            author()
        except _Stop:
            pass
        if dump_ops:
            S.op("sp", None, extra=dump_ops)
        S.emit(block, sems)
    nc._sched = S
    return nc


def _prep(inputs):
    import ml_dtypes
    f = lambda a: np.ascontiguousarray(np.asarray(a, dtype=np.float32))
    x = f(inputs["x"]); c = f(inputs["c"]); ctx = f(inputs["ctx"]); c_ctx = f(inputs["c_ctx"])
    w_in = f(inputs["w_in"])[0]
    conv_w = f(inputs["conv_w"])[0]
    a_log = f(inputs["a_log"])[0]; dt_bias = f(inputs["dt_bias"])[0]
    idx = np.arange(128)
    tri = np.stack([(idx[:, None] <= idx[None, :]), (idx[:, None] > idx[None, :]), (idx[:, None] >= idx[None, :]), (idx[:, None] < idx[None, :])]).astype(np.float32)
    negm = np.stack([(idx[:, None] <= idx[None, :]), (idx[None, :] < idx[:, None]), (idx[:, None] >= idx[None, :]), (idx[None, :] > idx[:, None])]).astype(np.float32) * -100.0
    blk = lambda n: (idx[:, None] // n == idx[None, :] // n)
    dcm = [blk(8)]
    for n in (16, 32, 64, 128):
        low = blk(n) & ((idx[:, None] % n) >= n // 2) & ((idx[None, :] % n) < n // 2)
        dcm += [low, low.T]
    dcm = np.stack(dcm).astype(np.float32).astype(ml_dtypes.bfloat16)
    common = {
        "dcm": dcm,
        "ada_w": f(inputs["ada_w"])[0], "ada_b": f(inputs["ada_b"]).reshape(1, -1),
        "gcols": np.ascontiguousarray(np.stack([f(inputs["norm_mix_g"])[0].reshape(8, 128).T, f(inputs["norm_ffn_g"])[0].reshape(8, 128).T], axis=-1)),
        "gng": f(inputs["gdn_norm_g"])[0].reshape(128, 1),
        "ident": np.eye(128, dtype=np.float32), "tri": tri, "negm": negm.astype(ml_dtypes.bfloat16),
        "w_rest": np.ascontiguousarray(w_in[:, COL_Z:]),
        "sgu_wT": np.ascontiguousarray(f(inputs["sgu_w"])[0].transpose(0, 2, 1)),
        "sgu_bb": np.ascontiguousarray(np.broadcast_to(f(inputs["sgu_b"])[0][None], (128, 8, 128))),
        "lng_c": np.ascontiguousarray(f(inputs["sgu_ln_g"])[0].reshape(8, 128).T),
        "lnb_bc": np.ascontiguousarray(np.broadcast_to(f(inputs["sgu_ln_b"])[0][None], (128, D))),
        "w_a": f(inputs["w_branch_a"])[0], "w_b": f(inputs["w_branch_b"])[0], "w_out": f(inputs["w_out"])[0],
        "rw": np.ascontiguousarray(np.concatenate([f(inputs["router_group_w"])[0], f(inputs["router_expert_w"])[0]], axis=1)),
        "rb": np.ascontiguousarray(np.broadcast_to(np.concatenate([f(inputs["router_group_b"])[0], f(inputs["router_expert_b"])[0]])[None], (128, 36))),
        "ew1": f(inputs["expert_w1"])[0], "ew3": f(inputs["expert_w3"])[0], "ew2": f(inputs["expert_w2"])[0],
        "fng_bc": np.ascontiguousarray(np.broadcast_to(f(inputs["final_norm_g"])[None], (128, D))),
    }
    maps = []
    for core in range(8):
        b, r = core // 4, core % 4
        heads = (2 * r, 2 * r + 1)
        qcols = np.concatenate([np.arange(base + h * 128, base + (h + 1) * 128) for base in (0, 1024, 2048) for h in heads])
        bacols = np.array([COL_BETA + d * 8 + h for d in range(2) for h in heads] + [COL_A + d * 8 + h for d in range(2) for h in heads])
        conv_c = np.ascontiguousarray(conv_w[:, qcols].reshape(5, 6, 128).transpose(2, 1, 0))
        gsc = np.concatenate([np.array([a_log[d, h] for d in range(2) for h in heads]), np.array([dt_bias[d, h] for d in range(2) for h in heads])]).astype(np.float32)
        qm = np.zeros((128, 4), np.float32); qm[:, r] = 1.0
        m = dict(common)
        m.update({
            "xb": x[b], "ctxb": ctx[b], "xo": np.ascontiguousarray(x[b, r * OWN:(r + 1) * OWN]),
            "cvT": np.ascontiguousarray(np.stack([c[b].reshape(8, 128).T, c_ctx.reshape(8, 128).T], axis=-1)),
            "w_qkv": np.ascontiguousarray(w_in[:, qcols]), "w_ba": np.ascontiguousarray(w_in[:, bacols]),
            "gsc66": np.ascontiguousarray(np.broadcast_to(gsc.reshape(1, 2, 1, 4), (128, 2, NT, 4))),
            "conv_c": conv_c, "gsc": np.ascontiguousarray(np.broadcast_to(gsc[None], (128, 8))), "qmask": qm,
        })
        maps.append(m)
    return maps


_NC = None


def kernel(**inputs):
    global _NC
    if _NC is None:
        _NC = build_program()
    maps = _prep(inputs)
    res = run_bass_kernel_spmd(_NC, maps, core_ids=list(range(8)))
    out = np.zeros((2, SEQ, D), np.float32)
    for core in range(8):
        b, r = core // 4, core % 4
        out[b, r * OWN:(r + 1) * OWN] = np.asarray(res.results[core]["y"], dtype=np.float32)
    return out
```
